# Optimizing a Trainium2 kernel written in Bass

```python
import math
import jax
import jax.numpy as jnp
from jax import lax
import numpy as np

D_MODEL = 1024
BATCH = 4
SEQ = 4096
DEPTH = 2

CTX_LEN = 256
GRID_W = 64
CONV_W = 3
NORM_EPS = 1e-6
F32 = jnp.float32

M_HEADS = 4
M_HEAD_DIM = 64
M_INNER = M_HEADS * M_HEAD_DIM
M_GROUPS = 2
M_STATE = 128
M_CONV_DIM = M_INNER + 2 * M_GROUPS * M_STATE
M_COLS = M_INNER + M_CONV_DIM + 2 * M_HEADS
M_CHUNK = 128

DN_HEADS = 4
DN_HEAD_K = 128
DN_HEAD_V = 128
DN_KD = DN_HEADS * DN_HEAD_K
DN_VD = DN_HEADS * DN_HEAD_V
DN_CONV_DIM = 2 * DN_KD + DN_VD
DN_COLS = DN_CONV_DIM + DN_VD + 4 * DN_HEADS
DN_CHUNK = 64

RW_HEADS = 4
RW_HEAD = 64
RW_DIM = RW_HEADS * RW_HEAD
RW_DECAY_LORA = 64
RW_ICLR_LORA = 64
RW_GATE_LORA = 128
RW_COLS = 3 * RW_DIM + 2 * RW_DECAY_LORA + RW_ICLR_LORA + RW_GATE_LORA
RW_LN_EPS = RW_HEAD * 1e-5

IN_COLS = M_COLS + DN_COLS + RW_COLS
MIX_OUT = M_INNER + DN_VD + RW_DIM

N_EXPERTS = 32
TOP_K = 4
D_FF = 1024
SWIGLU_ALPHA = 1.702
SWIGLU_LIMIT = 7.0
MOE_BLOCK = 256

kernel_name = 'hybrid_ssd_deltanet_rwkv7_moe_prefix_dit'


def split_cols(t, sizes):
    return jnp.split(t, [int(o) for o in np.cumsum(sizes)[:-1]], axis=-1)


def rms_norm(t, w, eps=NORM_EPS):
    tf = t.astype(F32)
    y = tf * lax.rsqrt(jnp.mean(tf * tf, axis=-1, keepdims=True) + eps)
    return (y * w).astype(t.dtype)


def l2norm(t, eps=1e-6):
    return t * lax.rsqrt(jnp.sum(t * t, axis=-1, keepdims=True) + eps)


def modulate(h, shift, scale):
    return h * (1 + scale) + shift


def centred_conv(t, w):
    half = CONV_W // 2
    n = t.shape[1]
    tp = jnp.pad(t, ((0, 0), (half, half), (0, 0)))
    out = tp[:, 0:n] * w[0]
    for j in range(1, CONV_W):
        out = out + tp[:, j:j + n] * w[j]
    return out


def to_col_major(t, rows):
    b, n, ch = t.shape
    return t.reshape(b, rows, GRID_W, ch).transpose(0, 2, 1, 3).reshape(b, n, ch)


def to_raster(t, rows):
    b, n, ch = t.shape
    return t.reshape(b, GRID_W, rows, ch).transpose(0, 2, 1, 3).reshape(b, n, ch)


def q_shift(t, rows):
    b, n, ch = t.shape
    g = t.reshape(b, rows, GRID_W, ch)
    from_left = jnp.pad(g[:, :, :-1], ((0, 0), (0, 0), (1, 0), (0, 0)))
    from_right = jnp.pad(g[:, :, 1:], ((0, 0), (0, 0), (0, 1), (0, 0)))
    from_up = jnp.pad(g[:, :-1], ((0, 0), (1, 0), (0, 0), (0, 0)))
    from_down = jnp.pad(g[:, 1:], ((0, 0), (0, 1), (0, 0), (0, 0)))
    sel = jnp.arange(ch) % 4
    out = jnp.where(sel == 0, from_left, jnp.where(sel == 1, from_right, jnp.where(sel == 2, from_up, from_down)))
    return out.reshape(b, n, ch)


def bi_shift(t):
    prev = jnp.pad(t[:, :-1], ((0, 0), (1, 0), (0, 0)))
    nxt = jnp.pad(t[:, 1:], ((0, 0), (0, 1), (0, 0)))
    return jnp.where(jnp.arange(t.shape[-1]) % 2 == 0, prev, nxt)


def two_segment_scan(scan_fn, ctx_args, lat_args, state0, reverse):
    flip = (lambda t: jnp.flip(t, axis=1)) if reverse else (lambda t: t)
    y_c, s_c = scan_fn(*[flip(t) for t in ctx_args], state0)
    y_l, _ = scan_fn(*[flip(t) for t in lat_args], s_c)
    return flip(y_c), flip(y_l)


def ssd_chunk_scan(xs, log_a, bm, cm, h0):
    b, n, H, P = xs.shape
    nc = n // M_CHUNK
    ch = lambda t: t.reshape((b, nc, M_CHUNK) + t.shape[2:])
    xs, log_a, bm, cm = ch(xs), ch(log_a), ch(bm), ch(cm)
    a_cum = jnp.cumsum(log_a, axis=2)
    idx = jnp.arange(M_CHUNK)
    incl = (idx[:, None] >= idx[None, :])[None, None, :, :, None]
    seg = jnp.exp(jnp.where(incl, a_cum[:, :, :, None, :] - a_cum[:, :, None, :, :], -jnp.inf))
    scores = jnp.einsum('bcihn,bcjhn->bcijh', cm, bm) * seg
    y_diag = jnp.einsum('bcijh,bcjhp->bcihp', scores, xs)
    to_end = jnp.exp(a_cum[:, :, -1:] - a_cum)
    states = jnp.einsum('bcjhn,bcjhp->bchpn', bm * to_end[..., None], xs)
    chunk_decay = jnp.exp(a_cum[:, :, -1])

    def step(h, inp):
        s, dec = inp
        return h * dec[:, :, None, None] + s, h

    h_last, h_in = lax.scan(step, h0, (jnp.moveaxis(states, 1, 0), jnp.moveaxis(chunk_decay, 1, 0)))
    h_in = jnp.moveaxis(h_in, 0, 1)
    y_off = jnp.einsum('bcihn,bchpn->bcihp', cm, h_in) * jnp.exp(a_cum)[..., None]
    return (y_diag + y_off).reshape(b, n, H, P), h_last


def gated_delta_chunk_scan(q, k, v, g, beta, s0):
    b, n, H, K = q.shape
    V = v.shape[-1]
    nc = n // DN_CHUNK
    ch = lambda t: t.reshape((b, nc, DN_CHUNK) + t.shape[2:])
    q, k, v, g, beta = ch(q), ch(k), ch(v), ch(g), ch(beta)
    g_cum = jnp.cumsum(g, axis=2)
    idx = jnp.arange(DN_CHUNK)
    incl = idx[:, None] >= idx[None, :]
    strict = idx[:, None] > idx[None, :]
    gh = jnp.moveaxis(g_cum, 3, 2)
    decay = jnp.exp(jnp.where(incl, gh[..., :, None] - gh[..., None, :], -jnp.inf))
    kb = k * beta[..., None]
    lower = jnp.where(strict, jnp.einsum('bcihk,bcjhk->bchij', kb, k) * decay, 0.0)
    eye = jnp.eye(DN_CHUNK, dtype=q.dtype)
    rhs = jnp.concatenate([v * beta[..., None], kb * jnp.exp(g_cum)[..., None]], axis=-1)
    rhs = jnp.moveaxis(rhs, 3, 2)
    sol = lax.linalg.triangular_solve(eye + lower, rhs, left_side=True, lower=True, unit_diagonal=True)
    u, w = sol[..., :V], sol[..., V:]
    attn = jnp.einsum('bcihk,bcjhk->bchij', q, k) * decay
    q_dec = jnp.moveaxis(q * jnp.exp(g_cum)[..., None], 3, 2)
    k_dec = jnp.moveaxis(k * jnp.exp(g_cum[:, :, -1:] - g_cum)[..., None], 3, 2)
    g_last = jnp.exp(g_cum[:, :, -1])

    def step(S, inp):
        u_i, w_i, attn_i, qd_i, kd_i, gl_i = inp
        v_new = u_i - jnp.einsum('bhck,bhkv->bhcv', w_i, S)
        o = jnp.einsum('bhck,bhkv->bhcv', qd_i, S) + jnp.einsum('bhij,bhjv->bhiv', attn_i, v_new)
        S = S * gl_i[..., None, None] + jnp.einsum('bhck,bhcv->bhkv', kd_i, v_new)
        return S, o

    xs = tuple(jnp.moveaxis(t, 1, 0) for t in (u, w, attn, q_dec, k_dec, g_last))
    s_last, o = lax.scan(step, s0, xs)
    o = jnp.transpose(o, (1, 0, 3, 2, 4)).reshape(b, n, H, V)
    return o, s_last


def rwkv7_scan(r, log_w, k, v, a_vec, b_vec, s0):
    def step(S, inp):
        r_t, w_t, k_t, v_t, a_t, b_t = inp
        sa = jnp.einsum('bhvk,bhk->bhv', S, a_t)
        S = S * w_t[:, :, None, :] + sa[..., None] * b_t[:, :, None, :] + v_t[..., None] * k_t[:, :, None, :]
        return S, jnp.einsum('bhvk,bhk->bhv', S, r_t)

    xs = tuple(jnp.moveaxis(t, 1, 0) for t in (r, jnp.exp(log_w), k, v, a_vec, b_vec))
    s_last, y = lax.scan(step, s0, xs)
    return jnp.moveaxis(y, 0, 1), s_last


def mamba2_mixer(p_c, p_l, conv_w, conv_b, dt_bias, a_log, d_skip, norm_w):
    a_neg = -jnp.exp(a_log.astype(F32))
    rep = M_HEADS // M_GROUPS

    def prep(p):
        b, n = p.shape[:2]
        z, xbc, dt_raw = split_cols(p, (M_INNER, M_CONV_DIM, 2 * M_HEADS))
        xbc = jax.nn.silu(centred_conv(xbc, conv_w) + conv_b)
        xs, bm, cm = split_cols(xbc, (M_INNER, M_GROUPS * M_STATE, M_GROUPS * M_STATE))
        grp = lambda t: jnp.repeat(t.astype(F32).reshape(b, n, M_GROUPS, M_STATE), rep, axis=2)
        return z, xs.astype(F32).reshape(b, n, M_HEADS, M_HEAD_DIM), grp(bm), grp(cm), dt_raw.astype(F32)

    def scan_args(direction, xs, bm, cm, dt_raw):
        dt = jax.nn.softplus(dt_raw[..., direction * M_HEADS:(direction + 1) * M_HEADS] + dt_bias[direction])
        return (xs * dt[..., None], dt * a_neg[direction], bm, cm)

    zc, xc, bc, cc, dtc = prep(p_c)
    zl, xl, bl, cl, dtl = prep(p_l)
    state0 = jnp.zeros((p_c.shape[0], M_HEADS, M_HEAD_DIM, M_STATE), F32)
    yc = xc * d_skip[:, None]
    yl = xl * d_skip[:, None]
    for direction in range(2):
        oc, ol = two_segment_scan(ssd_chunk_scan, scan_args(direction, xc, bc, cc, dtc),
                                  scan_args(direction, xl, bl, cl, dtl), state0, direction == 1)
        yc, yl = yc + oc, yl + ol

    def finish(y, z):
        b, n = z.shape[:2]
        y = y.reshape(b, n, M_INNER) * jax.nn.silu(z.astype(F32))
        y = y.reshape(b, n, M_GROUPS, M_INNER // M_GROUPS)
        y = y * lax.rsqrt(jnp.mean(y * y, axis=-1, keepdims=True) + 1e-5)
        return (y.reshape(b, n, M_INNER) * norm_w).astype(z.dtype)

    return finish(yc, zc), finish(yl, zl)


def gated_deltanet_mixer(p_c, p_l, rows, conv_w, dt_bias, a_log, norm_w):
    a_pos = jnp.exp(a_log.astype(F32))

    def prep(p):
        b, n = p.shape[:2]
        qkv, gate, beta_raw, a_raw = split_cols(p, (DN_CONV_DIM, DN_VD, 2 * DN_HEADS, 2 * DN_HEADS))
        qkv = jax.nn.silu(centred_conv(qkv, conv_w)).astype(F32)
        q, k, v = split_cols(qkv, (DN_KD, DN_KD, DN_VD))
        q = l2norm(q.reshape(b, n, DN_HEADS, DN_HEAD_K)) * DN_HEAD_K ** -0.5
        k = l2norm(k.reshape(b, n, DN_HEADS, DN_HEAD_K))
        v = v.reshape(b, n, DN_HEADS, DN_HEAD_V)
        return q, k, v, gate, beta_raw.astype(F32), a_raw.astype(F32)

    def scan_args(direction, q, k, v, beta_raw, a_raw):
        sl = slice(direction * DN_HEADS, (direction + 1) * DN_HEADS)
        beta = jax.nn.sigmoid(beta_raw[..., sl])
        g = -a_pos[direction] * jax.nn.softplus(a_raw[..., sl] + dt_bias[direction])
        return (q, k, v, g, beta)

    qc, kc, vc, gate_c, brc, arc = prep(p_c)
    ql, kl, vl, gate_l, brl, arl = prep(to_col_major(p_l, rows))
    state0 = jnp.zeros((p_c.shape[0], DN_HEADS, DN_HEAD_K, DN_HEAD_V), F32)
    oc, ol = 0.0, 0.0
    for direction in range(2):
        dc, dl = two_segment_scan(gated_delta_chunk_scan, scan_args(direction, qc, kc, vc, brc, arc),
                                  scan_args(direction, ql, kl, vl, brl, arl), state0, direction == 1)
        oc, ol = oc + dc, ol + dl

    def finish(o, gate):
        b, n = gate.shape[:2]
        o = o * lax.rsqrt(jnp.mean(o * o, axis=-1, keepdims=True) + 1e-6) * norm_w
        return (o.reshape(b, n, DN_VD) * jax.nn.silu(gate.astype(F32))).astype(gate.dtype)

    return finish(oc, gate_c), to_raster(finish(ol, gate_l), rows)


def rwkv7_mixer(p_c, p_l, rows, mu, w0, w2, a0, a2, g2, k_k, k_a, r_k, ln_w, ln_b):
    def prep(p, shifted):
        b, n = p.shape[:2]
        p = p + (shifted - p) * mu
        r, k, v, wd, ad, gd = split_cols(p, (RW_DIM, RW_DIM, RW_DIM, 2 * RW_DECAY_LORA, RW_ICLR_LORA, RW_GATE_LORA))
        heads = lambda t: t.astype(F32).reshape(b, n, RW_HEADS, RW_HEAD)
        a = jax.nn.sigmoid(a0 + ad @ a2)
        g = jax.nn.sigmoid(gd) @ g2
        kk = l2norm(heads(k * k_k))
        k = k * (1 + (a - 1) * k_a)
        log_w = [heads(-jnp.exp(-jax.nn.softplus(-(w0[j] + jnp.tanh(wd_j) @ w2[j]).astype(F32)) - 0.5))
                 for j, wd_j in enumerate(jnp.split(wd, 2, axis=-1))]
        return heads(r), heads(k), heads(v), kk, heads(a), g, log_w

    rc, kc, vc, kkc, ac, gc, lwc = prep(p_c, bi_shift(p_c))
    rl, kl, vl, kkl, al, gl, lwl = prep(p_l, q_shift(p_l, rows))
    state0 = jnp.zeros((p_c.shape[0], RW_HEADS, RW_HEAD, RW_HEAD), F32)
    yc, yl = 0.0, 0.0
    for j in range(2):
        oc, ol = two_segment_scan(rwkv7_scan, (rc, lwc[j], kc, vc, -kkc, kkc * ac),
                                  (rl, lwl[j], kl, vl, -kkl, kkl * al), state0, j == 1)
        yc, yl = yc + oc, yl + ol

    def finish(y, r, k, v, g):
        b, n = y.shape[:2]
        mean = jnp.mean(y, axis=-1, keepdims=True)
        var = jnp.mean(jnp.square(y - mean), axis=-1, keepdims=True)
        y = ((y - mean) * lax.rsqrt(var + RW_LN_EPS)).reshape(b, n, RW_DIM) * ln_w + ln_b
        bonus = (jnp.sum(r * k * r_k, axis=-1, keepdims=True) * v).reshape(b, n, RW_DIM)
        return ((y + bonus) * g).astype(p_c.dtype)

    return finish(yc, rc, kc, vc, gc), finish(yl, rl, kl, vl, gl)


def moe_ffn(h, w_router, b_router, w_gate_up, b_gate_up, w_down, b_down):
    n, d = h.shape
    logits = (h @ w_router).astype(F32) + b_router
    top_logit, top_e = lax.top_k(logits, TOP_K)
    gate = jax.nn.softmax(top_logit, axis=-1)
    nk = n * TOP_K
    flat_e = top_e.reshape(nk)
    flat_tok = jnp.arange(nk, dtype=jnp.int32) // TOP_K
    order = jnp.argsort(flat_e)
    sorted_e = flat_e[order]
    counts = jnp.bincount(flat_e, length=N_EXPERTS)
    padded = (counts + MOE_BLOCK - 1) // MOE_BLOCK * MOE_BLOCK
    pad_start = jnp.cumsum(padded) - padded
    start = jnp.cumsum(counts) - counts
    dest = pad_start[sorted_e] + jnp.arange(nk) - start[sorted_e]
    n_blocks = -(-nk // MOE_BLOCK) + N_EXPERTS
    slots = n_blocks * MOE_BLOCK
    slot_tok = jnp.full((slots,), n, jnp.int32).at[dest].set(flat_tok[order])
    slot_gate = jnp.zeros((slots,), h.dtype).at[dest].set(gate.reshape(nk)[order].astype(h.dtype))
    block_end = jnp.cumsum(padded) // MOE_BLOCK
    block_expert = jnp.minimum(jnp.searchsorted(block_end, jnp.arange(n_blocks), side='right'), N_EXPERTS - 1)
    h_pad = jnp.concatenate([h, jnp.zeros((1, d), h.dtype)], axis=0)

    def expert_block(args):
        tok, e = args
        gu = h_pad[tok] @ w_gate_up[e] + b_gate_up[e]
        g_, u_ = jnp.split(gu, 2, axis=-1)
        g_ = jnp.minimum(g_, SWIGLU_LIMIT)
        u_ = jnp.clip(u_, -SWIGLU_LIMIT, SWIGLU_LIMIT)
        act = g_ * jax.nn.sigmoid(SWIGLU_ALPHA * g_) * (u_ + 1)
        return act @ w_down[e] + b_down[e]

    y_slots = lax.map(expert_block, (slot_tok.reshape(n_blocks, MOE_BLOCK), block_expert))
    out = jnp.zeros((n + 1, d), h.dtype).at[slot_tok].add(y_slots.reshape(slots, d) * slot_gate[:, None])
    return out[:n]


def setup_inputs(seed: int = 0) -> dict:
    key = jax.random.key(seed)
    ks = iter(jax.random.split(key, 48))
    nrm = lambda shape, scale: scale * jax.random.normal(next(ks), shape, F32)
    unif = lambda shape, lo, hi: jax.random.uniform(next(ks), shape, F32, lo, hi)
    gain = lambda shape: 1.0 + nrm(shape, 0.01)

    def dt_bias(shape):
        dt = jnp.exp(unif(shape, math.log(1e-3), math.log(1e-1)))
        return dt + jnp.log(-jnp.expm1(-dt))

    L, D = DEPTH, D_MODEL
    return {
        'x': nrm((BATCH, SEQ, D), 1.0),
        'c': nrm((BATCH, D), 1.0),
        'ctx': nrm((BATCH, CTX_LEN, D), 1.0),
        'c_ctx': nrm((D,), 1.0),
        'w_mod': nrm((L, D, 6 * D), 0.5 * D ** -0.5),
        'b_mod': nrm((L, 6 * D), 0.01),
        'norm1_w': gain((L, D)),
        'norm2_w': gain((L, D)),
        'w_in': nrm((L, D, IN_COLS), D ** -0.5),
        'w_out': nrm((L, MIX_OUT, D), MIX_OUT ** -0.5),
        'm_conv_w': nrm((L, CONV_W, M_CONV_DIM), CONV_W ** -0.5),
        'm_conv_b': nrm((L, M_CONV_DIM), 0.01),
        'm_dt_bias': dt_bias((L, 2, M_HEADS)),
        'm_a_log': jnp.log(unif((L, 2, M_HEADS), 1.0, 16.0)),
        'm_d': 1.0 + nrm((L, M_HEADS), 0.1),
        'm_norm_w': gain((L, M_INNER)),
        'dn_conv_w': nrm((L, CONV_W, DN_CONV_DIM), CONV_W ** -0.5),
        'dn_dt_bias': dt_bias((L, 2, DN_HEADS)),
        'dn_a_log': jnp.log(unif((L, 2, DN_HEADS), 1.0, 16.0)),
        'dn_norm_w': gain((L, DN_HEAD_V)),
        'rw_mu': unif((L, RW_COLS), 0.0, 1.0),
        'rw_w0': unif((L, 2, RW_DIM), -5.0, 0.0),
        'rw_w2': nrm((L, 2, RW_DECAY_LORA, RW_DIM), 0.1 * RW_DECAY_LORA ** -0.5),
        'rw_a0': nrm((L, RW_DIM), 0.1),
        'rw_a2': nrm((L, RW_ICLR_LORA, RW_DIM), 0.1 * RW_ICLR_LORA ** -0.5),
        'rw_g2': nrm((L, RW_GATE_LORA, RW_DIM), RW_GATE_LORA ** -0.5),
        'rw_k_k': 0.85 + nrm((L, RW_DIM), 0.05),
        'rw_k_a': 1.0 + nrm((L, RW_DIM), 0.05),
        'rw_r_k': nrm((L, RW_HEADS, RW_HEAD), 0.1),
        'rw_ln_w': gain((L, RW_DIM)),
        'rw_ln_b': nrm((L, RW_DIM), 0.01),
        'w_router': nrm((L, D, N_EXPERTS), D ** -0.5),
        'b_router': nrm((L, N_EXPERTS), 0.01),
        'w_gate_up': nrm((L, N_EXPERTS, D, 2 * D_FF), D ** -0.5),
        'b_gate_up': nrm((L, N_EXPERTS, 2 * D_FF), 0.01),
        'w_down': nrm((L, N_EXPERTS, D_FF, D), D_FF ** -0.5),
        'b_down': nrm((L, N_EXPERTS, D), 0.01),
        'norm_f_w': gain((D,)),
    }


def reference(x, c, ctx, c_ctx, w_mod, b_mod, norm1_w, norm2_w, w_in, w_out,
              m_conv_w, m_conv_b, m_dt_bias, m_a_log, m_d, m_norm_w,
              dn_conv_w, dn_dt_bias, dn_a_log, dn_norm_w,
              rw_mu, rw_w0, rw_w2, rw_a0, rw_a2, rw_g2, rw_k_k, rw_k_a, rw_r_k, rw_ln_w, rw_ln_b,
              w_router, b_router, w_gate_up, b_gate_up, w_down, b_down, norm_f_w):
    seq, d = x.shape[1], x.shape[2]
    rows = seq // GRID_W
    x_l, x_c = x, ctx
    silu_c = jax.nn.silu(c)
    silu_cc = jax.nn.silu(c_ctx)
    for i in range(DEPTH):
        last = i == DEPTH - 1
        mod_l = jnp.split((silu_c @ w_mod[i] + b_mod[i])[:, None, :], 6, axis=-1)
        mod_c = jnp.split(silu_cc @ w_mod[i] + b_mod[i], 6, axis=-1)
        h_l = modulate(rms_norm(x_l, norm1_w[i]), mod_l[0], mod_l[1])
        h_c = modulate(rms_norm(x_c, norm1_w[i]), mod_c[0], mod_c[1])
        pa_l, pb_l, pc_l = split_cols(h_l @ w_in[i], (M_COLS, DN_COLS, RW_COLS))
        pa_c, pb_c, pc_c = split_cols(h_c @ w_in[i], (M_COLS, DN_COLS, RW_COLS))
        ya_c, ya_l = mamba2_mixer(pa_c, pa_l, m_conv_w[i], m_conv_b[i], m_dt_bias[i], m_a_log[i], m_d[i], m_norm_w[i])
        yb_c, yb_l = gated_deltanet_mixer(pb_c, pb_l, rows, dn_conv_w[i], dn_dt_bias[i], dn_a_log[i], dn_norm_w[i])
        yc_c, yc_l = rwkv7_mixer(pc_c, pc_l, rows, rw_mu[i], rw_w0[i], rw_w2[i], rw_a0[i], rw_a2[i], rw_g2[i],
                                 rw_k_k[i], rw_k_a[i], rw_r_k[i], rw_ln_w[i], rw_ln_b[i])
        x_l = x_l + mod_l[2] * (jnp.concatenate([ya_l, yb_l, yc_l], axis=-1) @ w_out[i])
        h2_l = modulate(rms_norm(x_l, norm2_w[i]), mod_l[3], mod_l[4])
        moe_args = (w_router[i], b_router[i], w_gate_up[i], b_gate_up[i], w_down[i], b_down[i])
        if last:
            x_l = x_l + mod_l[5] * moe_ffn(h2_l.reshape(-1, d), *moe_args).reshape(x_l.shape)
        else:
            x_c = x_c + mod_c[2] * (jnp.concatenate([ya_c, yb_c, yc_c], axis=-1) @ w_out[i])
            h2_c = modulate(rms_norm(x_c, norm2_w[i]), mod_c[3], mod_c[4])
            n_c = h2_c.shape[0] * h2_c.shape[1]
            f = moe_ffn(jnp.concatenate([h2_c.reshape(-1, d), h2_l.reshape(-1, d)], axis=0), *moe_args)
            x_c = x_c + mod_c[5] * f[:n_c].reshape(x_c.shape)
            x_l = x_l + mod_l[5] * f[n_c:].reshape(x_l.shape)
    return rms_norm(x_l, norm_f_w)
```

```python
import numpy as np
from contextlib import ExitStack
import concourse.bass as bass
import concourse.mybir as mybir
from concourse.bass_utils import run_bass_kernel_spmd

F32 = mybir.dt.float32
BF16 = mybir.dt.bfloat16
AF = mybir.ActivationFunctionType
ALU = mybir.AluOpType
AX = mybir.AxisListType

ENGS = ("pe", "dve", "act", "pool", "sp")
N_DMA_SEMS = 12


class Prog:
    def __init__(self, nc, same_engine_sync=None):
        import os
        if same_engine_sync is None:
            same_engine_sync = os.environ.get('SAMESYNC', '1') == '1'
        self.nc = nc
        self.es = ExitStack()
        self.ops = {e: [] for e in ENGS}
        self.cnt = {e: 0 for e in ENGS}
        self.sem = {}
        for e in ENGS:
            self.sem[e] = self.es.enter_context(nc.semaphore("s_" + e))
        self.dsem = {q: [self.es.enter_context(nc.semaphore(f"d_{q}{i}")) for i in range(N_DMA_SEMS)]
                     for q in ("sp", "pool", "act")}
        self.dsem_uses = {q: [0] * N_DMA_SEMS for q in ("sp", "pool", "act")}
        self.dsem_next = {q: 0 for q in ("sp", "pool", "act")}
        self.waited = {e: {} for e in ENGS}
        self.lastw = {}
        self.readers = {}
        self.same = same_engine_sync
        self.semobj = {}
        self.out_tokens = []
        self.nops = 0

    def sb(self, name, shape, dt=F32):
        return self.es.enter_context(self.nc.sbuf_tensor("sb_" + name, list(shape), dt))

    def ps(self, name, shape, dt=F32):
        return self.es.enter_context(self.nc.psum_tensor("ps_" + name, list(shape), dt))

    def _need(self, eng, tok, waits):
        if tok is None:
            return
        semkey, val, src = tok
        if src == eng and (not self.same or eng == "pe"):
            return
        if self.waited[eng].get(semkey, 0) >= val:
            return
        waits[semkey] = max(waits.get(semkey, 0), val)

    def _deps(self, eng, reads, writes):
        waits = {}
        for k in reads:
            self._need(eng, self.lastw.get(k), waits)
        for k in writes:
            self._need(eng, self.lastw.get(k), waits)
            for t in self.readers.get(k, ()):
                self._need(eng, t, waits)
        for semkey, val in waits.items():
            self.waited[eng][semkey] = val
            self.ops[eng].append(("wait", semkey, val))

    def _commit(self, tok, reads, writes):
        for k in reads:
            self.readers.setdefault(k, []).append(tok)
        for k in writes:
            self.lastw[k] = tok
            self.readers[k] = []

    def op(self, eng, fn, reads=(), writes=()):
        import os
        lim = os.environ.get("OPLIMIT")
        if lim is not None and self.nops >= int(lim):
            return None
        writes = list(writes) + [k for k in reads if isinstance(k, str) and k.startswith("bk")]
        reads = [k for k in reads if not (isinstance(k, str) and k.startswith("bk"))]
        self._deps(eng, reads, writes)
        self.cnt[eng] += 1
        tok = (("e", eng), self.cnt[eng], eng)
        self.ops[eng].append(("op", fn, ("e", eng), 1))
        self._commit(tok, reads, writes)
        self.nops += 1
        return tok

    def I(self, eng, meth, *args, r=(), w=(), **kw):
        return self.op(eng, lambda e: getattr(e, meth)(*args, **kw), reads=r, writes=w)

    def dma(self, q, out, in_, reads=(), writes=(), is_output=False, **kw):
        import os
        lim = os.environ.get("OPLIMIT")
        if lim is not None and self.nops >= int(lim) and not is_output:
            return None
        i = self.dsem_next[q]
        self.dsem_next[q] = (i + 1) % N_DMA_SEMS
        uses = self.dsem_uses[q][i]
        semkey = ("d", q, i)
        if uses > 0 and self.waited[q].get(semkey, 0) < 16 * uses:
            self.waited[q][semkey] = 16 * uses
            self.ops[q].append(("wait", semkey, 16 * uses))
        self._deps(q, reads, writes)
        self.dsem_uses[q][i] = uses + 1
        tok = (semkey, 16 * (uses + 1), None)
        self.ops[q].append(("op", lambda e: e.dma_start(out=out, in_=in_, **kw), semkey, 16))
        self._commit(tok, reads, writes)
        if is_output:
            self.out_tokens.append(tok)
        self.nops += 1
        return tok

    def idma(self, out, in_, out_idx=None, in_idx=None, reads=(), writes=(), is_output=False, **kw):
        q = "pool"
        i = self.dsem_next[q]
        self.dsem_next[q] = (i + 1) % N_DMA_SEMS
        uses = self.dsem_uses[q][i]
        semkey = ("d", q, i)
        if uses > 0 and self.waited[q].get(semkey, 0) < 16 * uses:
            self.waited[q][semkey] = 16 * uses
            self.ops[q].append(("wait", semkey, 16 * uses))
        self._deps(q, reads, writes)
        self.dsem_uses[q][i] = uses + 1
        tok = (semkey, 16 * (uses + 1), None)
        oo = bass.IndirectOffsetOnAxis(ap=out_idx, axis=0) if out_idx is not None else None
        io = bass.IndirectOffsetOnAxis(ap=in_idx, axis=0) if in_idx is not None else None
        self.ops[q].append(("op", lambda e: e.indirect_dma_start(out=out, out_offset=oo, in_=in_, in_offset=io, **kw), semkey, 16))
        self._commit(tok, reads, writes)
        if is_output:
            self.out_tokens.append(tok)
        self.nops += 1
        return tok

    def _semh(self, semkey):
        if semkey[0] == "e":
            return self.sem[semkey[1]]
        return self.dsem[semkey[1]][semkey[2]]

    def finish(self):
        for tok in self.out_tokens:
            semkey, val, _ = tok
            if self.waited["sp"].get(semkey, 0) < val:
                self.waited["sp"][semkey] = val
                self.ops["sp"].append(("wait", semkey, val))
        for e in ENGS:
            if self.cnt[e] > 0 and e != "sp":
                self.ops["sp"].append(("wait", ("e", e), self.cnt[e]))
        for q in ("sp", "pool", "act"):
            for i in range(N_DMA_SEMS):
                if self.dsem_uses[q][i] > 0:
                    self.ops["sp"].append(("wait", ("d", q, i), 16 * self.dsem_uses[q][i]))
        nc = self.nc
        with nc.Block() as block:
            def mk(ename):
                def body(e):
                    for item in self.ops[ename]:
                        if item[0] == "wait":
                            e.wait_ge(self._semh(item[1]), item[2])
                        else:
                            ins = item[1](e)
                            ins.then_inc(self._semh(item[2]), item[3])
                return body
            block.tensor(mk("pe"))
            block.vector(mk("dve"))
            block.scalar(mk("act"))
            block.gpsimd(mk("pool"))
            block.sync(mk("sp"))
        self.es.close()


def build_M():
    nc = bass.Bass("TRN2", target_bir_lowering=False)
    cs = nc.dram_tensor("cs", [128, 8, 5], F32, kind="ExternalInput").ap()
    wm = nc.dram_tensor("wm", [12, 128, 8, 128], F32, kind="ExternalInput").ap()
    bm = nc.dram_tensor("bm", [128, 12], F32, kind="ExternalInput").ap()
    out = nc.dram_tensor("modT", [128, 12, 5], F32, kind="ExternalOutput").ap()
    P = Prog(nc)
    cst = P.sb("cst", [128, 8, 5]); sg = P.sb("sg", [128, 8, 5]); sc = P.sb("sc", [128, 8, 5])
    bmt = P.sb("bmt", [128, 12]); ot = P.sb("ot", [128, 12, 5])
    wt = [P.sb(f"wt{i}", [128, 8, 128]) for i in range(2)]
    pp = [P.ps(f"pp{i}", [128, 8]) for i in range(2)]
    P.dma("sp", cst[:], cs, writes=["cst"])
    P.dma("sp", bmt[:], bm, writes=["bmt"])
    P.op("act", lambda e: e.activation(sg[:], cst[:], AF.Sigmoid), reads=["cst"], writes=["sg"])
    P.op("dve", lambda e: e.tensor_tensor(sc[:], cst[:], sg[:], ALU.mult), reads=["cst", "sg"], writes=["sc"])
    for j in range(12):
        w = wt[j % 2]; wk = f"wt{j%2}"; pk = f"pp{j%2}"; p_ = pp[j % 2]
        P.dma("sp", w[:], wm[j], writes=[wk])
        for k in range(8):
            P.op("pe", lambda e, w=w, k=k, p_=p_: e.matmul(p_[:, 0:5], w[:, k, :], sc[:, k, :], start=(k == 0), stop=(k == 7)),
                 reads=[wk, "sc"], writes=[pk])
        P.op("dve", lambda e, j=j, p_=p_: e.tensor_scalar(ot[:, j, :], p_[:, 0:5], bmt[:, j:j + 1], None, ALU.add),
             reads=[pk, "bmt"], writes=["ot"])
    P.dma("sp", out, ot[:], reads=["ot"], is_output=True)
    P.finish()
    return nc


NTOK = 2176
TILES = [(0, 128)] + [(128 + 512 * i, 512) for i in range(4)]
NCT = 33


def rms_modulate(P, xT, hT, mod, nw, ones_bf, shift_i, scale_i, hT32=None, tagp="n", psb=None, hkey="hT"):
    g = P.sb(tagp + "_g", [128, 8, 2]);
    for v, (sh, sci) in enumerate(zip(shift_i, scale_i)):
        P.op("dve", lambda e, v=v, sci=sci: e.scalar_tensor_tensor(g[:, :, v], mod[:, :, sci], 1.0, nw[:, :], ALU.add, ALU.mult),
             reads=["mod", "nw"], writes=[tagp + "_g"])
    sq = [P.sb(f"{tagp}_sq{i}", [128, 8, 512], BF16) for i in range(2)]
    ss = list(psb); ssk = [f"bk_{tagp}0", f"bk_{tagp}1"]
    rs = [P.sb(f"{tagp}_rs{i}", [128, 512]) for i in range(2)]
    tmp = [P.sb(f"{tagp}_tmp{i}", [128, 512]) for i in range(2)]
    ti = 0
    for it, (t0, n) in enumerate(TILES):
        b = it % 2
        v = 1 if it == 0 else 0
        for k in range(8):
            P.op("act", lambda e, k=k, b=b, t0=t0, n=n: e.activation(sq[b][:, k, 0:n], xT[:, k, t0:t0 + n], AF.Square),
                 reads=["xT"], writes=[f"{tagp}_sq{b}"])
        for k in range(8):
            P.op("pe", lambda e, k=k, b=b, n=n: e.matmul(ss[b][:, 0:n], ones_bf[:], sq[b][:, k, 0:n], start=(k == 0), stop=(k == 7)),
                 reads=[f"{tagp}_sq{b}", "ones_bf"], writes=[ssk[b]])
        P.op("dve", lambda e, b=b, n=n: e.tensor_scalar(rs[b][:, 0:n], ss[b][:, 0:n], 1.0 / 1024, 1e-6, ALU.mult, ALU.add),
             reads=[ssk[b]], writes=[f"{tagp}_rs{b}"])
        P.op("dve", lambda e, b=b, n=n: e.reciprocal(rs[b][:, 0:n], rs[b][:, 0:n]),
             reads=[f"{tagp}_rs{b}"], writes=[f"{tagp}_rs{b}"])
        P.op("act", lambda e, b=b, n=n: e.activation(rs[b][:, 0:n], rs[b][:, 0:n], AF.Sqrt),
             reads=[f"{tagp}_rs{b}"], writes=[f"{tagp}_rs{b}"])
        for k in range(8):
            tb = ti % 2; ti += 1
            P.op("dve", lambda e, k=k, b=b, tb=tb, t0=t0, n=n, v=v: e.scalar_tensor_tensor(
                tmp[tb][:, 0:n], xT[:, k, t0:t0 + n], g[:, k, v:v + 1], rs[b][:, 0:n], ALU.mult, ALU.mult),
                 reads=["xT", tagp + "_g", f"{tagp}_rs{b}"], writes=[f"{tagp}_tmp{tb}"])
            sh = shift_i[v]
            P.op("act", lambda e, k=k, tb=tb, t0=t0, n=n, sh=sh: e.activation(
                hT[:, k, t0:t0 + n], tmp[tb][:, 0:n], AF.Identity, bias=mod[:, k, sh:sh + 1]),
                 reads=[f"{tagp}_tmp{tb}", "mod"], writes=[hkey])
            if hT32 is not None:
                P.op("pool", lambda e, k=k, tb=tb, t0=t0, n=n, sh=sh: e.tensor_scalar(
                    hT32[:, k, t0:t0 + n], tmp[tb][:, 0:n], mod[:, k, sh:sh + 1], None, ALU.add),
                     reads=[f"{tagp}_tmp{tb}", "mod"], writes=["hT32"])


def build_A(first=False):
    nc = bass.Bass("TRN2", target_bir_lowering=False)
    xTd = nc.dram_tensor("xT", [128, 8, NTOK], F32, kind="ExternalInput").ap()
    modd = nc.dram_tensor("mod", [128, 8, 6], F32, kind="ExternalInput").ap()
    fTd = nc.dram_tensor("fT", [8, 128, 8, NTOK], F32, kind="ExternalInput").ap() if not first else None
    xoutd = nc.dram_tensor("xout", [128, 8, NTOK], F32, kind="ExternalOutput").ap()
    nwd = nc.dram_tensor("nw", [128, 8], F32, kind="ExternalInput").ap()
    wind = nc.dram_tensor("win", [128, 8, NCT * 128], F32, kind="ExternalInput").ap()
    pTd = nc.dram_tensor("pT", [NCT, 128, NTOK], F32, kind="ExternalOutput").ap()
    P = Prog(nc)
    xT = P.sb("xT", [128, 8, NTOK]); hT = P.sb("hT", [128, 8, NTOK], BF16)
    mod = P.sb("mod", [128, 8, 6]); nw = P.sb("nw", [128, 8])
    wbf = P.sb("wbf", [128, 8, NCT * 128], BF16)
    ones_bf = P.sb("ones_bf", [128, 128], BF16)
    P.op("pool", lambda e: e.memset(ones_bf[:], 1.0), writes=["ones_bf"])
    for k in range(8):
        P.dma("sp", xT[:, k, :], xTd[:, k, :], writes=["xT"])
    P.dma("sp", mod[:], modd, writes=["mod"])
    P.dma("sp", nw[:], nwd, writes=["nw"])
    for k in range(8):
        P.dma("pool", wbf[:, k, :], wind[:, k, :], writes=["wbf"])
    fT = P.sb("fT", [128, 512])
    for c, k in [(c, k) for c in range(0 if first else 8) for k in range(8)]:
        for (t0, n) in TILES:
            P.dma("sp", fT[:, 0:n], fTd[c, :, k, t0:t0 + n], writes=["fT"])
            gcol = 5 if t0 == 0 else 4
            P.I("dve", "scalar_tensor_tensor", xT[:, k, t0:t0 + n], fT[:, 0:n], mod[:, k, gcol:gcol + 1], xT[:, k, t0:t0 + n], ALU.mult, ALU.add,
                r=["fT", "mod", "xT"], w=["xT"])
    for k in range(8):
        P.dma("sp", xoutd[:, k, :], xT[:, k, :], reads=["xT"], is_output=True)
    pp = [P.ps(f"bank{i}", [128, 512]) for i in range(6)]
    rms_modulate(P, xT, hT, mod, nw, ones_bf, shift_i=(0, 2), scale_i=(1, 3), tagp="n1", psb=(pp[4], pp[5]))
    st = [P.sb(f"st{i}", [128, 512]) for i in range(4)]
    i = 0
    for ct in range(NCT):
        for (t0, n) in TILES:
            b = i % 4; i += 1
            for k in range(8):
                P.op("pe", lambda e, k=k, b=b, ct=ct, t0=t0, n=n: e.matmul(
                    pp[b][:, 0:n], wbf[:, k, ct * 128:(ct + 1) * 128], hT[:, k, t0:t0 + n], start=(k == 0), stop=(k == 7)),
                     reads=["wbf", "hT"], writes=[f"bk{b}"])
            if b % 2 == 0:
                P.op("dve", lambda e, b=b, n=n: e.tensor_copy(st[b][:, 0:n], pp[b][:, 0:n]), reads=[f"bk{b}"], writes=[f"st{b}"])
            else:
                P.op("act", lambda e, b=b, n=n: e.activation(st[b][:, 0:n], pp[b][:, 0:n], AF.Copy), reads=[f"bk{b}"], writes=[f"st{b}"])
            P.dma("sp", pTd[ct, :, t0:t0 + n], st[b][:, 0:n], reads=[f"st{b}"], is_output=True)
    P.finish()
    return nc


TSEQ = 4352
QS = 64
NCH = 34
SEGS = [(0, 256), (256, 4352)]


def conv_silu(P, dst, src, cw, cb, ti, key_dst, key_src, tmp, key_tmp, out_dt_tile=None):
    for (s, e_) in SEGS:
        if cb is not None:
            P.op("act", lambda e, s=s, e_=e_: e.activation(tmp[:, s:e_], src[:, s:e_], AF.Identity, bias=cb[:, ti:ti + 1], scale=cw[:, ti, 1:2]),
                 reads=[key_src, "cw", "cb"], writes=[key_tmp])
        else:
            P.op("act", lambda e, s=s, e_=e_: e.activation(tmp[:, s:e_], src[:, s:e_], AF.Copy, scale=cw[:, ti, 1:2]),
                 reads=[key_src, "cw"], writes=[key_tmp])
        P.op("dve", lambda e, s=s, e_=e_: e.scalar_tensor_tensor(tmp[:, s + 1:e_], src[:, s:e_ - 1], cw[:, ti, 0:1], tmp[:, s + 1:e_], ALU.mult, ALU.add),
             reads=[key_src, "cw", key_tmp], writes=[key_tmp])
        P.op("dve", lambda e, s=s, e_=e_: e.scalar_tensor_tensor(tmp[:, s:e_ - 1], src[:, s + 1:e_], cw[:, ti, 2:3], tmp[:, s:e_ - 1], ALU.mult, ALU.add),
             reads=[key_src, "cw", key_tmp], writes=[key_tmp])
    P.op("act", lambda e: e.activation(dst[:, :], tmp[:, :], AF.Silu), reads=[key_tmp], writes=[key_dst])


def load_consts(P, cd):
    c = {}
    for i, nm in enumerate(["tri_f", "tri_b", "nm_f", "nm_b", "ident"]):
        t = P.sb("c_" + nm, [128, 128]); P.dma("sp", t[:], cd[i], writes=["c_" + nm]); c[nm] = t
    ones = P.sb("c_ones", [128, 128]); P.op("pool", lambda e: e.memset(ones[:], 1.0), writes=["c_ones"]); c["ones"] = ones
    idb = P.sb("c_identb", [128, 128], BF16)
    P.op("dve", lambda e: e.tensor_copy(idb[:], c["ident"][:]), reads=["c_ident"], writes=["c_identb"]); c["identb"] = idb
    return c


def host_consts():
    k = np.arange(128)[:, None]; i = np.arange(128)[None, :]
    tri_f = (k <= i).astype(np.float32); tri_b = (k >= i).astype(np.float32)
    nm_f = np.where(i >= k, 0.0, -30000.0).astype(np.float32); nm_b = np.where(i <= k, 0.0, -30000.0).astype(np.float32)
    return np.stack([tri_f, tri_b, nm_f, nm_b, np.eye(128, dtype=np.float32)])


def build_B1(stage=99):
    nc = bass.Bass("TRN2", target_bir_lowering=False)
    D = lambda n, s: nc.dram_tensor(n, s, F32, kind="ExternalInput").ap()
    zTd = D("zT", [128, TSEQ]); xbcd = D("xbcT", [3, 128, TSEQ]); dtrd = D("dtr", [128, 4 * QS])
    dtbd = D("dtb", [128, 4 * QS]); alogd = D("alog", [128, 4 * QS]); cwd = D("cw", [128, 3, 3]); cbd = D("cb", [128, 3])
    dvd = D("dvec", [128, 1]); nwd = D("normw", [128, 1]); cd = D("consts", [5, 128, 128])
    yTd = nc.dram_tensor("yT", [128, TSEQ], F32, kind="ExternalOutput").ap()
    P = Prog(nc)
    C = load_consts(P, cd)
    raw = P.sb("raw", [128, TSEQ]); tmp = P.sb("tmp", [128, TSEQ])
    xT = P.sb("xT", [128, TSEQ]); B32 = P.sb("B32", [128, TSEQ]); C32 = P.sb("C32", [128, TSEQ])
    Bb = P.sb("Bb", [128, TSEQ], BF16); Cb = P.sb("Cb", [128, TSEQ], BF16)
    cw = P.sb("cw", [128, 3, 3]); cb = P.sb("cb", [128, 3]); dvec = P.sb("dvec", [128, 1]); normw = P.sb("normw", [128, 1])
    for t, d, k in ((cw, cwd, "cw"), (cb, cbd, "cb"), (dvec, dvd, "dvec"), (normw, nwd, "normw")):
        P.dma("sp", t[:], d, writes=[k])
    if stage == 0:
        P.op("dve", lambda e: e.tensor_copy(xT[:, 0:128], C["ident"][:]), reads=["c_ident"], writes=["xT"])
        P.op("dve", lambda e: e.tensor_scalar(xT[:, 128:256], C["tri_f"][:], cw[:, 0, 0:1], dvec[:, 0:1], ALU.mult, ALU.add), reads=["c_tri_f", "cw", "dvec"], writes=["xT"])
        P.dma("sp", yTd, xT[:], reads=["xT"], is_output=True); P.finish(); return nc
    import os
    NT = int(os.environ.get("NT", "3"))
    for ti, (dst, kd) in enumerate(((xT, "xT"), (B32, "B32"), (C32, "C32"))[:NT]):
        P.dma("sp", raw[:], xbcd[ti], writes=["raw"])
        conv_silu(P, dst, raw, cw, cb, ti, kd, "raw", tmp, "tmp")
    if NT == 3 and os.environ.get("NOCAST") is None:
        P.op("pool", lambda e: e.tensor_copy(Bb[:], B32[:]), reads=["B32"], writes=["Bb"])
        P.op("pool", lambda e: e.tensor_copy(Cb[:], C32[:]), reads=["C32"], writes=["Cb"])
    if stage == 1:
        P.dma("sp", yTd, xT[:], reads=["xT"], is_output=True); P.finish(); return nc
    dtr = P.sb("dtr", [128, 4 * QS]); dtb = P.sb("dtb", [128, 4 * QS]); alog = P.sb("alog", [128, 4 * QS])
    dt = P.sb("dt", [128, 4 * QS]); la = P.sb("la", [128, 4 * QS]); ncum = P.sb("ncum", [128, 4 * QS])
    wgt = P.sb("wgt", [128, 4 * QS]); dec = P.sb("dec", [128, 4 * QS])
    P.dma("sp", dtr[:], dtrd, writes=["dtr"]); P.dma("sp", dtb[:], dtbd, writes=["dtb"]); P.dma("sp", alog[:], alogd, writes=["alog"])
    P.op("dve", lambda e: e.tensor_tensor(dtr[:], dtr[:], dtb[:], ALU.add), reads=["dtr", "dtb"], writes=["dtr"])
    P.op("dve", lambda e: e.tensor_scalar(dtr[:], dtr[:], 60.0, None, ALU.min), reads=["dtr"], writes=["dtr"])
    P.op("act", lambda e: e.activation(dtr[:], dtr[:], AF.Exp), reads=["dtr"], writes=["dtr"])
    P.op("act", lambda e: e.activation(dt[:], dtr[:], AF.Ln, bias=1.0), reads=["dtr"], writes=["dt"])
    P.op("act", lambda e: e.activation(alog[:], alog[:], AF.Exp), reads=["alog"], writes=["alog"])
    P.op("dve", lambda e: e.scalar_tensor_tensor(la[:], dt[:], -1.0, alog[:], ALU.mult, ALU.mult), reads=["dt", "alog"], writes=["la"])
    bk = [P.ps(f"bank{i}", [128, 512]) for i in range(8)]
    pc = bk[0][:, 0:4 * QS]; pt = bk[1][:, 0:4 * QS]
    P.op("pe", lambda e: e.matmul(pc[:, 0:2 * QS], C["tri_f"][:], la[:, 0:2 * QS], start=True, stop=True), reads=["la", "c_tri_f"], writes=["bk0"])
    P.op("pe", lambda e: e.matmul(pc[:, 2 * QS:4 * QS], C["tri_b"][:], la[:, 2 * QS:4 * QS], start=True, stop=True), reads=["la", "c_tri_b"], writes=["bk0"])
    P.op("pe", lambda e: e.matmul(pt, C["ones"][:], la[:], start=True, stop=True), reads=["la", "c_ones"], writes=["bk1"])
    P.op("dve", lambda e: e.tensor_scalar(ncum[:], pc, -1.0, None, ALU.mult), reads=["bk0"], writes=["ncum"])
    P.op("dve", lambda e: e.tensor_tensor(wgt[:], pt, ncum[:], ALU.add), reads=["bk1", "ncum"], writes=["wgt"])
    P.op("act", lambda e: e.activation(wgt[:], wgt[:], AF.Exp), reads=["wgt"], writes=["wgt"])
    P.op("dve", lambda e: e.tensor_tensor(wgt[:], wgt[:], dt[:], ALU.mult), reads=["wgt", "dt"], writes=["wgt"])
    P.op("act", lambda e: e.activation(dec[:], pt, AF.Exp), reads=["bk1"], writes=["dec"])
    if stage == 2:
        for i_, (t_, k_) in enumerate(((dt, "dt"), (la, "la"), (ncum, "ncum"), (wgt, "wgt"), (dec, "dec"))):
            P.dma("sp", yTd[:, i_ * 256:(i_ + 1) * 256], t_[:], reads=[k_], is_output=True)
        for i_, (t_, k_) in enumerate(((B32, "B32"), (C32, "C32"), (xT, "xT"))):
            P.dma("sp", yTd[:, 1280 + i_ * 1024:1280 + (i_ + 1) * 1024], t_[:, 0:1024], reads=[k_], is_output=True)
        P.finish(); return nc
    xpad = [P.sb(f"xpad{h}", [128, NCH, 128], BF16) for h in range(2)]
    Btok = P.sb("Btok", [128, NCH, 128], BF16); xw = P.sb("xw", [128, NCH, 4, 64], BF16)
    for h in range(2):
        P.op("pool", lambda e, h=h: e.memset(xpad[h][:], 0.0), writes=[f"xpad{h}"])
    ptr = [bk[2][:, 0:128], bk[3][:, 0:128]]
    for c in range(NCH):
        sl = slice(c * 128, (c + 1) * 128)
        P.op("pe", lambda e, sl=sl: e.transpose(ptr[0], xT[:, sl], C["ident"][:]), reads=["xT", "c_ident"], writes=["bk2"])
        P.op("pe", lambda e, sl=sl: e.transpose(ptr[1], B32[:, sl], C["ident"][:]), reads=["B32", "c_ident"], writes=["bk3"])
        for h in range(2):
            P.op("act", lambda e, h=h, c=c: e.activation(xpad[h][:, c, h * 64:(h + 1) * 64], ptr[0][:, h * 64:(h + 1) * 64], AF.Copy),
                 reads=["bk2"], writes=[f"xpad{h}"])
        for q in range(4):
            h = q % 2
            P.op("dve", lambda e, q=q, h=h, c=c: e.tensor_scalar(xw[:, c, q, :], ptr[0][:, h * 64:(h + 1) * 64], wgt[:, q * QS + c:q * QS + c + 1], None, ALU.mult),
                 reads=["bk2", "wgt"], writes=["xw"])
        P.op("act", lambda e, c=c: e.activation(Btok[:, c, :], ptr[1], AF.Copy), reads=["bk3"], writes=["Btok"])
    if stage == 3:
        P.dma("sp", yTd, xT[:], reads=["xT"], is_output=True); P.finish(); return nc
    yacc = P.sb("yacc", [128, TSEQ])
    Hpad = [P.sb(f"Hpad{q}", [128, 128]) for q in range(4)]
    larep = [P.sb(f"larep{i}", [128, 128]) for i in range(2)]
    seg = [P.sb(f"seg{i}", [128, 128]) for i in range(2)]; Et = [P.sb(f"Et{i}", [128, 128]) for i in range(2)]
    STp = [P.sb(f"STp{i}", [128, 128], BF16) for i in range(2)]; CTs = [P.sb(f"CTs{i}", [128, 128]) for i in range(2)]
    psA = bk[0][:, 0:128]; psE = bk[1][:, 0:128]; psS = bk[4][:, 0:128]; psY = bk[5][:, 0:128]
    psH = [bk[6][:, 0:64], bk[7][:, 0:64]]
    import os
    for d in range(int(os.environ.get('ND', '2'))):
        tri = C["tri_f"] if d == 0 else C["tri_b"]; nm = C["nm_f"] if d == 0 else C["nm_b"]
        trik = "c_tri_f" if d == 0 else "c_tri_b"; nmk = "c_nm_f" if d == 0 else "c_nm_b"
        order = list(range(NCH)) if d == 0 else [1, 0] + list(range(NCH - 1, 1, -1))
        for hh in range(2):
            P.op("pool", lambda e, q=d * 2 + hh: e.memset(Hpad[q][:], 0.0), writes=[f"Hpad{d*2+hh}"])
        for c in order:
            sl = slice(c * 128, (c + 1) * 128)
            P.op("pe", lambda e, sl=sl: e.matmul(psS, Bb[:, sl], Cb[:, sl], start=True, stop=True), reads=["Bb", "Cb"], writes=["bk4"])
            for hh in range(2):
                q = d * 2 + hh
                P.op("pool", lambda e, q=q, c=c, hh=hh: e.tensor_scalar(larep[hh][:], C["ones"][:], la[:, q * QS + c:q * QS + c + 1], None, ALU.mult),
                     reads=["c_ones", "la"], writes=[f"larep{hh}"])
                P.op("pe", lambda e, hh=hh, tri=tri: e.matmul(psA, larep[hh][:], tri[:], start=True, stop=False), reads=[f"larep{hh}", trik], writes=["bk0"])
                P.op("pe", lambda e, nm=nm: e.matmul(psA, C["ident"][:], nm[:], start=False, stop=True), reads=["c_ident", nmk], writes=["bk0"])
                P.op("pe", lambda e, hh=hh, tri=tri: e.matmul(psE, larep[hh][:], tri[:], start=True, stop=True), reads=[f"larep{hh}", trik], writes=["bk1"])
                P.op("act", lambda e, q=q, c=c, hh=hh: e.activation(seg[hh][:], psA, AF.Exp, bias=ncum[:, q * QS + c:q * QS + c + 1]), reads=["bk0", "ncum"], writes=[f"seg{hh}"])
                P.op("act", lambda e, hh=hh: e.activation(Et[hh][:], psE, AF.Exp), reads=["bk1"], writes=[f"Et{hh}"])
                P.op("dve", lambda e, q=q, c=c, hh=hh: e.scalar_tensor_tensor(STp[hh][:], psS, dt[:, q * QS + c:q * QS + c + 1], seg[hh][:], ALU.mult, ALU.mult),
                     reads=["bk4", "dt", f"seg{hh}"], writes=[f"STp{hh}"])
                P.op("dve", lambda e, sl=sl, hh=hh: e.tensor_tensor(CTs[hh][:], C32[:, sl], Et[hh][:], ALU.mult), reads=["C32", f"Et{hh}"], writes=[f"CTs{hh}"])
            for hh in range(2):
                q = d * 2 + hh
                P.op("pe", lambda e, hh=hh, c=c: e.matmul(psY, xpad[hh][:, c, :], STp[hh][:], start=(hh == 0), stop=False),
                     reads=[f"xpad{hh}", f"STp{hh}"], writes=["bk5"])
            for hh in range(2):
                q = d * 2 + hh
                P.op("pe", lambda e, hh=hh, q=q: e.matmul(psY, Hpad[q][:], CTs[hh][:], start=False, stop=(hh == 1)),
                     reads=[f"Hpad{q}", f"CTs{hh}"], writes=["bk5"])
            for hh in range(2):
                q = d * 2 + hh
                P.op("pe", lambda e, hh=hh, q=q, c=c: e.matmul(psH[hh], Btok[:, c, :], xw[:, c, q, :], start=True, stop=True),
                     reads=["Btok", "xw"], writes=[f"bk{6+hh}"])
                P.op("dve", lambda e, hh=hh, q=q, c=c: e.scalar_tensor_tensor(
                    Hpad[q][:, hh * 64:(hh + 1) * 64], Hpad[q][:, hh * 64:(hh + 1) * 64], dec[:, q * QS + c:q * QS + c + 1], psH[hh], ALU.mult, ALU.add),
                     reads=[f"Hpad{q}", "dec", f"bk{6+hh}"], writes=[f"Hpad{q}"])
            if d == 0:
                P.op("dve", lambda e, sl=sl: e.scalar_tensor_tensor(yacc[:, sl], xT[:, sl], dvec[:, 0:1], psY, ALU.mult, ALU.add),
                     reads=["xT", "dvec", "bk5"], writes=[f"yacc{c}"])
            else:
                P.op("dve", lambda e, sl=sl: e.tensor_tensor(yacc[:, sl], yacc[:, sl], psY, ALU.add), reads=[f"yacc{c}", "bk5"], writes=[f"yacc{c}"])
    if stage == 4:
        P.dma("sp", yTd, yacc[:], reads=[f"yacc{c}" for c in range(NCH)], is_output=True); P.finish(); return nc
    zT = raw
    P.dma("sp", zT[:], zTd, writes=["raw"])
    P.op("act", lambda e: e.activation(tmp[:], zT[:], AF.Silu), reads=["raw"], writes=["tmp"])
    allc = [f"yacc{c}" for c in range(NCH)]
    P.op("dve", lambda e: e.tensor_tensor(yacc[:], yacc[:], tmp[:], ALU.mult), reads=allc + ["tmp"], writes=allc)
    P.op("act", lambda e: e.activation(tmp[:], yacc[:], AF.Square), reads=allc, writes=["tmp"])
    pss = [bk[2][:, 0:256], bk[3][:, 0:256]]; rs = [P.sb(f"rs{i}", [128, 256]) for i in range(2)]
    for i, t0 in enumerate(range(0, TSEQ, 256)):
        n = min(256, TSEQ - t0); b = i % 2
        P.op("pe", lambda e, b=b, t0=t0, n=n: e.matmul(pss[b][:, 0:n], C["ones"][:], tmp[:, t0:t0 + n], start=True, stop=True), reads=["tmp", "c_ones"], writes=[f"bk{2+b}"])
        P.op("dve", lambda e, b=b, n=n: e.tensor_scalar(rs[b][:, 0:n], pss[b][:, 0:n], 1.0 / 128, 1e-5, ALU.mult, ALU.add), reads=[f"bk{2+b}"], writes=[f"rs{b}"])
        P.op("dve", lambda e, b=b, n=n: e.reciprocal(rs[b][:, 0:n], rs[b][:, 0:n]), reads=[f"rs{b}"], writes=[f"rs{b}"])
        P.op("act", lambda e, b=b, n=n: e.activation(rs[b][:, 0:n], rs[b][:, 0:n], AF.Sqrt), reads=[f"rs{b}"], writes=[f"rs{b}"])
        P.op("dve", lambda e, b=b, t0=t0, n=n: e.scalar_tensor_tensor(xT[:, t0:t0 + n], yacc[:, t0:t0 + n], normw[:, 0:1], rs[b][:, 0:n], ALU.mult, ALU.mult),
             reads=allc + ["normw", f"rs{b}"], writes=["xT"])
    P.dma("sp", yTd, xT[:], reads=["xT"], is_output=True)
    P.finish()
    return nc


def host_B1_inputs(pa, L, b, hp, prm):
    p = pa[b]
    z = p[:, hp * 128:(hp + 1) * 128].T
    x = p[:, 256 + hp * 128:256 + (hp + 1) * 128].T
    Bm = p[:, 512 + hp * 128:512 + (hp + 1) * 128].T
    Cm = p[:, 768 + hp * 128:768 + (hp + 1) * 128].T
    cols = [1024 + d * 4 + 2 * hp + hh for d in range(2) for hh in range(2)]
    dtr = np.zeros((128, 4, QS), np.float32); dtr[:, :, :NCH] = p[:, cols].reshape(NCH, 128, 4).transpose(1, 2, 0); dtr = dtr.reshape(128, 4 * QS)
    bc = lambda v: np.broadcast_to(np.asarray(v, np.float32)[None, :, None], (128, 4, QS)).reshape(128, 4 * QS)
    dtb = bc([prm['m_dt_bias'][L, d, 2 * hp + hh] for d in range(2) for hh in range(2)])
    alog = bc([prm['m_a_log'][L, d, 2 * hp + hh] for d in range(2) for hh in range(2)])
    cwfull = prm['m_conv_w'][L]; cbfull = prm['m_conv_b'][L]
    offs = [hp * 128, 256 + hp * 128, 512 + hp * 128]
    cw = np.stack([cwfull[:, o:o + 128].T for o in offs], 1)
    cb = np.stack([cbfull[o:o + 128] for o in offs], 1)
    dvec = np.repeat(prm['m_d'][L, 2 * hp:2 * hp + 2], 64)[:, None]
    normw = prm['m_norm_w'][L, hp * 128:(hp + 1) * 128][:, None]
    A = np.ascontiguousarray
    return {"zT": A(z), "xbcT": A(np.stack([x, Bm, Cm])), "dtr": A(dtr), "dtb": A(dtb), "alog": A(alog), "cw": A(cw), "cb": A(cb),
            "dvec": A(dvec), "normw": A(normw), "consts": host_consts()}


NPK = 34


def host_consts64():
    k = np.arange(128)[:, None]; i = np.arange(128)[None, :]
    same = (k // 64) == (i // 64)
    f = lambda m: m.astype(np.float32)
    tri_f = f(same & (k <= i)); tri_b = f(same & (k >= i))
    nm_f = np.where(same & (i >= k), 0.0, -30000.0); nm_b = np.where(same & (i <= k), 0.0, -30000.0)
    pms_f = np.where(same & (i < k), 0.0, 30000.0); pms_b = np.where(same & (i > k), 0.0, 30000.0)
    blk = f(same); selA = f(np.broadcast_to(k < 64, (128, 128))); selB = f(np.broadcast_to(k >= 64, (128, 128)))
    inc_f = f(same & (i < k)); inc_b = f(same & (i > k))
    return np.stack([np.eye(128), tri_f, tri_b, nm_f, nm_b, pms_f, pms_b, blk, selA, selB, inc_f, inc_b]).astype(np.float32)

C64_NAMES = ["ident", "tri_f", "tri_b", "nm_f", "nm_b", "pms_f", "pms_b", "blk", "selA", "selB", "sl", "su"]


def load_consts64(P, cd):
    c = {}
    for i, nm in enumerate(C64_NAMES):
        t = P.sb("c_" + nm, [128, 128]); P.dma("sp", t[:], cd[i], writes=["c_" + nm]); c[nm] = t
    ones = P.sb("c_ones", [128, 128]); P.I("pool", "memset", ones[:], 1.0, w=["c_ones"]); c["ones"] = ones
    return c


def tri_inverse_apply(P, C, Lm, X, ncolsX, bk, tg):
    Pt = [P.sb(f"{tg}P{i}", [128, 128]) for i in range(2)] if not hasattr(P, "_tri_" + tg) else getattr(P, "_tri_" + tg)[0]
    Qt = [P.sb(f"{tg}Q{i}", [128, 128]) for i in range(2)] if not hasattr(P, "_tri_" + tg) else getattr(P, "_tri_" + tg)[1]
    setattr(P, "_tri_" + tg, (Pt, Qt))
    (pP, kP), (pQ, kQ), (pT, kT), (pX, kX) = bk["P"], bk["Q"], bk["T"], bk["X"]
    ident = C["ident"]
    P.I("pe", "transpose", pT[:, 0:128], Lm[:], ident[:], r=[tg + "L", "c_ident"], w=[kT])
    P.I("act", "activation", Qt[0][:], pT[:, 0:128], AF.Copy, r=[kT], w=[f"{tg}Q0"])
    P.I("pe", "matmul", pX[:, 0:ncolsX], Qt[0][:], X[:], start=True, stop=True, r=[f"{tg}Q0", tg + "X"], w=[kX])
    P.I("dve", "tensor_tensor", X[:], X[:], pX[:, 0:ncolsX], ALU.subtract, r=[tg + "X", kX], w=[tg + "X"])
    Pc, Pk, Qc, Qk = Lm, tg + "L", Qt[0], f"{tg}Q0"
    for lvl in range(1, 6):
        a = lvl % 2
        P.I("pe", "matmul", pQ[:, 0:128], Pc[:], Qc[:], start=True, stop=True, r=[Pk, Qk], w=[kQ])
        if lvl < 5:
            P.I("pe", "matmul", pP[:, 0:128], Qc[:], Pc[:], start=True, stop=True, r=[Pk, Qk], w=[kP])
            P.I("dve", "tensor_copy", Pt[a][:], pP[:, 0:128], r=[kP], w=[f"{tg}P{a}"])
        P.I("act", "activation", Qt[a][:], pQ[:, 0:128], AF.Copy, r=[kQ], w=[f"{tg}Q{a}"])
        Pc, Pk, Qc, Qk = Pt[a], f"{tg}P{a}", Qt[a], f"{tg}Q{a}"
        P.I("pe", "matmul", pX[:, 0:ncolsX], Qc[:], X[:], start=True, stop=True, r=[Qk, tg + "X"], w=[kX])
        P.I("dve", "tensor_tensor", X[:], X[:], pX[:, 0:ncolsX], ALU.add, r=[tg + "X", kX], w=[tg + "X"])


def build_B2():
    nc = bass.Bass("TRN2", target_bir_lowering=False)
    D = lambda n, s: nc.dram_tensor(n, s, F32, kind="ExternalInput").ap()
    qkvd = D("qkvT", [3, 128, TSEQ]); gated = D("gate", [128, NPK, 128]); tabd = D("tab", [4, 128, 2 * QS])
    cwd = D("cw", [128, 3, 3]); nwd = D("normw", [128, 128]); cd = D("consts", [len(C64_NAMES), 128, 128])
    yd = nc.dram_tensor("y", [128, NPK, 128], F32, kind="ExternalOutput").ap()
    P = Prog(nc)
    C = load_consts64(P, cd)
    bkt = [P.ps(f"bank{i}", [128, 512]) for i in range(8)]
    BK = lambda i: (bkt[i], f"bk{i}")
    raw = P.sb("raw", [128, TSEQ]); tmp = P.sb("tmp", [128, TSEQ])
    qT = P.sb("qT", [128, TSEQ]); kT = P.sb("kT", [128, TSEQ])
    cw = P.sb("cw", [128, 3, 3]); P.dma("sp", cw[:], cwd, writes=["cw"])
    normw = P.sb("normw", [128, 128]); P.dma("sp", normw[:], nwd, writes=["normw"])
    ktok = P.sb("ktok", [128, NPK, 128]); vtok = P.sb("vtok", [128, NPK, 128]); oacc = P.sb("oacc", [128, NPK, 128])
    rs = [P.sb(f"rs{i}", [128, 256]) for i in range(2)]
    for ti, (dst, kd) in enumerate(((qT, "qT"), (kT, "kT"), (raw, "raw"))):
        P.dma("sp", raw[:], qkvd[ti], writes=["raw"])
        conv_silu(P, dst, raw, cw, None, ti, kd, "raw", tmp, "tmp")
        if ti < 2:
            P.I("act", "activation", tmp[:], dst[:], AF.Square, r=[kd], w=["tmp"])
            for i, t0 in enumerate(range(0, TSEQ, 256)):
                b = i % 2; (pa, pk) = BK(b)
                P.I("pe", "matmul", pa[:, 0:256], C["ones"][:], tmp[:, t0:t0 + 256], start=True, stop=True, r=["tmp", "c_ones"], w=[pk])
                P.I("dve", "tensor_scalar", rs[b][:], pa[:, 0:256], 1e-6, None, ALU.add, r=[pk], w=[f"rs{b}"])
                P.I("dve", "reciprocal", rs[b][:], rs[b][:], r=[f"rs{b}"], w=[f"rs{b}"])
                P.I("act", "activation", rs[b][:], rs[b][:], AF.Sqrt, r=[f"rs{b}"], w=[f"rs{b}"])
                sc = 128.0 ** -0.5 if ti == 0 else 1.0
                P.I("dve", "scalar_tensor_tensor", dst[:, t0:t0 + 256], dst[:, t0:t0 + 256], sc, rs[b][:], ALU.mult, ALU.mult,
                    r=[kd, f"rs{b}"], w=[kd])
    vT = raw
    for c in range(NPK):
        sl = slice(c * 128, (c + 1) * 128)
        for src, sk, dst, dk, bi in ((kT, "kT", ktok, "ktok", 0), (vT, "raw", vtok, "vtok", 1)):
            (pa, pk) = BK(bi)
            P.I("pe", "transpose", pa[:, 0:128], src[:, sl], C["ident"][:], r=[sk, "c_ident"], w=[pk])
            P.I("act" if bi else "dve", "activation" if bi else "tensor_copy", dst[:, c, :], pa[:, 0:128], *([AF.Copy] if bi else []), r=[pk], w=[dk])
    W2 = 2 * QS
    tb = {n: P.sb("t_" + n, [128, W2]) for n in ("braw", "araw", "dtb", "alog", "beta", "g", "gc", "ngc", "egc", "toend", "glA", "glB", "bw")}
    for i, n in enumerate(("braw", "araw", "dtb", "alog")):
        P.dma("sp", tb[n][:], tabd[i], writes=["t_" + n])
    P.I("act", "activation", tb["beta"][:], tb["braw"][:], AF.Sigmoid, r=["t_braw"], w=["t_beta"])
    P.I("dve", "tensor_tensor", tb["araw"][:], tb["araw"][:], tb["dtb"][:], ALU.add, r=["t_araw", "t_dtb"], w=["t_araw"])
    P.I("dve", "tensor_scalar", tb["araw"][:], tb["araw"][:], 60.0, None, ALU.min, r=["t_araw"], w=["t_araw"])
    P.I("act", "activation", tb["araw"][:], tb["araw"][:], AF.Exp, r=["t_araw"], w=["t_araw"])
    P.I("act", "activation", tb["araw"][:], tb["araw"][:], AF.Ln, bias=1.0, r=["t_araw"], w=["t_araw"])
    P.I("act", "activation", tb["alog"][:], tb["alog"][:], AF.Exp, r=["t_alog"], w=["t_alog"])
    P.I("dve", "scalar_tensor_tensor", tb["g"][:], tb["araw"][:], -1.0, tb["alog"][:], ALU.mult, ALU.mult, r=["t_araw", "t_alog"], w=["t_g"])
    (p0, k0), (p1, k1), (p2, k2), (p3, k3) = BK(0), BK(1), BK(2), BK(3)
    P.I("pe", "matmul", p0[:, 0:QS], C["tri_f"][:], tb["g"][:, 0:QS], start=True, stop=True, r=["t_g", "c_tri_f"], w=[k0])
    P.I("pe", "matmul", p0[:, QS:W2], C["tri_b"][:], tb["g"][:, QS:W2], start=True, stop=True, r=["t_g", "c_tri_b"], w=[k0])
    P.I("pe", "matmul", p1[:, 0:W2], C["blk"][:], tb["g"][:], start=True, stop=True, r=["t_g", "c_blk"], w=[k1])
    P.I("pe", "matmul", p2[:, 0:W2], C["selA"][:], tb["g"][:], start=True, stop=True, r=["t_g", "c_selA"], w=[k2])
    P.I("pe", "matmul", p3[:, 0:W2], C["selB"][:], tb["g"][:], start=True, stop=True, r=["t_g", "c_selB"], w=[k3])
    P.I("dve", "tensor_copy", tb["gc"][:], p0[:, 0:W2], r=[k0], w=["t_gc"])
    P.I("dve", "tensor_scalar", tb["ngc"][:], tb["gc"][:], -1.0, None, ALU.mult, r=["t_gc"], w=["t_ngc"])
    P.I("act", "activation", tb["egc"][:], tb["gc"][:], AF.Exp, r=["t_gc"], w=["t_egc"])
    P.I("dve", "tensor_tensor", tb["toend"][:], p1[:, 0:W2], tb["gc"][:], ALU.subtract, r=[k1, "t_gc"], w=["t_toend"])
    P.I("act", "activation", tb["toend"][:], tb["toend"][:], AF.Exp, r=["t_toend"], w=["t_toend"])
    P.I("act", "activation", tb["glA"][:], p2[:, 0:W2], AF.Exp, r=[k2], w=["t_glA"])
    P.I("act", "activation", tb["glB"][:], p3[:, 0:W2], AF.Exp, r=[k3], w=["t_glB"])
    P.I("dve", "tensor_tensor", tb["bw"][:], tb["beta"][:], tb["egc"][:], ALU.mult, r=["t_beta", "t_egc"], w=["t_bw"])
    S = P.sb("S", [128, 128]); grep = P.sb("grep", [128, 128])
    DmT = P.sb("DmT", [128, 128]); DmS = P.sb("DmS", [128, 128]); Et = P.sb("Et", [128, 128])
    attnT = P.sb("attnT", [128, 128]); Lm = P.sb("dnL", [128, 128]); qdT = P.sb("qdT", [128, 128])
    X = P.sb("dnX", [128, 256]); kdec = P.sb("kdec", [128, 128]); wT = P.sb("wT", [128, 128]); vnew = P.sb("vnew", [128, 128])
    bkinv = {"P": BK(0), "Q": BK(1), "T": BK(2), "X": BK(3)}
    for d in range(2):
        sfx = "_f" if d == 0 else "_b"
        tri, nm, pms = C["tri" + sfx], C["nm" + sfx], C["pms" + sfx]
        order = list(range(NPK)) if d == 0 else [1, 0] + list(range(NPK - 1, 1, -1))
        P.I("pool", "memset", S[:], 0.0, w=["S"])
        for c in order:
            sl = slice(c * 128, (c + 1) * 128); col = d * QS + c; cs = slice(col, col + 1)
            (pG, kG), (pA, kA), (pD1, kD1), (pD2, kD2), (pE, kE), (pV, kV), (pO, kO), (pS, kS) = [BK(i) for i in range(8)]
            P.I("pe", "matmul", pG[:, 0:128], kT[:, sl], kT[:, sl], start=True, stop=True, r=["kT"], w=[kG])
            P.I("pe", "matmul", pA[:, 0:128], kT[:, sl], qT[:, sl], start=True, stop=True, r=["kT", "qT"], w=[kA])
            P.I("pool", "tensor_scalar", grep[:], C["ones"][:], tb["g"][:, cs], None, ALU.mult, r=["c_ones", "t_g"], w=["grep"])
            P.I("pe", "matmul", pD1[:, 0:128], grep[:], tri[:], start=True, stop=False, r=["grep", "c_tri" + sfx], w=[kD1])
            P.I("pe", "matmul", pD1[:, 0:128], C["ident"][:], nm[:], start=False, stop=True, r=["c_ident", "c_nm" + sfx], w=[kD1])
            P.I("pe", "matmul", pD2[:, 0:128], grep[:], tri[:], start=True, stop=False, r=["grep", "c_tri" + sfx], w=[kD2])
            P.I("pe", "matmul", pD2[:, 0:128], C["ident"][:], pms[:], start=False, stop=True, r=["c_ident", "c_pms" + sfx], w=[kD2])
            P.I("pe", "matmul", pE[:, 0:128], grep[:], tri[:], start=True, stop=True, r=["grep", "c_tri" + sfx], w=[kE])
            P.I("act", "activation", DmT[:], pD1[:, 0:128], AF.Exp, bias=tb["ngc"][:, cs], r=[kD1, "t_ngc"], w=["DmT"])
            P.I("act", "activation", DmS[:], pD2[:, 0:128], AF.Exp, bias=tb["gc"][:, cs], scale=-1.0, r=[kD2, "t_gc"], w=["DmS"])
            P.I("act", "activation", Et[:], pE[:, 0:128], AF.Exp, r=[kE], w=["Et"])
            P.I("dve", "tensor_tensor", attnT[:], pA[:, 0:128], DmT[:], ALU.mult, r=[kA, "DmT"], w=["attnT"])
            P.I("dve", "scalar_tensor_tensor", Lm[:], pG[:, 0:128], tb["beta"][:, cs], DmS[:], ALU.mult, ALU.mult, r=[kG, "t_beta", "DmS"], w=["dnL"])
            P.I("dve", "tensor_tensor", qdT[:], qT[:, sl], Et[:], ALU.mult, r=["qT", "Et"], w=["qdT"])
            P.I("dve", "tensor_scalar", X[:, 0:128], vtok[:, c, :], tb["beta"][:, cs], None, ALU.mult, r=["vtok", "t_beta"], w=["dnX"])
            P.I("dve", "tensor_scalar", X[:, 128:256], ktok[:, c, :], tb["bw"][:, cs], None, ALU.mult, r=["ktok", "t_bw"], w=["dnX"])
            P.I("pool", "tensor_scalar", kdec[:], ktok[:, c, :], tb["toend"][:, cs], None, ALU.mult, r=["ktok", "t_toend"], w=["kdec"])
            tri_inverse_apply(P, C, Lm, X, 256, bkinv, "dn")
            P.I("pe", "transpose", pD1[:, 0:128], X[:, 128:256], C["ident"][:], r=["dnX", "c_ident"], w=[kD1])
            P.I("act", "activation", wT[:], pD1[:, 0:128], AF.Copy, r=[kD1], w=["wT"])
            for half in ((0, 1) if d == 0 else (1, 0)):
                rows = slice(half * 64, (half + 1) * 64)
                gl = tb["glA"] if half == 0 else tb["glB"]; glk = "t_glA" if half == 0 else "t_glB"
                P.I("pe", "matmul", pV[:, 0:128], wT[:], S[:], start=True, stop=True, r=["wT", "S"], w=[kV])
                P.I("dve", "tensor_tensor", vnew[rows, :], X[rows, 0:128], pV[rows, 0:128], ALU.subtract, r=["dnX", kV], w=["vnew"])
                P.I("pe", "matmul", pO[:, 0:128], qdT[:], S[:], start=True, stop=False, r=["qdT", "S"], w=[kO])
                P.I("pe", "matmul", pO[:, 0:128], attnT[rows, :], vnew[rows, :], start=False, stop=True, r=["attnT", "vnew"], w=[kO])
                if d == 0:
                    P.I("act", "activation", oacc[rows, c, :], pO[rows, 0:128], AF.Copy, r=[kO], w=[f"oacc{c}"])
                else:
                    P.I("dve", "tensor_tensor", oacc[rows, c, :], oacc[rows, c, :], pO[rows, 0:128], ALU.add, r=[kO, f"oacc{c}"], w=[f"oacc{c}"])
                P.I("pe", "matmul", pS[:, 0:128], kdec[rows, :], vnew[rows, :], start=True, stop=True, r=["kdec", "vnew"], w=[kS])
                P.I("dve", "scalar_tensor_tensor", S[:], S[:], gl[:, cs], pS[:, 0:128], ALU.mult, ALU.add, r=["S", glk, kS], w=["S"])
    allo = [f"oacc{c}" for c in range(NPK)]
    gate = P.sb("gate", [128, NPK, 128]); sq = P.sb("sq", [128, NPK, 128]); ss = P.sb("ss", [128, NPK])
    P.dma("sp", gate[:], gated, writes=["gate"])
    P.I("act", "activation", gate[:], gate[:], AF.Silu, r=["gate"], w=["gate"])
    P.I("act", "activation", sq[:], oacc[:], AF.Square, r=allo, w=["sq"])
    P.I("dve", "tensor_reduce", ss[:], sq[:], AX.X, ALU.add, r=["sq"], w=["ss"])
    P.I("dve", "tensor_scalar", ss[:], ss[:], 1.0 / 128, 1e-6, ALU.mult, ALU.add, r=["ss"], w=["ss"])
    P.I("dve", "reciprocal", ss[:], ss[:], r=["ss"], w=["ss"])
    P.I("act", "activation", ss[:], ss[:], AF.Sqrt, r=["ss"], w=["ss"])
    for c in range(NPK):
        P.I("dve", "scalar_tensor_tensor", sq[:, c, :], oacc[:, c, :], ss[:, c:c + 1], normw[:], ALU.mult, ALU.mult, r=allo + ["ss", "normw"], w=["sq"])
    P.I("dve", "tensor_tensor", sq[:], sq[:], gate[:], ALU.mult, r=["sq", "gate"], w=["sq"])
    P.dma("sp", yd, sq[:], reads=["sq"], is_output=True)
    P.finish()
    return nc


def colmajor_perm():
    t = np.arange(4096).reshape(64, 64)
    return t.T.reshape(-1)


def host_B2_inputs(pb, L, b, head, prm):
    perm = np.concatenate([np.arange(256), 256 + colmajor_perm()])
    p = pb[b][perm]
    q = p[:, head * 128:(head + 1) * 128].T; k = p[:, 512 + head * 128:512 + (head + 1) * 128].T
    v = p[:, 1024 + head * 128:1024 + (head + 1) * 128].T
    gate = p[:, 1536 + head * 128:1536 + (head + 1) * 128].reshape(NPK, 128, 128).transpose(1, 0, 2)
    def tabl(cols):
        t = np.zeros((128, 2, QS), np.float32); t[:, :, :NPK] = p[:, cols].reshape(NPK, 128, 2).transpose(1, 2, 0); return t.reshape(128, 2 * QS)
    braw = tabl([2048 + d * 4 + head for d in range(2)]); araw = tabl([2056 + d * 4 + head for d in range(2)])
    bc = lambda v_: np.broadcast_to(np.asarray(v_, np.float32)[None, :, None], (128, 2, QS)).reshape(128, 2 * QS)
    dtb = bc(prm['dn_dt_bias'][L, :, head]); alog = bc(prm['dn_a_log'][L, :, head])
    cwf = prm['dn_conv_w'][L]
    cw = np.stack([cwf[:, o + head * 128:o + (head + 1) * 128].T for o in (0, 512, 1024)], 1)
    normw = np.broadcast_to(prm['dn_norm_w'][L][None, :], (128, 128))
    A = lambda a: np.ascontiguousarray(a, dtype=np.float32)
    return {"qkvT": A(np.stack([q, k, v])), "gate": A(gate), "tab": A(np.stack([braw, araw, dtb, alog])), "cw": A(cw), "normw": A(normw),
            "consts": host_consts64()}


def host_B2_output(y):
    yy = y.transpose(1, 0, 2).reshape(TSEQ, 128)
    out = np.empty_like(yy)
    perm = np.concatenate([np.arange(256), 256 + colmajor_perm()])
    out[perm] = yy
    return out


def build_B3():
    nc = bass.Bass("TRN2", target_bir_lowering=False)
    D = lambda n, s: nc.dram_tensor(n, s, F32, kind="ExternalInput").ap()
    p64d = D("p64", [4, 64, TSEQ]); p128d = D("p128", [2, 128, TSEQ]); mu64d = D("mu64", [4, 64, 8]); mu128d = D("mu128", [2, 128, 8])
    pvd = D("pv", [64, 8]); a2d = D("a2h", [64, 64]); g2d = D("g2h", [128, 64]); w2d = D("w2pad", [2, 128, 64])
    cd = D("consts", [len(C64_NAMES), 128, 128])
    yd = nc.dram_tensor("y", [64, TSEQ], F32, kind="ExternalOutput").ap()
    P = Prog(nc)
    C = load_consts64(P, cd)
    bkt = [P.ps(f"bank{i}", [128, 512]) for i in range(8)]
    BK = lambda i: (bkt[i], f"bk{i}")
    raw = P.sb("raw", [128, TSEQ]); mix = P.sb("mix", [128, TSEQ])
    mu64 = P.sb("mu64", [64, 4, 8]); mu128 = P.sb("mu128", [128, 2, 8]); pv = P.sb("pv", [64, 8])
    for i in range(4):
        P.dma("sp", mu64[:, i, :], mu64d[i], writes=["mu64"])
    for i in range(2):
        P.dma("sp", mu128[:, i, :], mu128d[i], writes=["mu128"])
    P.dma("sp", pv[:], pvd, writes=["pv"])
    a2h = P.sb("a2h", [64, 64]); g2h = P.sb("g2h", [128, 64]); w2p = P.sb("w2p", [128, 2, 64])
    P.dma("sp", a2h[:], a2d, writes=["a2h"]); P.dma("sp", g2h[:], g2d, writes=["g2h"])
    for j in range(2):
        P.dma("sp", w2p[:, j, :], w2d[j], writes=["w2p"])
    omm64 = P.sb("omm64", [64, 4]); omm128 = P.sb("omm128", [128, 2])
    P.I("dve", "tensor_scalar", omm64[:], mu64[:, :, 0], -1.0, 1.0, ALU.mult, ALU.add, r=["mu64"], w=["omm64"])
    P.I("dve", "tensor_scalar", omm128[:], mu128[:, :, 0], -1.0, 1.0, ALU.mult, ALU.add, r=["mu128"], w=["omm128"])

    def token_mix(dst, dk, src_d, npart, mu, muk, omm, ommk, ti):
        R = slice(0, npart)
        P.dma("sp", raw[R, :], src_d, writes=["raw"])
        P.I("dve", "tensor_scalar", dst[R, :], raw[R, :], omm[R, ti:ti + 1], None, ALU.mult, r=["raw", ommk], w=[dk])
        def acc(o0, o1, i0, i1, mcol, eng="dve"):
            P.I(eng, "scalar_tensor_tensor", dst[R, o0:o1], raw[R, i0:i1], mu[R, ti, mcol:mcol + 1], dst[R, o0:o1], ALU.mult, ALU.add,
                r=["raw", muk, dk], w=[dk])
        acc(1, 256, 0, 255, 5); acc(0, 255, 1, 256, 6)
        acc(256 + 64, TSEQ, 256, TSEQ - 64, 3); acc(256, TSEQ - 64, 256 + 64, TSEQ, 4)
        dl = dst[R, 256:TSEQ].rearrange("p (r c) -> p r c", c=64); rl = raw[R, 256:TSEQ].rearrange("p (r c) -> p r c", c=64)
        P.I("dve", "scalar_tensor_tensor", dl[:, :, 1:64], rl[:, :, 0:63], mu[R, ti, 1:2], dl[:, :, 1:64], ALU.mult, ALU.add, r=["raw", muk, dk], w=[dk])
        P.I("dve", "scalar_tensor_tensor", dl[:, :, 0:63], rl[:, :, 1:64], mu[R, ti, 2:3], dl[:, :, 0:63], ALU.mult, ALU.add, r=["raw", muk, dk], w=[dk])

    rT = P.sb("rT", [64, TSEQ]); kT = P.sb("kT", [64, TSEQ]); vT = P.sb("vT", [64, TSEQ]); aT = P.sb("aT", [64, TSEQ])
    gT = P.sb("gT", [64, TSEQ]); bT = P.sb("bT", [64, TSEQ]); lwT = [P.sb(f"lwT{j}", [64, TSEQ]) for j in range(2)]
    token_mix(rT, "rT", p64d[0], 64, mu64, "mu64", omm64, "omm64", 0)
    token_mix(kT, "kT", p64d[1], 64, mu64, "mu64", omm64, "omm64", 1)
    token_mix(vT, "vT", p64d[2], 64, mu64, "mu64", omm64, "omm64", 2)
    NB = 256
    token_mix(mix, "mix", p64d[3], 64, mu64, "mu64", omm64, "omm64", 3)
    for i, t0 in enumerate(range(0, TSEQ, NB)):
        (pa, pk) = BK(i % 2)
        P.I("pe", "matmul", pa[0:64, 0:NB], a2h[:], mix[0:64, t0:t0 + NB], start=True, stop=True, r=["a2h", "mix"], w=[pk])
        P.I("act", "activation", aT[:, t0:t0 + NB], pa[0:64, 0:NB], AF.Sigmoid, bias=pv[:, 0:1], r=[pk, "pv"], w=["aT"])
    token_mix(mix, "mix", p128d[0], 128, mu128, "mu128", omm128, "omm128", 0)
    P.I("act", "activation", mix[:], mix[:], AF.Tanh, r=["mix"], w=["mix"])
    for j in range(2):
        for i, t0 in enumerate(range(0, TSEQ, NB)):
            (pa, pk) = BK(i % 2)
            P.I("pe", "matmul", pa[0:64, 0:NB], w2p[:, j, :], mix[:, t0:t0 + NB], start=True, stop=True, r=["w2p", "mix"], w=[pk])
            P.I("act", "activation", lwT[j][:, t0:t0 + NB], pa[0:64, 0:NB], AF.Sigmoid, bias=pv[:, 3 + j:4 + j], r=[pk, "pv"], w=[f"lwT{j}"])
        P.I("dve", "tensor_scalar", lwT[j][:], lwT[j][:], -float(np.exp(-0.5)), None, ALU.mult, r=[f"lwT{j}"], w=[f"lwT{j}"])
    token_mix(mix, "mix", p128d[1], 128, mu128, "mu128", omm128, "omm128", 1)
    P.I("act", "activation", mix[:], mix[:], AF.Sigmoid, r=["mix"], w=["mix"])
    for i, t0 in enumerate(range(0, TSEQ, NB)):
        (pa, pk) = BK(i % 2)
        P.I("pe", "matmul", pa[0:64, 0:NB], g2h[:], mix[:, t0:t0 + NB], start=True, stop=True, r=["g2h", "mix"], w=[pk])
        P.I("act", "activation", gT[:, t0:t0 + NB], pa[0:64, 0:NB], AF.Copy, r=[pk], w=["gT"])
    kk = mix
    P.I("dve", "tensor_scalar", kk[0:64, :], kT[:], pv[:, 1:2], None, ALU.mult, r=["kT", "pv"], w=["mix"])
    P.I("act", "activation", raw[0:64, :], kk[0:64, :], AF.Square, r=["mix"], w=["raw"])
    rs = [P.sb(f"rs{i}", [64, NB]) for i in range(2)]
    for i, t0 in enumerate(range(0, TSEQ, NB)):
        b = i % 2; (pa, pk) = BK(b)
        P.I("pe", "matmul", pa[0:64, 0:NB], C["ones"][0:64, 0:64], raw[0:64, t0:t0 + NB], start=True, stop=True, r=["raw", "c_ones"], w=[pk])
        P.I("dve", "tensor_scalar", rs[b][:], pa[0:64, 0:NB], 1e-6, None, ALU.add, r=[pk], w=[f"rs{b}"])
        P.I("dve", "reciprocal", rs[b][:], rs[b][:], r=[f"rs{b}"], w=[f"rs{b}"])
        P.I("act", "activation", rs[b][:], rs[b][:], AF.Sqrt, r=[f"rs{b}"], w=[f"rs{b}"])
        P.I("dve", "tensor_tensor", kk[0:64, t0:t0 + NB], kk[0:64, t0:t0 + NB], rs[b][:], ALU.mult, r=["mix", f"rs{b}"], w=["mix"])
    P.I("dve", "tensor_tensor", bT[:], kk[0:64, :], aT[:], ALU.mult, r=["mix", "aT"], w=["bT"])
    P.I("dve", "tensor_scalar", kk[0:64, :], kk[0:64, :], -1.0, None, ALU.mult, r=["mix"], w=["mix"])
    P.I("dve", "tensor_scalar", aT[:], aT[:], -1.0, pv[:, 2:3], ALU.add, ALU.mult, r=["aT", "pv"], w=["aT"])
    P.I("dve", "scalar_tensor_tensor", kT[:], aT[:], 1.0, kT[:], ALU.add, ALU.mult, r=["aT", "kT"], w=["kT"])
    avT = kk
    oacc = P.sb("oacc", [128, NPK, 64])
    H = P.sb("H", [64, 64])
    T_ = lambda n, sh: P.sb(n, sh)
    lwtok = T_("lwtok", [128, 64]); ea_tok = T_("ea_tok", [128, 64]); te_tok = T_("te_tok", [128, 64])
    ep = T_("ep", [64, 128]); em = T_("em", [64, 128]); eaT = T_("eaT", [64, 128])
    atl = T_("atl", [64, 128]); btl = T_("btl", [64, 128]); ktl = T_("ktl", [64, 128]); rtl = T_("rtl", [64, 128])
    Lm = T_("rwL", [128, 128]); AakT = T_("AakT", [128, 128]); ArbT = T_("ArbT", [128, 128]); ArkT = T_("ArkT", [128, 128])
    X = T_("rwX", [128, 128]); Bh = T_("Bh", [128, 64]); Kh = T_("Kh", [128, 64]); W1T = T_("W1T", [64, 128]); U = T_("U", [128, 64])
    pc2 = T_("pc2", [64, 2])
    tk = {n: T_(n + "_t", [128, 64]) for n in ("av", "b", "k", "v")}
    bkinv = {"P": BK(0), "Q": BK(1), "T": BK(2), "X": BK(3)}
    for d in range(2):
        sfx = "_f" if d == 0 else "_b"
        tri = C["tri" + sfx]; trik = "c_tri" + sfx
        m_strict_ts = C["sl"] if d == 0 else C["su"]; mk_ts = "c_sl" if d == 0 else "c_su"
        m_strict_st = C["su"] if d == 0 else C["sl"]; mk_st = "c_su" if d == 0 else "c_sl"
        m_incl_st = C["tri_f"] if d == 0 else C["tri_b"]; mk_in = trik
        order = list(range(NPK)) if d == 0 else [1, 0] + list(range(NPK - 1, 1, -1))
        P.I("pool", "memset", H[:], 0.0, w=["H"])
        for c in order:
            sl = slice(c * 128, (c + 1) * 128)
            (p0, k0), (p1, k1), (p2, k2), (p3, k3), (p4, k4), (p5, k5), (p6, k6), (p7, k7) = [BK(i) for i in range(8)]
            lw = lwT[d]; lwk = f"lwT{d}"
            for ii, (n, src, sk) in enumerate((("av", avT, "mix"), ("b", bT, "bT"), ("k", kT, "kT"), ("v", vT, "vT"))):
                (pa, pk) = BK(4 + ii)
                P.I("pe", "transpose", pa[:, 0:64], src[0:64, sl], C["ident"][0:64, 0:64], r=[sk, "c_ident"], w=[pk])
                if ii % 2:
                    P.I("act", "activation", tk[n][:], pa[:, 0:64], AF.Copy, r=[pk], w=[n + "_t"])
                else:
                    P.I("dve", "tensor_copy", tk[n][:], pa[:, 0:64], r=[pk], w=[n + "_t"])
            P.I("pe", "transpose", p0[:, 0:64], lw[:, sl], C["ident"][0:64, 0:64], r=[lwk, "c_ident"], w=[k0])
            P.I("dve", "tensor_copy", lwtok[:], p0[:, 0:64], r=[k0], w=["lwtok"])
            P.I("pe", "matmul", p1[:, 0:64], tri[:], lwtok[:], start=True, stop=True, r=[trik, "lwtok"], w=[k1])
            P.I("pe", "matmul", p2[:, 0:64], C["blk"][:], lwtok[:], start=True, stop=True, r=["c_blk", "lwtok"], w=[k2])
            P.I("pe", "matmul", p3[0:64, 0:128], lwtok[:], tri[:], start=True, stop=True, r=[trik, "lwtok"], w=[k3])
            P.I("dve", "tensor_tensor", ea_tok[:], p1[:, 0:64], lwtok[:], ALU.subtract, r=[k1, "lwtok"], w=["ea_tok"])
            P.I("act", "activation", ea_tok[:], ea_tok[:], AF.Exp, r=["ea_tok"], w=["ea_tok"])
            P.I("dve", "tensor_copy", te_tok[:], p1[:, 0:64], r=[k1], w=["te_tok"])
            P.I("dve", "tensor_tensor", te_tok[:], p2[:, 0:64], te_tok[:], ALU.subtract, r=[k2, "te_tok"], w=["te_tok"])
            P.I("act", "activation", te_tok[:], te_tok[:], AF.Exp, r=["te_tok"], w=["te_tok"])
            P.I("act", "activation", ep[:], p3[0:64, 0:128], AF.Exp, r=[k3], w=["ep"])
            P.I("act", "activation", em[:], p3[0:64, 0:128], AF.Exp, scale=-1.0, r=[k3], w=["em"])
            P.I("dve", "tensor_tensor", eaT[:], p3[0:64, 0:128], lw[:, sl], ALU.subtract, r=[k3, lwk], w=["eaT"])
            P.I("act", "activation", eaT[:], eaT[:], AF.Exp, r=["eaT"], w=["eaT"])
            cA, cB = (63, 127) if d == 0 else (0, 64)
            P.I("act", "activation", pc2[:, 0:1], p3[0:64, cA:cA + 1], AF.Exp, r=[k3], w=["pc2"])
            P.I("act", "activation", pc2[:, 1:2], p3[0:64, cB:cB + 1], AF.Exp, r=[k3], w=["pc2"])
            P.I("dve", "tensor_tensor", atl[:], avT[0:64, sl], eaT[:], ALU.mult, r=["mix", "eaT"], w=["atl"])
            P.I("dve", "tensor_tensor", btl[:], bT[:, sl], em[:], ALU.mult, r=["bT", "em"], w=["btl"])
            P.I("pool", "tensor_tensor", ktl[:], kT[:, sl], em[:], ALU.mult, r=["kT", "em"], w=["ktl"])
            P.I("pool", "tensor_tensor", rtl[:], rT[:, sl], ep[:], ALU.mult, r=["rT", "ep"], w=["rtl"])
            P.I("pe", "matmul", p4[:, 0:128], atl[:], btl[:], start=True, stop=True, r=["atl", "btl"], w=[k4])
            P.I("pe", "matmul", p5[:, 0:128], ktl[:], atl[:], start=True, stop=True, r=["ktl", "atl"], w=[k5])
            P.I("pe", "matmul", p6[:, 0:128], btl[:], rtl[:], start=True, stop=True, r=["btl", "rtl"], w=[k6])
            P.I("pe", "matmul", p7[:, 0:128], ktl[:], rtl[:], start=True, stop=True, r=["ktl", "rtl"], w=[k7])
            P.I("dve", "scalar_tensor_tensor", Lm[:], p4[:, 0:128], -1.0, m_strict_ts[:], ALU.mult, ALU.mult, r=[k4, mk_ts], w=["rwL"])
            P.I("dve", "tensor_tensor", AakT[:], p5[:, 0:128], m_strict_st[:], ALU.mult, r=[k5, mk_st], w=["AakT"])
            P.I("dve", "tensor_tensor", ArbT[:], p6[:, 0:128], m_incl_st[:], ALU.mult, r=[k6, mk_in], w=["ArbT"])
            P.I("dve", "tensor_tensor", ArkT[:], p7[:, 0:128], m_incl_st[:], ALU.mult, r=[k7, mk_in], w=["ArkT"])
            P.I("dve", "tensor_tensor", X[:, 0:64], tk["av"][:], ea_tok[:], ALU.mult, r=["av_t", "ea_tok"], w=["rwX"])
            P.I("pe", "matmul", p4[:, 0:64], AakT[:], tk["v"][:], start=True, stop=True, r=["AakT", "v_t"], w=[k4])
            P.I("act", "activation", X[:, 64:128], p4[:, 0:64], AF.Copy, r=[k4], w=["rwX"])
            P.I("pool", "tensor_tensor", Bh[:], tk["b"][:], te_tok[:], ALU.mult, r=["b_t", "te_tok"], w=["Bh"])
            P.I("pool", "tensor_tensor", Kh[:], tk["k"][:], te_tok[:], ALU.mult, r=["k_t", "te_tok"], w=["Kh"])
            tri_inverse_apply(P, C, Lm, X, 128, bkinv, "rw")
            P.I("pe", "transpose", p2[0:64, 0:128], X[:, 0:64], C["ident"][:], r=["rwX", "c_ident"], w=[k2])
            P.I("act", "activation", W1T[:], p2[0:64, 0:128], AF.Copy, r=[k2], w=["W1T"])
            for half in ((0, 1) if d == 0 else (1, 0)):
                rows = slice(half * 64, (half + 1) * 64)
                P.I("pe", "matmul", p5[:, 0:64], W1T[:], H[:], start=True, stop=True, r=["W1T", "H"], w=[k5])
                P.I("dve", "tensor_tensor", U[rows, :], X[rows, 64:128], p5[rows, 0:64], ALU.add, r=["rwX", k5], w=["U"])
                P.I("pe", "matmul", p6[:, 0:64], rtl[:], H[:], start=True, stop=False, r=["rtl", "H"], w=[k6])
                P.I("pe", "matmul", p6[:, 0:64], ArbT[rows, :], U[rows, :], start=False, stop=False, r=["ArbT", "U"], w=[k6])
                P.I("pe", "matmul", p6[:, 0:64], ArkT[rows, :], tk["v"][rows, :], start=False, stop=True, r=["ArkT", "v_t"], w=[k6])
                if d == 0:
                    P.I("act", "activation", oacc[rows, c, :], p6[rows, 0:64], AF.Copy, r=[k6], w=[f"oacc{c}"])
                else:
                    P.I("dve", "tensor_tensor", oacc[rows, c, :], oacc[rows, c, :], p6[rows, 0:64], ALU.add, r=[k6, f"oacc{c}"], w=[f"oacc{c}"])
                P.I("pe", "matmul", p7[0:64, 0:64], Bh[rows, :], U[rows, :], start=True, stop=False, r=["Bh", "U"], w=[k7])
                P.I("pe", "matmul", p7[0:64, 0:64], Kh[rows, :], tk["v"][rows, :], start=False, stop=True, r=["Kh", "v_t"], w=[k7])
                P.I("dve", "scalar_tensor_tensor", H[:], H[:], pc2[:, half:half + 1], p7[0:64, 0:64], ALU.mult, ALU.add, r=["H", "pc2", k7], w=["H"])
    allo = [f"oacc{c}" for c in range(NPK)]
    yT = raw; t1 = aT; t2 = bT
    for c in range(NPK):
        (pa, pk) = BK(c % 4)
        P.I("pe", "transpose", pa[0:64, 0:128], oacc[:, c, :], C["ident"][:], r=allo + ["c_ident"], w=[pk])
        P.I("act" if c % 2 else "dve", "activation" if c % 2 else "tensor_copy", yT[0:64, c * 128:(c + 1) * 128], pa[0:64, 0:128], *([AF.Copy] if c % 2 else []),
            r=[pk], w=["raw"])
    on64 = C["ones"][0:64, 0:64]
    for i, t0 in enumerate(range(0, TSEQ, NB)):
        ts = slice(t0, t0 + NB); b = i % 2
        (pm, km), (pvv, kvv), (pb, kb) = BK(b * 3), BK(b * 3 + 1), BK(b * 3 + 2)
        P.I("pe", "matmul", pm[0:64, 0:NB], on64, yT[0:64, ts], start=True, stop=True, r=["raw", "c_ones"], w=[km])
        P.I("dve", "scalar_tensor_tensor", yT[0:64, ts], pm[0:64, 0:NB], -1.0 / 64, yT[0:64, ts], ALU.mult, ALU.add, r=[km, "raw"], w=["raw"])
        P.I("act", "activation", t1[:, ts], yT[0:64, ts], AF.Square, r=["raw"], w=["aT"])
        P.I("pe", "matmul", pvv[0:64, 0:NB], on64, t1[:, ts], start=True, stop=True, r=["aT", "c_ones"], w=[kvv])
        P.I("dve", "tensor_scalar", rs[b][:], pvv[0:64, 0:NB], 1.0 / 64, 64e-5, ALU.mult, ALU.add, r=[kvv], w=[f"rs{b}"])
        P.I("dve", "reciprocal", rs[b][:], rs[b][:], r=[f"rs{b}"], w=[f"rs{b}"])
        P.I("act", "activation", rs[b][:], rs[b][:], AF.Sqrt, r=[f"rs{b}"], w=[f"rs{b}"])
        P.I("dve", "scalar_tensor_tensor", yT[0:64, ts], yT[0:64, ts], pv[:, 5:6], rs[b][:], ALU.mult, ALU.mult, r=["raw", "pv", f"rs{b}"], w=["raw"])
        P.I("dve", "scalar_tensor_tensor", t2[:, ts], rT[:, ts], pv[:, 7:8], kT[:, ts], ALU.mult, ALU.mult, r=["rT", "pv", "kT"], w=["bT"])
        P.I("pe", "matmul", pb[0:64, 0:NB], on64, t2[:, ts], start=True, stop=True, r=["bT", "c_ones"], w=[kb])
        P.I("dve", "tensor_tensor", t2[:, ts], pb[0:64, 0:NB], vT[:, ts], ALU.mult, r=[kb, "vT"], w=["bT"])
        P.I("dve", "scalar_tensor_tensor", yT[0:64, ts], yT[0:64, ts], pv[:, 6:7], t2[:, ts], ALU.add, ALU.add, r=["raw", "pv", "bT"], w=["raw"])
        P.I("dve", "tensor_tensor", yT[0:64, ts], yT[0:64, ts], gT[:, ts], ALU.mult, r=["raw", "gT"], w=["raw"])
    P.dma("sp", yd, yT[0:64, :], reads=["raw"], is_output=True)
    P.finish()
    return nc


def host_B3_inputs(pc_, L, b, head, prm):
    p = pc_[b]
    hc = slice(head * 64, (head + 1) * 64)
    mu = prm['rw_mu'][L]
    def seg(o, n):
        cols = np.arange(o, o + n); m = mu[cols]; cl = cols % 4
        tab = np.zeros((n, 8), np.float32)
        tab[:, 0] = m
        for j in range(4):
            tab[:, 1 + j] = np.where(cl == j, m, 0.0)
        tab[:, 5] = np.where(cl % 2 == 0, m, 0.0); tab[:, 6] = np.where(cl % 2 == 1, m, 0.0)
        return p[:, cols].T, tab
    s64 = [seg(head * 64, 64), seg(256 + head * 64, 64), seg(512 + head * 64, 64), seg(896, 64)]
    s128 = [seg(768, 128), seg(960, 128)]
    pv = np.zeros((64, 8), np.float32)
    pv[:, 0] = prm['rw_a0'][L][hc]; pv[:, 1] = prm['rw_k_k'][L][hc]; pv[:, 2] = prm['rw_k_a'][L][hc]
    pv[:, 3] = prm['rw_w0'][L][0][hc]; pv[:, 4] = prm['rw_w0'][L][1][hc]
    pv[:, 5] = prm['rw_ln_w'][L][hc]; pv[:, 6] = prm['rw_ln_b'][L][hc]; pv[:, 7] = prm['rw_r_k'][L][head]
    w2pad = np.zeros((2, 128, 64), np.float32)
    for j in range(2):
        w2pad[j, j * 64:(j + 1) * 64] = prm['rw_w2'][L][j][:, hc]
    A = lambda a: np.ascontiguousarray(a, dtype=np.float32)
    return {"p64": A(np.stack([s[0] for s in s64])), "p128": A(np.stack([s[0] for s in s128])),
            "mu64": A(np.stack([s[1] for s in s64])), "mu128": A(np.stack([s[1] for s in s128])),
            "pv": pv, "a2h": A(prm['rw_a2'][L][:, hc]), "g2h": A(prm['rw_g2'][L][:, hc]), "w2pad": w2pad,
            "consts": host_consts64()}


NTT = 17


def build_C1():
    nc = bass.Bass("TRN2", target_bir_lowering=False)
    D = lambda n, s: nc.dram_tensor(n, s, F32, kind="ExternalInput").ap()
    xTd = D("xT", [128, 8, NTOK]); yTd = D("yT", [128, 8, NTOK]); woutd = D("wout", [128, 8, 1024])
    modd = D("mod", [128, 8, 6]); nwd = D("nw", [128, 8]); wrd = D("wr", [128, 8, 32]); brd = D("br", [128, 32])
    O = lambda n, s: nc.dram_tensor(n, s, F32, kind="ExternalOutput").ap()
    xmd = O("xmT", [128, 8, NTOK]); h2d = O("h2T", [128, 8, NTOK]); Gd = O("G", [128, NTT, 32])
    P = Prog(nc)
    xT = P.sb("xT", [128, 8, NTOK]); ybf = P.sb("ybf", [128, 8, NTOK], BF16)
    hT32 = xT
    mod = P.sb("mod", [128, 8, 6]); nw = P.sb("nw", [128, 8]); wbf = P.sb("wbf", [128, 8, 1024], BF16)
    wr = P.sb("wr", [128, 8, 32]); br = P.sb("br", [128, 32])
    ones_bf = P.sb("ones_bf", [128, 128], BF16)
    P.I("pool", "memset", ones_bf[:], 1.0, w=["ones_bf"])
    for k in range(8):
        P.dma("sp", xT[:, k, :], xTd[:, k, :], writes=["xT"])
        P.dma("pool", ybf[:, k, :], yTd[:, k, :], writes=["ybf"])
        P.dma("pool", wbf[:, k, :], woutd[:, k, :], writes=["wbf"])
    for t, d, kk in ((mod, modd, "mod"), (nw, nwd, "nw"), (wr, wrd, "wr"), (br, brd, "br")):
        P.dma("sp", t[:], d, writes=[kk])
    pp = [P.ps(f"bank{i}", [128, 512]) for i in range(8)]
    i = 0
    for m in range(8):
        for it, (t0, n) in enumerate(TILES):
            b = i % 4; i += 1
            for k in range(8):
                P.I("pe", "matmul", pp[b][:, 0:n], wbf[:, k, m * 128:(m + 1) * 128], ybf[:, k, t0:t0 + n], start=(k == 0), stop=(k == 7),
                    r=["wbf", "ybf"], w=[f"bk{b}"])
            gcol = 3 if it == 0 else 0
            P.I("dve", "scalar_tensor_tensor", xT[:, m, t0:t0 + n], pp[b][:, 0:n], mod[:, m, gcol:gcol + 1], xT[:, m, t0:t0 + n], ALU.mult, ALU.add,
                r=[f"bk{b}", "mod", "xT"], w=["xT"])
    for k in range(8):
        P.dma("sp", xmd[:, k, :], xT[:, k, :], reads=["xT"], is_output=True)
    rms_modulate(P, xT, xT, mod, nw, ones_bf, shift_i=(1, 4), scale_i=(2, 5), tagp="n2", psb=(pp[4], pp[5]), hkey="xT")
    for k in range(8):
        P.dma("sp", h2d[:, k, :], hT32[:, k, :], reads=["xT"], is_output=True)
    G = P.sb("G", [128, NTT, 32]); lg = P.sb("lg", [128, NTT, 32]); m8 = P.sb("m8", [128, NTT, 8]); nmx = P.sb("nmx", [128, NTT])
    msk = P.sb("msk", [128, NTT, 32]); ssum = P.sb("ssum", [128, NTT])
    for tt in range(NTT):
        b = 6 + tt % 2
        for k in range(8):
            P.I("pe", "matmul", pp[b][:, 0:32], hT32[:, k, tt * 128:(tt + 1) * 128], wr[:, k, :], start=(k == 0), stop=(k == 7),
                r=["xT", "wr"], w=[f"bk{b}"])
        P.I("dve", "tensor_tensor", lg[:, tt, :], pp[b][:, 0:32], br[:], ALU.add, r=[f"bk{b}", "br"], w=["lg"])
        P.I("dve", "max", m8[:, tt, :], lg[:, tt, :], r=["lg"], w=["m8"])
        P.I("dve", "tensor_scalar", msk[:, tt, :], lg[:, tt, :], m8[:, tt, 3:4], None, ALU.is_ge, r=["lg", "m8"], w=["msk"])
        P.I("dve", "tensor_scalar", nmx[:, tt:tt + 1], m8[:, tt, 0:1], -1.0, None, ALU.mult, r=["m8"], w=["nmx"])
        P.I("act", "activation", G[:, tt, :], lg[:, tt, :], AF.Exp, bias=nmx[:, tt:tt + 1], r=["lg", "nmx"], w=["G"])
        P.I("dve", "tensor_tensor", G[:, tt, :], G[:, tt, :], msk[:, tt, :], ALU.mult, r=["G", "msk"], w=["G"])
        P.I("dve", "tensor_reduce", ssum[:, tt:tt + 1], G[:, tt, :], AX.X, ALU.add, r=["G"], w=["ssum"])
        P.I("dve", "reciprocal", ssum[:, tt:tt + 1], ssum[:, tt:tt + 1], r=["ssum"], w=["ssum"])
        P.I("dve", "tensor_scalar", G[:, tt, :], G[:, tt, :], ssum[:, tt:tt + 1], None, ALU.mult, r=["G", "ssum"], w=["G"])
    P.dma("sp", Gd, G[:], reads=["G"], is_output=True)
    P.finish()
    return nc


NT2 = 1088
T2 = [(0, 512), (512, 512), (1024, 64)]
ST2 = [(i * 128, 128) for i in range(8)] + [(1024, 64)]


def build_C2(NB=16, NE=4):
    nc = bass.Bass("TRN2", target_bir_lowering=False)
    D = lambda n, s: nc.dram_tensor(n, s, F32, kind="ExternalInput").ap()
    h2d = D("h2T", [128, 8, NB * NT2]); Gd = D("G", [128, NB, 9, NE]); wgud = D("wgu", [NE, 128, 8, 2048]); wdd = D("wd", [NE, 128, 8, 1024])
    bgud = D("bgu", [128, NE, 16]); bdd = D("bd", [NE, 1024]); idd = D("ident", [128, 128])
    fd = nc.dram_tensor("f", [NB, 128, 9, 1024], F32, kind="ExternalOutput").ap()
    P = Prog(nc)
    hbf = [P.sb(f"hbf{i}", [128, 8, NT2], BF16) for i in range(2)]
    G = P.sb("G", [128, NB, 9, NE]); bgu = P.sb("bgu", [128, NE, 16]); bd = P.sb("bd", [NE, 1024]); ident = P.sb("ident", [128, 128])
    P.dma("sp", G[:], Gd, writes=["G"]); P.dma("sp", bgu[:], bgud, writes=["bgu"]); P.dma("sp", bd[:], bdd, writes=["bd"]); P.dma("sp", ident[:], idd, writes=["ident"])
    wgu = [P.sb(f"wgu{i}", [128, 8, 2048], BF16) for i in range(2)]; wd = [P.sb(f"wd{i}", [128, 8, 1024], BF16) for i in range(2)]
    act = P.sb("act", [128, 8, NT2], BF16); acc = P.sb("acc", [128, 9, 1024])
    gc_ = [P.sb(f"gc{i}", [128, 512]) for i in range(2)]; sg = [P.sb(f"sg{i}", [128, 512]) for i in range(2)]
    uc = [P.sb(f"uc{i}", [128, 512]) for i in range(2)]
    GT = P.sb("GT", [NE, 128])
    pp = [P.ps(f"bank{i}", [128, 512]) for i in range(8)]

    wgus = [nc.dram_tensor(f"wgu_bf{e}", [128, 8, 2048], BF16).ap() for e in range(NE)]
    wds = [nc.dram_tensor(f"wd_bf{e}", [128, 8, 1024], BF16).ap() for e in range(NE)]
    for e in range(NE):
        for k in range(8):
            P.dma("pool", wgus[e][:, k, :], wgud[e, :, k, :], writes=[f"wgus{e}"])
            P.dma("pool", wds[e][:, k, :], wdd[e, :, k, :], writes=[f"wds{e}"])

    def load_w(j):
        e = j % NE; b = j % 2
        for k in range(0, 8, 2):
            P.dma("sp", wgu[b][:, k:k + 2, :], wgus[e][:, k:k + 2, :], reads=[f"wgus{e}"], writes=[f"wgu{b}"])
        for k in range(0, 8, 4):
            P.dma("act", wd[b][:, k:k + 4, :], wds[e][:, k:k + 4, :], reads=[f"wds{e}"], writes=[f"wd{b}"])

    def load_h(blk):
        for k in range(8):
            P.dma("pool", hbf[blk % 2][:, k, :], h2d[:, k, blk * NT2:(blk + 1) * NT2], writes=[f"hbf{blk%2}"])
    load_h(0); load_w(0)
    it = 0; jt = 0; j = 0
    for blk in range(NB):
        hb = hbf[blk % 2]; hk = f"hbf{blk%2}"
        if blk + 1 < NB:
            load_h(blk + 1)
        for e in range(NE):
            b = j % 2
            if j + 1 < NB * NE:
                load_w(j + 1)
            j += 1
            for fc in range(8):
                for (t0, n) in T2:
                    s = it % 2; it += 1
                    pg, pu = pp[2 * s], pp[2 * s + 1]; kg, ku = f"bk{2*s}", f"bk{2*s+1}"
                    for k in range(8):
                        P.I("pe", "matmul", pg[:, 0:n], wgu[b][:, k, fc * 128:(fc + 1) * 128], hb[:, k, t0:t0 + n], start=(k == 0), stop=(k == 7),
                            r=[f"wgu{b}", hk], w=[kg])
                    for k in range(8):
                        P.I("pe", "matmul", pu[:, 0:n], wgu[b][:, k, 1024 + fc * 128:1024 + (fc + 1) * 128], hb[:, k, t0:t0 + n], start=(k == 0), stop=(k == 7),
                            r=[f"wgu{b}", hk], w=[ku])
                    P.I("dve", "tensor_scalar", gc_[s][:, 0:n], pg[:, 0:n], bgu[:, e, fc:fc + 1], 7.0, ALU.add, ALU.min, r=[kg, "bgu"], w=[f"gc{s}"])
                    P.I("act", "activation", sg[s][:, 0:n], gc_[s][:, 0:n], AF.Sigmoid, scale=1.702, r=[f"gc{s}"], w=[f"sg{s}"])
                    P.I("dve", "tensor_scalar", uc[s][:, 0:n], pu[:, 0:n], bgu[:, e, 8 + fc:9 + fc], 7.0, ALU.add, ALU.min, r=[ku, "bgu"], w=[f"uc{s}"])
                    P.I("dve", "tensor_scalar", uc[s][:, 0:n], uc[s][:, 0:n], -7.0, 1.0, ALU.max, ALU.add, r=[f"uc{s}"], w=[f"uc{s}"])
                    P.I("dve", "tensor_tensor", gc_[s][:, 0:n], gc_[s][:, 0:n], sg[s][:, 0:n], ALU.mult, r=[f"gc{s}", f"sg{s}"], w=[f"gc{s}"])
                    P.I("dve", "tensor_tensor", act[:, fc, t0:t0 + n], gc_[s][:, 0:n], uc[s][:, 0:n], ALU.mult, r=[f"gc{s}", f"uc{s}"], w=["act"])
            for si, (s0, sn) in enumerate(ST2):
                for half in range(2):
                    pb = 4 + jt % 4; jt += 1
                    hs = slice(half * 512, (half + 1) * 512)
                    for fc in range(8):
                        P.I("pe", "matmul", pp[pb][0:sn, 0:512], act[:, fc, s0:s0 + sn], wd[b][:, fc, hs], start=(fc == 0), stop=(fc == 7),
                            r=["act", f"wd{b}"], w=[f"bk{pb}"])
                    if e == 0:
                        P.I("dve", "tensor_scalar", acc[0:sn, si, hs], pp[pb][0:sn, 0:512], G[0:sn, blk, si, e:e + 1], None, ALU.mult,
                            r=[f"bk{pb}", "G"], w=["acc"])
                    else:
                        P.I("dve", "scalar_tensor_tensor", acc[0:sn, si, hs], pp[pb][0:sn, 0:512], G[0:sn, blk, si, e:e + 1], acc[0:sn, si, hs],
                            ALU.mult, ALU.add, r=[f"bk{pb}", "G", "acc"], w=["acc"])
        for si, (s0, sn) in enumerate(ST2):
            P.I("pe", "transpose", pp[0][0:NE, 0:sn], G[0:sn, blk, si, :], ident[0:sn, 0:sn], r=["G", "ident"], w=["bk0"])
            P.I("dve", "tensor_copy", GT[:, 0:sn], pp[0][0:NE, 0:sn], r=["bk0"], w=["GT"])
            for half in range(2):
                pb = 1 + half; hs = slice(half * 512, (half + 1) * 512)
                P.I("pe", "matmul", pp[pb][0:sn, 0:512], GT[:, 0:sn], bd[:, hs], start=True, stop=True, r=["GT", "bd"], w=[f"bk{pb}"])
                P.I("dve", "tensor_tensor", acc[0:sn, si, hs], acc[0:sn, si, hs], pp[pb][0:sn, 0:512], ALU.add, r=[f"bk{pb}", "acc"], w=["acc"])
        P.I("pool", "memset", acc[64:128, 8, :], 0.0, r=["acc"], w=["acc"]) if blk == 0 else None
        P.dma("sp", fd[blk], acc[:], reads=["acc"], is_output=True)
    P.finish()
    return nc


def build_D():
    nc = bass.Bass("TRN2", target_bir_lowering=False)
    D = lambda n, s: nc.dram_tensor(n, s, F32, kind="ExternalInput").ap()
    xmd = D("xmT", [128, 8, NTOK]); fTd = D("fT", [8, 128, 8, NTOK]); modd = D("mod", [128, 8, 2]); nwd = D("nw", [128, 8])
    od = nc.dram_tensor("oT", [128, 8, NTOK], F32, kind="ExternalOutput").ap()
    P = Prog(nc)
    xT = P.sb("xT", [128, 8, NTOK]); fT = P.sb("fT", [128, 8, NTOK]); mod = P.sb("mod", [128, 8, 2]); nw = P.sb("nw", [128, 8])
    ones_bf = P.sb("ones_bf", [128, 128], BF16)
    P.I("pool", "memset", ones_bf[:], 1.0, w=["ones_bf"])
    for k in range(8):
        P.dma("sp", xT[:, k, :], xmd[:, k, :], writes=["xT"])
    P.dma("sp", mod[:], modd, writes=["mod"]); P.dma("sp", nw[:], nwd, writes=["nw"])
    for c in range(8):
        for k in range(8):
            P.dma("sp", fT[:, k, :], fTd[c, :, k, :], writes=["fT"])
        add_gated(P, xT, fT, mod, 0, 1)
    sq = [P.sb(f"sq{i}", [128, 8, 512], BF16) for i in range(2)]; rs = [P.sb(f"rs{i}", [128, 512]) for i in range(2)]
    ss = [P.ps(f"bank{i}", [128, 512]) for i in range(2)]
    for it, (t0, n) in enumerate(TILES):
        b = it % 2
        for k in range(8):
            P.I("act", "activation", sq[b][:, k, 0:n], xT[:, k, t0:t0 + n], AF.Square, r=["xT"], w=[f"sq{b}"])
        for k in range(8):
            P.I("pe", "matmul", ss[b][:, 0:n], ones_bf[:], sq[b][:, k, 0:n], start=(k == 0), stop=(k == 7), r=[f"sq{b}", "ones_bf"], w=[f"bk{b}"])
        P.I("dve", "tensor_scalar", rs[b][:, 0:n], ss[b][:, 0:n], 1.0 / 1024, 1e-6, ALU.mult, ALU.add, r=[f"bk{b}"], w=[f"rs{b}"])
        P.I("dve", "reciprocal", rs[b][:, 0:n], rs[b][:, 0:n], r=[f"rs{b}"], w=[f"rs{b}"])
        P.I("act", "activation", rs[b][:, 0:n], rs[b][:, 0:n], AF.Sqrt, r=[f"rs{b}"], w=[f"rs{b}"])
        for k in range(8):
            P.I("dve", "scalar_tensor_tensor", fT[:, k, t0:t0 + n], xT[:, k, t0:t0 + n], nw[:, k:k + 1], rs[b][:, 0:n], ALU.mult, ALU.mult,
                r=["xT", "nw", f"rs{b}"], w=["fT"])
    for k in range(8):
        P.dma("sp", od[:, k, :], fT[:, k, :], reads=["fT"], is_output=True)
    P.finish()
    return nc


def add_gated(P, xT, fT, mod, col_l, col_c):
    for k in range(8):
        P.I("dve", "scalar_tensor_tensor", xT[:, k, 0:128], fT[:, k, 0:128], mod[:, k, col_c:col_c + 1], xT[:, k, 0:128], ALU.mult, ALU.add,
            r=["fT", "mod", "xT"], w=["xT"])
        P.I("dve", "scalar_tensor_tensor", xT[:, k, 128:NTOK], fT[:, k, 128:NTOK], mod[:, k, col_l:col_l + 1], xT[:, k, 128:NTOK], ALU.mult, ALU.add,
            r=["fT", "mod", "xT"], w=["xT"])


I32 = mybir.dt.int32


def build_C2s(NTILE=136, NE=4, CAP=2560):
    NTOKA = NTILE * 128; NJ = CAP // 128; NJH = NJ // 2; HALF = CAP // 2
    T3 = [(t0, min(512, HALF - t0)) for t0 in range(0, HALF, 512)]
    nc = bass.Bass("TRN2", target_bir_lowering=False)
    D = lambda n, s: nc.dram_tensor(n, s, F32, kind="ExternalInput").ap()
    h2d = D("h2tok", [NTOKA + 128, 1024]); Gd = D("Gm", [128, NTILE, NE]); tokd = D("tokid", [128, NTILE]); Ld = D("lst", [128, 128])
    padd = D("padtab", [128, NJ, 2]); idd = D("ident", [128, 128]); dumpd = D("dump", [128, 1])
    wgud = D("wgu", [NE, 128, 8, 2048]); wdd = D("wd", [NE, 128, 8, 1024]); bgud = D("bgu", [128, NE, 16]); bdbd = D("bdb", [NE, 128, 1024])
    fd = nc.dram_tensor("fpart", [NTOKA + 128, 1024], F32, kind="ExternalOutput").ap()
    tab = [nc.dram_tensor(f"slot_tab{e}", [CAP + 128, 2], F32).ap() for e in range(NE)]
    P = Prog(nc)
    pp = [P.ps(f"bank{i}", [128, 512]) for i in range(8)]
    z = P.sb("z", [128, 1024]); ones = P.sb("ones", [128, 128]); lst = P.sb("lst", [128, 128]); ident = P.sb("ident", [128, 128])
    P.I("pool", "memset", z[:], 0.0, w=["z"]); P.I("pool", "memset", ones[:], 1.0, w=["ones"])
    P.dma("sp", lst[:], Ld, writes=["lst"]); P.dma("sp", ident[:], idd, writes=["ident"])
    for r in range(NTILE + 1):
        P.dma("sp", fd[r * 128:(r + 1) * 128, :], z[:], reads=["z"], writes=["fpart"])
    Gm = P.sb("Gm", [128, NTILE, NE]); tokid = P.sb("tokid", [128, NTILE]); padt = P.sb("padt", [128, NJ, 2]); bgu = P.sb("bgu", [128, NE, 16])
    P.dma("act", Gm[:], Gd, writes=["Gm"]); P.dma("act", tokid[:], tokd, writes=["tokid"]); P.dma("act", padt[:], padd, writes=["padt"])
    P.dma("act", bgu[:], bgud, writes=["bgu"])
    dump = P.sb("dump", [128, 1]); P.dma("act", dump[:], dumpd, writes=["dump"])
    wgu = [P.sb(f"wgu{i}", [128, 8, 2048], BF16) for i in range(2)]; wd = [P.sb(f"wd{i}", [128, 8, 1024], BF16) for i in range(2)]
    bdb = [P.sb(f"bdb{i}", [128, 1024]) for i in range(2)]
    hsel = P.sb("hsel", [128, 8, HALF], BF16); act = P.sb("act", [128, 8, HALF], BF16)
    hg = [P.sb(f"hg{i}", [128, 1024]) for i in range(2)]; yst = [P.sb(f"yst{i}", [128, 1024]) for i in range(2)]
    gc_ = [P.sb(f"gc{i}", [128, 512]) for i in range(2)]; sg = [P.sb(f"sg{i}", [128, 512]) for i in range(2)]
    uc = [P.sb(f"uc{i}", [128, 512]) for i in range(2)]

    def load_w(e):
        b = e % 2
        for k in range(8):
            P.dma("pool", wgu[b][:, k, :], wgud[e, :, k, :], writes=[f"wgu{b}"])
        for k in range(8):
            P.dma("pool", wd[b][:, k, :], wdd[e, :, k, :], writes=[f"wd{b}"])
        P.dma("act", bdb[b][:], bdbd[e], writes=[f"bdb{b}"])
    load_w(0)
    m = P.sb("m", [128, NE, NTILE]); cs = P.sb("cs", [128, NE, NTILE]); inc = P.sb("inc", [128, NE, NTILE]); rk = P.sb("rk", [128, NE, NTILE])
    idx = P.sb("idx", [128, NE, NTILE], I32); pr = P.sb("pr", [128, NTILE, NE, 2]); onesw = P.sb("onesw", [128, NTILE])
    P.I("pool", "memset", onesw[:], 1.0, w=["onesw"])
    for e in range(NE):
        P.I("dve", "tensor_scalar", m[:, e, :], Gm[:, :, e], 0.0, None, ALU.is_gt, r=["Gm"], w=["m"])
        P.I("pe", "matmul", pp[0][:, 0:NTILE], lst[:], m[:, e, :], start=True, stop=True, r=["lst", "m"], w=["bk0"])
        P.I("pe", "matmul", pp[1][:, 0:NTILE], ones[:], m[:, e, :], start=True, stop=True, r=["ones", "m"], w=["bk1"])
        P.I("act", "activation", cs[:, e, :], pp[1][:, 0:NTILE], AF.Copy, r=["bk1"], w=["cs"])
        P.I("dve", "tensor_tensor_scan", inc[:, e, :], onesw[:], cs[:, e, :], 0.0, ALU.mult, ALU.add, r=["onesw", "cs"], w=["inc"])
        P.I("dve", "tensor_tensor", rk[:, e, :], pp[0][:, 0:NTILE], inc[:, e, :], ALU.add, r=["bk0", "inc"], w=["rk"])
        P.I("dve", "tensor_tensor", rk[:, e, :], rk[:, e, :], cs[:, e, :], ALU.subtract, r=["rk", "cs"], w=["rk"])
        P.I("dve", "tensor_scalar", cs[:, e, :], rk[:, e, :], float(CAP), None, ALU.is_lt, r=["rk", "cs"], w=["cs"])
        P.I("dve", "tensor_tensor", cs[:, e, :], cs[:, e, :], m[:, e, :], ALU.mult, r=["cs", "m"], w=["cs"])
        P.I("dve", "tensor_scalar", rk[:, e, :], rk[:, e, :], dump[:, 0:1], None, ALU.subtract, r=["rk", "dump"], w=["rk"])
        P.I("dve", "tensor_tensor", rk[:, e, :], rk[:, e, :], cs[:, e, :], ALU.mult, r=["rk", "cs"], w=["rk"])
        P.I("dve", "tensor_scalar", rk[:, e, :], rk[:, e, :], dump[:, 0:1], None, ALU.add, r=["rk", "dump"], w=["rk"])
        P.I("dve", "tensor_copy", idx[:, e, :], rk[:, e, :], r=["rk"], w=["idx"])
        P.I("pool", "tensor_copy", pr[:, :, e, 0], tokid[:], r=["tokid"], w=["pr"])
        P.I("pool", "tensor_copy", pr[:, :, e, 1], Gm[:, :, e], r=["Gm"], w=["pr"])
    for e in range(NE):
        P.dma("act", tab[e][0:CAP, :].rearrange("(p j) c -> p j c", j=NJ), padt[:], reads=["padt"], writes=[f"tabinit{e}"])
    for t in range(NTILE):
        for e in range(NE):
            P.idma(tab[e], pr[:, t, e, :], out_idx=idx[:, e, t:t + 1], reads=["pr", "idx", f"tabinit{e}"], writes=[f"sc{e}_{t}"])
    tabsb = P.sb("tabsb", [128, NE, NJ, 2]); tok_i = P.sb("tok_i", [128, NE, NJ], I32); gate = P.sb("gate", [128, NE, NJ])
    for e in range(NE):
        P.dma("act", tabsb[:, e, :, :], tab[e][0:CAP, :].rearrange("(p j) c -> p j c", j=NJ), reads=[f"sc{e}_{t}" for t in range(NTILE)], writes=["tabsb"])
    P.I("dve", "tensor_copy", tok_i[:], tabsb[:, :, :, 0], r=["tabsb"], w=["tok_i"])
    P.I("dve", "tensor_copy", gate[:], tabsb[:, :, :, 1], r=["tabsb"], w=["gate"])
    it = 0; jt = 0; gi = 0; ti = 0
    for e in range(NE):
        b = e % 2
        if e + 1 < NE:
            load_w(e + 1)
        for half in range(2):
            for jj in range(NJH):
                j = half * NJH + jj; g = gi % 2; gi += 1
                P.idma(hg[g][:], h2d, in_idx=tok_i[:, e, j:j + 1], reads=["tok_i"], writes=[f"hg{g}"])
                for k in range(8):
                    pb = 4 + ti % 4; ti += 1
                    P.I("pe", "transpose", pp[pb][:, 0:128], hg[g][:, k * 128:(k + 1) * 128], ident[:], r=[f"hg{g}", "ident"], w=[f"bk{pb}"])
                    if ti % 2:
                        P.I("act", "activation", hsel[:, k, jj * 128:(jj + 1) * 128], pp[pb][:, 0:128], AF.Copy, r=[f"bk{pb}"], w=["hsel"])
                    else:
                        P.I("dve", "tensor_copy", hsel[:, k, jj * 128:(jj + 1) * 128], pp[pb][:, 0:128], r=[f"bk{pb}"], w=["hsel"])
            for fc in range(8):
                for (t0, n) in T3:
                    s = it % 2; it += 1
                    pg, pu = pp[2 * s], pp[2 * s + 1]; kg, ku = f"bk{2*s}", f"bk{2*s+1}"
                    for k in range(8):
                        P.I("pe", "matmul", pg[:, 0:n], wgu[b][:, k, fc * 128:(fc + 1) * 128], hsel[:, k, t0:t0 + n], start=(k == 0), stop=(k == 7),
                            r=[f"wgu{b}", "hsel"], w=[kg])
                    for k in range(8):
                        P.I("pe", "matmul", pu[:, 0:n], wgu[b][:, k, 1024 + fc * 128:1024 + (fc + 1) * 128], hsel[:, k, t0:t0 + n], start=(k == 0), stop=(k == 7),
                            r=[f"wgu{b}", "hsel"], w=[ku])
                    P.I("dve", "tensor_scalar", gc_[s][:, 0:n], pg[:, 0:n], bgu[:, e, fc:fc + 1], 7.0, ALU.add, ALU.min, r=[kg, "bgu"], w=[f"gc{s}"])
                    P.I("act", "activation", sg[s][:, 0:n], gc_[s][:, 0:n], AF.Sigmoid, scale=1.702, r=[f"gc{s}"], w=[f"sg{s}"])
                    P.I("dve", "tensor_scalar", uc[s][:, 0:n], pu[:, 0:n], bgu[:, e, 8 + fc:9 + fc], 7.0, ALU.add, ALU.min, r=[ku, "bgu"], w=[f"uc{s}"])
                    P.I("dve", "tensor_scalar", uc[s][:, 0:n], uc[s][:, 0:n], -7.0, 1.0, ALU.max, ALU.add, r=[f"uc{s}"], w=[f"uc{s}"])
                    P.I("dve", "tensor_tensor", gc_[s][:, 0:n], gc_[s][:, 0:n], sg[s][:, 0:n], ALU.mult, r=[f"gc{s}", f"sg{s}"], w=[f"gc{s}"])
                    P.I("dve", "tensor_tensor", act[:, fc, t0:t0 + n], gc_[s][:, 0:n], uc[s][:, 0:n], ALU.mult, r=[f"gc{s}", f"uc{s}"], w=["act"])
            for jj in range(NJH):
                j = half * NJH + jj; y = jt % 2
                for hh in range(2):
                    pb = 4 + jt % 4; jt += 1
                    hs = slice(hh * 512, (hh + 1) * 512)
                    for fc in range(8):
                        P.I("pe", "matmul", pp[pb][:, 0:512], act[:, fc, jj * 128:(jj + 1) * 128], wd[b][:, fc, hs], start=(fc == 0), stop=(fc == 7),
                            r=["act", f"wd{b}"], w=[f"bk{pb}"])
                    P.I("dve", "tensor_tensor", yst[jj % 2][:, hs], pp[pb][:, 0:512], bdb[b][:, hs], ALU.add, r=[f"bk{pb}", f"bdb{b}"], w=[f"yst{jj%2}"])
                P.I("pool", "tensor_scalar", yst[jj % 2][:], yst[jj % 2][:], gate[:, e, j:j + 1], None, ALU.mult, r=[f"yst{jj%2}", "gate"], w=[f"yst{jj%2}"])
                P.idma(fd, yst[jj % 2][:], out_idx=tok_i[:, e, j:j + 1], reads=[f"yst{jj%2}", "tok_i", "fpart"], writes=["fpart"], is_output=True, compute_op=ALU.add)
    P.finish()
    return nc


def host_C2s_consts(NTILE=136, CAP=2560):
    NJ = CAP // 128
    tokid = (np.arange(NTILE)[None, :] * 128 + np.arange(128)[:, None]).astype(np.float32)
    p_ = np.arange(128)[:, None]; q_ = np.arange(128)[None, :]
    lst = (p_ < q_).astype(np.float32)
    padtab = np.zeros((128, NJ, 2), np.float32); padtab[:, :, 0] = NTILE * 128 + np.arange(128)[:, None]
    return {"tokid": tokid, "lst": lst, "padtab": padtab, "ident": np.eye(128, dtype=np.float32), "dump": (CAP + np.arange(128, dtype=np.float32))[:, None]}


def _fm(tok):
    return np.ascontiguousarray(tok.T.reshape(8, 128, -1).transpose(1, 0, 2))


def _tok(fm):
    return fm.transpose(2, 1, 0).reshape(fm.shape[2], -1)


def _vec(v):
    return np.ascontiguousarray(np.asarray(v, np.float32).reshape(8, 128).T)


_PROGS = {}
MOE_SPARSE = False


def _prog(name, builder, *a):
    key = (name,) + a
    if key not in _PROGS:
        _PROGS[key] = builder(*a)
    return _PROGS[key]


def _run(nc, maps):
    res = run_bass_kernel_spmd(nc, maps, core_ids=list(range(len(maps))))
    return res.results


def kernel(**inp):
    prm = {k: np.asarray(v, dtype=np.float32) for k, v in inp.items()}
    x, c, ctx, c_ctx = prm['x'], prm['c'], prm['ctx'], prm['c_ctx']
    NC = 8
    A_ = lambda a: np.ascontiguousarray(a, dtype=np.float32)
    cs = np.zeros((128, 8, 5), np.float32)
    for v in range(4):
        cs[:, :, v] = _vec(c[v])
    cs[:, :, 4] = _vec(c_ctx)
    items = [(l, fc) for l in range(2) for fc in range(48)]
    maps = []
    for i in range(NC):
        its = items[i * 12:(i + 1) * 12]
        wm = np.stack([prm['w_mod'][l][:, fc * 128:(fc + 1) * 128].reshape(8, 128, 128).transpose(1, 0, 2) for l, fc in its])
        bm = np.stack([prm['b_mod'][l][fc * 128:(fc + 1) * 128] for l, fc in its], 1)
        maps.append({"cs": cs, "wm": A_(wm), "bm": A_(bm)})
    res = _run(_prog("M", build_M), maps)
    modv = {}
    for i in range(NC):
        for j, (l, fc) in enumerate(items[i * 12:(i + 1) * 12]):
            modv[(l, fc)] = res[i]["modT"][:, j, :]
    mod6 = [[np.stack([modv[(l, i6 * 8 + k)] for k in range(8)], 1) for i6 in range(6)] for l in range(2)]

    def core_tokens(arr_c, arr_l, b, half):
        return np.concatenate([arr_c[b, half * 128:(half + 1) * 128], arr_l[b, half * 2048:(half + 1) * 2048]], 0)

    xm = [_fm(core_tokens(ctx, x, j // 2, j % 2)) for j in range(NC)]
    fparts = None
    consts64 = host_consts64()
    for l in range(2):
        win = np.zeros((1024, NCT * 128), np.float32); win[:, :4184] = prm['w_in'][l]
        win = A_(win.reshape(8, 128, NCT * 128).transpose(1, 0, 2)); nw1 = _vec(prm['norm1_w'][l])
        maps = []
        for j in range(NC):
            b = j // 2
            g5l = mod6[l - 1][5][:, :, b] if l > 0 else np.zeros((128, 8), np.float32)
            g5c = mod6[l - 1][5][:, :, 4] if l > 0 else np.zeros((128, 8), np.float32)
            mod = np.stack([mod6[l][0][:, :, b], mod6[l][1][:, :, b], mod6[l][0][:, :, 4], mod6[l][1][:, :, 4], g5l, g5c], -1)
            m = {"xT": xm[j], "mod": A_(mod), "nw": nw1, "win": win}
            if l > 0:
                m["fT"] = A_(np.stack([_fm(fparts[cc][j * NTOK:(j + 1) * NTOK]) for cc in range(NC)]))
            maps.append(m)
        res = _run(_prog("A", build_A, l == 0), maps)
        xcur = [res[j]["xout"] for j in range(NC)]
        ptok = [res[j]["pT"].reshape(NCT * 128, NTOK).T[:, :4184] for j in range(NC)]
        del res
        pfull = np.stack([np.concatenate([ptok[2 * b][:128], ptok[2 * b + 1][:128], ptok[2 * b][128:], ptok[2 * b + 1][128:]], 0) for b in range(4)])
        del ptok
        yall = np.zeros((4, TSEQ, 1024), np.float32)
        pa = pfull[:, :, 0:1032]
        res = _run(_prog("B1", build_B1), [host_B1_inputs(pa, l, j // 2, j % 2, prm) for j in range(NC)])
        for j in range(NC):
            yall[j // 2, :, (j % 2) * 128:(j % 2 + 1) * 128] = res[j]["yT"].T
        pb = pfull[:, :, 1032:3096]
        for rnd in range(2):
            its = [(i // 4, i % 4) for i in range(rnd * 8, rnd * 8 + 8)]
            maps = [host_B2_inputs(pb, l, b, h, prm) for b, h in its]
            for m in maps:
                m["consts"] = consts64
            res = _run(_prog("B2", build_B2), maps)
            for (b, h), r in zip(its, res):
                yall[b, :, 256 + h * 128:256 + (h + 1) * 128] = host_B2_output(r["y"])
        pc = pfull[:, :, 3096:4184]
        for rnd in range(2):
            its = [(i // 4, i % 4) for i in range(rnd * 8, rnd * 8 + 8)]
            maps = [host_B3_inputs(pc, l, b, h, prm) for b, h in its]
            for m in maps:
                m["consts"] = consts64
            res = _run(_prog("B3", build_B3), maps)
            for (b, h), r in zip(its, res):
                yall[b, :, 768 + h * 64:768 + (h + 1) * 64] = r["y"].T
        del pfull
        wout = A_(prm['w_out'][l].reshape(8, 128, 1024).transpose(1, 0, 2)); nw2 = _vec(prm['norm2_w'][l])
        wr = A_(prm['w_router'][l].reshape(8, 128, 32).transpose(1, 0, 2)); br = A_(np.broadcast_to(prm['b_router'][l][None], (128, 32)))
        maps = []
        for j in range(NC):
            b, half = j // 2, j % 2
            ytok = np.concatenate([yall[b, half * 128:(half + 1) * 128], yall[b, 256 + half * 2048:256 + (half + 1) * 2048]], 0)
            mod = np.stack([mod6[l][2][:, :, b], mod6[l][3][:, :, b], mod6[l][4][:, :, b], mod6[l][2][:, :, 4], mod6[l][3][:, :, 4], mod6[l][4][:, :, 4]], -1)
            maps.append({"xT": xcur[j], "yT": _fm(ytok), "wout": wout, "mod": A_(mod), "nw": nw2, "wr": wr, "br": br})
        res = _run(_prog("C1", build_C1), maps)
        xm = [res[j]["xmT"] for j in range(NC)]
        if MOE_SPARSE:
            h2tok = np.concatenate([_tok(res[j]["h2T"]) for j in range(NC)] + [np.zeros((128, 1024), np.float32)], 0)
        else:
            h2all = np.ascontiguousarray(np.concatenate([res[j]["h2T"] for j in range(NC)], 2))
        Gall = np.concatenate([res[j]["G"].transpose(1, 0, 2).reshape(NTOK, 32) for j in range(NC)], 0)
        del res, yall
        if MOE_SPARSE:
            maps = []
            c2c = host_C2s_consts()
            for cc in range(NC):
                es = slice(4 * cc, 4 * cc + 4)
                m_ = {"h2tok": h2tok, "Gm": A_(Gall[:, es].reshape(136, 128, 4).transpose(1, 0, 2)),
                      "wgu": A_(prm['w_gate_up'][l][es].reshape(4, 8, 128, 2048).transpose(0, 2, 1, 3)),
                      "wd": A_(prm['w_down'][l][es].reshape(4, 8, 128, 1024).transpose(0, 2, 1, 3)),
                      "bgu": A_(prm['b_gate_up'][l][es].reshape(4, 16, 128).transpose(2, 0, 1)),
                      "bdb": A_(np.broadcast_to(prm['b_down'][l][es][:, None, :], (4, 128, 1024)))}
                m_.update(c2c)
                maps.append(m_)
            res = _run(_prog("C2s", build_C2s), maps)
            del maps, h2tok
            fparts = [res[cc]["fpart"][:16 * NT2] for cc in range(NC)]
            del res
        else:
            maps = []
            ident = np.eye(128, dtype=np.float32)
            for cc in range(NC):
                es = slice(4 * cc, 4 * cc + 4)
                Gp = np.zeros((16, 1152, 4), np.float32); Gp[:, :NT2] = Gall[:, es].reshape(16, NT2, 4)
                maps.append({"h2T": h2all, "G": A_(Gp.reshape(16, 9, 128, 4).transpose(2, 0, 1, 3)),
                             "wgu": A_(prm['w_gate_up'][l][es].reshape(4, 8, 128, 2048).transpose(0, 2, 1, 3)),
                             "wd": A_(prm['w_down'][l][es].reshape(4, 8, 128, 1024).transpose(0, 2, 1, 3)),
                             "bgu": A_(prm['b_gate_up'][l][es].reshape(4, 16, 128).transpose(2, 0, 1)), "bd": A_(prm['b_down'][l][es]), "ident": ident})
            res = _run(_prog("C2", build_C2), maps)
            del maps, h2all
            fparts = [res[cc]["f"].transpose(0, 2, 1, 3).reshape(16, 1152, 1024)[:, :NT2].reshape(16 * NT2, 1024) for cc in range(NC)]
            del res
    nwf = _vec(prm['norm_f_w'])
    maps = []
    for j in range(NC):
        b = j // 2
        mod = np.stack([mod6[1][5][:, :, b], mod6[1][5][:, :, 4]], -1)
        maps.append({"xmT": xm[j], "fT": A_(np.stack([_fm(fparts[cc][j * NTOK:(j + 1) * NTOK]) for cc in range(NC)])), "mod": A_(mod), "nw": nwf})
    res = _run(_prog("D", build_D), maps)
    out = np.zeros((4, 4096, 1024), np.float32)
    for j in range(NC):
        b, half = j // 2, j % 2
        out[b, half * 2048:(half + 1) * 2048] = _tok(res[j]["oT"])[128:]
    return out
```

```python
import numpy as np
from contextlib import ExitStack
import concourse.bass as bass
import concourse.mybir as mybir
from concourse.bass_utils import run_bass_kernel_spmd

F32 = mybir.dt.float32
BF16 = mybir.dt.bfloat16
AF = mybir.ActivationFunctionType
ALU = mybir.AluOpType
AX = mybir.AxisListType

ENGS = ("pe", "dve", "act", "pool", "sp")
N_DMA_SEMS = 12


class Prog:
    def __init__(self, nc, same_engine_sync=None):
        import os
        if same_engine_sync is None:
            same_engine_sync = os.environ.get('SAMESYNC', '1') == '1'
        self.nc = nc
        self.es = ExitStack()
        self.ops = {e: [] for e in ENGS}
        self.cnt = {e: 0 for e in ENGS}
        self.sem = {}
        for e in ENGS:
            self.sem[e] = self.es.enter_context(nc.semaphore("s_" + e))
        self.dsem = {q: [self.es.enter_context(nc.semaphore(f"d_{q}{i}")) for i in range(N_DMA_SEMS)]
                     for q in ("sp", "pool", "act")}
        self.dsem_uses = {q: [0] * N_DMA_SEMS for q in ("sp", "pool", "act")}
        self.dsem_next = {q: 0 for q in ("sp", "pool", "act")}
        self.waited = {e: {} for e in ENGS}
        self.lastw = {}
        self.readers = {}
        self.same = same_engine_sync
        self.semobj = {}
        self.out_tokens = []
        self.nops = 0

    def sb(self, name, shape, dt=F32):
        return self.es.enter_context(self.nc.sbuf_tensor("sb_" + name, list(shape), dt))

    def ps(self, name, shape, dt=F32):
        return self.es.enter_context(self.nc.psum_tensor("ps_" + name, list(shape), dt))

    def _need(self, eng, tok, waits):
        if tok is None:
            return
        semkey, val, src = tok
        if src == eng and (not self.same or eng == "pe"):
            return
        if self.waited[eng].get(semkey, 0) >= val:
            return
        waits[semkey] = max(waits.get(semkey, 0), val)

    def _deps(self, eng, reads, writes):
        waits = {}
        for k in reads:
            self._need(eng, self.lastw.get(k), waits)
        for k in writes:
            self._need(eng, self.lastw.get(k), waits)
            for t in self.readers.get(k, ()):
                self._need(eng, t, waits)
        for semkey, val in waits.items():
            self.waited[eng][semkey] = val
            self.ops[eng].append(("wait", semkey, val))

    def _commit(self, tok, reads, writes):
        for k in reads:
            self.readers.setdefault(k, []).append(tok)
        for k in writes:
            self.lastw[k] = tok
            self.readers[k] = []

    def op(self, eng, fn, reads=(), writes=()):
        import os
        lim = os.environ.get("OPLIMIT")
        if lim is not None and self.nops >= int(lim):
            return None
        writes = list(writes) + [k for k in reads if isinstance(k, str) and k.startswith("bk")]
        reads = [k for k in reads if not (isinstance(k, str) and k.startswith("bk"))]
        self._deps(eng, reads, writes)
        self.cnt[eng] += 1
        tok = (("e", eng), self.cnt[eng], eng)
        self.ops[eng].append(("op", fn, ("e", eng), 1))
        self._commit(tok, reads, writes)
        self.nops += 1
        return tok

    def I(self, eng, meth, *args, r=(), w=(), **kw):
        return self.op(eng, lambda e: getattr(e, meth)(*args, **kw), reads=r, writes=w)

    def dma(self, q, out, in_, reads=(), writes=(), is_output=False, **kw):
        import os
        lim = os.environ.get("OPLIMIT")
        if lim is not None and self.nops >= int(lim) and not is_output:
            return None
        i = self.dsem_next[q]
        self.dsem_next[q] = (i + 1) % N_DMA_SEMS
        uses = self.dsem_uses[q][i]
        semkey = ("d", q, i)
        if uses > 0 and self.waited[q].get(semkey, 0) < 16 * uses:
            self.waited[q][semkey] = 16 * uses
            self.ops[q].append(("wait", semkey, 16 * uses))
        self._deps(q, reads, writes)
        self.dsem_uses[q][i] = uses + 1
        tok = (semkey, 16 * (uses + 1), None)
        self.ops[q].append(("op", lambda e: e.dma_start(out=out, in_=in_, **kw), semkey, 16))
        self._commit(tok, reads, writes)
        if is_output:
            self.out_tokens.append(tok)
        self.nops += 1
        return tok

    def idma(self, out, in_, out_idx=None, in_idx=None, reads=(), writes=(), is_output=False, **kw):
        q = "pool"
        i = self.dsem_next[q]
        self.dsem_next[q] = (i + 1) % N_DMA_SEMS
        uses = self.dsem_uses[q][i]
        semkey = ("d", q, i)
        if uses > 0 and self.waited[q].get(semkey, 0) < 16 * uses:
            self.waited[q][semkey] = 16 * uses
            self.ops[q].append(("wait", semkey, 16 * uses))
        self._deps(q, reads, writes)
        self.dsem_uses[q][i] = uses + 1
        tok = (semkey, 16 * (uses + 1), None)
        oo = bass.IndirectOffsetOnAxis(ap=out_idx, axis=0) if out_idx is not None else None
        io = bass.IndirectOffsetOnAxis(ap=in_idx, axis=0) if in_idx is not None else None
        self.ops[q].append(("op", lambda e: e.indirect_dma_start(out=out, out_offset=oo, in_=in_, in_offset=io, **kw), semkey, 16))
        self._commit(tok, reads, writes)
        if is_output:
            self.out_tokens.append(tok)
        self.nops += 1
        return tok

    def _semh(self, semkey):
        if semkey[0] == "e":
            return self.sem[semkey[1]]
        return self.dsem[semkey[1]][semkey[2]]

    def finish(self):
        for tok in self.out_tokens:
            semkey, val, _ = tok
            if self.waited["sp"].get(semkey, 0) < val:
                self.waited["sp"][semkey] = val
                self.ops["sp"].append(("wait", semkey, val))
        for e in ENGS:
            if self.cnt[e] > 0 and e != "sp":
                self.ops["sp"].append(("wait", ("e", e), self.cnt[e]))
        for q in ("sp", "pool", "act"):
            for i in range(N_DMA_SEMS):
                if self.dsem_uses[q][i] > 0:
                    self.ops["sp"].append(("wait", ("d", q, i), 16 * self.dsem_uses[q][i]))
        nc = self.nc
        with nc.Block() as block:
            def mk(ename):
                def body(e):
                    for item in self.ops[ename]:
                        if item[0] == "wait":
                            e.wait_ge(self._semh(item[1]), item[2])
                        else:
                            ins = item[1](e)
                            ins.then_inc(self._semh(item[2]), item[3])
                return body
            block.tensor(mk("pe"))
            block.vector(mk("dve"))
            block.scalar(mk("act"))
            block.gpsimd(mk("pool"))
            block.sync(mk("sp"))
        self.es.close()


def build_M():
    nc = bass.Bass("TRN2", target_bir_lowering=False)
    cs = nc.dram_tensor("cs", [128, 8, 5], F32, kind="ExternalInput").ap()
    wm = nc.dram_tensor("wm", [12, 128, 8, 128], F32, kind="ExternalInput").ap()
    bm = nc.dram_tensor("bm", [128, 12], F32, kind="ExternalInput").ap()
    out = nc.dram_tensor("modT", [128, 12, 5], F32, kind="ExternalOutput").ap()
    P = Prog(nc)
    cst = P.sb("cst", [128, 8, 5]); sg = P.sb("sg", [128, 8, 5]); sc = P.sb("sc", [128, 8, 5])
    bmt = P.sb("bmt", [128, 12]); ot = P.sb("ot", [128, 12, 5])
    wt = [P.sb(f"wt{i}", [128, 8, 128]) for i in range(2)]
    pp = [P.ps(f"pp{i}", [128, 8]) for i in range(2)]
    P.dma("sp", cst[:], cs, writes=["cst"])
    P.dma("sp", bmt[:], bm, writes=["bmt"])
    P.op("act", lambda e: e.activation(sg[:], cst[:], AF.Sigmoid), reads=["cst"], writes=["sg"])
    P.op("dve", lambda e: e.tensor_tensor(sc[:], cst[:], sg[:], ALU.mult), reads=["cst", "sg"], writes=["sc"])
    for j in range(12):
        w = wt[j % 2]; wk = f"wt{j%2}"; pk = f"pp{j%2}"; p_ = pp[j % 2]
        P.dma("sp", w[:], wm[j], writes=[wk])
        for k in range(8):
            P.op("pe", lambda e, w=w, k=k, p_=p_: e.matmul(p_[:, 0:5], w[:, k, :], sc[:, k, :], start=(k == 0), stop=(k == 7)),
                 reads=[wk, "sc"], writes=[pk])
        P.op("dve", lambda e, j=j, p_=p_: e.tensor_scalar(ot[:, j, :], p_[:, 0:5], bmt[:, j:j + 1], None, ALU.add),
             reads=[pk, "bmt"], writes=["ot"])
    P.dma("sp", out, ot[:], reads=["ot"], is_output=True)
    P.finish()
    return nc


NTOK = 2176
TILES = [(0, 128)] + [(128 + 512 * i, 512) for i in range(4)]
NCT = 33


def rms_modulate(P, xT, hT, mod, nw, ones_bf, shift_i, scale_i, hT32=None, tagp="n", psb=None, hkey="hT"):
    g = P.sb(tagp + "_g", [128, 8, 2]);
    for v, (sh, sci) in enumerate(zip(shift_i, scale_i)):
        P.op("dve", lambda e, v=v, sci=sci: e.scalar_tensor_tensor(g[:, :, v], mod[:, :, sci], 1.0, nw[:, :], ALU.add, ALU.mult),
             reads=["mod", "nw"], writes=[tagp + "_g"])
    sq = [P.sb(f"{tagp}_sq{i}", [128, 8, 512], BF16) for i in range(2)]
    ss = list(psb); ssk = [f"bk_{tagp}0", f"bk_{tagp}1"]
    rs = [P.sb(f"{tagp}_rs{i}", [128, 512]) for i in range(2)]
    tmp = [P.sb(f"{tagp}_tmp{i}", [128, 512]) for i in range(2)]
    ti = 0
    for it, (t0, n) in enumerate(TILES):
        b = it % 2
        v = 1 if it == 0 else 0
        for k in range(8):
            P.op("act", lambda e, k=k, b=b, t0=t0, n=n: e.activation(sq[b][:, k, 0:n], xT[:, k, t0:t0 + n], AF.Square),
                 reads=["xT"], writes=[f"{tagp}_sq{b}"])
        for k in range(8):
            P.op("pe", lambda e, k=k, b=b, n=n: e.matmul(ss[b][:, 0:n], ones_bf[:], sq[b][:, k, 0:n], start=(k == 0), stop=(k == 7)),
                 reads=[f"{tagp}_sq{b}", "ones_bf"], writes=[ssk[b]])
        P.op("dve", lambda e, b=b, n=n: e.tensor_scalar(rs[b][:, 0:n], ss[b][:, 0:n], 1.0 / 1024, 1e-6, ALU.mult, ALU.add),
             reads=[ssk[b]], writes=[f"{tagp}_rs{b}"])
        P.op("dve", lambda e, b=b, n=n: e.reciprocal(rs[b][:, 0:n], rs[b][:, 0:n]),
             reads=[f"{tagp}_rs{b}"], writes=[f"{tagp}_rs{b}"])
        P.op("act", lambda e, b=b, n=n: e.activation(rs[b][:, 0:n], rs[b][:, 0:n], AF.Sqrt),
             reads=[f"{tagp}_rs{b}"], writes=[f"{tagp}_rs{b}"])
        for k in range(8):
            tb = ti % 2; ti += 1
            P.op("dve", lambda e, k=k, b=b, tb=tb, t0=t0, n=n, v=v: e.scalar_tensor_tensor(
                tmp[tb][:, 0:n], xT[:, k, t0:t0 + n], g[:, k, v:v + 1], rs[b][:, 0:n], ALU.mult, ALU.mult),
                 reads=["xT", tagp + "_g", f"{tagp}_rs{b}"], writes=[f"{tagp}_tmp{tb}"])
            sh = shift_i[v]
            P.op("act", lambda e, k=k, tb=tb, t0=t0, n=n, sh=sh: e.activation(
                hT[:, k, t0:t0 + n], tmp[tb][:, 0:n], AF.Identity, bias=mod[:, k, sh:sh + 1]),
                 reads=[f"{tagp}_tmp{tb}", "mod"], writes=[hkey])
            if hT32 is not None:
                P.op("pool", lambda e, k=k, tb=tb, t0=t0, n=n, sh=sh: e.tensor_scalar(
                    hT32[:, k, t0:t0 + n], tmp[tb][:, 0:n], mod[:, k, sh:sh + 1], None, ALU.add),
                     reads=[f"{tagp}_tmp{tb}", "mod"], writes=["hT32"])


def build_A(first=False):
    nc = bass.Bass("TRN2", target_bir_lowering=False)
    xTd = nc.dram_tensor("xT", [128, 8, NTOK], F32, kind="ExternalInput").ap()
    modd = nc.dram_tensor("mod", [128, 8, 6], F32, kind="ExternalInput").ap()
    fTd = nc.dram_tensor("fT", [8, 128, 8, NTOK], F32, kind="ExternalInput").ap() if not first else None
    xoutd = nc.dram_tensor("xout", [128, 8, NTOK], F32, kind="ExternalOutput").ap()
    nwd = nc.dram_tensor("nw", [128, 8], F32, kind="ExternalInput").ap()
    wind = nc.dram_tensor("win", [128, 8, NCT * 128], F32, kind="ExternalInput").ap()
    pTd = nc.dram_tensor("pT", [NCT, 128, NTOK], F32, kind="ExternalOutput").ap()
    P = Prog(nc)
    xT = P.sb("xT", [128, 8, NTOK]); hT = P.sb("hT", [128, 8, NTOK], BF16)
    mod = P.sb("mod", [128, 8, 6]); nw = P.sb("nw", [128, 8])
    wbf = P.sb("wbf", [128, 8, NCT * 128], BF16)
    ones_bf = P.sb("ones_bf", [128, 128], BF16)
    P.op("pool", lambda e: e.memset(ones_bf[:], 1.0), writes=["ones_bf"])
    for k in range(8):
        P.dma("sp", xT[:, k, :], xTd[:, k, :], writes=["xT"])
    P.dma("sp", mod[:], modd, writes=["mod"])
    P.dma("sp", nw[:], nwd, writes=["nw"])
    for k in range(8):
        P.dma("pool", wbf[:, k, :], wind[:, k, :], writes=["wbf"])
    fT = P.sb("fT", [128, 512])
    for c, k in [(c, k) for c in range(0 if first else 8) for k in range(8)]:
        for (t0, n) in TILES:
            P.dma("sp", fT[:, 0:n], fTd[c, :, k, t0:t0 + n], writes=["fT"])
            gcol = 5 if t0 == 0 else 4
            P.I("dve", "scalar_tensor_tensor", xT[:, k, t0:t0 + n], fT[:, 0:n], mod[:, k, gcol:gcol + 1], xT[:, k, t0:t0 + n], ALU.mult, ALU.add,
                r=["fT", "mod", "xT"], w=["xT"])
    for k in range(8):
        P.dma("sp", xoutd[:, k, :], xT[:, k, :], reads=["xT"], is_output=True)
    pp = [P.ps(f"bank{i}", [128, 512]) for i in range(6)]
    rms_modulate(P, xT, hT, mod, nw, ones_bf, shift_i=(0, 2), scale_i=(1, 3), tagp="n1", psb=(pp[4], pp[5]))
    st = [P.sb(f"st{i}", [128, 512]) for i in range(4)]
    i = 0
    for ct in range(NCT):
        for (t0, n) in TILES:
            b = i % 4; i += 1
            for k in range(8):
                P.op("pe", lambda e, k=k, b=b, ct=ct, t0=t0, n=n: e.matmul(
                    pp[b][:, 0:n], wbf[:, k, ct * 128:(ct + 1) * 128], hT[:, k, t0:t0 + n], start=(k == 0), stop=(k == 7)),
                     reads=["wbf", "hT"], writes=[f"bk{b}"])
            if b % 2 == 0:
                P.op("dve", lambda e, b=b, n=n: e.tensor_copy(st[b][:, 0:n], pp[b][:, 0:n]), reads=[f"bk{b}"], writes=[f"st{b}"])
            else:
                P.op("act", lambda e, b=b, n=n: e.activation(st[b][:, 0:n], pp[b][:, 0:n], AF.Copy), reads=[f"bk{b}"], writes=[f"st{b}"])
            P.dma("sp", pTd[ct, :, t0:t0 + n], st[b][:, 0:n], reads=[f"st{b}"], is_output=True)
    P.finish()
    return nc


TSEQ = 4352
QS = 64
NCH = 34
SEGS = [(0, 256), (256, 4352)]


def conv_silu(P, dst, src, cw, cb, ti, key_dst, key_src, tmp, key_tmp, out_dt_tile=None):
    for (s, e_) in SEGS:
        if cb is not None:
            P.op("act", lambda e, s=s, e_=e_: e.activation(tmp[:, s:e_], src[:, s:e_], AF.Identity, bias=cb[:, ti:ti + 1], scale=cw[:, ti, 1:2]),
                 reads=[key_src, "cw", "cb"], writes=[key_tmp])
        else:
            P.op("act", lambda e, s=s, e_=e_: e.activation(tmp[:, s:e_], src[:, s:e_], AF.Copy, scale=cw[:, ti, 1:2]),
                 reads=[key_src, "cw"], writes=[key_tmp])
        P.op("dve", lambda e, s=s, e_=e_: e.scalar_tensor_tensor(tmp[:, s + 1:e_], src[:, s:e_ - 1], cw[:, ti, 0:1], tmp[:, s + 1:e_], ALU.mult, ALU.add),
             reads=[key_src, "cw", key_tmp], writes=[key_tmp])
        P.op("dve", lambda e, s=s, e_=e_: e.scalar_tensor_tensor(tmp[:, s:e_ - 1], src[:, s + 1:e_], cw[:, ti, 2:3], tmp[:, s:e_ - 1], ALU.mult, ALU.add),
             reads=[key_src, "cw", key_tmp], writes=[key_tmp])
    P.op("act", lambda e: e.activation(dst[:, :], tmp[:, :], AF.Silu), reads=[key_tmp], writes=[key_dst])


def load_consts(P, cd):
    c = {}
    for i, nm in enumerate(["tri_f", "tri_b", "nm_f", "nm_b", "ident"]):
        t = P.sb("c_" + nm, [128, 128]); P.dma("sp", t[:], cd[i], writes=["c_" + nm]); c[nm] = t
    ones = P.sb("c_ones", [128, 128]); P.op("pool", lambda e: e.memset(ones[:], 1.0), writes=["c_ones"]); c["ones"] = ones
    idb = P.sb("c_identb", [128, 128], BF16)
    P.op("dve", lambda e: e.tensor_copy(idb[:], c["ident"][:]), reads=["c_ident"], writes=["c_identb"]); c["identb"] = idb
    return c


def host_consts():
    k = np.arange(128)[:, None]; i = np.arange(128)[None, :]
    tri_f = (k <= i).astype(np.float32); tri_b = (k >= i).astype(np.float32)
    nm_f = np.where(i >= k, 0.0, -30000.0).astype(np.float32); nm_b = np.where(i <= k, 0.0, -30000.0).astype(np.float32)
    return np.stack([tri_f, tri_b, nm_f, nm_b, np.eye(128, dtype=np.float32)])


def build_B1(stage=99):
    nc = bass.Bass("TRN2", target_bir_lowering=False)
    D = lambda n, s: nc.dram_tensor(n, s, F32, kind="ExternalInput").ap()
    zTd = D("zT", [128, TSEQ]); xbcd = D("xbcT", [3, 128, TSEQ]); dtrd = D("dtr", [128, 4 * QS])
    dtbd = D("dtb", [128, 4 * QS]); alogd = D("alog", [128, 4 * QS]); cwd = D("cw", [128, 3, 3]); cbd = D("cb", [128, 3])
    dvd = D("dvec", [128, 1]); nwd = D("normw", [128, 1]); cd = D("consts", [5, 128, 128])
    yTd = nc.dram_tensor("yT", [128, TSEQ], F32, kind="ExternalOutput").ap()
    P = Prog(nc)
    C = load_consts(P, cd)
    raw = P.sb("raw", [128, TSEQ]); tmp = P.sb("tmp", [128, TSEQ])
    xT = P.sb("xT", [128, TSEQ]); B32 = P.sb("B32", [128, TSEQ]); C32 = P.sb("C32", [128, TSEQ])
    Bb = P.sb("Bb", [128, TSEQ], BF16); Cb = P.sb("Cb", [128, TSEQ], BF16)
    cw = P.sb("cw", [128, 3, 3]); cb = P.sb("cb", [128, 3]); dvec = P.sb("dvec", [128, 1]); normw = P.sb("normw", [128, 1])
    for t, d, k in ((cw, cwd, "cw"), (cb, cbd, "cb"), (dvec, dvd, "dvec"), (normw, nwd, "normw")):
        P.dma("sp", t[:], d, writes=[k])
    if stage == 0:
        P.op("dve", lambda e: e.tensor_copy(xT[:, 0:128], C["ident"][:]), reads=["c_ident"], writes=["xT"])
        P.op("dve", lambda e: e.tensor_scalar(xT[:, 128:256], C["tri_f"][:], cw[:, 0, 0:1], dvec[:, 0:1], ALU.mult, ALU.add), reads=["c_tri_f", "cw", "dvec"], writes=["xT"])
        P.dma("sp", yTd, xT[:], reads=["xT"], is_output=True); P.finish(); return nc
    import os
    NT = int(os.environ.get("NT", "3"))
    for ti, (dst, kd) in enumerate(((xT, "xT"), (B32, "B32"), (C32, "C32"))[:NT]):
        P.dma("sp", raw[:], xbcd[ti], writes=["raw"])
        conv_silu(P, dst, raw, cw, cb, ti, kd, "raw", tmp, "tmp")
    if NT == 3 and os.environ.get("NOCAST") is None:
        P.op("pool", lambda e: e.tensor_copy(Bb[:], B32[:]), reads=["B32"], writes=["Bb"])
        P.op("pool", lambda e: e.tensor_copy(Cb[:], C32[:]), reads=["C32"], writes=["Cb"])
    if stage == 1:
        P.dma("sp", yTd, xT[:], reads=["xT"], is_output=True); P.finish(); return nc
    dtr = P.sb("dtr", [128, 4 * QS]); dtb = P.sb("dtb", [128, 4 * QS]); alog = P.sb("alog", [128, 4 * QS])
    dt = P.sb("dt", [128, 4 * QS]); la = P.sb("la", [128, 4 * QS]); ncum = P.sb("ncum", [128, 4 * QS])
    wgt = P.sb("wgt", [128, 4 * QS]); dec = P.sb("dec", [128, 4 * QS])
    P.dma("sp", dtr[:], dtrd, writes=["dtr"]); P.dma("sp", dtb[:], dtbd, writes=["dtb"]); P.dma("sp", alog[:], alogd, writes=["alog"])
    P.op("dve", lambda e: e.tensor_tensor(dtr[:], dtr[:], dtb[:], ALU.add), reads=["dtr", "dtb"], writes=["dtr"])
    P.op("dve", lambda e: e.tensor_scalar(dtr[:], dtr[:], 60.0, None, ALU.min), reads=["dtr"], writes=["dtr"])
    P.op("act", lambda e: e.activation(dtr[:], dtr[:], AF.Exp), reads=["dtr"], writes=["dtr"])
    P.op("act", lambda e: e.activation(dt[:], dtr[:], AF.Ln, bias=1.0), reads=["dtr"], writes=["dt"])
    P.op("act", lambda e: e.activation(alog[:], alog[:], AF.Exp), reads=["alog"], writes=["alog"])
    P.op("dve", lambda e: e.scalar_tensor_tensor(la[:], dt[:], -1.0, alog[:], ALU.mult, ALU.mult), reads=["dt", "alog"], writes=["la"])
    bk = [P.ps(f"bank{i}", [128, 512]) for i in range(8)]
    pc = bk[0][:, 0:4 * QS]; pt = bk[1][:, 0:4 * QS]
    P.op("pe", lambda e: e.matmul(pc[:, 0:2 * QS], C["tri_f"][:], la[:, 0:2 * QS], start=True, stop=True), reads=["la", "c_tri_f"], writes=["bk0"])
    P.op("pe", lambda e: e.matmul(pc[:, 2 * QS:4 * QS], C["tri_b"][:], la[:, 2 * QS:4 * QS], start=True, stop=True), reads=["la", "c_tri_b"], writes=["bk0"])
    P.op("pe", lambda e: e.matmul(pt, C["ones"][:], la[:], start=True, stop=True), reads=["la", "c_ones"], writes=["bk1"])
    P.op("dve", lambda e: e.tensor_scalar(ncum[:], pc, -1.0, None, ALU.mult), reads=["bk0"], writes=["ncum"])
    P.op("dve", lambda e: e.tensor_tensor(wgt[:], pt, ncum[:], ALU.add), reads=["bk1", "ncum"], writes=["wgt"])
    P.op("act", lambda e: e.activation(wgt[:], wgt[:], AF.Exp), reads=["wgt"], writes=["wgt"])
    P.op("dve", lambda e: e.tensor_tensor(wgt[:], wgt[:], dt[:], ALU.mult), reads=["wgt", "dt"], writes=["wgt"])
    P.op("act", lambda e: e.activation(dec[:], pt, AF.Exp), reads=["bk1"], writes=["dec"])
    if stage == 2:
        for i_, (t_, k_) in enumerate(((dt, "dt"), (la, "la"), (ncum, "ncum"), (wgt, "wgt"), (dec, "dec"))):
            P.dma("sp", yTd[:, i_ * 256:(i_ + 1) * 256], t_[:], reads=[k_], is_output=True)
        for i_, (t_, k_) in enumerate(((B32, "B32"), (C32, "C32"), (xT, "xT"))):
            P.dma("sp", yTd[:, 1280 + i_ * 1024:1280 + (i_ + 1) * 1024], t_[:, 0:1024], reads=[k_], is_output=True)
        P.finish(); return nc
    xpad = [P.sb(f"xpad{h}", [128, NCH, 128], BF16) for h in range(2)]
    Btok = P.sb("Btok", [128, NCH, 128], BF16); xw = P.sb("xw", [128, NCH, 4, 64], BF16)
    for h in range(2):
        P.op("pool", lambda e, h=h: e.memset(xpad[h][:], 0.0), writes=[f"xpad{h}"])
    ptr = [bk[2][:, 0:128], bk[3][:, 0:128]]
    for c in range(NCH):
        sl = slice(c * 128, (c + 1) * 128)
        P.op("pe", lambda e, sl=sl: e.transpose(ptr[0], xT[:, sl], C["ident"][:]), reads=["xT", "c_ident"], writes=["bk2"])
        P.op("pe", lambda e, sl=sl: e.transpose(ptr[1], B32[:, sl], C["ident"][:]), reads=["B32", "c_ident"], writes=["bk3"])
        for h in range(2):
            P.op("act", lambda e, h=h, c=c: e.activation(xpad[h][:, c, h * 64:(h + 1) * 64], ptr[0][:, h * 64:(h + 1) * 64], AF.Copy),
                 reads=["bk2"], writes=[f"xpad{h}"])
        for q in range(4):
            h = q % 2
            P.op("dve", lambda e, q=q, h=h, c=c: e.tensor_scalar(xw[:, c, q, :], ptr[0][:, h * 64:(h + 1) * 64], wgt[:, q * QS + c:q * QS + c + 1], None, ALU.mult),
                 reads=["bk2", "wgt"], writes=["xw"])
        P.op("act", lambda e, c=c: e.activation(Btok[:, c, :], ptr[1], AF.Copy), reads=["bk3"], writes=["Btok"])
    if stage == 3:
        P.dma("sp", yTd, xT[:], reads=["xT"], is_output=True); P.finish(); return nc
    yacc = P.sb("yacc", [128, TSEQ])
    Hpad = [P.sb(f"Hpad{q}", [128, 128]) for q in range(4)]
    larep = [P.sb(f"larep{i}", [128, 128]) for i in range(2)]
    seg = [P.sb(f"seg{i}", [128, 128]) for i in range(2)]; Et = [P.sb(f"Et{i}", [128, 128]) for i in range(2)]
    STp = [P.sb(f"STp{i}", [128, 128], BF16) for i in range(2)]; CTs = [P.sb(f"CTs{i}", [128, 128]) for i in range(2)]
    psA = bk[0][:, 0:128]; psE = bk[1][:, 0:128]; psS = bk[4][:, 0:128]; psY = bk[5][:, 0:128]
    psH = [bk[6][:, 0:64], bk[7][:, 0:64]]
    import os
    for d in range(int(os.environ.get('ND', '2'))):
        tri = C["tri_f"] if d == 0 else C["tri_b"]; nm = C["nm_f"] if d == 0 else C["nm_b"]
        trik = "c_tri_f" if d == 0 else "c_tri_b"; nmk = "c_nm_f" if d == 0 else "c_nm_b"
        order = list(range(NCH)) if d == 0 else [1, 0] + list(range(NCH - 1, 1, -1))
        for hh in range(2):
            P.op("pool", lambda e, q=d * 2 + hh: e.memset(Hpad[q][:], 0.0), writes=[f"Hpad{d*2+hh}"])
        for c in order:
            sl = slice(c * 128, (c + 1) * 128)
            P.op("pe", lambda e, sl=sl: e.matmul(psS, Bb[:, sl], Cb[:, sl], start=True, stop=True), reads=["Bb", "Cb"], writes=["bk4"])
            for hh in range(2):
                q = d * 2 + hh
                P.op("pool", lambda e, q=q, c=c, hh=hh: e.tensor_scalar(larep[hh][:], C["ones"][:], la[:, q * QS + c:q * QS + c + 1], None, ALU.mult),
                     reads=["c_ones", "la"], writes=[f"larep{hh}"])
                P.op("pe", lambda e, hh=hh, tri=tri: e.matmul(psA, larep[hh][:], tri[:], start=True, stop=False), reads=[f"larep{hh}", trik], writes=["bk0"])
                P.op("pe", lambda e, nm=nm: e.matmul(psA, C["ident"][:], nm[:], start=False, stop=True), reads=["c_ident", nmk], writes=["bk0"])
                P.op("pe", lambda e, hh=hh, tri=tri: e.matmul(psE, larep[hh][:], tri[:], start=True, stop=True), reads=[f"larep{hh}", trik], writes=["bk1"])
                P.op("act", lambda e, q=q, c=c, hh=hh: e.activation(seg[hh][:], psA, AF.Exp, bias=ncum[:, q * QS + c:q * QS + c + 1]), reads=["bk0", "ncum"], writes=[f"seg{hh}"])
                P.op("act", lambda e, hh=hh: e.activation(Et[hh][:], psE, AF.Exp), reads=["bk1"], writes=[f"Et{hh}"])
                P.op("dve", lambda e, q=q, c=c, hh=hh: e.scalar_tensor_tensor(STp[hh][:], psS, dt[:, q * QS + c:q * QS + c + 1], seg[hh][:], ALU.mult, ALU.mult),
                     reads=["bk4", "dt", f"seg{hh}"], writes=[f"STp{hh}"])
                P.op("dve", lambda e, sl=sl, hh=hh: e.tensor_tensor(CTs[hh][:], C32[:, sl], Et[hh][:], ALU.mult), reads=["C32", f"Et{hh}"], writes=[f"CTs{hh}"])
            for hh in range(2):
                q = d * 2 + hh
                P.op("pe", lambda e, hh=hh, c=c: e.matmul(psY, xpad[hh][:, c, :], STp[hh][:], start=(hh == 0), stop=False),
                     reads=[f"xpad{hh}", f"STp{hh}"], writes=["bk5"])
            for hh in range(2):
                q = d * 2 + hh
                P.op("pe", lambda e, hh=hh, q=q: e.matmul(psY, Hpad[q][:], CTs[hh][:], start=False, stop=(hh == 1)),
                     reads=[f"Hpad{q}", f"CTs{hh}"], writes=["bk5"])
            for hh in range(2):
                q = d * 2 + hh
                P.op("pe", lambda e, hh=hh, q=q, c=c: e.matmul(psH[hh], Btok[:, c, :], xw[:, c, q, :], start=True, stop=True),
                     reads=["Btok", "xw"], writes=[f"bk{6+hh}"])
                P.op("dve", lambda e, hh=hh, q=q, c=c: e.scalar_tensor_tensor(
                    Hpad[q][:, hh * 64:(hh + 1) * 64], Hpad[q][:, hh * 64:(hh + 1) * 64], dec[:, q * QS + c:q * QS + c + 1], psH[hh], ALU.mult, ALU.add),
                     reads=[f"Hpad{q}", "dec", f"bk{6+hh}"], writes=[f"Hpad{q}"])
            if d == 0:
                P.op("dve", lambda e, sl=sl: e.scalar_tensor_tensor(yacc[:, sl], xT[:, sl], dvec[:, 0:1], psY, ALU.mult, ALU.add),
                     reads=["xT", "dvec", "bk5"], writes=[f"yacc{c}"])
            else:
                P.op("dve", lambda e, sl=sl: e.tensor_tensor(yacc[:, sl], yacc[:, sl], psY, ALU.add), reads=[f"yacc{c}", "bk5"], writes=[f"yacc{c}"])
    if stage == 4:
        P.dma("sp", yTd, yacc[:], reads=[f"yacc{c}" for c in range(NCH)], is_output=True); P.finish(); return nc
    zT = raw
    P.dma("sp", zT[:], zTd, writes=["raw"])
    P.op("act", lambda e: e.activation(tmp[:], zT[:], AF.Silu), reads=["raw"], writes=["tmp"])
    allc = [f"yacc{c}" for c in range(NCH)]
    P.op("dve", lambda e: e.tensor_tensor(yacc[:], yacc[:], tmp[:], ALU.mult), reads=allc + ["tmp"], writes=allc)
    P.op("act", lambda e: e.activation(tmp[:], yacc[:], AF.Square), reads=allc, writes=["tmp"])
    pss = [bk[2][:, 0:256], bk[3][:, 0:256]]; rs = [P.sb(f"rs{i}", [128, 256]) for i in range(2)]
    for i, t0 in enumerate(range(0, TSEQ, 256)):
        n = min(256, TSEQ - t0); b = i % 2
        P.op("pe", lambda e, b=b, t0=t0, n=n: e.matmul(pss[b][:, 0:n], C["ones"][:], tmp[:, t0:t0 + n], start=True, stop=True), reads=["tmp", "c_ones"], writes=[f"bk{2+b}"])
        P.op("dve", lambda e, b=b, n=n: e.tensor_scalar(rs[b][:, 0:n], pss[b][:, 0:n], 1.0 / 128, 1e-5, ALU.mult, ALU.add), reads=[f"bk{2+b}"], writes=[f"rs{b}"])
        P.op("dve", lambda e, b=b, n=n: e.reciprocal(rs[b][:, 0:n], rs[b][:, 0:n]), reads=[f"rs{b}"], writes=[f"rs{b}"])
        P.op("act", lambda e, b=b, n=n: e.activation(rs[b][:, 0:n], rs[b][:, 0:n], AF.Sqrt), reads=[f"rs{b}"], writes=[f"rs{b}"])
        P.op("dve", lambda e, b=b, t0=t0, n=n: e.scalar_tensor_tensor(xT[:, t0:t0 + n], yacc[:, t0:t0 + n], normw[:, 0:1], rs[b][:, 0:n], ALU.mult, ALU.mult),
             reads=allc + ["normw", f"rs{b}"], writes=["xT"])
    P.dma("sp", yTd, xT[:], reads=["xT"], is_output=True)
    P.finish()
    return nc


def host_B1_inputs(pa, L, b, hp, prm):
    p = pa[b]
    z = p[:, hp * 128:(hp + 1) * 128].T
    x = p[:, 256 + hp * 128:256 + (hp + 1) * 128].T
    Bm = p[:, 512 + hp * 128:512 + (hp + 1) * 128].T
    Cm = p[:, 768 + hp * 128:768 + (hp + 1) * 128].T
    cols = [1024 + d * 4 + 2 * hp + hh for d in range(2) for hh in range(2)]
    dtr = np.zeros((128, 4, QS), np.float32); dtr[:, :, :NCH] = p[:, cols].reshape(NCH, 128, 4).transpose(1, 2, 0); dtr = dtr.reshape(128, 4 * QS)
    bc = lambda v: np.broadcast_to(np.asarray(v, np.float32)[None, :, None], (128, 4, QS)).reshape(128, 4 * QS)
    dtb = bc([prm['m_dt_bias'][L, d, 2 * hp + hh] for d in range(2) for hh in range(2)])
    alog = bc([prm['m_a_log'][L, d, 2 * hp + hh] for d in range(2) for hh in range(2)])
    cwfull = prm['m_conv_w'][L]; cbfull = prm['m_conv_b'][L]
    offs = [hp * 128, 256 + hp * 128, 512 + hp * 128]
    cw = np.stack([cwfull[:, o:o + 128].T for o in offs], 1)
    cb = np.stack([cbfull[o:o + 128] for o in offs], 1)
    dvec = np.repeat(prm['m_d'][L, 2 * hp:2 * hp + 2], 64)[:, None]
    normw = prm['m_norm_w'][L, hp * 128:(hp + 1) * 128][:, None]
    A = np.ascontiguousarray
    return {"zT": A(z), "xbcT": A(np.stack([x, Bm, Cm])), "dtr": A(dtr), "dtb": A(dtb), "alog": A(alog), "cw": A(cw), "cb": A(cb),
            "dvec": A(dvec), "normw": A(normw), "consts": host_consts()}


NPK = 34


def host_consts64():
    k = np.arange(128)[:, None]; i = np.arange(128)[None, :]
    same = (k // 64) == (i // 64)
    f = lambda m: m.astype(np.float32)
    tri_f = f(same & (k <= i)); tri_b = f(same & (k >= i))
    nm_f = np.where(same & (i >= k), 0.0, -30000.0); nm_b = np.where(same & (i <= k), 0.0, -30000.0)
    pms_f = np.where(same & (i < k), 0.0, 30000.0); pms_b = np.where(same & (i > k), 0.0, 30000.0)
    blk = f(same); selA = f(np.broadcast_to(k < 64, (128, 128))); selB = f(np.broadcast_to(k >= 64, (128, 128)))
    inc_f = f(same & (i < k)); inc_b = f(same & (i > k))
    return np.stack([np.eye(128), tri_f, tri_b, nm_f, nm_b, pms_f, pms_b, blk, selA, selB, inc_f, inc_b]).astype(np.float32)

C64_NAMES = ["ident", "tri_f", "tri_b", "nm_f", "nm_b", "pms_f", "pms_b", "blk", "selA", "selB", "sl", "su"]


def load_consts64(P, cd):
    c = {}
    for i, nm in enumerate(C64_NAMES):
        t = P.sb("c_" + nm, [128, 128]); P.dma("sp", t[:], cd[i], writes=["c_" + nm]); c[nm] = t
    ones = P.sb("c_ones", [128, 128]); P.I("pool", "memset", ones[:], 1.0, w=["c_ones"]); c["ones"] = ones
    return c


def tri_inverse_apply(P, C, Lm, X, ncolsX, bk, tg):
    Pt = [P.sb(f"{tg}P{i}", [128, 128]) for i in range(2)] if not hasattr(P, "_tri_" + tg) else getattr(P, "_tri_" + tg)[0]
    Qt = [P.sb(f"{tg}Q{i}", [128, 128]) for i in range(2)] if not hasattr(P, "_tri_" + tg) else getattr(P, "_tri_" + tg)[1]
    setattr(P, "_tri_" + tg, (Pt, Qt))
    (pP, kP), (pQ, kQ), (pT, kT), (pX, kX) = bk["P"], bk["Q"], bk["T"], bk["X"]
    ident = C["ident"]
    P.I("pe", "transpose", pT[:, 0:128], Lm[:], ident[:], r=[tg + "L", "c_ident"], w=[kT])
    P.I("act", "activation", Qt[0][:], pT[:, 0:128], AF.Copy, r=[kT], w=[f"{tg}Q0"])
    P.I("pe", "matmul", pX[:, 0:ncolsX], Qt[0][:], X[:], start=True, stop=True, r=[f"{tg}Q0", tg + "X"], w=[kX])
    P.I("dve", "tensor_tensor", X[:], X[:], pX[:, 0:ncolsX], ALU.subtract, r=[tg + "X", kX], w=[tg + "X"])
    Pc, Pk, Qc, Qk = Lm, tg + "L", Qt[0], f"{tg}Q0"
    for lvl in range(1, 6):
        a = lvl % 2
        P.I("pe", "matmul", pQ[:, 0:128], Pc[:], Qc[:], start=True, stop=True, r=[Pk, Qk], w=[kQ])
        if lvl < 5:
            P.I("pe", "matmul", pP[:, 0:128], Qc[:], Pc[:], start=True, stop=True, r=[Pk, Qk], w=[kP])
            P.I("dve", "tensor_copy", Pt[a][:], pP[:, 0:128], r=[kP], w=[f"{tg}P{a}"])
        P.I("act", "activation", Qt[a][:], pQ[:, 0:128], AF.Copy, r=[kQ], w=[f"{tg}Q{a}"])
        Pc, Pk, Qc, Qk = Pt[a], f"{tg}P{a}", Qt[a], f"{tg}Q{a}"
        P.I("pe", "matmul", pX[:, 0:ncolsX], Qc[:], X[:], start=True, stop=True, r=[Qk, tg + "X"], w=[kX])
        P.I("dve", "tensor_tensor", X[:], X[:], pX[:, 0:ncolsX], ALU.add, r=[tg + "X", kX], w=[tg + "X"])


def build_B2():
    nc = bass.Bass("TRN2", target_bir_lowering=False)
    D = lambda n, s: nc.dram_tensor(n, s, F32, kind="ExternalInput").ap()
    qkvd = D("qkvT", [3, 128, TSEQ]); gated = D("gate", [128, NPK, 128]); tabd = D("tab", [4, 128, 2 * QS])
    cwd = D("cw", [128, 3, 3]); nwd = D("normw", [128, 128]); cd = D("consts", [len(C64_NAMES), 128, 128])
    yd = nc.dram_tensor("y", [128, NPK, 128], F32, kind="ExternalOutput").ap()
    P = Prog(nc)
    C = load_consts64(P, cd)
    bkt = [P.ps(f"bank{i}", [128, 512]) for i in range(8)]
    BK = lambda i: (bkt[i], f"bk{i}")
    raw = P.sb("raw", [128, TSEQ]); tmp = P.sb("tmp", [128, TSEQ])
    qT = P.sb("qT", [128, TSEQ]); kT = P.sb("kT", [128, TSEQ])
    cw = P.sb("cw", [128, 3, 3]); P.dma("sp", cw[:], cwd, writes=["cw"])
    normw = P.sb("normw", [128, 128]); P.dma("sp", normw[:], nwd, writes=["normw"])
    ktok = P.sb("ktok", [128, NPK, 128]); vtok = P.sb("vtok", [128, NPK, 128]); oacc = P.sb("oacc", [128, NPK, 128])
    rs = [P.sb(f"rs{i}", [128, 256]) for i in range(2)]
    for ti, (dst, kd) in enumerate(((qT, "qT"), (kT, "kT"), (raw, "raw"))):
        P.dma("sp", raw[:], qkvd[ti], writes=["raw"])
        conv_silu(P, dst, raw, cw, None, ti, kd, "raw", tmp, "tmp")
        if ti < 2:
            P.I("act", "activation", tmp[:], dst[:], AF.Square, r=[kd], w=["tmp"])
            for i, t0 in enumerate(range(0, TSEQ, 256)):
                b = i % 2; (pa, pk) = BK(b)
                P.I("pe", "matmul", pa[:, 0:256], C["ones"][:], tmp[:, t0:t0 + 256], start=True, stop=True, r=["tmp", "c_ones"], w=[pk])
                P.I("dve", "tensor_scalar", rs[b][:], pa[:, 0:256], 1e-6, None, ALU.add, r=[pk], w=[f"rs{b}"])
                P.I("dve", "reciprocal", rs[b][:], rs[b][:], r=[f"rs{b}"], w=[f"rs{b}"])
                P.I("act", "activation", rs[b][:], rs[b][:], AF.Sqrt, r=[f"rs{b}"], w=[f"rs{b}"])
                sc = 128.0 ** -0.5 if ti == 0 else 1.0
                P.I("dve", "scalar_tensor_tensor", dst[:, t0:t0 + 256], dst[:, t0:t0 + 256], sc, rs[b][:], ALU.mult, ALU.mult,
                    r=[kd, f"rs{b}"], w=[kd])
    vT = raw
    for c in range(NPK):
        sl = slice(c * 128, (c + 1) * 128)
        for src, sk, dst, dk, bi in ((kT, "kT", ktok, "ktok", 0), (vT, "raw", vtok, "vtok", 1)):
            (pa, pk) = BK(bi)
            P.I("pe", "transpose", pa[:, 0:128], src[:, sl], C["ident"][:], r=[sk, "c_ident"], w=[pk])
            P.I("act" if bi else "dve", "activation" if bi else "tensor_copy", dst[:, c, :], pa[:, 0:128], *([AF.Copy] if bi else []), r=[pk], w=[dk])
    W2 = 2 * QS
    tb = {n: P.sb("t_" + n, [128, W2]) for n in ("braw", "araw", "dtb", "alog", "beta", "g", "gc", "ngc", "egc", "toend", "glA", "glB", "bw")}
    for i, n in enumerate(("braw", "araw", "dtb", "alog")):
        P.dma("sp", tb[n][:], tabd[i], writes=["t_" + n])
    P.I("act", "activation", tb["beta"][:], tb["braw"][:], AF.Sigmoid, r=["t_braw"], w=["t_beta"])
    P.I("dve", "tensor_tensor", tb["araw"][:], tb["araw"][:], tb["dtb"][:], ALU.add, r=["t_araw", "t_dtb"], w=["t_araw"])
    P.I("dve", "tensor_scalar", tb["araw"][:], tb["araw"][:], 60.0, None, ALU.min, r=["t_araw"], w=["t_araw"])
    P.I("act", "activation", tb["araw"][:], tb["araw"][:], AF.Exp, r=["t_araw"], w=["t_araw"])
    P.I("act", "activation", tb["araw"][:], tb["araw"][:], AF.Ln, bias=1.0, r=["t_araw"], w=["t_araw"])
    P.I("act", "activation", tb["alog"][:], tb["alog"][:], AF.Exp, r=["t_alog"], w=["t_alog"])
    P.I("dve", "scalar_tensor_tensor", tb["g"][:], tb["araw"][:], -1.0, tb["alog"][:], ALU.mult, ALU.mult, r=["t_araw", "t_alog"], w=["t_g"])
    (p0, k0), (p1, k1), (p2, k2), (p3, k3) = BK(0), BK(1), BK(2), BK(3)
    P.I("pe", "matmul", p0[:, 0:QS], C["tri_f"][:], tb["g"][:, 0:QS], start=True, stop=True, r=["t_g", "c_tri_f"], w=[k0])
    P.I("pe", "matmul", p0[:, QS:W2], C["tri_b"][:], tb["g"][:, QS:W2], start=True, stop=True, r=["t_g", "c_tri_b"], w=[k0])
    P.I("pe", "matmul", p1[:, 0:W2], C["blk"][:], tb["g"][:], start=True, stop=True, r=["t_g", "c_blk"], w=[k1])
    P.I("pe", "matmul", p2[:, 0:W2], C["selA"][:], tb["g"][:], start=True, stop=True, r=["t_g", "c_selA"], w=[k2])
    P.I("pe", "matmul", p3[:, 0:W2], C["selB"][:], tb["g"][:], start=True, stop=True, r=["t_g", "c_selB"], w=[k3])
    P.I("dve", "tensor_copy", tb["gc"][:], p0[:, 0:W2], r=[k0], w=["t_gc"])
    P.I("dve", "tensor_scalar", tb["ngc"][:], tb["gc"][:], -1.0, None, ALU.mult, r=["t_gc"], w=["t_ngc"])
    P.I("act", "activation", tb["egc"][:], tb["gc"][:], AF.Exp, r=["t_gc"], w=["t_egc"])
    P.I("dve", "tensor_tensor", tb["toend"][:], p1[:, 0:W2], tb["gc"][:], ALU.subtract, r=[k1, "t_gc"], w=["t_toend"])
    P.I("act", "activation", tb["toend"][:], tb["toend"][:], AF.Exp, r=["t_toend"], w=["t_toend"])
    P.I("act", "activation", tb["glA"][:], p2[:, 0:W2], AF.Exp, r=[k2], w=["t_glA"])
    P.I("act", "activation", tb["glB"][:], p3[:, 0:W2], AF.Exp, r=[k3], w=["t_glB"])
    P.I("dve", "tensor_tensor", tb["bw"][:], tb["beta"][:], tb["egc"][:], ALU.mult, r=["t_beta", "t_egc"], w=["t_bw"])
    S = P.sb("S", [128, 128]); grep = P.sb("grep", [128, 128])
    DmT = P.sb("DmT", [128, 128]); DmS = P.sb("DmS", [128, 128]); Et = P.sb("Et", [128, 128])
    attnT = P.sb("attnT", [128, 128]); Lm = P.sb("dnL", [128, 128]); qdT = P.sb("qdT", [128, 128])
    X = P.sb("dnX", [128, 256]); kdec = P.sb("kdec", [128, 128]); wT = P.sb("wT", [128, 128]); vnew = P.sb("vnew", [128, 128])
    bkinv = {"P": BK(0), "Q": BK(1), "T": BK(2), "X": BK(3)}
    for d in range(2):
        sfx = "_f" if d == 0 else "_b"
        tri, nm, pms = C["tri" + sfx], C["nm" + sfx], C["pms" + sfx]
        order = list(range(NPK)) if d == 0 else [1, 0] + list(range(NPK - 1, 1, -1))
        P.I("pool", "memset", S[:], 0.0, w=["S"])
        for c in order:
            sl = slice(c * 128, (c + 1) * 128); col = d * QS + c; cs = slice(col, col + 1)
            (pG, kG), (pA, kA), (pD1, kD1), (pD2, kD2), (pE, kE), (pV, kV), (pO, kO), (pS, kS) = [BK(i) for i in range(8)]
            P.I("pe", "matmul", pG[:, 0:128], kT[:, sl], kT[:, sl], start=True, stop=True, r=["kT"], w=[kG])
            P.I("pe", "matmul", pA[:, 0:128], kT[:, sl], qT[:, sl], start=True, stop=True, r=["kT", "qT"], w=[kA])
            P.I("pool", "tensor_scalar", grep[:], C["ones"][:], tb["g"][:, cs], None, ALU.mult, r=["c_ones", "t_g"], w=["grep"])
            P.I("pe", "matmul", pD1[:, 0:128], grep[:], tri[:], start=True, stop=False, r=["grep", "c_tri" + sfx], w=[kD1])
            P.I("pe", "matmul", pD1[:, 0:128], C["ident"][:], nm[:], start=False, stop=True, r=["c_ident", "c_nm" + sfx], w=[kD1])
            P.I("pe", "matmul", pD2[:, 0:128], grep[:], tri[:], start=True, stop=False, r=["grep", "c_tri" + sfx], w=[kD2])
            P.I("pe", "matmul", pD2[:, 0:128], C["ident"][:], pms[:], start=False, stop=True, r=["c_ident", "c_pms" + sfx], w=[kD2])
            P.I("pe", "matmul", pE[:, 0:128], grep[:], tri[:], start=True, stop=True, r=["grep", "c_tri" + sfx], w=[kE])
            P.I("act", "activation", DmT[:], pD1[:, 0:128], AF.Exp, bias=tb["ngc"][:, cs], r=[kD1, "t_ngc"], w=["DmT"])
            P.I("act", "activation", DmS[:], pD2[:, 0:128], AF.Exp, bias=tb["gc"][:, cs], scale=-1.0, r=[kD2, "t_gc"], w=["DmS"])
            P.I("act", "activation", Et[:], pE[:, 0:128], AF.Exp, r=[kE], w=["Et"])
            P.I("dve", "tensor_tensor", attnT[:], pA[:, 0:128], DmT[:], ALU.mult, r=[kA, "DmT"], w=["attnT"])
            P.I("dve", "scalar_tensor_tensor", Lm[:], pG[:, 0:128], tb["beta"][:, cs], DmS[:], ALU.mult, ALU.mult, r=[kG, "t_beta", "DmS"], w=["dnL"])
            P.I("dve", "tensor_tensor", qdT[:], qT[:, sl], Et[:], ALU.mult, r=["qT", "Et"], w=["qdT"])
            P.I("dve", "tensor_scalar", X[:, 0:128], vtok[:, c, :], tb["beta"][:, cs], None, ALU.mult, r=["vtok", "t_beta"], w=["dnX"])
            P.I("dve", "tensor_scalar", X[:, 128:256], ktok[:, c, :], tb["bw"][:, cs], None, ALU.mult, r=["ktok", "t_bw"], w=["dnX"])
            P.I("pool", "tensor_scalar", kdec[:], ktok[:, c, :], tb["toend"][:, cs], None, ALU.mult, r=["ktok", "t_toend"], w=["kdec"])
            tri_inverse_apply(P, C, Lm, X, 256, bkinv, "dn")
            P.I("pe", "transpose", pD1[:, 0:128], X[:, 128:256], C["ident"][:], r=["dnX", "c_ident"], w=[kD1])
            P.I("act", "activation", wT[:], pD1[:, 0:128], AF.Copy, r=[kD1], w=["wT"])
            for half in ((0, 1) if d == 0 else (1, 0)):
                rows = slice(half * 64, (half + 1) * 64)
                gl = tb["glA"] if half == 0 else tb["glB"]; glk = "t_glA" if half == 0 else "t_glB"
                P.I("pe", "matmul", pV[:, 0:128], wT[:], S[:], start=True, stop=True, r=["wT", "S"], w=[kV])
                P.I("dve", "tensor_tensor", vnew[rows, :], X[rows, 0:128], pV[rows, 0:128], ALU.subtract, r=["dnX", kV], w=["vnew"])
                P.I("pe", "matmul", pO[:, 0:128], qdT[:], S[:], start=True, stop=False, r=["qdT", "S"], w=[kO])
                P.I("pe", "matmul", pO[:, 0:128], attnT[rows, :], vnew[rows, :], start=False, stop=True, r=["attnT", "vnew"], w=[kO])
                if d == 0:
                    P.I("act", "activation", oacc[rows, c, :], pO[rows, 0:128], AF.Copy, r=[kO], w=[f"oacc{c}"])
                else:
                    P.I("dve", "tensor_tensor", oacc[rows, c, :], oacc[rows, c, :], pO[rows, 0:128], ALU.add, r=[kO, f"oacc{c}"], w=[f"oacc{c}"])
                P.I("pe", "matmul", pS[:, 0:128], kdec[rows, :], vnew[rows, :], start=True, stop=True, r=["kdec", "vnew"], w=[kS])
                P.I("dve", "scalar_tensor_tensor", S[:], S[:], gl[:, cs], pS[:, 0:128], ALU.mult, ALU.add, r=["S", glk, kS], w=["S"])
    allo = [f"oacc{c}" for c in range(NPK)]
    gate = P.sb("gate", [128, NPK, 128]); sq = P.sb("sq", [128, NPK, 128]); ss = P.sb("ss", [128, NPK])
    P.dma("sp", gate[:], gated, writes=["gate"])
    P.I("act", "activation", gate[:], gate[:], AF.Silu, r=["gate"], w=["gate"])
    P.I("act", "activation", sq[:], oacc[:], AF.Square, r=allo, w=["sq"])
    P.I("dve", "tensor_reduce", ss[:], sq[:], AX.X, ALU.add, r=["sq"], w=["ss"])
    P.I("dve", "tensor_scalar", ss[:], ss[:], 1.0 / 128, 1e-6, ALU.mult, ALU.add, r=["ss"], w=["ss"])
    P.I("dve", "reciprocal", ss[:], ss[:], r=["ss"], w=["ss"])
    P.I("act", "activation", ss[:], ss[:], AF.Sqrt, r=["ss"], w=["ss"])
    for c in range(NPK):
        P.I("dve", "scalar_tensor_tensor", sq[:, c, :], oacc[:, c, :], ss[:, c:c + 1], normw[:], ALU.mult, ALU.mult, r=allo + ["ss", "normw"], w=["sq"])
    P.I("dve", "tensor_tensor", sq[:], sq[:], gate[:], ALU.mult, r=["sq", "gate"], w=["sq"])
    P.dma("sp", yd, sq[:], reads=["sq"], is_output=True)
    P.finish()
    return nc


def colmajor_perm():
    t = np.arange(4096).reshape(64, 64)
    return t.T.reshape(-1)


def host_B2_inputs(pb, L, b, head, prm):
    perm = np.concatenate([np.arange(256), 256 + colmajor_perm()])
    p = pb[b][perm]
    q = p[:, head * 128:(head + 1) * 128].T; k = p[:, 512 + head * 128:512 + (head + 1) * 128].T
    v = p[:, 1024 + head * 128:1024 + (head + 1) * 128].T
    gate = p[:, 1536 + head * 128:1536 + (head + 1) * 128].reshape(NPK, 128, 128).transpose(1, 0, 2)
    def tabl(cols):
        t = np.zeros((128, 2, QS), np.float32); t[:, :, :NPK] = p[:, cols].reshape(NPK, 128, 2).transpose(1, 2, 0); return t.reshape(128, 2 * QS)
    braw = tabl([2048 + d * 4 + head for d in range(2)]); araw = tabl([2056 + d * 4 + head for d in range(2)])
    bc = lambda v_: np.broadcast_to(np.asarray(v_, np.float32)[None, :, None], (128, 2, QS)).reshape(128, 2 * QS)
    dtb = bc(prm['dn_dt_bias'][L, :, head]); alog = bc(prm['dn_a_log'][L, :, head])
    cwf = prm['dn_conv_w'][L]
    cw = np.stack([cwf[:, o + head * 128:o + (head + 1) * 128].T for o in (0, 512, 1024)], 1)
    normw = np.broadcast_to(prm['dn_norm_w'][L][None, :], (128, 128))
    A = lambda a: np.ascontiguousarray(a, dtype=np.float32)
    return {"qkvT": A(np.stack([q, k, v])), "gate": A(gate), "tab": A(np.stack([braw, araw, dtb, alog])), "cw": A(cw), "normw": A(normw),
            "consts": host_consts64()}


def host_B2_output(y):
    yy = y.transpose(1, 0, 2).reshape(TSEQ, 128)
    out = np.empty_like(yy)
    perm = np.concatenate([np.arange(256), 256 + colmajor_perm()])
    out[perm] = yy
    return out


def build_B3():
    nc = bass.Bass("TRN2", target_bir_lowering=False)
    D = lambda n, s: nc.dram_tensor(n, s, F32, kind="ExternalInput").ap()
    p64d = D("p64", [4, 64, TSEQ]); p128d = D("p128", [2, 128, TSEQ]); mu64d = D("mu64", [4, 64, 8]); mu128d = D("mu128", [2, 128, 8])
    pvd = D("pv", [64, 8]); a2d = D("a2h", [64, 64]); g2d = D("g2h", [128, 64]); w2d = D("w2pad", [2, 128, 64])
    cd = D("consts", [len(C64_NAMES), 128, 128])
    yd = nc.dram_tensor("y", [64, TSEQ], F32, kind="ExternalOutput").ap()
    P = Prog(nc)
    C = load_consts64(P, cd)
    bkt = [P.ps(f"bank{i}", [128, 512]) for i in range(8)]
    BK = lambda i: (bkt[i], f"bk{i}")
    raw = P.sb("raw", [128, TSEQ]); mix = P.sb("mix", [128, TSEQ])
    mu64 = P.sb("mu64", [64, 4, 8]); mu128 = P.sb("mu128", [128, 2, 8]); pv = P.sb("pv", [64, 8])
    for i in range(4):
        P.dma("sp", mu64[:, i, :], mu64d[i], writes=["mu64"])
    for i in range(2):
        P.dma("sp", mu128[:, i, :], mu128d[i], writes=["mu128"])
    P.dma("sp", pv[:], pvd, writes=["pv"])
    a2h = P.sb("a2h", [64, 64]); g2h = P.sb("g2h", [128, 64]); w2p = P.sb("w2p", [128, 2, 64])
    P.dma("sp", a2h[:], a2d, writes=["a2h"]); P.dma("sp", g2h[:], g2d, writes=["g2h"])
    for j in range(2):
        P.dma("sp", w2p[:, j, :], w2d[j], writes=["w2p"])
    omm64 = P.sb("omm64", [64, 4]); omm128 = P.sb("omm128", [128, 2])
    P.I("dve", "tensor_scalar", omm64[:], mu64[:, :, 0], -1.0, 1.0, ALU.mult, ALU.add, r=["mu64"], w=["omm64"])
    P.I("dve", "tensor_scalar", omm128[:], mu128[:, :, 0], -1.0, 1.0, ALU.mult, ALU.add, r=["mu128"], w=["omm128"])

    def token_mix(dst, dk, src_d, npart, mu, muk, omm, ommk, ti):
        R = slice(0, npart)
        P.dma("sp", raw[R, :], src_d, writes=["raw"])
        P.I("dve", "tensor_scalar", dst[R, :], raw[R, :], omm[R, ti:ti + 1], None, ALU.mult, r=["raw", ommk], w=[dk])
        def acc(o0, o1, i0, i1, mcol, eng="dve"):
            P.I(eng, "scalar_tensor_tensor", dst[R, o0:o1], raw[R, i0:i1], mu[R, ti, mcol:mcol + 1], dst[R, o0:o1], ALU.mult, ALU.add,
                r=["raw", muk, dk], w=[dk])
        acc(1, 256, 0, 255, 5); acc(0, 255, 1, 256, 6)
        acc(256 + 64, TSEQ, 256, TSEQ - 64, 3); acc(256, TSEQ - 64, 256 + 64, TSEQ, 4)
        dl = dst[R, 256:TSEQ].rearrange("p (r c) -> p r c", c=64); rl = raw[R, 256:TSEQ].rearrange("p (r c) -> p r c", c=64)
        P.I("dve", "scalar_tensor_tensor", dl[:, :, 1:64], rl[:, :, 0:63], mu[R, ti, 1:2], dl[:, :, 1:64], ALU.mult, ALU.add, r=["raw", muk, dk], w=[dk])
        P.I("dve", "scalar_tensor_tensor", dl[:, :, 0:63], rl[:, :, 1:64], mu[R, ti, 2:3], dl[:, :, 0:63], ALU.mult, ALU.add, r=["raw", muk, dk], w=[dk])

    rT = P.sb("rT", [64, TSEQ]); kT = P.sb("kT", [64, TSEQ]); vT = P.sb("vT", [64, TSEQ]); aT = P.sb("aT", [64, TSEQ])
    gT = P.sb("gT", [64, TSEQ]); bT = P.sb("bT", [64, TSEQ]); lwT = [P.sb(f"lwT{j}", [64, TSEQ]) for j in range(2)]
    token_mix(rT, "rT", p64d[0], 64, mu64, "mu64", omm64, "omm64", 0)
    token_mix(kT, "kT", p64d[1], 64, mu64, "mu64", omm64, "omm64", 1)
    token_mix(vT, "vT", p64d[2], 64, mu64, "mu64", omm64, "omm64", 2)
    NB = 256
    token_mix(mix, "mix", p64d[3], 64, mu64, "mu64", omm64, "omm64", 3)
    for i, t0 in enumerate(range(0, TSEQ, NB)):
        (pa, pk) = BK(i % 2)
        P.I("pe", "matmul", pa[0:64, 0:NB], a2h[:], mix[0:64, t0:t0 + NB], start=True, stop=True, r=["a2h", "mix"], w=[pk])
        P.I("act", "activation", aT[:, t0:t0 + NB], pa[0:64, 0:NB], AF.Sigmoid, bias=pv[:, 0:1], r=[pk, "pv"], w=["aT"])
    token_mix(mix, "mix", p128d[0], 128, mu128, "mu128", omm128, "omm128", 0)
    P.I("act", "activation", mix[:], mix[:], AF.Tanh, r=["mix"], w=["mix"])
    for j in range(2):
        for i, t0 in enumerate(range(0, TSEQ, NB)):
            (pa, pk) = BK(i % 2)
            P.I("pe", "matmul", pa[0:64, 0:NB], w2p[:, j, :], mix[:, t0:t0 + NB], start=True, stop=True, r=["w2p", "mix"], w=[pk])
            P.I("act", "activation", lwT[j][:, t0:t0 + NB], pa[0:64, 0:NB], AF.Sigmoid, bias=pv[:, 3 + j:4 + j], r=[pk, "pv"], w=[f"lwT{j}"])
        P.I("dve", "tensor_scalar", lwT[j][:], lwT[j][:], -float(np.exp(-0.5)), None, ALU.mult, r=[f"lwT{j}"], w=[f"lwT{j}"])
    token_mix(mix, "mix", p128d[1], 128, mu128, "mu128", omm128, "omm128", 1)
    P.I("act", "activation", mix[:], mix[:], AF.Sigmoid, r=["mix"], w=["mix"])
    for i, t0 in enumerate(range(0, TSEQ, NB)):
        (pa, pk) = BK(i % 2)
        P.I("pe", "matmul", pa[0:64, 0:NB], g2h[:], mix[:, t0:t0 + NB], start=True, stop=True, r=["g2h", "mix"], w=[pk])
        P.I("act", "activation", gT[:, t0:t0 + NB], pa[0:64, 0:NB], AF.Copy, r=[pk], w=["gT"])
    kk = mix
    P.I("dve", "tensor_scalar", kk[0:64, :], kT[:], pv[:, 1:2], None, ALU.mult, r=["kT", "pv"], w=["mix"])
    P.I("act", "activation", raw[0:64, :], kk[0:64, :], AF.Square, r=["mix"], w=["raw"])
    rs = [P.sb(f"rs{i}", [64, NB]) for i in range(2)]
    for i, t0 in enumerate(range(0, TSEQ, NB)):
        b = i % 2; (pa, pk) = BK(b)
        P.I("pe", "matmul", pa[0:64, 0:NB], C["ones"][0:64, 0:64], raw[0:64, t0:t0 + NB], start=True, stop=True, r=["raw", "c_ones"], w=[pk])
        P.I("dve", "tensor_scalar", rs[b][:], pa[0:64, 0:NB], 1e-6, None, ALU.add, r=[pk], w=[f"rs{b}"])
        P.I("dve", "reciprocal", rs[b][:], rs[b][:], r=[f"rs{b}"], w=[f"rs{b}"])
        P.I("act", "activation", rs[b][:], rs[b][:], AF.Sqrt, r=[f"rs{b}"], w=[f"rs{b}"])
        P.I("dve", "tensor_tensor", kk[0:64, t0:t0 + NB], kk[0:64, t0:t0 + NB], rs[b][:], ALU.mult, r=["mix", f"rs{b}"], w=["mix"])
    P.I("dve", "tensor_tensor", bT[:], kk[0:64, :], aT[:], ALU.mult, r=["mix", "aT"], w=["bT"])
    P.I("dve", "tensor_scalar", kk[0:64, :], kk[0:64, :], -1.0, None, ALU.mult, r=["mix"], w=["mix"])
    P.I("dve", "tensor_scalar", aT[:], aT[:], -1.0, pv[:, 2:3], ALU.add, ALU.mult, r=["aT", "pv"], w=["aT"])
    P.I("dve", "scalar_tensor_tensor", kT[:], aT[:], 1.0, kT[:], ALU.add, ALU.mult, r=["aT", "kT"], w=["kT"])
    avT = kk
    oacc = P.sb("oacc", [128, NPK, 64])
    H = P.sb("H", [64, 64])
    T_ = lambda n, sh: P.sb(n, sh)
    lwtok = T_("lwtok", [128, 64]); ea_tok = T_("ea_tok", [128, 64]); te_tok = T_("te_tok", [128, 64])
    ep = T_("ep", [64, 128]); em = T_("em", [64, 128]); eaT = T_("eaT", [64, 128])
    atl = T_("atl", [64, 128]); btl = T_("btl", [64, 128]); ktl = T_("ktl", [64, 128]); rtl = T_("rtl", [64, 128])
    Lm = T_("rwL", [128, 128]); AakT = T_("AakT", [128, 128]); ArbT = T_("ArbT", [128, 128]); ArkT = T_("ArkT", [128, 128])
    X = T_("rwX", [128, 128]); Bh = T_("Bh", [128, 64]); Kh = T_("Kh", [128, 64]); W1T = T_("W1T", [64, 128]); U = T_("U", [128, 64])
    pc2 = T_("pc2", [64, 2])
    tk = {n: T_(n + "_t", [128, 64]) for n in ("av", "b", "k", "v")}
    bkinv = {"P": BK(0), "Q": BK(1), "T": BK(2), "X": BK(3)}
    for d in range(2):
        sfx = "_f" if d == 0 else "_b"
        tri = C["tri" + sfx]; trik = "c_tri" + sfx
        m_strict_ts = C["sl"] if d == 0 else C["su"]; mk_ts = "c_sl" if d == 0 else "c_su"
        m_strict_st = C["su"] if d == 0 else C["sl"]; mk_st = "c_su" if d == 0 else "c_sl"
        m_incl_st = C["tri_f"] if d == 0 else C["tri_b"]; mk_in = trik
        order = list(range(NPK)) if d == 0 else [1, 0] + list(range(NPK - 1, 1, -1))
        P.I("pool", "memset", H[:], 0.0, w=["H"])
        for c in order:
            sl = slice(c * 128, (c + 1) * 128)
            (p0, k0), (p1, k1), (p2, k2), (p3, k3), (p4, k4), (p5, k5), (p6, k6), (p7, k7) = [BK(i) for i in range(8)]
            lw = lwT[d]; lwk = f"lwT{d}"
            for ii, (n, src, sk) in enumerate((("av", avT, "mix"), ("b", bT, "bT"), ("k", kT, "kT"), ("v", vT, "vT"))):
                (pa, pk) = BK(4 + ii)
                P.I("pe", "transpose", pa[:, 0:64], src[0:64, sl], C["ident"][0:64, 0:64], r=[sk, "c_ident"], w=[pk])
                if ii % 2:
                    P.I("act", "activation", tk[n][:], pa[:, 0:64], AF.Copy, r=[pk], w=[n + "_t"])
                else:
                    P.I("dve", "tensor_copy", tk[n][:], pa[:, 0:64], r=[pk], w=[n + "_t"])
            P.I("pe", "transpose", p0[:, 0:64], lw[:, sl], C["ident"][0:64, 0:64], r=[lwk, "c_ident"], w=[k0])
            P.I("dve", "tensor_copy", lwtok[:], p0[:, 0:64], r=[k0], w=["lwtok"])
            P.I("pe", "matmul", p1[:, 0:64], tri[:], lwtok[:], start=True, stop=True, r=[trik, "lwtok"], w=[k1])
            P.I("pe", "matmul", p2[:, 0:64], C["blk"][:], lwtok[:], start=True, stop=True, r=["c_blk", "lwtok"], w=[k2])
            P.I("pe", "matmul", p3[0:64, 0:128], lwtok[:], tri[:], start=True, stop=True, r=[trik, "lwtok"], w=[k3])
            P.I("dve", "tensor_tensor", ea_tok[:], p1[:, 0:64], lwtok[:], ALU.subtract, r=[k1, "lwtok"], w=["ea_tok"])
            P.I("act", "activation", ea_tok[:], ea_tok[:], AF.Exp, r=["ea_tok"], w=["ea_tok"])
            P.I("dve", "tensor_copy", te_tok[:], p1[:, 0:64], r=[k1], w=["te_tok"])
            P.I("dve", "tensor_tensor", te_tok[:], p2[:, 0:64], te_tok[:], ALU.subtract, r=[k2, "te_tok"], w=["te_tok"])
            P.I("act", "activation", te_tok[:], te_tok[:], AF.Exp, r=["te_tok"], w=["te_tok"])
            P.I("act", "activation", ep[:], p3[0:64, 0:128], AF.Exp, r=[k3], w=["ep"])
            P.I("act", "activation", em[:], p3[0:64, 0:128], AF.Exp, scale=-1.0, r=[k3], w=["em"])
            P.I("dve", "tensor_tensor", eaT[:], p3[0:64, 0:128], lw[:, sl], ALU.subtract, r=[k3, lwk], w=["eaT"])
            P.I("act", "activation", eaT[:], eaT[:], AF.Exp, r=["eaT"], w=["eaT"])
            cA, cB = (63, 127) if d == 0 else (0, 64)
            P.I("act", "activation", pc2[:, 0:1], p3[0:64, cA:cA + 1], AF.Exp, r=[k3], w=["pc2"])
            P.I("act", "activation", pc2[:, 1:2], p3[0:64, cB:cB + 1], AF.Exp, r=[k3], w=["pc2"])
            P.I("dve", "tensor_tensor", atl[:], avT[0:64, sl], eaT[:], ALU.mult, r=["mix", "eaT"], w=["atl"])
            P.I("dve", "tensor_tensor", btl[:], bT[:, sl], em[:], ALU.mult, r=["bT", "em"], w=["btl"])
            P.I("pool", "tensor_tensor", ktl[:], kT[:, sl], em[:], ALU.mult, r=["kT", "em"], w=["ktl"])
            P.I("pool", "tensor_tensor", rtl[:], rT[:, sl], ep[:], ALU.mult, r=["rT", "ep"], w=["rtl"])
            P.I("pe", "matmul", p4[:, 0:128], atl[:], btl[:], start=True, stop=True, r=["atl", "btl"], w=[k4])
            P.I("pe", "matmul", p5[:, 0:128], ktl[:], atl[:], start=True, stop=True, r=["ktl", "atl"], w=[k5])
            P.I("pe", "matmul", p6[:, 0:128], btl[:], rtl[:], start=True, stop=True, r=["btl", "rtl"], w=[k6])
            P.I("pe", "matmul", p7[:, 0:128], ktl[:], rtl[:], start=True, stop=True, r=["ktl", "rtl"], w=[k7])
            P.I("dve", "scalar_tensor_tensor", Lm[:], p4[:, 0:128], -1.0, m_strict_ts[:], ALU.mult, ALU.mult, r=[k4, mk_ts], w=["rwL"])
            P.I("dve", "tensor_tensor", AakT[:], p5[:, 0:128], m_strict_st[:], ALU.mult, r=[k5, mk_st], w=["AakT"])
            P.I("dve", "tensor_tensor", ArbT[:], p6[:, 0:128], m_incl_st[:], ALU.mult, r=[k6, mk_in], w=["ArbT"])
            P.I("dve", "tensor_tensor", ArkT[:], p7[:, 0:128], m_incl_st[:], ALU.mult, r=[k7, mk_in], w=["ArkT"])
            P.I("dve", "tensor_tensor", X[:, 0:64], tk["av"][:], ea_tok[:], ALU.mult, r=["av_t", "ea_tok"], w=["rwX"])
            P.I("pe", "matmul", p4[:, 0:64], AakT[:], tk["v"][:], start=True, stop=True, r=["AakT", "v_t"], w=[k4])
            P.I("act", "activation", X[:, 64:128], p4[:, 0:64], AF.Copy, r=[k4], w=["rwX"])
            P.I("pool", "tensor_tensor", Bh[:], tk["b"][:], te_tok[:], ALU.mult, r=["b_t", "te_tok"], w=["Bh"])
            P.I("pool", "tensor_tensor", Kh[:], tk["k"][:], te_tok[:], ALU.mult, r=["k_t", "te_tok"], w=["Kh"])
            tri_inverse_apply(P, C, Lm, X, 128, bkinv, "rw")
            P.I("pe", "transpose", p2[0:64, 0:128], X[:, 0:64], C["ident"][:], r=["rwX", "c_ident"], w=[k2])
            P.I("act", "activation", W1T[:], p2[0:64, 0:128], AF.Copy, r=[k2], w=["W1T"])
            for half in ((0, 1) if d == 0 else (1, 0)):
                rows = slice(half * 64, (half + 1) * 64)
                P.I("pe", "matmul", p5[:, 0:64], W1T[:], H[:], start=True, stop=True, r=["W1T", "H"], w=[k5])
                P.I("dve", "tensor_tensor", U[rows, :], X[rows, 64:128], p5[rows, 0:64], ALU.add, r=["rwX", k5], w=["U"])
                P.I("pe", "matmul", p6[:, 0:64], rtl[:], H[:], start=True, stop=False, r=["rtl", "H"], w=[k6])
                P.I("pe", "matmul", p6[:, 0:64], ArbT[rows, :], U[rows, :], start=False, stop=False, r=["ArbT", "U"], w=[k6])
                P.I("pe", "matmul", p6[:, 0:64], ArkT[rows, :], tk["v"][rows, :], start=False, stop=True, r=["ArkT", "v_t"], w=[k6])
                if d == 0:
                    P.I("act", "activation", oacc[rows, c, :], p6[rows, 0:64], AF.Copy, r=[k6], w=[f"oacc{c}"])
                else:
                    P.I("dve", "tensor_tensor", oacc[rows, c, :], oacc[rows, c, :], p6[rows, 0:64], ALU.add, r=[k6, f"oacc{c}"], w=[f"oacc{c}"])
                P.I("pe", "matmul", p7[0:64, 0:64], Bh[rows, :], U[rows, :], start=True, stop=False, r=["Bh", "U"], w=[k7])
                P.I("pe", "matmul", p7[0:64, 0:64], Kh[rows, :], tk["v"][rows, :], start=False, stop=True, r=["Kh", "v_t"], w=[k7])
                P.I("dve", "scalar_tensor_tensor", H[:], H[:], pc2[:, half:half + 1], p7[0:64, 0:64], ALU.mult, ALU.add, r=["H", "pc2", k7], w=["H"])
    allo = [f"oacc{c}" for c in range(NPK)]
    yT = raw; t1 = aT; t2 = bT
    for c in range(NPK):
        (pa, pk) = BK(c % 4)
        P.I("pe", "transpose", pa[0:64, 0:128], oacc[:, c, :], C["ident"][:], r=allo + ["c_ident"], w=[pk])
        P.I("act" if c % 2 else "dve", "activation" if c % 2 else "tensor_copy", yT[0:64, c * 128:(c + 1) * 128], pa[0:64, 0:128], *([AF.Copy] if c % 2 else []),
            r=[pk], w=["raw"])
    on64 = C["ones"][0:64, 0:64]
    for i, t0 in enumerate(range(0, TSEQ, NB)):
        ts = slice(t0, t0 + NB); b = i % 2
        (pm, km), (pvv, kvv), (pb, kb) = BK(b * 3), BK(b * 3 + 1), BK(b * 3 + 2)
        P.I("pe", "matmul", pm[0:64, 0:NB], on64, yT[0:64, ts], start=True, stop=True, r=["raw", "c_ones"], w=[km])
        P.I("dve", "scalar_tensor_tensor", yT[0:64, ts], pm[0:64, 0:NB], -1.0 / 64, yT[0:64, ts], ALU.mult, ALU.add, r=[km, "raw"], w=["raw"])
        P.I("act", "activation", t1[:, ts], yT[0:64, ts], AF.Square, r=["raw"], w=["aT"])
        P.I("pe", "matmul", pvv[0:64, 0:NB], on64, t1[:, ts], start=True, stop=True, r=["aT", "c_ones"], w=[kvv])
        P.I("dve", "tensor_scalar", rs[b][:], pvv[0:64, 0:NB], 1.0 / 64, 64e-5, ALU.mult, ALU.add, r=[kvv], w=[f"rs{b}"])
        P.I("dve", "reciprocal", rs[b][:], rs[b][:], r=[f"rs{b}"], w=[f"rs{b}"])
        P.I("act", "activation", rs[b][:], rs[b][:], AF.Sqrt, r=[f"rs{b}"], w=[f"rs{b}"])
        P.I("dve", "scalar_tensor_tensor", yT[0:64, ts], yT[0:64, ts], pv[:, 5:6], rs[b][:], ALU.mult, ALU.mult, r=["raw", "pv", f"rs{b}"], w=["raw"])
        P.I("dve", "scalar_tensor_tensor", t2[:, ts], rT[:, ts], pv[:, 7:8], kT[:, ts], ALU.mult, ALU.mult, r=["rT", "pv", "kT"], w=["bT"])
        P.I("pe", "matmul", pb[0:64, 0:NB], on64, t2[:, ts], start=True, stop=True, r=["bT", "c_ones"], w=[kb])
        P.I("dve", "tensor_tensor", t2[:, ts], pb[0:64, 0:NB], vT[:, ts], ALU.mult, r=[kb, "vT"], w=["bT"])
        P.I("dve", "scalar_tensor_tensor", yT[0:64, ts], yT[0:64, ts], pv[:, 6:7], t2[:, ts], ALU.add, ALU.add, r=["raw", "pv", "bT"], w=["raw"])
        P.I("dve", "tensor_tensor", yT[0:64, ts], yT[0:64, ts], gT[:, ts], ALU.mult, r=["raw", "gT"], w=["raw"])
    P.dma("sp", yd, yT[0:64, :], reads=["raw"], is_output=True)
    P.finish()
    return nc


def host_B3_inputs(pc_, L, b, head, prm):
    p = pc_[b]
    hc = slice(head * 64, (head + 1) * 64)
    mu = prm['rw_mu'][L]
    def seg(o, n):
        cols = np.arange(o, o + n); m = mu[cols]; cl = cols % 4
        tab = np.zeros((n, 8), np.float32)
        tab[:, 0] = m
        for j in range(4):
            tab[:, 1 + j] = np.where(cl == j, m, 0.0)
        tab[:, 5] = np.where(cl % 2 == 0, m, 0.0); tab[:, 6] = np.where(cl % 2 == 1, m, 0.0)
        return p[:, cols].T, tab
    s64 = [seg(head * 64, 64), seg(256 + head * 64, 64), seg(512 + head * 64, 64), seg(896, 64)]
    s128 = [seg(768, 128), seg(960, 128)]
    pv = np.zeros((64, 8), np.float32)
    pv[:, 0] = prm['rw_a0'][L][hc]; pv[:, 1] = prm['rw_k_k'][L][hc]; pv[:, 2] = prm['rw_k_a'][L][hc]
    pv[:, 3] = prm['rw_w0'][L][0][hc]; pv[:, 4] = prm['rw_w0'][L][1][hc]
    pv[:, 5] = prm['rw_ln_w'][L][hc]; pv[:, 6] = prm['rw_ln_b'][L][hc]; pv[:, 7] = prm['rw_r_k'][L][head]
    w2pad = np.zeros((2, 128, 64), np.float32)
    for j in range(2):
        w2pad[j, j * 64:(j + 1) * 64] = prm['rw_w2'][L][j][:, hc]
    A = lambda a: np.ascontiguousarray(a, dtype=np.float32)
    return {"p64": A(np.stack([s[0] for s in s64])), "p128": A(np.stack([s[0] for s in s128])),
            "mu64": A(np.stack([s[1] for s in s64])), "mu128": A(np.stack([s[1] for s in s128])),
            "pv": pv, "a2h": A(prm['rw_a2'][L][:, hc]), "g2h": A(prm['rw_g2'][L][:, hc]), "w2pad": w2pad,
            "consts": host_consts64()}


NTT = 17


def build_C1():
    nc = bass.Bass("TRN2", target_bir_lowering=False)
    D = lambda n, s: nc.dram_tensor(n, s, F32, kind="ExternalInput").ap()
    xTd = D("xT", [128, 8, NTOK]); yTd = D("yT", [128, 8, NTOK]); woutd = D("wout", [128, 8, 1024])
    modd = D("mod", [128, 8, 6]); nwd = D("nw", [128, 8]); wrd = D("wr", [128, 8, 32]); brd = D("br", [128, 32])
    O = lambda n, s: nc.dram_tensor(n, s, F32, kind="ExternalOutput").ap()
    xmd = O("xmT", [128, 8, NTOK]); h2d = O("h2T", [128, 8, NTOK]); Gd = O("G", [128, NTT, 32])
    P = Prog(nc)
    xT = P.sb("xT", [128, 8, NTOK]); ybf = P.sb("ybf", [128, 8, NTOK], BF16)
    hT32 = xT
    mod = P.sb("mod", [128, 8, 6]); nw = P.sb("nw", [128, 8]); wbf = P.sb("wbf", [128, 8, 1024], BF16)
    wr = P.sb("wr", [128, 8, 32]); br = P.sb("br", [128, 32])
    ones_bf = P.sb("ones_bf", [128, 128], BF16)
    P.I("pool", "memset", ones_bf[:], 1.0, w=["ones_bf"])
    for k in range(8):
        P.dma("sp", xT[:, k, :], xTd[:, k, :], writes=["xT"])
        P.dma("pool", ybf[:, k, :], yTd[:, k, :], writes=["ybf"])
        P.dma("pool", wbf[:, k, :], woutd[:, k, :], writes=["wbf"])
    for t, d, kk in ((mod, modd, "mod"), (nw, nwd, "nw"), (wr, wrd, "wr"), (br, brd, "br")):
        P.dma("sp", t[:], d, writes=[kk])
    pp = [P.ps(f"bank{i}", [128, 512]) for i in range(8)]
    i = 0
    for m in range(8):
        for it, (t0, n) in enumerate(TILES):
            b = i % 4; i += 1
            for k in range(8):
                P.I("pe", "matmul", pp[b][:, 0:n], wbf[:, k, m * 128:(m + 1) * 128], ybf[:, k, t0:t0 + n], start=(k == 0), stop=(k == 7),
                    r=["wbf", "ybf"], w=[f"bk{b}"])
            gcol = 3 if it == 0 else 0
            P.I("dve", "scalar_tensor_tensor", xT[:, m, t0:t0 + n], pp[b][:, 0:n], mod[:, m, gcol:gcol + 1], xT[:, m, t0:t0 + n], ALU.mult, ALU.add,
                r=[f"bk{b}", "mod", "xT"], w=["xT"])
    for k in range(8):
        P.dma("sp", xmd[:, k, :], xT[:, k, :], reads=["xT"], is_output=True)
    rms_modulate(P, xT, xT, mod, nw, ones_bf, shift_i=(1, 4), scale_i=(2, 5), tagp="n2", psb=(pp[4], pp[5]), hkey="xT")
    for k in range(8):
        P.dma("sp", h2d[:, k, :], hT32[:, k, :], reads=["xT"], is_output=True)
    G = P.sb("G", [128, NTT, 32]); lg = P.sb("lg", [128, NTT, 32]); m8 = P.sb("m8", [128, NTT, 8]); nmx = P.sb("nmx", [128, NTT])
    msk = P.sb("msk", [128, NTT, 32]); ssum = P.sb("ssum", [128, NTT])
    for tt in range(NTT):
        b = 6 + tt % 2
        for k in range(8):
            P.I("pe", "matmul", pp[b][:, 0:32], hT32[:, k, tt * 128:(tt + 1) * 128], wr[:, k, :], start=(k == 0), stop=(k == 7),
                r=["xT", "wr"], w=[f"bk{b}"])
        P.I("dve", "tensor_tensor", lg[:, tt, :], pp[b][:, 0:32], br[:], ALU.add, r=[f"bk{b}", "br"], w=["lg"])
        P.I("dve", "max", m8[:, tt, :], lg[:, tt, :], r=["lg"], w=["m8"])
        P.I("dve", "tensor_scalar", msk[:, tt, :], lg[:, tt, :], m8[:, tt, 3:4], None, ALU.is_ge, r=["lg", "m8"], w=["msk"])
        P.I("dve", "tensor_scalar", nmx[:, tt:tt + 1], m8[:, tt, 0:1], -1.0, None, ALU.mult, r=["m8"], w=["nmx"])
        P.I("act", "activation", G[:, tt, :], lg[:, tt, :], AF.Exp, bias=nmx[:, tt:tt + 1], r=["lg", "nmx"], w=["G"])
        P.I("dve", "tensor_tensor", G[:, tt, :], G[:, tt, :], msk[:, tt, :], ALU.mult, r=["G", "msk"], w=["G"])
        P.I("dve", "tensor_reduce", ssum[:, tt:tt + 1], G[:, tt, :], AX.X, ALU.add, r=["G"], w=["ssum"])
        P.I("dve", "reciprocal", ssum[:, tt:tt + 1], ssum[:, tt:tt + 1], r=["ssum"], w=["ssum"])
        P.I("dve", "tensor_scalar", G[:, tt, :], G[:, tt, :], ssum[:, tt:tt + 1], None, ALU.mult, r=["G", "ssum"], w=["G"])
    P.dma("sp", Gd, G[:], reads=["G"], is_output=True)
    P.finish()
    return nc


NT2 = 1088
T2 = [(0, 512), (512, 512), (1024, 64)]
ST2 = [(i * 128, 128) for i in range(8)] + [(1024, 64)]


def build_C2(NB=16, NE=4):
    nc = bass.Bass("TRN2", target_bir_lowering=False)
    D = lambda n, s: nc.dram_tensor(n, s, F32, kind="ExternalInput").ap()
    h2d = D("h2T", [128, 8, NB * NT2]); Gd = D("G", [128, NB, 9, NE]); wgud = D("wgu", [NE, 128, 8, 2048]); wdd = D("wd", [NE, 128, 8, 1024])
    bgud = D("bgu", [128, NE, 16]); bdd = D("bd", [NE, 1024]); idd = D("ident", [128, 128])
    fd = nc.dram_tensor("f", [NB, 128, 9, 1024], F32, kind="ExternalOutput").ap()
    P = Prog(nc)
    hbf = [P.sb(f"hbf{i}", [128, 8, NT2], BF16) for i in range(2)]
    G = P.sb("G", [128, NB, 9, NE]); bgu = P.sb("bgu", [128, NE, 16]); bd = P.sb("bd", [NE, 1024]); ident = P.sb("ident", [128, 128])
    P.dma("sp", G[:], Gd, writes=["G"]); P.dma("sp", bgu[:], bgud, writes=["bgu"]); P.dma("sp", bd[:], bdd, writes=["bd"]); P.dma("sp", ident[:], idd, writes=["ident"])
    wgu = [P.sb(f"wgu{i}", [128, 8, 2048], BF16) for i in range(2)]; wd = [P.sb(f"wd{i}", [128, 8, 1024], BF16) for i in range(2)]
    act = P.sb("act", [128, 8, NT2], BF16); acc = P.sb("acc", [128, 9, 1024])
    gc_ = [P.sb(f"gc{i}", [128, 512]) for i in range(2)]; sg = [P.sb(f"sg{i}", [128, 512]) for i in range(2)]
    uc = [P.sb(f"uc{i}", [128, 512]) for i in range(2)]
    GT = P.sb("GT", [NE, 128])
    pp = [P.ps(f"bank{i}", [128, 512]) for i in range(8)]

    wgus = [nc.dram_tensor(f"wgu_bf{e}", [128, 8, 2048], BF16).ap() for e in range(NE)]
    wds = [nc.dram_tensor(f"wd_bf{e}", [128, 8, 1024], BF16).ap() for e in range(NE)]
    for e in range(NE):
        for k in range(8):
            P.dma("pool", wgus[e][:, k, :], wgud[e, :, k, :], writes=[f"wgus{e}"])
            P.dma("pool", wds[e][:, k, :], wdd[e, :, k, :], writes=[f"wds{e}"])

    def load_w(j):
        e = j % NE; b = j % 2
        for k in range(0, 8, 2):
            P.dma("sp", wgu[b][:, k:k + 2, :], wgus[e][:, k:k + 2, :], reads=[f"wgus{e}"], writes=[f"wgu{b}"])
        for k in range(0, 8, 4):
            P.dma("act", wd[b][:, k:k + 4, :], wds[e][:, k:k + 4, :], reads=[f"wds{e}"], writes=[f"wd{b}"])

    def load_h(blk):
        for k in range(8):
            P.dma("pool", hbf[blk % 2][:, k, :], h2d[:, k, blk * NT2:(blk + 1) * NT2], writes=[f"hbf{blk%2}"])
    load_h(0); load_w(0)
    it = 0; jt = 0; j = 0
    for blk in range(NB):
        hb = hbf[blk % 2]; hk = f"hbf{blk%2}"
        if blk + 1 < NB:
            load_h(blk + 1)
        for e in range(NE):
            b = j % 2
            if j + 1 < NB * NE:
                load_w(j + 1)
            j += 1
            for fc in range(8):
                for (t0, n) in T2:
                    s = it % 2; it += 1
                    pg, pu = pp[2 * s], pp[2 * s + 1]; kg, ku = f"bk{2*s}", f"bk{2*s+1}"
                    for k in range(8):
                        P.I("pe", "matmul", pg[:, 0:n], wgu[b][:, k, fc * 128:(fc + 1) * 128], hb[:, k, t0:t0 + n], start=(k == 0), stop=(k == 7),
                            r=[f"wgu{b}", hk], w=[kg])
                    for k in range(8):
                        P.I("pe", "matmul", pu[:, 0:n], wgu[b][:, k, 1024 + fc * 128:1024 + (fc + 1) * 128], hb[:, k, t0:t0 + n], start=(k == 0), stop=(k == 7),
                            r=[f"wgu{b}", hk], w=[ku])
                    P.I("dve", "tensor_scalar", gc_[s][:, 0:n], pg[:, 0:n], bgu[:, e, fc:fc + 1], 7.0, ALU.add, ALU.min, r=[kg, "bgu"], w=[f"gc{s}"])
                    P.I("act", "activation", sg[s][:, 0:n], gc_[s][:, 0:n], AF.Sigmoid, scale=1.702, r=[f"gc{s}"], w=[f"sg{s}"])
                    P.I("dve", "tensor_scalar", uc[s][:, 0:n], pu[:, 0:n], bgu[:, e, 8 + fc:9 + fc], 7.0, ALU.add, ALU.min, r=[ku, "bgu"], w=[f"uc{s}"])
                    P.I("dve", "tensor_scalar", uc[s][:, 0:n], uc[s][:, 0:n], -7.0, 1.0, ALU.max, ALU.add, r=[f"uc{s}"], w=[f"uc{s}"])
                    P.I("dve", "tensor_tensor", gc_[s][:, 0:n], gc_[s][:, 0:n], sg[s][:, 0:n], ALU.mult, r=[f"gc{s}", f"sg{s}"], w=[f"gc{s}"])
                    P.I("dve", "tensor_tensor", act[:, fc, t0:t0 + n], gc_[s][:, 0:n], uc[s][:, 0:n], ALU.mult, r=[f"gc{s}", f"uc{s}"], w=["act"])
            for si, (s0, sn) in enumerate(ST2):
                for half in range(2):
                    pb = 4 + jt % 4; jt += 1
                    hs = slice(half * 512, (half + 1) * 512)
                    for fc in range(8):
                        P.I("pe", "matmul", pp[pb][0:sn, 0:512], act[:, fc, s0:s0 + sn], wd[b][:, fc, hs], start=(fc == 0), stop=(fc == 7),
                            r=["act", f"wd{b}"], w=[f"bk{pb}"])
                    if e == 0:
                        P.I("dve", "tensor_scalar", acc[0:sn, si, hs], pp[pb][0:sn, 0:512], G[0:sn, blk, si, e:e + 1], None, ALU.mult,
                            r=[f"bk{pb}", "G"], w=["acc"])
                    else:
                        P.I("dve", "scalar_tensor_tensor", acc[0:sn, si, hs], pp[pb][0:sn, 0:512], G[0:sn, blk, si, e:e + 1], acc[0:sn, si, hs],
                            ALU.mult, ALU.add, r=[f"bk{pb}", "G", "acc"], w=["acc"])
        for si, (s0, sn) in enumerate(ST2):
            P.I("pe", "transpose", pp[0][0:NE, 0:sn], G[0:sn, blk, si, :], ident[0:sn, 0:sn], r=["G", "ident"], w=["bk0"])
            P.I("dve", "tensor_copy", GT[:, 0:sn], pp[0][0:NE, 0:sn], r=["bk0"], w=["GT"])
            for half in range(2):
                pb = 1 + half; hs = slice(half * 512, (half + 1) * 512)
                P.I("pe", "matmul", pp[pb][0:sn, 0:512], GT[:, 0:sn], bd[:, hs], start=True, stop=True, r=["GT", "bd"], w=[f"bk{pb}"])
                P.I("dve", "tensor_tensor", acc[0:sn, si, hs], acc[0:sn, si, hs], pp[pb][0:sn, 0:512], ALU.add, r=[f"bk{pb}", "acc"], w=["acc"])
        P.I("pool", "memset", acc[64:128, 8, :], 0.0, r=["acc"], w=["acc"]) if blk == 0 else None
        P.dma("sp", fd[blk], acc[:], reads=["acc"], is_output=True)
    P.finish()
    return nc


def build_D():
    nc = bass.Bass("TRN2", target_bir_lowering=False)
    D = lambda n, s: nc.dram_tensor(n, s, F32, kind="ExternalInput").ap()
    xmd = D("xmT", [128, 8, NTOK]); fTd = D("fT", [8, 128, 8, NTOK]); modd = D("mod", [128, 8, 2]); nwd = D("nw", [128, 8])
    od = nc.dram_tensor("oT", [128, 8, NTOK], F32, kind="ExternalOutput").ap()
    P = Prog(nc)
    xT = P.sb("xT", [128, 8, NTOK]); fT = P.sb("fT", [128, 8, NTOK]); mod = P.sb("mod", [128, 8, 2]); nw = P.sb("nw", [128, 8])
    ones_bf = P.sb("ones_bf", [128, 128], BF16)
    P.I("pool", "memset", ones_bf[:], 1.0, w=["ones_bf"])
    for k in range(8):
        P.dma("sp", xT[:, k, :], xmd[:, k, :], writes=["xT"])
    P.dma("sp", mod[:], modd, writes=["mod"]); P.dma("sp", nw[:], nwd, writes=["nw"])
    for c in range(8):
        for k in range(8):
            P.dma("sp", fT[:, k, :], fTd[c, :, k, :], writes=["fT"])
        add_gated(P, xT, fT, mod, 0, 1)
    sq = [P.sb(f"sq{i}", [128, 8, 512], BF16) for i in range(2)]; rs = [P.sb(f"rs{i}", [128, 512]) for i in range(2)]
    ss = [P.ps(f"bank{i}", [128, 512]) for i in range(2)]
    for it, (t0, n) in enumerate(TILES):
        b = it % 2
        for k in range(8):
            P.I("act", "activation", sq[b][:, k, 0:n], xT[:, k, t0:t0 + n], AF.Square, r=["xT"], w=[f"sq{b}"])
        for k in range(8):
            P.I("pe", "matmul", ss[b][:, 0:n], ones_bf[:], sq[b][:, k, 0:n], start=(k == 0), stop=(k == 7), r=[f"sq{b}", "ones_bf"], w=[f"bk{b}"])
        P.I("dve", "tensor_scalar", rs[b][:, 0:n], ss[b][:, 0:n], 1.0 / 1024, 1e-6, ALU.mult, ALU.add, r=[f"bk{b}"], w=[f"rs{b}"])
        P.I("dve", "reciprocal", rs[b][:, 0:n], rs[b][:, 0:n], r=[f"rs{b}"], w=[f"rs{b}"])
        P.I("act", "activation", rs[b][:, 0:n], rs[b][:, 0:n], AF.Sqrt, r=[f"rs{b}"], w=[f"rs{b}"])
        for k in range(8):
            P.I("dve", "scalar_tensor_tensor", fT[:, k, t0:t0 + n], xT[:, k, t0:t0 + n], nw[:, k:k + 1], rs[b][:, 0:n], ALU.mult, ALU.mult,
                r=["xT", "nw", f"rs{b}"], w=["fT"])
    for k in range(8):
        P.dma("sp", od[:, k, :], fT[:, k, :], reads=["fT"], is_output=True)
    P.finish()
    return nc


def add_gated(P, xT, fT, mod, col_l, col_c):
    for k in range(8):
        P.I("dve", "scalar_tensor_tensor", xT[:, k, 0:128], fT[:, k, 0:128], mod[:, k, col_c:col_c + 1], xT[:, k, 0:128], ALU.mult, ALU.add,
            r=["fT", "mod", "xT"], w=["xT"])
        P.I("dve", "scalar_tensor_tensor", xT[:, k, 128:NTOK], fT[:, k, 128:NTOK], mod[:, k, col_l:col_l + 1], xT[:, k, 128:NTOK], ALU.mult, ALU.add,
            r=["fT", "mod", "xT"], w=["xT"])


I32 = mybir.dt.int32


def build_C2s(NTILE=136, NE=4, CAP=4608, NPASS=4):
    NTOKA = NTILE * 128; NJ = CAP // 128; NJH = NJ // NPASS; HALF = CAP // NPASS
    T3 = [(t0, min(512, HALF - t0)) for t0 in range(0, HALF, 512)]
    nc = bass.Bass("TRN2", target_bir_lowering=False)
    D = lambda n, s: nc.dram_tensor(n, s, F32, kind="ExternalInput").ap()
    h2d = D("h2tok", [NTOKA + 128, 1024]); Gd = D("Gm", [128, NTILE, NE]); tokd = D("tokid", [128, NTILE]); Ld = D("lst", [128, 128])
    padd = D("padtab", [128, NJ, 2]); idd = D("ident", [128, 128]); dumpd = D("dump", [128, 1])
    wgud = D("wgu", [NE, 128, 8, 2048]); wdd = D("wd", [NE, 128, 8, 1024]); bgud = D("bgu", [128, NE, 16]); bdbd = D("bdb", [NE, 128, 1024])
    fd = nc.dram_tensor("fpart", [NTOKA + 128, 1024], F32, kind="ExternalOutput").ap()
    tab = [nc.dram_tensor(f"slot_tab{e}", [CAP + 128, 2], F32).ap() for e in range(NE)]
    P = Prog(nc)
    pp = [P.ps(f"bank{i}", [128, 512]) for i in range(8)]
    z = P.sb("z", [128, 1024]); ones = P.sb("ones", [128, 128]); lst = P.sb("lst", [128, 128]); ident = P.sb("ident", [128, 128])
    P.I("pool", "memset", z[:], 0.0, w=["z"]); P.I("pool", "memset", ones[:], 1.0, w=["ones"])
    P.dma("sp", lst[:], Ld, writes=["lst"]); P.dma("sp", ident[:], idd, writes=["ident"])
    for r in range(NTILE + 1):
        P.dma("sp", fd[r * 128:(r + 1) * 128, :], z[:], reads=["z"], writes=["fpart"])
    Gm = P.sb("Gm", [128, NTILE, NE]); tokid = P.sb("tokid", [128, NTILE]); padt = P.sb("padt", [128, NJ, 2]); bgu = P.sb("bgu", [128, NE, 16])
    P.dma("act", Gm[:], Gd, writes=["Gm"]); P.dma("act", tokid[:], tokd, writes=["tokid"]); P.dma("act", padt[:], padd, writes=["padt"])
    P.dma("act", bgu[:], bgud, writes=["bgu"])
    dump = P.sb("dump", [128, 1]); P.dma("act", dump[:], dumpd, writes=["dump"])
    wgu = [P.sb(f"wgu{i}", [128, 8, 2048], BF16) for i in range(2)]; wd = [P.sb(f"wd{i}", [128, 8, 1024], BF16) for i in range(2)]
    bdb = [P.sb(f"bdb{i}", [128, 1024]) for i in range(2)]
    hsel = P.sb("hsel", [128, 8, HALF], BF16); act = P.sb("act", [128, 8, HALF], BF16)
    hg = [P.sb(f"hg{i}", [128, 1024]) for i in range(2)]; yst = [P.sb(f"yst{i}", [128, 1024]) for i in range(2)]
    gc_ = [P.sb(f"gc{i}", [128, 512]) for i in range(2)]; sg = [P.sb(f"sg{i}", [128, 512]) for i in range(2)]
    uc = [P.sb(f"uc{i}", [128, 512]) for i in range(2)]

    def load_w(e):
        b = e % 2
        for k in range(8):
            P.dma("pool", wgu[b][:, k, :], wgud[e, :, k, :], writes=[f"wgu{b}"])
        for k in range(8):
            P.dma("pool", wd[b][:, k, :], wdd[e, :, k, :], writes=[f"wd{b}"])
        P.dma("act", bdb[b][:], bdbd[e], writes=[f"bdb{b}"])
    load_w(0)
    m = P.sb("m", [128, NE, NTILE]); cs = P.sb("cs", [128, NE, NTILE]); inc = P.sb("inc", [128, NE, NTILE]); rk = P.sb("rk", [128, NE, NTILE])
    idx = P.sb("idx", [128, NE, NTILE], I32); pr = P.sb("pr", [128, NTILE, NE, 2]); onesw = P.sb("onesw", [128, NTILE])
    P.I("pool", "memset", onesw[:], 1.0, w=["onesw"])
    for e in range(NE):
        P.I("dve", "tensor_scalar", m[:, e, :], Gm[:, :, e], 0.0, None, ALU.is_gt, r=["Gm"], w=["m"])
        P.I("pe", "matmul", pp[0][:, 0:NTILE], lst[:], m[:, e, :], start=True, stop=True, r=["lst", "m"], w=["bk0"])
        P.I("pe", "matmul", pp[1][:, 0:NTILE], ones[:], m[:, e, :], start=True, stop=True, r=["ones", "m"], w=["bk1"])
        P.I("act", "activation", cs[:, e, :], pp[1][:, 0:NTILE], AF.Copy, r=["bk1"], w=["cs"])
        P.I("dve", "tensor_tensor_scan", inc[:, e, :], onesw[:], cs[:, e, :], 0.0, ALU.mult, ALU.add, r=["onesw", "cs"], w=["inc"])
        P.I("dve", "tensor_tensor", rk[:, e, :], pp[0][:, 0:NTILE], inc[:, e, :], ALU.add, r=["bk0", "inc"], w=["rk"])
        P.I("dve", "tensor_tensor", rk[:, e, :], rk[:, e, :], cs[:, e, :], ALU.subtract, r=["rk", "cs"], w=["rk"])
        P.I("dve", "tensor_scalar", cs[:, e, :], rk[:, e, :], float(CAP), None, ALU.is_lt, r=["rk", "cs"], w=["cs"])
        P.I("dve", "tensor_tensor", cs[:, e, :], cs[:, e, :], m[:, e, :], ALU.mult, r=["cs", "m"], w=["cs"])
        P.I("dve", "tensor_scalar", rk[:, e, :], rk[:, e, :], dump[:, 0:1], None, ALU.subtract, r=["rk", "dump"], w=["rk"])
        P.I("dve", "tensor_tensor", rk[:, e, :], rk[:, e, :], cs[:, e, :], ALU.mult, r=["rk", "cs"], w=["rk"])
        P.I("dve", "tensor_scalar", rk[:, e, :], rk[:, e, :], dump[:, 0:1], None, ALU.add, r=["rk", "dump"], w=["rk"])
        P.I("dve", "tensor_copy", idx[:, e, :], rk[:, e, :], r=["rk"], w=["idx"])
        P.I("pool", "tensor_copy", pr[:, :, e, 0], tokid[:], r=["tokid"], w=["pr"])
        P.I("pool", "tensor_copy", pr[:, :, e, 1], Gm[:, :, e], r=["Gm"], w=["pr"])
    for e in range(NE):
        P.dma("act", tab[e][0:CAP, :].rearrange("(p j) c -> p j c", j=NJ), padt[:], reads=["padt"], writes=[f"tabinit{e}"])
    for t in range(NTILE):
        for e in range(NE):
            P.idma(tab[e], pr[:, t, e, :], out_idx=idx[:, e, t:t + 1], reads=["pr", "idx", f"tabinit{e}"], writes=[f"sc{e}_{t}"])
    tabsb = P.sb("tabsb", [128, NE, NJ, 2]); tok_i = P.sb("tok_i", [128, NE, NJ], I32); gate = P.sb("gate", [128, NE, NJ])
    for e in range(NE):
        P.dma("act", tabsb[:, e, :, :], tab[e][0:CAP, :].rearrange("(p j) c -> p j c", j=NJ), reads=[f"sc{e}_{t}" for t in range(NTILE)], writes=["tabsb"])
    P.I("dve", "tensor_copy", tok_i[:], tabsb[:, :, :, 0], r=["tabsb"], w=["tok_i"])
    P.I("dve", "tensor_copy", gate[:], tabsb[:, :, :, 1], r=["tabsb"], w=["gate"])
    it = 0; jt = 0; gi = 0; ti = 0
    for e in range(NE):
        b = e % 2
        if e + 1 < NE:
            load_w(e + 1)
        for half in range(NPASS):
            for jj in range(NJH):
                j = half * NJH + jj; g = gi % 2; gi += 1
                P.idma(hg[g][:], h2d, in_idx=tok_i[:, e, j:j + 1], reads=["tok_i"], writes=[f"hg{g}"])
                for k in range(8):
                    pb = 4 + ti % 4; ti += 1
                    P.I("pe", "transpose", pp[pb][:, 0:128], hg[g][:, k * 128:(k + 1) * 128], ident[:], r=[f"hg{g}", "ident"], w=[f"bk{pb}"])
                    if ti % 2:
                        P.I("act", "activation", hsel[:, k, jj * 128:(jj + 1) * 128], pp[pb][:, 0:128], AF.Copy, r=[f"bk{pb}"], w=["hsel"])
                    else:
                        P.I("dve", "tensor_copy", hsel[:, k, jj * 128:(jj + 1) * 128], pp[pb][:, 0:128], r=[f"bk{pb}"], w=["hsel"])
            for fc in range(8):
                for (t0, n) in T3:
                    s = it % 2; it += 1
                    pg, pu = pp[2 * s], pp[2 * s + 1]; kg, ku = f"bk{2*s}", f"bk{2*s+1}"
                    for k in range(8):
                        P.I("pe", "matmul", pg[:, 0:n], wgu[b][:, k, fc * 128:(fc + 1) * 128], hsel[:, k, t0:t0 + n], start=(k == 0), stop=(k == 7),
                            r=[f"wgu{b}", "hsel"], w=[kg])
                    for k in range(8):
                        P.I("pe", "matmul", pu[:, 0:n], wgu[b][:, k, 1024 + fc * 128:1024 + (fc + 1) * 128], hsel[:, k, t0:t0 + n], start=(k == 0), stop=(k == 7),
                            r=[f"wgu{b}", "hsel"], w=[ku])
                    P.I("dve", "tensor_scalar", gc_[s][:, 0:n], pg[:, 0:n], bgu[:, e, fc:fc + 1], 7.0, ALU.add, ALU.min, r=[kg, "bgu"], w=[f"gc{s}"])
                    P.I("act", "activation", sg[s][:, 0:n], gc_[s][:, 0:n], AF.Sigmoid, scale=1.702, r=[f"gc{s}"], w=[f"sg{s}"])
                    P.I("dve", "tensor_scalar", uc[s][:, 0:n], pu[:, 0:n], bgu[:, e, 8 + fc:9 + fc], 7.0, ALU.add, ALU.min, r=[ku, "bgu"], w=[f"uc{s}"])
                    P.I("dve", "tensor_scalar", uc[s][:, 0:n], uc[s][:, 0:n], -7.0, 1.0, ALU.max, ALU.add, r=[f"uc{s}"], w=[f"uc{s}"])
                    P.I("dve", "tensor_tensor", gc_[s][:, 0:n], gc_[s][:, 0:n], sg[s][:, 0:n], ALU.mult, r=[f"gc{s}", f"sg{s}"], w=[f"gc{s}"])
                    P.I("dve", "tensor_tensor", act[:, fc, t0:t0 + n], gc_[s][:, 0:n], uc[s][:, 0:n], ALU.mult, r=[f"gc{s}", f"uc{s}"], w=["act"])
            for jj in range(NJH):
                j = half * NJH + jj; y = jt % 2
                for hh in range(2):
                    pb = 4 + jt % 4; jt += 1
                    hs = slice(hh * 512, (hh + 1) * 512)
                    for fc in range(8):
                        P.I("pe", "matmul", pp[pb][:, 0:512], act[:, fc, jj * 128:(jj + 1) * 128], wd[b][:, fc, hs], start=(fc == 0), stop=(fc == 7),
                            r=["act", f"wd{b}"], w=[f"bk{pb}"])
                    P.I("dve", "tensor_tensor", yst[jj % 2][:, hs], pp[pb][:, 0:512], bdb[b][:, hs], ALU.add, r=[f"bk{pb}", f"bdb{b}"], w=[f"yst{jj%2}"])
                P.I("pool", "tensor_scalar", yst[jj % 2][:], yst[jj % 2][:], gate[:, e, j:j + 1], None, ALU.mult, r=[f"yst{jj%2}", "gate"], w=[f"yst{jj%2}"])
                P.idma(fd, yst[jj % 2][:], out_idx=tok_i[:, e, j:j + 1], reads=[f"yst{jj%2}", "tok_i", "fpart"], writes=["fpart"], is_output=True, compute_op=ALU.add)
    P.finish()
    return nc


def host_C2s_consts(NTILE=136, CAP=4608):
    NJ = CAP // 128
    tokid = (np.arange(NTILE)[None, :] * 128 + np.arange(128)[:, None]).astype(np.float32)
    p_ = np.arange(128)[:, None]; q_ = np.arange(128)[None, :]
    lst = (p_ < q_).astype(np.float32)
    padtab = np.zeros((128, NJ, 2), np.float32); padtab[:, :, 0] = NTILE * 128 + np.arange(128)[:, None]
    return {"tokid": tokid, "lst": lst, "padtab": padtab, "ident": np.eye(128, dtype=np.float32), "dump": (CAP + np.arange(128, dtype=np.float32))[:, None]}


def _fm(tok):
    return np.ascontiguousarray(tok.T.reshape(8, 128, -1).transpose(1, 0, 2))


def _tok(fm):
    return fm.transpose(2, 1, 0).reshape(fm.shape[2], -1)


def _vec(v):
    return np.ascontiguousarray(np.asarray(v, np.float32).reshape(8, 128).T)


_PROGS = {}
C2S_CAP = 4608
MOE_SPARSE = True


def _prog(name, builder, *a):
    key = (name,) + a
    if key not in _PROGS:
        _PROGS[key] = builder(*a)
    return _PROGS[key]


def _run(nc, maps):
    res = run_bass_kernel_spmd(nc, maps, core_ids=list(range(len(maps))))
    return res.results


def kernel(**inp):
    prm = {k: np.asarray(v, dtype=np.float32) for k, v in inp.items()}
    x, c, ctx, c_ctx = prm['x'], prm['c'], prm['ctx'], prm['c_ctx']
    NC = 8
    A_ = lambda a: np.ascontiguousarray(a, dtype=np.float32)
    cs = np.zeros((128, 8, 5), np.float32)
    for v in range(4):
        cs[:, :, v] = _vec(c[v])
    cs[:, :, 4] = _vec(c_ctx)
    items = [(l, fc) for l in range(2) for fc in range(48)]
    maps = []
    for i in range(NC):
        its = items[i * 12:(i + 1) * 12]
        wm = np.stack([prm['w_mod'][l][:, fc * 128:(fc + 1) * 128].reshape(8, 128, 128).transpose(1, 0, 2) for l, fc in its])
        bm = np.stack([prm['b_mod'][l][fc * 128:(fc + 1) * 128] for l, fc in its], 1)
        maps.append({"cs": cs, "wm": A_(wm), "bm": A_(bm)})
    res = _run(_prog("M", build_M), maps)
    modv = {}
    for i in range(NC):
        for j, (l, fc) in enumerate(items[i * 12:(i + 1) * 12]):
            modv[(l, fc)] = res[i]["modT"][:, j, :]
    mod6 = [[np.stack([modv[(l, i6 * 8 + k)] for k in range(8)], 1) for i6 in range(6)] for l in range(2)]

    def core_tokens(arr_c, arr_l, b, half):
        return np.concatenate([arr_c[b, half * 128:(half + 1) * 128], arr_l[b, half * 2048:(half + 1) * 2048]], 0)

    xm = [_fm(core_tokens(ctx, x, j // 2, j % 2)) for j in range(NC)]
    fparts = None
    consts64 = host_consts64()
    for l in range(2):
        win = np.zeros((1024, NCT * 128), np.float32); win[:, :4184] = prm['w_in'][l]
        win = A_(win.reshape(8, 128, NCT * 128).transpose(1, 0, 2)); nw1 = _vec(prm['norm1_w'][l])
        maps = []
        for j in range(NC):
            b = j // 2
            g5l = mod6[l - 1][5][:, :, b] if l > 0 else np.zeros((128, 8), np.float32)
            g5c = mod6[l - 1][5][:, :, 4] if l > 0 else np.zeros((128, 8), np.float32)
            mod = np.stack([mod6[l][0][:, :, b], mod6[l][1][:, :, b], mod6[l][0][:, :, 4], mod6[l][1][:, :, 4], g5l, g5c], -1)
            m = {"xT": xm[j], "mod": A_(mod), "nw": nw1, "win": win}
            if l > 0:
                m["fT"] = A_(np.stack([_fm(fparts[cc][j * NTOK:(j + 1) * NTOK]) for cc in range(NC)]))
            maps.append(m)
        res = _run(_prog("A", build_A, l == 0), maps)
        xcur = [res[j]["xout"] for j in range(NC)]
        ptok = [res[j]["pT"].reshape(NCT * 128, NTOK).T[:, :4184] for j in range(NC)]
        del res
        pfull = np.stack([np.concatenate([ptok[2 * b][:128], ptok[2 * b + 1][:128], ptok[2 * b][128:], ptok[2 * b + 1][128:]], 0) for b in range(4)])
        del ptok
        yall = np.zeros((4, TSEQ, 1024), np.float32)
        pa = pfull[:, :, 0:1032]
        res = _run(_prog("B1", build_B1), [host_B1_inputs(pa, l, j // 2, j % 2, prm) for j in range(NC)])
        for j in range(NC):
            yall[j // 2, :, (j % 2) * 128:(j % 2 + 1) * 128] = res[j]["yT"].T
        pb = pfull[:, :, 1032:3096]
        for rnd in range(2):
            its = [(i // 4, i % 4) for i in range(rnd * 8, rnd * 8 + 8)]
            maps = [host_B2_inputs(pb, l, b, h, prm) for b, h in its]
            for m in maps:
                m["consts"] = consts64
            res = _run(_prog("B2", build_B2), maps)
            for (b, h), r in zip(its, res):
                yall[b, :, 256 + h * 128:256 + (h + 1) * 128] = host_B2_output(r["y"])
        pc = pfull[:, :, 3096:4184]
        for rnd in range(2):
            its = [(i // 4, i % 4) for i in range(rnd * 8, rnd * 8 + 8)]
            maps = [host_B3_inputs(pc, l, b, h, prm) for b, h in its]
            for m in maps:
                m["consts"] = consts64
            res = _run(_prog("B3", build_B3), maps)
            for (b, h), r in zip(its, res):
                yall[b, :, 768 + h * 64:768 + (h + 1) * 64] = r["y"].T
        del pfull
        wout = A_(prm['w_out'][l].reshape(8, 128, 1024).transpose(1, 0, 2)); nw2 = _vec(prm['norm2_w'][l])
        wr = A_(prm['w_router'][l].reshape(8, 128, 32).transpose(1, 0, 2)); br = A_(np.broadcast_to(prm['b_router'][l][None], (128, 32)))
        maps = []
        for j in range(NC):
            b, half = j // 2, j % 2
            ytok = np.concatenate([yall[b, half * 128:(half + 1) * 128], yall[b, 256 + half * 2048:256 + (half + 1) * 2048]], 0)
            mod = np.stack([mod6[l][2][:, :, b], mod6[l][3][:, :, b], mod6[l][4][:, :, b], mod6[l][2][:, :, 4], mod6[l][3][:, :, 4], mod6[l][4][:, :, 4]], -1)
            maps.append({"xT": xcur[j], "yT": _fm(ytok), "wout": wout, "mod": A_(mod), "nw": nw2, "wr": wr, "br": br})
        res = _run(_prog("C1", build_C1), maps)
        xm = [res[j]["xmT"] for j in range(NC)]
        Gall = np.concatenate([res[j]["G"].transpose(1, 0, 2).reshape(NTOK, 32) for j in range(NC)], 0)
        loads = np.count_nonzero(Gall, axis=0)
        use_sparse = MOE_SPARSE and int(loads.max()) <= C2S_CAP
        print(f"[moe] layer {l}: max expert load {int(loads.max())} (mean {float(loads.mean()):.0f}) -> {'dispatch' if use_sparse else 'dense'}", flush=True)
        if use_sparse:
            h2tok = np.concatenate([_tok(res[j]["h2T"]) for j in range(NC)] + [np.zeros((128, 1024), np.float32)], 0)
        else:
            h2all = np.ascontiguousarray(np.concatenate([res[j]["h2T"] for j in range(NC)], 2))
        del res, yall
        if use_sparse:
            maps = []
            c2c = host_C2s_consts()
            for cc in range(NC):
                es = slice(4 * cc, 4 * cc + 4)
                m_ = {"h2tok": h2tok, "Gm": A_(Gall[:, es].reshape(136, 128, 4).transpose(1, 0, 2)),
                      "wgu": A_(prm['w_gate_up'][l][es].reshape(4, 8, 128, 2048).transpose(0, 2, 1, 3)),
                      "wd": A_(prm['w_down'][l][es].reshape(4, 8, 128, 1024).transpose(0, 2, 1, 3)),
                      "bgu": A_(prm['b_gate_up'][l][es].reshape(4, 16, 128).transpose(2, 0, 1)),
                      "bdb": A_(np.broadcast_to(prm['b_down'][l][es][:, None, :], (4, 128, 1024)))}
                m_.update(c2c)
                maps.append(m_)
            res = _run(_prog("C2s", build_C2s), maps)
            del maps, h2tok
            fparts = [res[cc]["fpart"][:16 * NT2] for cc in range(NC)]
            del res
        else:
            maps = []
            ident = np.eye(128, dtype=np.float32)
            for cc in range(NC):
                es = slice(4 * cc, 4 * cc + 4)
                Gp = np.zeros((16, 1152, 4), np.float32); Gp[:, :NT2] = Gall[:, es].reshape(16, NT2, 4)
                maps.append({"h2T": h2all, "G": A_(Gp.reshape(16, 9, 128, 4).transpose(2, 0, 1, 3)),
                             "wgu": A_(prm['w_gate_up'][l][es].reshape(4, 8, 128, 2048).transpose(0, 2, 1, 3)),
                             "wd": A_(prm['w_down'][l][es].reshape(4, 8, 128, 1024).transpose(0, 2, 1, 3)),
                             "bgu": A_(prm['b_gate_up'][l][es].reshape(4, 16, 128).transpose(2, 0, 1)), "bd": A_(prm['b_down'][l][es]), "ident": ident})
            res = _run(_prog("C2", build_C2), maps)
            del maps, h2all
            fparts = [res[cc]["f"].transpose(0, 2, 1, 3).reshape(16, 1152, 1024)[:, :NT2].reshape(16 * NT2, 1024) for cc in range(NC)]
            del res
    nwf = _vec(prm['norm_f_w'])
    maps = []
    for j in range(NC):
        b = j // 2
        mod = np.stack([mod6[1][5][:, :, b], mod6[1][5][:, :, 4]], -1)
        maps.append({"xmT": xm[j], "fT": A_(np.stack([_fm(fparts[cc][j * NTOK:(j + 1) * NTOK]) for cc in range(NC)])), "mod": A_(mod), "nw": nwf})
    res = _run(_prog("D", build_D), maps)
    out = np.zeros((4, 4096, 1024), np.float32)
    for j in range(NC):
        b, half = j // 2, j % 2
        out[b, half * 2048:(half + 1) * 2048] = _tok(res[j]["oT"])[128:]
    return out
```

```python
import numpy as np
from contextlib import ExitStack
import concourse.bass as bass
import concourse.mybir as mybir
from concourse.bass_utils import run_bass_kernel_spmd

F32 = mybir.dt.float32
BF16 = mybir.dt.bfloat16
AF = mybir.ActivationFunctionType
ALU = mybir.AluOpType
AX = mybir.AxisListType

ENGS = ("pe", "dve", "act", "pool", "sp")
N_DMA_SEMS = 12


class Prog:
    def __init__(self, nc, same_engine_sync=None):
        import os
        if same_engine_sync is None:
            same_engine_sync = os.environ.get('SAMESYNC', '1') == '1'
        self.nc = nc
        self.es = ExitStack()
        self.ops = {e: [] for e in ENGS}
        self.cnt = {e: 0 for e in ENGS}
        self.sem = {}
        for e in ENGS:
            self.sem[e] = self.es.enter_context(nc.semaphore("s_" + e))
        self.dsem = {q: [self.es.enter_context(nc.semaphore(f"d_{q}{i}")) for i in range(N_DMA_SEMS)]
                     for q in ("sp", "pool", "act")}
        self.dsem_uses = {q: [0] * N_DMA_SEMS for q in ("sp", "pool", "act")}
        self.dsem_next = {q: 0 for q in ("sp", "pool", "act")}
        self.waited = {e: {} for e in ENGS}
        self.lastw = {}
        self.readers = {}
        self.same = same_engine_sync
        self.semobj = {}
        self.out_tokens = []
        self.nops = 0

    def sb(self, name, shape, dt=F32):
        return self.es.enter_context(self.nc.sbuf_tensor("sb_" + name, list(shape), dt))

    def ps(self, name, shape, dt=F32):
        return self.es.enter_context(self.nc.psum_tensor("ps_" + name, list(shape), dt))

    def _need(self, eng, tok, waits):
        if tok is None:
            return
        semkey, val, src = tok
        if src == eng and (not self.same or eng == "pe"):
            return
        if self.waited[eng].get(semkey, 0) >= val:
            return
        waits[semkey] = max(waits.get(semkey, 0), val)

    def _deps(self, eng, reads, writes):
        waits = {}
        for k in reads:
            self._need(eng, self.lastw.get(k), waits)
        for k in writes:
            self._need(eng, self.lastw.get(k), waits)
            for t in self.readers.get(k, ()):
                self._need(eng, t, waits)
        for semkey, val in waits.items():
            self.waited[eng][semkey] = val
            self.ops[eng].append(("wait", semkey, val))

    def _commit(self, tok, reads, writes):
        for k in reads:
            self.readers.setdefault(k, []).append(tok)
        for k in writes:
            self.lastw[k] = tok
            self.readers[k] = []

    def op(self, eng, fn, reads=(), writes=()):
        import os
        lim = os.environ.get("OPLIMIT")
        if lim is not None and self.nops >= int(lim):
            return None
        writes = list(writes) + [k for k in reads if isinstance(k, str) and k.startswith("bk")]
        reads = [k for k in reads if not (isinstance(k, str) and k.startswith("bk"))]
        self._deps(eng, reads, writes)
        self.cnt[eng] += 1
        tok = (("e", eng), self.cnt[eng], eng)
        self.ops[eng].append(("op", fn, ("e", eng), 1))
        self._commit(tok, reads, writes)
        self.nops += 1
        return tok

    def I(self, eng, meth, *args, r=(), w=(), **kw):
        return self.op(eng, lambda e: getattr(e, meth)(*args, **kw), reads=r, writes=w)

    def dma(self, q, out, in_, reads=(), writes=(), is_output=False, **kw):
        import os
        lim = os.environ.get("OPLIMIT")
        if lim is not None and self.nops >= int(lim) and not is_output:
            return None
        i = self.dsem_next[q]
        self.dsem_next[q] = (i + 1) % N_DMA_SEMS
        uses = self.dsem_uses[q][i]
        semkey = ("d", q, i)
        if uses > 0 and self.waited[q].get(semkey, 0) < 16 * uses:
            self.waited[q][semkey] = 16 * uses
            self.ops[q].append(("wait", semkey, 16 * uses))
        self._deps(q, reads, writes)
        self.dsem_uses[q][i] = uses + 1
        tok = (semkey, 16 * (uses + 1), None)
        self.ops[q].append(("op", lambda e: e.dma_start(out=out, in_=in_, **kw), semkey, 16))
        self._commit(tok, reads, writes)
        if is_output:
            self.out_tokens.append(tok)
        self.nops += 1
        return tok

    def idma(self, out, in_, out_idx=None, in_idx=None, reads=(), writes=(), is_output=False, **kw):
        q = "pool"
        i = self.dsem_next[q]
        self.dsem_next[q] = (i + 1) % N_DMA_SEMS
        uses = self.dsem_uses[q][i]
        semkey = ("d", q, i)
        if uses > 0 and self.waited[q].get(semkey, 0) < 16 * uses:
            self.waited[q][semkey] = 16 * uses
            self.ops[q].append(("wait", semkey, 16 * uses))
        self._deps(q, reads, writes)
        self.dsem_uses[q][i] = uses + 1
        tok = (semkey, 16 * (uses + 1), None)
        oo = bass.IndirectOffsetOnAxis(ap=out_idx, axis=0) if out_idx is not None else None
        io = bass.IndirectOffsetOnAxis(ap=in_idx, axis=0) if in_idx is not None else None
        self.ops[q].append(("op", lambda e: e.indirect_dma_start(out=out, out_offset=oo, in_=in_, in_offset=io, **kw), semkey, 16))
        self._commit(tok, reads, writes)
        if is_output:
            self.out_tokens.append(tok)
        self.nops += 1
        return tok

    def _semh(self, semkey):
        if semkey[0] == "e":
            return self.sem[semkey[1]]
        return self.dsem[semkey[1]][semkey[2]]

    def finish(self):
        for tok in self.out_tokens:
            semkey, val, _ = tok
            if self.waited["sp"].get(semkey, 0) < val:
                self.waited["sp"][semkey] = val
                self.ops["sp"].append(("wait", semkey, val))
        for e in ENGS:
            if self.cnt[e] > 0 and e != "sp":
                self.ops["sp"].append(("wait", ("e", e), self.cnt[e]))
        for q in ("sp", "pool", "act"):
            for i in range(N_DMA_SEMS):
                if self.dsem_uses[q][i] > 0:
                    self.ops["sp"].append(("wait", ("d", q, i), 16 * self.dsem_uses[q][i]))
        nc = self.nc
        with nc.Block() as block:
            def mk(ename):
                def body(e):
                    for item in self.ops[ename]:
                        if item[0] == "wait":
                            e.wait_ge(self._semh(item[1]), item[2])
                        else:
                            ins = item[1](e)
                            ins.then_inc(self._semh(item[2]), item[3])
                return body
            block.tensor(mk("pe"))
            block.vector(mk("dve"))
            block.scalar(mk("act"))
            block.gpsimd(mk("pool"))
            block.sync(mk("sp"))
        self.es.close()


def build_M():
    nc = bass.Bass("TRN2", target_bir_lowering=False)
    cs = nc.dram_tensor("cs", [128, 8, 5], F32, kind="ExternalInput").ap()
    wm = nc.dram_tensor("wm", [12, 128, 8, 128], F32, kind="ExternalInput").ap()
    bm = nc.dram_tensor("bm", [128, 12], F32, kind="ExternalInput").ap()
    out = nc.dram_tensor("modT", [128, 12, 5], F32, kind="ExternalOutput").ap()
    P = Prog(nc)
    cst = P.sb("cst", [128, 8, 5]); sg = P.sb("sg", [128, 8, 5]); sc = P.sb("sc", [128, 8, 5])
    bmt = P.sb("bmt", [128, 12]); ot = P.sb("ot", [128, 12, 5])
    wt = [P.sb(f"wt{i}", [128, 8, 128]) for i in range(2)]
    pp = [P.ps(f"pp{i}", [128, 8]) for i in range(2)]
    P.dma("sp", cst[:], cs, writes=["cst"])
    P.dma("sp", bmt[:], bm, writes=["bmt"])
    P.op("act", lambda e: e.activation(sg[:], cst[:], AF.Sigmoid), reads=["cst"], writes=["sg"])
    P.op("dve", lambda e: e.tensor_tensor(sc[:], cst[:], sg[:], ALU.mult), reads=["cst", "sg"], writes=["sc"])
    for j in range(12):
        w = wt[j % 2]; wk = f"wt{j%2}"; pk = f"pp{j%2}"; p_ = pp[j % 2]
        P.dma("sp", w[:], wm[j], writes=[wk])
        for k in range(8):
            P.op("pe", lambda e, w=w, k=k, p_=p_: e.matmul(p_[:, 0:5], w[:, k, :], sc[:, k, :], start=(k == 0), stop=(k == 7)),
                 reads=[wk, "sc"], writes=[pk])
        P.op("dve", lambda e, j=j, p_=p_: e.tensor_scalar(ot[:, j, :], p_[:, 0:5], bmt[:, j:j + 1], None, ALU.add),
             reads=[pk, "bmt"], writes=["ot"])
    P.dma("sp", out, ot[:], reads=["ot"], is_output=True)
    P.finish()
    return nc


NTOK = 2176
TILES = [(0, 128)] + [(128 + 512 * i, 512) for i in range(4)]
NCT = 33


def rms_modulate(P, xT, hT, mod, nw, ones_bf, shift_i, scale_i, hT32=None, tagp="n", psb=None, hkey="hT"):
    g = P.sb(tagp + "_g", [128, 8, 2]);
    for v, (sh, sci) in enumerate(zip(shift_i, scale_i)):
        P.op("dve", lambda e, v=v, sci=sci: e.scalar_tensor_tensor(g[:, :, v], mod[:, :, sci], 1.0, nw[:, :], ALU.add, ALU.mult),
             reads=["mod", "nw"], writes=[tagp + "_g"])
    sq = [P.sb(f"{tagp}_sq{i}", [128, 8, 512], BF16) for i in range(2)]
    ss = list(psb); ssk = [f"bk_{tagp}0", f"bk_{tagp}1"]
    rs = [P.sb(f"{tagp}_rs{i}", [128, 512]) for i in range(2)]
    tmp = [P.sb(f"{tagp}_tmp{i}", [128, 512]) for i in range(2)]
    ti = 0
    for it, (t0, n) in enumerate(TILES):
        b = it % 2
        v = 1 if it == 0 else 0
        for k in range(8):
            P.op("act", lambda e, k=k, b=b, t0=t0, n=n: e.activation(sq[b][:, k, 0:n], xT[:, k, t0:t0 + n], AF.Square),
                 reads=["xT"], writes=[f"{tagp}_sq{b}"])
        for k in range(8):
            P.op("pe", lambda e, k=k, b=b, n=n: e.matmul(ss[b][:, 0:n], ones_bf[:], sq[b][:, k, 0:n], start=(k == 0), stop=(k == 7)),
                 reads=[f"{tagp}_sq{b}", "ones_bf"], writes=[ssk[b]])
        P.op("dve", lambda e, b=b, n=n: e.tensor_scalar(rs[b][:, 0:n], ss[b][:, 0:n], 1.0 / 1024, 1e-6, ALU.mult, ALU.add),
             reads=[ssk[b]], writes=[f"{tagp}_rs{b}"])
        P.op("dve", lambda e, b=b, n=n: e.reciprocal(rs[b][:, 0:n], rs[b][:, 0:n]),
             reads=[f"{tagp}_rs{b}"], writes=[f"{tagp}_rs{b}"])
        P.op("act", lambda e, b=b, n=n: e.activation(rs[b][:, 0:n], rs[b][:, 0:n], AF.Sqrt),
             reads=[f"{tagp}_rs{b}"], writes=[f"{tagp}_rs{b}"])
        for k in range(8):
            tb = ti % 2; ti += 1
            P.op("dve", lambda e, k=k, b=b, tb=tb, t0=t0, n=n, v=v: e.scalar_tensor_tensor(
                tmp[tb][:, 0:n], xT[:, k, t0:t0 + n], g[:, k, v:v + 1], rs[b][:, 0:n], ALU.mult, ALU.mult),
                 reads=["xT", tagp + "_g", f"{tagp}_rs{b}"], writes=[f"{tagp}_tmp{tb}"])
            sh = shift_i[v]
            P.op("act", lambda e, k=k, tb=tb, t0=t0, n=n, sh=sh: e.activation(
                hT[:, k, t0:t0 + n], tmp[tb][:, 0:n], AF.Identity, bias=mod[:, k, sh:sh + 1]),
                 reads=[f"{tagp}_tmp{tb}", "mod"], writes=[hkey])
            if hT32 is not None:
                P.op("pool", lambda e, k=k, tb=tb, t0=t0, n=n, sh=sh: e.tensor_scalar(
                    hT32[:, k, t0:t0 + n], tmp[tb][:, 0:n], mod[:, k, sh:sh + 1], None, ALU.add),
                     reads=[f"{tagp}_tmp{tb}", "mod"], writes=["hT32"])


def build_A(first=False):
    nc = bass.Bass("TRN2", target_bir_lowering=False)
    xTd = nc.dram_tensor("xT", [128, 8, NTOK], F32, kind="ExternalInput").ap()
    modd = nc.dram_tensor("mod", [128, 8, 6], F32, kind="ExternalInput").ap()
    fTd = nc.dram_tensor("fT", [8, 128, 8, NTOK], F32, kind="ExternalInput").ap() if not first else None
    xoutd = nc.dram_tensor("xout", [128, 8, NTOK], F32, kind="ExternalOutput").ap()
    nwd = nc.dram_tensor("nw", [128, 8], F32, kind="ExternalInput").ap()
    wind = nc.dram_tensor("win", [128, 8, NCT * 128], F32, kind="ExternalInput").ap()
    pTd = nc.dram_tensor("pT", [NCT, 128, NTOK], F32, kind="ExternalOutput").ap()
    P = Prog(nc)
    xT = P.sb("xT", [128, 8, NTOK]); hT = P.sb("hT", [128, 8, NTOK], BF16)
    mod = P.sb("mod", [128, 8, 6]); nw = P.sb("nw", [128, 8])
    wbf = P.sb("wbf", [128, 8, NCT * 128], BF16)
    ones_bf = P.sb("ones_bf", [128, 128], BF16)
    P.op("pool", lambda e: e.memset(ones_bf[:], 1.0), writes=["ones_bf"])
    for k in range(8):
        P.dma("sp", xT[:, k, :], xTd[:, k, :], writes=["xT"])
    P.dma("sp", mod[:], modd, writes=["mod"])
    P.dma("sp", nw[:], nwd, writes=["nw"])
    for k in range(8):
        P.dma("pool", wbf[:, k, :], wind[:, k, :], writes=["wbf"])
    fT = P.sb("fT", [128, 512])
    for c, k in [(c, k) for c in range(0 if first else 8) for k in range(8)]:
        for (t0, n) in TILES:
            P.dma("sp", fT[:, 0:n], fTd[c, :, k, t0:t0 + n], writes=["fT"])
            gcol = 5 if t0 == 0 else 4
            P.I("dve", "scalar_tensor_tensor", xT[:, k, t0:t0 + n], fT[:, 0:n], mod[:, k, gcol:gcol + 1], xT[:, k, t0:t0 + n], ALU.mult, ALU.add,
                r=["fT", "mod", "xT"], w=["xT"])
    for k in range(8):
        P.dma("sp", xoutd[:, k, :], xT[:, k, :], reads=["xT"], is_output=True)
    pp = [P.ps(f"bank{i}", [128, 512]) for i in range(6)]
    rms_modulate(P, xT, hT, mod, nw, ones_bf, shift_i=(0, 2), scale_i=(1, 3), tagp="n1", psb=(pp[4], pp[5]))
    st = [P.sb(f"st{i}", [128, 512]) for i in range(4)]
    i = 0
    for ct in range(NCT):
        for (t0, n) in TILES:
            b = i % 4; i += 1
            for k in range(8):
                P.op("pe", lambda e, k=k, b=b, ct=ct, t0=t0, n=n: e.matmul(
                    pp[b][:, 0:n], wbf[:, k, ct * 128:(ct + 1) * 128], hT[:, k, t0:t0 + n], start=(k == 0), stop=(k == 7)),
                     reads=["wbf", "hT"], writes=[f"bk{b}"])
            if b % 2 == 0:
                P.op("dve", lambda e, b=b, n=n: e.tensor_copy(st[b][:, 0:n], pp[b][:, 0:n]), reads=[f"bk{b}"], writes=[f"st{b}"])
            else:
                P.op("act", lambda e, b=b, n=n: e.activation(st[b][:, 0:n], pp[b][:, 0:n], AF.Copy), reads=[f"bk{b}"], writes=[f"st{b}"])
            P.dma("sp", pTd[ct, :, t0:t0 + n], st[b][:, 0:n], reads=[f"st{b}"], is_output=True)
    P.finish()
    return nc


TSEQ = 4352
QS = 64
NCH = 34
SEGS = [(0, 256), (256, 4352)]


def conv_silu(P, dst, src, cw, cb, ti, key_dst, key_src, tmp, key_tmp, out_dt_tile=None):
    for (s, e_) in SEGS:
        if cb is not None:
            P.op("act", lambda e, s=s, e_=e_: e.activation(tmp[:, s:e_], src[:, s:e_], AF.Identity, bias=cb[:, ti:ti + 1], scale=cw[:, ti, 1:2]),
                 reads=[key_src, "cw", "cb"], writes=[key_tmp])
        else:
            P.op("act", lambda e, s=s, e_=e_: e.activation(tmp[:, s:e_], src[:, s:e_], AF.Copy, scale=cw[:, ti, 1:2]),
                 reads=[key_src, "cw"], writes=[key_tmp])
        P.op("dve", lambda e, s=s, e_=e_: e.scalar_tensor_tensor(tmp[:, s + 1:e_], src[:, s:e_ - 1], cw[:, ti, 0:1], tmp[:, s + 1:e_], ALU.mult, ALU.add),
             reads=[key_src, "cw", key_tmp], writes=[key_tmp])
        P.op("dve", lambda e, s=s, e_=e_: e.scalar_tensor_tensor(tmp[:, s:e_ - 1], src[:, s + 1:e_], cw[:, ti, 2:3], tmp[:, s:e_ - 1], ALU.mult, ALU.add),
             reads=[key_src, "cw", key_tmp], writes=[key_tmp])
    P.op("act", lambda e: e.activation(dst[:, :], tmp[:, :], AF.Silu), reads=[key_tmp], writes=[key_dst])


def load_consts(P, cd):
    c = {}
    for i, nm in enumerate(["tri_f", "tri_b", "nm_f", "nm_b", "ident"]):
        t = P.sb("c_" + nm, [128, 128]); P.dma("sp", t[:], cd[i], writes=["c_" + nm]); c[nm] = t
    ones = P.sb("c_ones", [128, 128]); P.op("pool", lambda e: e.memset(ones[:], 1.0), writes=["c_ones"]); c["ones"] = ones
    idb = P.sb("c_identb", [128, 128], BF16)
    P.op("dve", lambda e: e.tensor_copy(idb[:], c["ident"][:]), reads=["c_ident"], writes=["c_identb"]); c["identb"] = idb
    return c


def host_consts():
    k = np.arange(128)[:, None]; i = np.arange(128)[None, :]
    tri_f = (k <= i).astype(np.float32); tri_b = (k >= i).astype(np.float32)
    nm_f = np.where(i >= k, 0.0, -30000.0).astype(np.float32); nm_b = np.where(i <= k, 0.0, -30000.0).astype(np.float32)
    return np.stack([tri_f, tri_b, nm_f, nm_b, np.eye(128, dtype=np.float32)])


def build_B1(stage=99):
    nc = bass.Bass("TRN2", target_bir_lowering=False)
    D = lambda n, s: nc.dram_tensor(n, s, F32, kind="ExternalInput").ap()
    zTd = D("zT", [128, TSEQ]); xbcd = D("xbcT", [3, 128, TSEQ]); dtrd = D("dtr", [128, 4 * QS])
    dtbd = D("dtb", [128, 4 * QS]); alogd = D("alog", [128, 4 * QS]); cwd = D("cw", [128, 3, 3]); cbd = D("cb", [128, 3])
    dvd = D("dvec", [128, 1]); nwd = D("normw", [128, 1]); cd = D("consts", [5, 128, 128])
    yTd = nc.dram_tensor("yT", [128, TSEQ], F32, kind="ExternalOutput").ap()
    P = Prog(nc)
    C = load_consts(P, cd)
    raw = P.sb("raw", [128, TSEQ]); tmp = P.sb("tmp", [128, TSEQ])
    xT = P.sb("xT", [128, TSEQ]); B32 = P.sb("B32", [128, TSEQ]); C32 = P.sb("C32", [128, TSEQ])
    Bb = P.sb("Bb", [128, TSEQ], BF16); Cb = P.sb("Cb", [128, TSEQ], BF16)
    cw = P.sb("cw", [128, 3, 3]); cb = P.sb("cb", [128, 3]); dvec = P.sb("dvec", [128, 1]); normw = P.sb("normw", [128, 1])
    for t, d, k in ((cw, cwd, "cw"), (cb, cbd, "cb"), (dvec, dvd, "dvec"), (normw, nwd, "normw")):
        P.dma("sp", t[:], d, writes=[k])
    if stage == 0:
        P.op("dve", lambda e: e.tensor_copy(xT[:, 0:128], C["ident"][:]), reads=["c_ident"], writes=["xT"])
        P.op("dve", lambda e: e.tensor_scalar(xT[:, 128:256], C["tri_f"][:], cw[:, 0, 0:1], dvec[:, 0:1], ALU.mult, ALU.add), reads=["c_tri_f", "cw", "dvec"], writes=["xT"])
        P.dma("sp", yTd, xT[:], reads=["xT"], is_output=True); P.finish(); return nc
    import os
    NT = int(os.environ.get("NT", "3"))
    for ti, (dst, kd) in enumerate(((xT, "xT"), (B32, "B32"), (C32, "C32"))[:NT]):
        P.dma("sp", raw[:], xbcd[ti], writes=["raw"])
        conv_silu(P, dst, raw, cw, cb, ti, kd, "raw", tmp, "tmp")
    if NT == 3 and os.environ.get("NOCAST") is None:
        P.op("pool", lambda e: e.tensor_copy(Bb[:], B32[:]), reads=["B32"], writes=["Bb"])
        P.op("pool", lambda e: e.tensor_copy(Cb[:], C32[:]), reads=["C32"], writes=["Cb"])
    if stage == 1:
        P.dma("sp", yTd, xT[:], reads=["xT"], is_output=True); P.finish(); return nc
    dtr = P.sb("dtr", [128, 4 * QS]); dtb = P.sb("dtb", [128, 4 * QS]); alog = P.sb("alog", [128, 4 * QS])
    dt = P.sb("dt", [128, 4 * QS]); la = P.sb("la", [128, 4 * QS]); ncum = P.sb("ncum", [128, 4 * QS])
    wgt = P.sb("wgt", [128, 4 * QS]); dec = P.sb("dec", [128, 4 * QS])
    P.dma("sp", dtr[:], dtrd, writes=["dtr"]); P.dma("sp", dtb[:], dtbd, writes=["dtb"]); P.dma("sp", alog[:], alogd, writes=["alog"])
    P.op("dve", lambda e: e.tensor_tensor(dtr[:], dtr[:], dtb[:], ALU.add), reads=["dtr", "dtb"], writes=["dtr"])
    P.op("dve", lambda e: e.tensor_scalar(dtr[:], dtr[:], 60.0, None, ALU.min), reads=["dtr"], writes=["dtr"])
    P.op("act", lambda e: e.activation(dtr[:], dtr[:], AF.Exp), reads=["dtr"], writes=["dtr"])
    P.op("act", lambda e: e.activation(dt[:], dtr[:], AF.Ln, bias=1.0), reads=["dtr"], writes=["dt"])
    P.op("act", lambda e: e.activation(alog[:], alog[:], AF.Exp), reads=["alog"], writes=["alog"])
    P.op("dve", lambda e: e.scalar_tensor_tensor(la[:], dt[:], -1.0, alog[:], ALU.mult, ALU.mult), reads=["dt", "alog"], writes=["la"])
    bk = [P.ps(f"bank{i}", [128, 512]) for i in range(8)]
    pc = bk[0][:, 0:4 * QS]; pt = bk[1][:, 0:4 * QS]
    P.op("pe", lambda e: e.matmul(pc[:, 0:2 * QS], C["tri_f"][:], la[:, 0:2 * QS], start=True, stop=True), reads=["la", "c_tri_f"], writes=["bk0"])
    P.op("pe", lambda e: e.matmul(pc[:, 2 * QS:4 * QS], C["tri_b"][:], la[:, 2 * QS:4 * QS], start=True, stop=True), reads=["la", "c_tri_b"], writes=["bk0"])
    P.op("pe", lambda e: e.matmul(pt, C["ones"][:], la[:], start=True, stop=True), reads=["la", "c_ones"], writes=["bk1"])
    P.op("dve", lambda e: e.tensor_scalar(ncum[:], pc, -1.0, None, ALU.mult), reads=["bk0"], writes=["ncum"])
    P.op("dve", lambda e: e.tensor_tensor(wgt[:], pt, ncum[:], ALU.add), reads=["bk1", "ncum"], writes=["wgt"])
    P.op("act", lambda e: e.activation(wgt[:], wgt[:], AF.Exp), reads=["wgt"], writes=["wgt"])
    P.op("dve", lambda e: e.tensor_tensor(wgt[:], wgt[:], dt[:], ALU.mult), reads=["wgt", "dt"], writes=["wgt"])
    P.op("act", lambda e: e.activation(dec[:], pt, AF.Exp), reads=["bk1"], writes=["dec"])
    if stage == 2:
        for i_, (t_, k_) in enumerate(((dt, "dt"), (la, "la"), (ncum, "ncum"), (wgt, "wgt"), (dec, "dec"))):
            P.dma("sp", yTd[:, i_ * 256:(i_ + 1) * 256], t_[:], reads=[k_], is_output=True)
        for i_, (t_, k_) in enumerate(((B32, "B32"), (C32, "C32"), (xT, "xT"))):
            P.dma("sp", yTd[:, 1280 + i_ * 1024:1280 + (i_ + 1) * 1024], t_[:, 0:1024], reads=[k_], is_output=True)
        P.finish(); return nc
    xpad = [P.sb(f"xpad{h}", [128, NCH, 128], BF16) for h in range(2)]
    Btok = P.sb("Btok", [128, NCH, 128], BF16); xw = P.sb("xw", [128, NCH, 4, 64], BF16)
    for h in range(2):
        P.op("pool", lambda e, h=h: e.memset(xpad[h][:], 0.0), writes=[f"xpad{h}"])
    ptr = [bk[2][:, 0:128], bk[3][:, 0:128]]
    for c in range(NCH):
        sl = slice(c * 128, (c + 1) * 128)
        P.op("pe", lambda e, sl=sl: e.transpose(ptr[0], xT[:, sl], C["ident"][:]), reads=["xT", "c_ident"], writes=["bk2"])
        P.op("pe", lambda e, sl=sl: e.transpose(ptr[1], B32[:, sl], C["ident"][:]), reads=["B32", "c_ident"], writes=["bk3"])
        for h in range(2):
            P.op("act", lambda e, h=h, c=c: e.activation(xpad[h][:, c, h * 64:(h + 1) * 64], ptr[0][:, h * 64:(h + 1) * 64], AF.Copy),
                 reads=["bk2"], writes=[f"xpad{h}"])
        for q in range(4):
            h = q % 2
            P.op("dve", lambda e, q=q, h=h, c=c: e.tensor_scalar(xw[:, c, q, :], ptr[0][:, h * 64:(h + 1) * 64], wgt[:, q * QS + c:q * QS + c + 1], None, ALU.mult),
                 reads=["bk2", "wgt"], writes=["xw"])
        P.op("act", lambda e, c=c: e.activation(Btok[:, c, :], ptr[1], AF.Copy), reads=["bk3"], writes=["Btok"])
    if stage == 3:
        P.dma("sp", yTd, xT[:], reads=["xT"], is_output=True); P.finish(); return nc
    yacc = P.sb("yacc", [128, TSEQ])
    Hpad = [P.sb(f"Hpad{q}", [128, 128]) for q in range(4)]
    larep = [P.sb(f"larep{i}", [128, 128]) for i in range(2)]
    seg = [P.sb(f"seg{i}", [128, 128]) for i in range(2)]; Et = [P.sb(f"Et{i}", [128, 128]) for i in range(2)]
    STp = [P.sb(f"STp{i}", [128, 128], BF16) for i in range(2)]; CTs = [P.sb(f"CTs{i}", [128, 128]) for i in range(2)]
    psA = bk[0][:, 0:128]; psE = bk[1][:, 0:128]; psS = bk[4][:, 0:128]; psY = bk[5][:, 0:128]
    psH = [bk[6][:, 0:64], bk[7][:, 0:64]]
    import os
    for d in range(int(os.environ.get('ND', '2'))):
        tri = C["tri_f"] if d == 0 else C["tri_b"]; nm = C["nm_f"] if d == 0 else C["nm_b"]
        trik = "c_tri_f" if d == 0 else "c_tri_b"; nmk = "c_nm_f" if d == 0 else "c_nm_b"
        order = list(range(NCH)) if d == 0 else [1, 0] + list(range(NCH - 1, 1, -1))
        for hh in range(2):
            P.op("pool", lambda e, q=d * 2 + hh: e.memset(Hpad[q][:], 0.0), writes=[f"Hpad{d*2+hh}"])
        for c in order:
            sl = slice(c * 128, (c + 1) * 128)
            P.op("pe", lambda e, sl=sl: e.matmul(psS, Bb[:, sl], Cb[:, sl], start=True, stop=True), reads=["Bb", "Cb"], writes=["bk4"])
            for hh in range(2):
                q = d * 2 + hh
                P.op("pool", lambda e, q=q, c=c, hh=hh: e.tensor_scalar(larep[hh][:], C["ones"][:], la[:, q * QS + c:q * QS + c + 1], None, ALU.mult),
                     reads=["c_ones", "la"], writes=[f"larep{hh}"])
                P.op("pe", lambda e, hh=hh, tri=tri: e.matmul(psA, larep[hh][:], tri[:], start=True, stop=False), reads=[f"larep{hh}", trik], writes=["bk0"])
                P.op("pe", lambda e, nm=nm: e.matmul(psA, C["ident"][:], nm[:], start=False, stop=True), reads=["c_ident", nmk], writes=["bk0"])
                P.op("pe", lambda e, hh=hh, tri=tri: e.matmul(psE, larep[hh][:], tri[:], start=True, stop=True), reads=[f"larep{hh}", trik], writes=["bk1"])
                P.op("act", lambda e, q=q, c=c, hh=hh: e.activation(seg[hh][:], psA, AF.Exp, bias=ncum[:, q * QS + c:q * QS + c + 1]), reads=["bk0", "ncum"], writes=[f"seg{hh}"])
                P.op("act", lambda e, hh=hh: e.activation(Et[hh][:], psE, AF.Exp), reads=["bk1"], writes=[f"Et{hh}"])
                P.op("dve", lambda e, q=q, c=c, hh=hh: e.scalar_tensor_tensor(STp[hh][:], psS, dt[:, q * QS + c:q * QS + c + 1], seg[hh][:], ALU.mult, ALU.mult),
                     reads=["bk4", "dt", f"seg{hh}"], writes=[f"STp{hh}"])
                P.op("dve", lambda e, sl=sl, hh=hh: e.tensor_tensor(CTs[hh][:], C32[:, sl], Et[hh][:], ALU.mult), reads=["C32", f"Et{hh}"], writes=[f"CTs{hh}"])
            for hh in range(2):
                q = d * 2 + hh
                P.op("pe", lambda e, hh=hh, c=c: e.matmul(psY, xpad[hh][:, c, :], STp[hh][:], start=(hh == 0), stop=False),
                     reads=[f"xpad{hh}", f"STp{hh}"], writes=["bk5"])
            for hh in range(2):
                q = d * 2 + hh
                P.op("pe", lambda e, hh=hh, q=q: e.matmul(psY, Hpad[q][:], CTs[hh][:], start=False, stop=(hh == 1)),
                     reads=[f"Hpad{q}", f"CTs{hh}"], writes=["bk5"])
            for hh in range(2):
                q = d * 2 + hh
                P.op("pe", lambda e, hh=hh, q=q, c=c: e.matmul(psH[hh], Btok[:, c, :], xw[:, c, q, :], start=True, stop=True),
                     reads=["Btok", "xw"], writes=[f"bk{6+hh}"])
                P.op("dve", lambda e, hh=hh, q=q, c=c: e.scalar_tensor_tensor(
                    Hpad[q][:, hh * 64:(hh + 1) * 64], Hpad[q][:, hh * 64:(hh + 1) * 64], dec[:, q * QS + c:q * QS + c + 1], psH[hh], ALU.mult, ALU.add),
                     reads=[f"Hpad{q}", "dec", f"bk{6+hh}"], writes=[f"Hpad{q}"])
            if d == 0:
                P.op("dve", lambda e, sl=sl: e.scalar_tensor_tensor(yacc[:, sl], xT[:, sl], dvec[:, 0:1], psY, ALU.mult, ALU.add),
                     reads=["xT", "dvec", "bk5"], writes=[f"yacc{c}"])
            else:
                P.op("dve", lambda e, sl=sl: e.tensor_tensor(yacc[:, sl], yacc[:, sl], psY, ALU.add), reads=[f"yacc{c}", "bk5"], writes=[f"yacc{c}"])
    if stage == 4:
        P.dma("sp", yTd, yacc[:], reads=[f"yacc{c}" for c in range(NCH)], is_output=True); P.finish(); return nc
    zT = raw
    P.dma("sp", zT[:], zTd, writes=["raw"])
    P.op("act", lambda e: e.activation(tmp[:], zT[:], AF.Silu), reads=["raw"], writes=["tmp"])
    allc = [f"yacc{c}" for c in range(NCH)]
    P.op("dve", lambda e: e.tensor_tensor(yacc[:], yacc[:], tmp[:], ALU.mult), reads=allc + ["tmp"], writes=allc)
    P.op("act", lambda e: e.activation(tmp[:], yacc[:], AF.Square), reads=allc, writes=["tmp"])
    pss = [bk[2][:, 0:256], bk[3][:, 0:256]]; rs = [P.sb(f"rs{i}", [128, 256]) for i in range(2)]
    for i, t0 in enumerate(range(0, TSEQ, 256)):
        n = min(256, TSEQ - t0); b = i % 2
        P.op("pe", lambda e, b=b, t0=t0, n=n: e.matmul(pss[b][:, 0:n], C["ones"][:], tmp[:, t0:t0 + n], start=True, stop=True), reads=["tmp", "c_ones"], writes=[f"bk{2+b}"])
        P.op("dve", lambda e, b=b, n=n: e.tensor_scalar(rs[b][:, 0:n], pss[b][:, 0:n], 1.0 / 128, 1e-5, ALU.mult, ALU.add), reads=[f"bk{2+b}"], writes=[f"rs{b}"])
        P.op("dve", lambda e, b=b, n=n: e.reciprocal(rs[b][:, 0:n], rs[b][:, 0:n]), reads=[f"rs{b}"], writes=[f"rs{b}"])
        P.op("act", lambda e, b=b, n=n: e.activation(rs[b][:, 0:n], rs[b][:, 0:n], AF.Sqrt), reads=[f"rs{b}"], writes=[f"rs{b}"])
        P.op("dve", lambda e, b=b, t0=t0, n=n: e.scalar_tensor_tensor(xT[:, t0:t0 + n], yacc[:, t0:t0 + n], normw[:, 0:1], rs[b][:, 0:n], ALU.mult, ALU.mult),
             reads=allc + ["normw", f"rs{b}"], writes=["xT"])
    P.dma("sp", yTd, xT[:], reads=["xT"], is_output=True)
    P.finish()
    return nc


def host_B1_inputs(pa, L, b, hp, prm):
    p = pa[b]
    z = p[:, hp * 128:(hp + 1) * 128].T
    x = p[:, 256 + hp * 128:256 + (hp + 1) * 128].T
    Bm = p[:, 512 + hp * 128:512 + (hp + 1) * 128].T
    Cm = p[:, 768 + hp * 128:768 + (hp + 1) * 128].T
    cols = [1024 + d * 4 + 2 * hp + hh for d in range(2) for hh in range(2)]
    dtr = np.zeros((128, 4, QS), np.float32); dtr[:, :, :NCH] = p[:, cols].reshape(NCH, 128, 4).transpose(1, 2, 0); dtr = dtr.reshape(128, 4 * QS)
    bc = lambda v: np.broadcast_to(np.asarray(v, np.float32)[None, :, None], (128, 4, QS)).reshape(128, 4 * QS)
    dtb = bc([prm['m_dt_bias'][L, d, 2 * hp + hh] for d in range(2) for hh in range(2)])
    alog = bc([prm['m_a_log'][L, d, 2 * hp + hh] for d in range(2) for hh in range(2)])
    cwfull = prm['m_conv_w'][L]; cbfull = prm['m_conv_b'][L]
    offs = [hp * 128, 256 + hp * 128, 512 + hp * 128]
    cw = np.stack([cwfull[:, o:o + 128].T for o in offs], 1)
    cb = np.stack([cbfull[o:o + 128] for o in offs], 1)
    dvec = np.repeat(prm['m_d'][L, 2 * hp:2 * hp + 2], 64)[:, None]
    normw = prm['m_norm_w'][L, hp * 128:(hp + 1) * 128][:, None]
    A = np.ascontiguousarray
    return {"zT": A(z), "xbcT": A(np.stack([x, Bm, Cm])), "dtr": A(dtr), "dtb": A(dtb), "alog": A(alog), "cw": A(cw), "cb": A(cb),
            "dvec": A(dvec), "normw": A(normw), "consts": host_consts()}


NPK = 34


def host_consts64():
    k = np.arange(128)[:, None]; i = np.arange(128)[None, :]
    same = (k // 64) == (i // 64)
    f = lambda m: m.astype(np.float32)
    tri_f = f(same & (k <= i)); tri_b = f(same & (k >= i))
    nm_f = np.where(same & (i >= k), 0.0, -30000.0); nm_b = np.where(same & (i <= k), 0.0, -30000.0)
    pms_f = np.where(same & (i < k), 0.0, 30000.0); pms_b = np.where(same & (i > k), 0.0, 30000.0)
    blk = f(same); selA = f(np.broadcast_to(k < 64, (128, 128))); selB = f(np.broadcast_to(k >= 64, (128, 128)))
    inc_f = f(same & (i < k)); inc_b = f(same & (i > k))
    return np.stack([np.eye(128), tri_f, tri_b, nm_f, nm_b, pms_f, pms_b, blk, selA, selB, inc_f, inc_b]).astype(np.float32)

C64_NAMES = ["ident", "tri_f", "tri_b", "nm_f", "nm_b", "pms_f", "pms_b", "blk", "selA", "selB", "sl", "su"]


def load_consts64(P, cd):
    c = {}
    for i, nm in enumerate(C64_NAMES):
        t = P.sb("c_" + nm, [128, 128]); P.dma("sp", t[:], cd[i], writes=["c_" + nm]); c[nm] = t
    ones = P.sb("c_ones", [128, 128]); P.I("pool", "memset", ones[:], 1.0, w=["c_ones"]); c["ones"] = ones
    return c


def tri_inverse_apply(P, C, Lm, X, ncolsX, bk, tg):
    Pt = [P.sb(f"{tg}P{i}", [128, 128]) for i in range(2)] if not hasattr(P, "_tri_" + tg) else getattr(P, "_tri_" + tg)[0]
    Qt = [P.sb(f"{tg}Q{i}", [128, 128]) for i in range(2)] if not hasattr(P, "_tri_" + tg) else getattr(P, "_tri_" + tg)[1]
    setattr(P, "_tri_" + tg, (Pt, Qt))
    (pP, kP), (pQ, kQ), (pT, kT), (pX, kX) = bk["P"], bk["Q"], bk["T"], bk["X"]
    ident = C["ident"]
    P.I("pe", "transpose", pT[:, 0:128], Lm[:], ident[:], r=[tg + "L", "c_ident"], w=[kT])
    P.I("act", "activation", Qt[0][:], pT[:, 0:128], AF.Copy, r=[kT], w=[f"{tg}Q0"])
    P.I("pe", "matmul", pX[:, 0:ncolsX], Qt[0][:], X[:], start=True, stop=True, r=[f"{tg}Q0", tg + "X"], w=[kX])
    P.I("dve", "tensor_tensor", X[:], X[:], pX[:, 0:ncolsX], ALU.subtract, r=[tg + "X", kX], w=[tg + "X"])
    Pc, Pk, Qc, Qk = Lm, tg + "L", Qt[0], f"{tg}Q0"
    for lvl in range(1, 6):
        a = lvl % 2
        P.I("pe", "matmul", pQ[:, 0:128], Pc[:], Qc[:], start=True, stop=True, r=[Pk, Qk], w=[kQ])
        if lvl < 5:
            P.I("pe", "matmul", pP[:, 0:128], Qc[:], Pc[:], start=True, stop=True, r=[Pk, Qk], w=[kP])
            P.I("dve", "tensor_copy", Pt[a][:], pP[:, 0:128], r=[kP], w=[f"{tg}P{a}"])
        P.I("act", "activation", Qt[a][:], pQ[:, 0:128], AF.Copy, r=[kQ], w=[f"{tg}Q{a}"])
        Pc, Pk, Qc, Qk = Pt[a], f"{tg}P{a}", Qt[a], f"{tg}Q{a}"
        P.I("pe", "matmul", pX[:, 0:ncolsX], Qc[:], X[:], start=True, stop=True, r=[Qk, tg + "X"], w=[kX])
        P.I("dve", "tensor_tensor", X[:], X[:], pX[:, 0:ncolsX], ALU.add, r=[tg + "X", kX], w=[tg + "X"])


def build_B2():
    nc = bass.Bass("TRN2", target_bir_lowering=False)
    D = lambda n, s: nc.dram_tensor(n, s, F32, kind="ExternalInput").ap()
    qkvd = D("qkvT", [3, 128, TSEQ]); gated = D("gate", [128, NPK, 128]); tabd = D("tab", [4, 128, 2 * QS])
    cwd = D("cw", [128, 3, 3]); nwd = D("normw", [128, 128]); cd = D("consts", [len(C64_NAMES), 128, 128])
    yd = nc.dram_tensor("y", [128, NPK, 128], F32, kind="ExternalOutput").ap()
    P = Prog(nc)
    C = load_consts64(P, cd)
    bkt = [P.ps(f"bank{i}", [128, 512]) for i in range(8)]
    BK = lambda i: (bkt[i], f"bk{i}")
    raw = P.sb("raw", [128, TSEQ]); tmp = P.sb("tmp", [128, TSEQ])
    qT = P.sb("qT", [128, TSEQ]); kT = P.sb("kT", [128, TSEQ])
    cw = P.sb("cw", [128, 3, 3]); P.dma("sp", cw[:], cwd, writes=["cw"])
    normw = P.sb("normw", [128, 128]); P.dma("sp", normw[:], nwd, writes=["normw"])
    ktok = P.sb("ktok", [128, NPK, 128]); vtok = P.sb("vtok", [128, NPK, 128]); oacc = P.sb("oacc", [128, NPK, 128])
    rs = [P.sb(f"rs{i}", [128, 256]) for i in range(2)]
    for ti, (dst, kd) in enumerate(((qT, "qT"), (kT, "kT"), (raw, "raw"))):
        P.dma("sp", raw[:], qkvd[ti], writes=["raw"])
        conv_silu(P, dst, raw, cw, None, ti, kd, "raw", tmp, "tmp")
        if ti < 2:
            P.I("act", "activation", tmp[:], dst[:], AF.Square, r=[kd], w=["tmp"])
            for i, t0 in enumerate(range(0, TSEQ, 256)):
                b = i % 2; (pa, pk) = BK(b)
                P.I("pe", "matmul", pa[:, 0:256], C["ones"][:], tmp[:, t0:t0 + 256], start=True, stop=True, r=["tmp", "c_ones"], w=[pk])
                P.I("dve", "tensor_scalar", rs[b][:], pa[:, 0:256], 1e-6, None, ALU.add, r=[pk], w=[f"rs{b}"])
                P.I("dve", "reciprocal", rs[b][:], rs[b][:], r=[f"rs{b}"], w=[f"rs{b}"])
                P.I("act", "activation", rs[b][:], rs[b][:], AF.Sqrt, r=[f"rs{b}"], w=[f"rs{b}"])
                sc = 128.0 ** -0.5 if ti == 0 else 1.0
                P.I("dve", "scalar_tensor_tensor", dst[:, t0:t0 + 256], dst[:, t0:t0 + 256], sc, rs[b][:], ALU.mult, ALU.mult,
                    r=[kd, f"rs{b}"], w=[kd])
    vT = raw
    for c in range(NPK):
        sl = slice(c * 128, (c + 1) * 128)
        for src, sk, dst, dk, bi in ((kT, "kT", ktok, "ktok", 0), (vT, "raw", vtok, "vtok", 1)):
            (pa, pk) = BK(bi)
            P.I("pe", "transpose", pa[:, 0:128], src[:, sl], C["ident"][:], r=[sk, "c_ident"], w=[pk])
            P.I("act" if bi else "dve", "activation" if bi else "tensor_copy", dst[:, c, :], pa[:, 0:128], *([AF.Copy] if bi else []), r=[pk], w=[dk])
    W2 = 2 * QS
    tb = {n: P.sb("t_" + n, [128, W2]) for n in ("braw", "araw", "dtb", "alog", "beta", "g", "gc", "ngc", "egc", "toend", "glA", "glB", "bw")}
    for i, n in enumerate(("braw", "araw", "dtb", "alog")):
        P.dma("sp", tb[n][:], tabd[i], writes=["t_" + n])
    P.I("act", "activation", tb["beta"][:], tb["braw"][:], AF.Sigmoid, r=["t_braw"], w=["t_beta"])
    P.I("dve", "tensor_tensor", tb["araw"][:], tb["araw"][:], tb["dtb"][:], ALU.add, r=["t_araw", "t_dtb"], w=["t_araw"])
    P.I("dve", "tensor_scalar", tb["araw"][:], tb["araw"][:], 60.0, None, ALU.min, r=["t_araw"], w=["t_araw"])
    P.I("act", "activation", tb["araw"][:], tb["araw"][:], AF.Exp, r=["t_araw"], w=["t_araw"])
    P.I("act", "activation", tb["araw"][:], tb["araw"][:], AF.Ln, bias=1.0, r=["t_araw"], w=["t_araw"])
    P.I("act", "activation", tb["alog"][:], tb["alog"][:], AF.Exp, r=["t_alog"], w=["t_alog"])
    P.I("dve", "scalar_tensor_tensor", tb["g"][:], tb["araw"][:], -1.0, tb["alog"][:], ALU.mult, ALU.mult, r=["t_araw", "t_alog"], w=["t_g"])
    (p0, k0), (p1, k1), (p2, k2), (p3, k3) = BK(0), BK(1), BK(2), BK(3)
    P.I("pe", "matmul", p0[:, 0:QS], C["tri_f"][:], tb["g"][:, 0:QS], start=True, stop=True, r=["t_g", "c_tri_f"], w=[k0])
    P.I("pe", "matmul", p0[:, QS:W2], C["tri_b"][:], tb["g"][:, QS:W2], start=True, stop=True, r=["t_g", "c_tri_b"], w=[k0])
    P.I("pe", "matmul", p1[:, 0:W2], C["blk"][:], tb["g"][:], start=True, stop=True, r=["t_g", "c_blk"], w=[k1])
    P.I("pe", "matmul", p2[:, 0:W2], C["selA"][:], tb["g"][:], start=True, stop=True, r=["t_g", "c_selA"], w=[k2])
    P.I("pe", "matmul", p3[:, 0:W2], C["selB"][:], tb["g"][:], start=True, stop=True, r=["t_g", "c_selB"], w=[k3])
    P.I("dve", "tensor_copy", tb["gc"][:], p0[:, 0:W2], r=[k0], w=["t_gc"])
    P.I("dve", "tensor_scalar", tb["ngc"][:], tb["gc"][:], -1.0, None, ALU.mult, r=["t_gc"], w=["t_ngc"])
    P.I("act", "activation", tb["egc"][:], tb["gc"][:], AF.Exp, r=["t_gc"], w=["t_egc"])
    P.I("dve", "tensor_tensor", tb["toend"][:], p1[:, 0:W2], tb["gc"][:], ALU.subtract, r=[k1, "t_gc"], w=["t_toend"])
    P.I("act", "activation", tb["toend"][:], tb["toend"][:], AF.Exp, r=["t_toend"], w=["t_toend"])
    P.I("act", "activation", tb["glA"][:], p2[:, 0:W2], AF.Exp, r=[k2], w=["t_glA"])
    P.I("act", "activation", tb["glB"][:], p3[:, 0:W2], AF.Exp, r=[k3], w=["t_glB"])
    P.I("dve", "tensor_tensor", tb["bw"][:], tb["beta"][:], tb["egc"][:], ALU.mult, r=["t_beta", "t_egc"], w=["t_bw"])
    S = P.sb("S", [128, 128]); grep = P.sb("grep", [128, 128])
    DmT = P.sb("DmT", [128, 128]); DmS = P.sb("DmS", [128, 128]); Et = P.sb("Et", [128, 128])
    attnT = P.sb("attnT", [128, 128]); Lm = P.sb("dnL", [128, 128]); qdT = P.sb("qdT", [128, 128])
    X = P.sb("dnX", [128, 256]); kdec = P.sb("kdec", [128, 128]); wT = P.sb("wT", [128, 128]); vnew = P.sb("vnew", [128, 128])
    bkinv = {"P": BK(0), "Q": BK(1), "T": BK(2), "X": BK(3)}
    for d in range(2):
        sfx = "_f" if d == 0 else "_b"
        tri, nm, pms = C["tri" + sfx], C["nm" + sfx], C["pms" + sfx]
        order = list(range(NPK)) if d == 0 else [1, 0] + list(range(NPK - 1, 1, -1))
        P.I("pool", "memset", S[:], 0.0, w=["S"])
        for c in order:
            sl = slice(c * 128, (c + 1) * 128); col = d * QS + c; cs = slice(col, col + 1)
            (pG, kG), (pA, kA), (pD1, kD1), (pD2, kD2), (pE, kE), (pV, kV), (pO, kO), (pS, kS) = [BK(i) for i in range(8)]
            P.I("pe", "matmul", pG[:, 0:128], kT[:, sl], kT[:, sl], start=True, stop=True, r=["kT"], w=[kG])
            P.I("pe", "matmul", pA[:, 0:128], kT[:, sl], qT[:, sl], start=True, stop=True, r=["kT", "qT"], w=[kA])
            P.I("pool", "tensor_scalar", grep[:], C["ones"][:], tb["g"][:, cs], None, ALU.mult, r=["c_ones", "t_g"], w=["grep"])
            P.I("pe", "matmul", pD1[:, 0:128], grep[:], tri[:], start=True, stop=False, r=["grep", "c_tri" + sfx], w=[kD1])
            P.I("pe", "matmul", pD1[:, 0:128], C["ident"][:], nm[:], start=False, stop=True, r=["c_ident", "c_nm" + sfx], w=[kD1])
            P.I("pe", "matmul", pD2[:, 0:128], grep[:], tri[:], start=True, stop=False, r=["grep", "c_tri" + sfx], w=[kD2])
            P.I("pe", "matmul", pD2[:, 0:128], C["ident"][:], pms[:], start=False, stop=True, r=["c_ident", "c_pms" + sfx], w=[kD2])
            P.I("pe", "matmul", pE[:, 0:128], grep[:], tri[:], start=True, stop=True, r=["grep", "c_tri" + sfx], w=[kE])
            P.I("act", "activation", DmT[:], pD1[:, 0:128], AF.Exp, bias=tb["ngc"][:, cs], r=[kD1, "t_ngc"], w=["DmT"])
            P.I("act", "activation", DmS[:], pD2[:, 0:128], AF.Exp, bias=tb["gc"][:, cs], scale=-1.0, r=[kD2, "t_gc"], w=["DmS"])
            P.I("act", "activation", Et[:], pE[:, 0:128], AF.Exp, r=[kE], w=["Et"])
            P.I("dve", "tensor_tensor", attnT[:], pA[:, 0:128], DmT[:], ALU.mult, r=[kA, "DmT"], w=["attnT"])
            P.I("dve", "scalar_tensor_tensor", Lm[:], pG[:, 0:128], tb["beta"][:, cs], DmS[:], ALU.mult, ALU.mult, r=[kG, "t_beta", "DmS"], w=["dnL"])
            P.I("dve", "tensor_tensor", qdT[:], qT[:, sl], Et[:], ALU.mult, r=["qT", "Et"], w=["qdT"])
            P.I("dve", "tensor_scalar", X[:, 0:128], vtok[:, c, :], tb["beta"][:, cs], None, ALU.mult, r=["vtok", "t_beta"], w=["dnX"])
            P.I("dve", "tensor_scalar", X[:, 128:256], ktok[:, c, :], tb["bw"][:, cs], None, ALU.mult, r=["ktok", "t_bw"], w=["dnX"])
            P.I("pool", "tensor_scalar", kdec[:], ktok[:, c, :], tb["toend"][:, cs], None, ALU.mult, r=["ktok", "t_toend"], w=["kdec"])
            tri_inverse_apply(P, C, Lm, X, 256, bkinv, "dn")
            P.I("pe", "transpose", pD1[:, 0:128], X[:, 128:256], C["ident"][:], r=["dnX", "c_ident"], w=[kD1])
            P.I("act", "activation", wT[:], pD1[:, 0:128], AF.Copy, r=[kD1], w=["wT"])
            for half in ((0, 1) if d == 0 else (1, 0)):
                rows = slice(half * 64, (half + 1) * 64)
                gl = tb["glA"] if half == 0 else tb["glB"]; glk = "t_glA" if half == 0 else "t_glB"
                P.I("pe", "matmul", pV[:, 0:128], wT[:], S[:], start=True, stop=True, r=["wT", "S"], w=[kV])
                P.I("dve", "tensor_tensor", vnew[rows, :], X[rows, 0:128], pV[rows, 0:128], ALU.subtract, r=["dnX", kV], w=["vnew"])
                P.I("pe", "matmul", pO[:, 0:128], qdT[:], S[:], start=True, stop=False, r=["qdT", "S"], w=[kO])
                P.I("pe", "matmul", pO[:, 0:128], attnT[rows, :], vnew[rows, :], start=False, stop=True, r=["attnT", "vnew"], w=[kO])
                if d == 0:
                    P.I("act", "activation", oacc[rows, c, :], pO[rows, 0:128], AF.Copy, r=[kO], w=[f"oacc{c}"])
                else:
                    P.I("dve", "tensor_tensor", oacc[rows, c, :], oacc[rows, c, :], pO[rows, 0:128], ALU.add, r=[kO, f"oacc{c}"], w=[f"oacc{c}"])
                P.I("pe", "matmul", pS[:, 0:128], kdec[rows, :], vnew[rows, :], start=True, stop=True, r=["kdec", "vnew"], w=[kS])
                P.I("dve", "scalar_tensor_tensor", S[:], S[:], gl[:, cs], pS[:, 0:128], ALU.mult, ALU.add, r=["S", glk, kS], w=["S"])
    allo = [f"oacc{c}" for c in range(NPK)]
    gate = P.sb("gate", [128, NPK, 128]); sq = P.sb("sq", [128, NPK, 128]); ss = P.sb("ss", [128, NPK])
    P.dma("sp", gate[:], gated, writes=["gate"])
    P.I("act", "activation", gate[:], gate[:], AF.Silu, r=["gate"], w=["gate"])
    P.I("act", "activation", sq[:], oacc[:], AF.Square, r=allo, w=["sq"])
    P.I("dve", "tensor_reduce", ss[:], sq[:], AX.X, ALU.add, r=["sq"], w=["ss"])
    P.I("dve", "tensor_scalar", ss[:], ss[:], 1.0 / 128, 1e-6, ALU.mult, ALU.add, r=["ss"], w=["ss"])
    P.I("dve", "reciprocal", ss[:], ss[:], r=["ss"], w=["ss"])
    P.I("act", "activation", ss[:], ss[:], AF.Sqrt, r=["ss"], w=["ss"])
    for c in range(NPK):
        P.I("dve", "scalar_tensor_tensor", sq[:, c, :], oacc[:, c, :], ss[:, c:c + 1], normw[:], ALU.mult, ALU.mult, r=allo + ["ss", "normw"], w=["sq"])
    P.I("dve", "tensor_tensor", sq[:], sq[:], gate[:], ALU.mult, r=["sq", "gate"], w=["sq"])
    P.dma("sp", yd, sq[:], reads=["sq"], is_output=True)
    P.finish()
    return nc


def colmajor_perm():
    t = np.arange(4096).reshape(64, 64)
    return t.T.reshape(-1)


def host_B2_inputs(pb, L, b, head, prm):
    perm = np.concatenate([np.arange(256), 256 + colmajor_perm()])
    p = pb[b][perm]
    q = p[:, head * 128:(head + 1) * 128].T; k = p[:, 512 + head * 128:512 + (head + 1) * 128].T
    v = p[:, 1024 + head * 128:1024 + (head + 1) * 128].T
    gate = p[:, 1536 + head * 128:1536 + (head + 1) * 128].reshape(NPK, 128, 128).transpose(1, 0, 2)
    def tabl(cols):
        t = np.zeros((128, 2, QS), np.float32); t[:, :, :NPK] = p[:, cols].reshape(NPK, 128, 2).transpose(1, 2, 0); return t.reshape(128, 2 * QS)
    braw = tabl([2048 + d * 4 + head for d in range(2)]); araw = tabl([2056 + d * 4 + head for d in range(2)])
    bc = lambda v_: np.broadcast_to(np.asarray(v_, np.float32)[None, :, None], (128, 2, QS)).reshape(128, 2 * QS)
    dtb = bc(prm['dn_dt_bias'][L, :, head]); alog = bc(prm['dn_a_log'][L, :, head])
    cwf = prm['dn_conv_w'][L]
    cw = np.stack([cwf[:, o + head * 128:o + (head + 1) * 128].T for o in (0, 512, 1024)], 1)
    normw = np.broadcast_to(prm['dn_norm_w'][L][None, :], (128, 128))
    A = lambda a: np.ascontiguousarray(a, dtype=np.float32)
    return {"qkvT": A(np.stack([q, k, v])), "gate": A(gate), "tab": A(np.stack([braw, araw, dtb, alog])), "cw": A(cw), "normw": A(normw),
            "consts": host_consts64()}


def host_B2_output(y):
    yy = y.transpose(1, 0, 2).reshape(TSEQ, 128)
    out = np.empty_like(yy)
    perm = np.concatenate([np.arange(256), 256 + colmajor_perm()])
    out[perm] = yy
    return out


def build_B3():
    nc = bass.Bass("TRN2", target_bir_lowering=False)
    D = lambda n, s: nc.dram_tensor(n, s, F32, kind="ExternalInput").ap()
    p64d = D("p64", [4, 64, TSEQ]); p128d = D("p128", [2, 128, TSEQ]); mu64d = D("mu64", [4, 64, 8]); mu128d = D("mu128", [2, 128, 8])
    pvd = D("pv", [64, 8]); a2d = D("a2h", [64, 64]); g2d = D("g2h", [128, 64]); w2d = D("w2pad", [2, 128, 64])
    cd = D("consts", [len(C64_NAMES), 128, 128])
    yd = nc.dram_tensor("y", [64, TSEQ], F32, kind="ExternalOutput").ap()
    P = Prog(nc)
    C = load_consts64(P, cd)
    bkt = [P.ps(f"bank{i}", [128, 512]) for i in range(8)]
    BK = lambda i: (bkt[i], f"bk{i}")
    raw = P.sb("raw", [128, TSEQ]); mix = P.sb("mix", [128, TSEQ])
    mu64 = P.sb("mu64", [64, 4, 8]); mu128 = P.sb("mu128", [128, 2, 8]); pv = P.sb("pv", [64, 8])
    for i in range(4):
        P.dma("sp", mu64[:, i, :], mu64d[i], writes=["mu64"])
    for i in range(2):
        P.dma("sp", mu128[:, i, :], mu128d[i], writes=["mu128"])
    P.dma("sp", pv[:], pvd, writes=["pv"])
    a2h = P.sb("a2h", [64, 64]); g2h = P.sb("g2h", [128, 64]); w2p = P.sb("w2p", [128, 2, 64])
    P.dma("sp", a2h[:], a2d, writes=["a2h"]); P.dma("sp", g2h[:], g2d, writes=["g2h"])
    for j in range(2):
        P.dma("sp", w2p[:, j, :], w2d[j], writes=["w2p"])
    omm64 = P.sb("omm64", [64, 4]); omm128 = P.sb("omm128", [128, 2])
    P.I("dve", "tensor_scalar", omm64[:], mu64[:, :, 0], -1.0, 1.0, ALU.mult, ALU.add, r=["mu64"], w=["omm64"])
    P.I("dve", "tensor_scalar", omm128[:], mu128[:, :, 0], -1.0, 1.0, ALU.mult, ALU.add, r=["mu128"], w=["omm128"])

    def token_mix(dst, dk, src_d, npart, mu, muk, omm, ommk, ti):
        R = slice(0, npart)
        P.dma("sp", raw[R, :], src_d, writes=["raw"])
        P.I("dve", "tensor_scalar", dst[R, :], raw[R, :], omm[R, ti:ti + 1], None, ALU.mult, r=["raw", ommk], w=[dk])
        def acc(o0, o1, i0, i1, mcol, eng="dve"):
            P.I(eng, "scalar_tensor_tensor", dst[R, o0:o1], raw[R, i0:i1], mu[R, ti, mcol:mcol + 1], dst[R, o0:o1], ALU.mult, ALU.add,
                r=["raw", muk, dk], w=[dk])
        acc(1, 256, 0, 255, 5); acc(0, 255, 1, 256, 6)
        acc(256 + 64, TSEQ, 256, TSEQ - 64, 3); acc(256, TSEQ - 64, 256 + 64, TSEQ, 4)
        dl = dst[R, 256:TSEQ].rearrange("p (r c) -> p r c", c=64); rl = raw[R, 256:TSEQ].rearrange("p (r c) -> p r c", c=64)
        P.I("dve", "scalar_tensor_tensor", dl[:, :, 1:64], rl[:, :, 0:63], mu[R, ti, 1:2], dl[:, :, 1:64], ALU.mult, ALU.add, r=["raw", muk, dk], w=[dk])
        P.I("dve", "scalar_tensor_tensor", dl[:, :, 0:63], rl[:, :, 1:64], mu[R, ti, 2:3], dl[:, :, 0:63], ALU.mult, ALU.add, r=["raw", muk, dk], w=[dk])

    rT = P.sb("rT", [64, TSEQ]); kT = P.sb("kT", [64, TSEQ]); vT = P.sb("vT", [64, TSEQ]); aT = P.sb("aT", [64, TSEQ])
    gT = P.sb("gT", [64, TSEQ]); bT = P.sb("bT", [64, TSEQ]); lwT = [P.sb(f"lwT{j}", [64, TSEQ]) for j in range(2)]
    token_mix(rT, "rT", p64d[0], 64, mu64, "mu64", omm64, "omm64", 0)
    token_mix(kT, "kT", p64d[1], 64, mu64, "mu64", omm64, "omm64", 1)
    token_mix(vT, "vT", p64d[2], 64, mu64, "mu64", omm64, "omm64", 2)
    NB = 256
    token_mix(mix, "mix", p64d[3], 64, mu64, "mu64", omm64, "omm64", 3)
    for i, t0 in enumerate(range(0, TSEQ, NB)):
        (pa, pk) = BK(i % 2)
        P.I("pe", "matmul", pa[0:64, 0:NB], a2h[:], mix[0:64, t0:t0 + NB], start=True, stop=True, r=["a2h", "mix"], w=[pk])
        P.I("act", "activation", aT[:, t0:t0 + NB], pa[0:64, 0:NB], AF.Sigmoid, bias=pv[:, 0:1], r=[pk, "pv"], w=["aT"])
    token_mix(mix, "mix", p128d[0], 128, mu128, "mu128", omm128, "omm128", 0)
    P.I("act", "activation", mix[:], mix[:], AF.Tanh, r=["mix"], w=["mix"])
    for j in range(2):
        for i, t0 in enumerate(range(0, TSEQ, NB)):
            (pa, pk) = BK(i % 2)
            P.I("pe", "matmul", pa[0:64, 0:NB], w2p[:, j, :], mix[:, t0:t0 + NB], start=True, stop=True, r=["w2p", "mix"], w=[pk])
            P.I("act", "activation", lwT[j][:, t0:t0 + NB], pa[0:64, 0:NB], AF.Sigmoid, bias=pv[:, 3 + j:4 + j], r=[pk, "pv"], w=[f"lwT{j}"])
        P.I("dve", "tensor_scalar", lwT[j][:], lwT[j][:], -float(np.exp(-0.5)), None, ALU.mult, r=[f"lwT{j}"], w=[f"lwT{j}"])
    token_mix(mix, "mix", p128d[1], 128, mu128, "mu128", omm128, "omm128", 1)
    P.I("act", "activation", mix[:], mix[:], AF.Sigmoid, r=["mix"], w=["mix"])
    for i, t0 in enumerate(range(0, TSEQ, NB)):
        (pa, pk) = BK(i % 2)
        P.I("pe", "matmul", pa[0:64, 0:NB], g2h[:], mix[:, t0:t0 + NB], start=True, stop=True, r=["g2h", "mix"], w=[pk])
        P.I("act", "activation", gT[:, t0:t0 + NB], pa[0:64, 0:NB], AF.Copy, r=[pk], w=["gT"])
    kk = mix
    P.I("dve", "tensor_scalar", kk[0:64, :], kT[:], pv[:, 1:2], None, ALU.mult, r=["kT", "pv"], w=["mix"])
    P.I("act", "activation", raw[0:64, :], kk[0:64, :], AF.Square, r=["mix"], w=["raw"])
    rs = [P.sb(f"rs{i}", [64, NB]) for i in range(2)]
    for i, t0 in enumerate(range(0, TSEQ, NB)):
        b = i % 2; (pa, pk) = BK(b)
        P.I("pe", "matmul", pa[0:64, 0:NB], C["ones"][0:64, 0:64], raw[0:64, t0:t0 + NB], start=True, stop=True, r=["raw", "c_ones"], w=[pk])
        P.I("dve", "tensor_scalar", rs[b][:], pa[0:64, 0:NB], 1e-6, None, ALU.add, r=[pk], w=[f"rs{b}"])
        P.I("dve", "reciprocal", rs[b][:], rs[b][:], r=[f"rs{b}"], w=[f"rs{b}"])
        P.I("act", "activation", rs[b][:], rs[b][:], AF.Sqrt, r=[f"rs{b}"], w=[f"rs{b}"])
        P.I("dve", "tensor_tensor", kk[0:64, t0:t0 + NB], kk[0:64, t0:t0 + NB], rs[b][:], ALU.mult, r=["mix", f"rs{b}"], w=["mix"])
    P.I("dve", "tensor_tensor", bT[:], kk[0:64, :], aT[:], ALU.mult, r=["mix", "aT"], w=["bT"])
    P.I("dve", "tensor_scalar", kk[0:64, :], kk[0:64, :], -1.0, None, ALU.mult, r=["mix"], w=["mix"])
    P.I("dve", "tensor_scalar", aT[:], aT[:], -1.0, pv[:, 2:3], ALU.add, ALU.mult, r=["aT", "pv"], w=["aT"])
    P.I("dve", "scalar_tensor_tensor", kT[:], aT[:], 1.0, kT[:], ALU.add, ALU.mult, r=["aT", "kT"], w=["kT"])
    avT = kk
    oacc = P.sb("oacc", [128, NPK, 64])
    H = P.sb("H", [64, 64])
    T_ = lambda n, sh: P.sb(n, sh)
    lwtok = T_("lwtok", [128, 64]); ea_tok = T_("ea_tok", [128, 64]); te_tok = T_("te_tok", [128, 64])
    ep = T_("ep", [64, 128]); em = T_("em", [64, 128]); eaT = T_("eaT", [64, 128])
    atl = T_("atl", [64, 128]); btl = T_("btl", [64, 128]); ktl = T_("ktl", [64, 128]); rtl = T_("rtl", [64, 128])
    Lm = T_("rwL", [128, 128]); AakT = T_("AakT", [128, 128]); ArbT = T_("ArbT", [128, 128]); ArkT = T_("ArkT", [128, 128])
    X = T_("rwX", [128, 128]); Bh = T_("Bh", [128, 64]); Kh = T_("Kh", [128, 64]); W1T = T_("W1T", [64, 128]); U = T_("U", [128, 64])
    pc2 = T_("pc2", [64, 2])
    tk = {n: T_(n + "_t", [128, 64]) for n in ("av", "b", "k", "v")}
    bkinv = {"P": BK(0), "Q": BK(1), "T": BK(2), "X": BK(3)}
    for d in range(2):
        sfx = "_f" if d == 0 else "_b"
        tri = C["tri" + sfx]; trik = "c_tri" + sfx
        m_strict_ts = C["sl"] if d == 0 else C["su"]; mk_ts = "c_sl" if d == 0 else "c_su"
        m_strict_st = C["su"] if d == 0 else C["sl"]; mk_st = "c_su" if d == 0 else "c_sl"
        m_incl_st = C["tri_f"] if d == 0 else C["tri_b"]; mk_in = trik
        order = list(range(NPK)) if d == 0 else [1, 0] + list(range(NPK - 1, 1, -1))
        P.I("pool", "memset", H[:], 0.0, w=["H"])
        for c in order:
            sl = slice(c * 128, (c + 1) * 128)
            (p0, k0), (p1, k1), (p2, k2), (p3, k3), (p4, k4), (p5, k5), (p6, k6), (p7, k7) = [BK(i) for i in range(8)]
            lw = lwT[d]; lwk = f"lwT{d}"
            for ii, (n, src, sk) in enumerate((("av", avT, "mix"), ("b", bT, "bT"), ("k", kT, "kT"), ("v", vT, "vT"))):
                (pa, pk) = BK(4 + ii)
                P.I("pe", "transpose", pa[:, 0:64], src[0:64, sl], C["ident"][0:64, 0:64], r=[sk, "c_ident"], w=[pk])
                if ii % 2:
                    P.I("act", "activation", tk[n][:], pa[:, 0:64], AF.Copy, r=[pk], w=[n + "_t"])
                else:
                    P.I("dve", "tensor_copy", tk[n][:], pa[:, 0:64], r=[pk], w=[n + "_t"])
            P.I("pe", "transpose", p0[:, 0:64], lw[:, sl], C["ident"][0:64, 0:64], r=[lwk, "c_ident"], w=[k0])
            P.I("dve", "tensor_copy", lwtok[:], p0[:, 0:64], r=[k0], w=["lwtok"])
            P.I("pe", "matmul", p1[:, 0:64], tri[:], lwtok[:], start=True, stop=True, r=[trik, "lwtok"], w=[k1])
            P.I("pe", "matmul", p2[:, 0:64], C["blk"][:], lwtok[:], start=True, stop=True, r=["c_blk", "lwtok"], w=[k2])
            P.I("pe", "matmul", p3[0:64, 0:128], lwtok[:], tri[:], start=True, stop=True, r=[trik, "lwtok"], w=[k3])
            P.I("dve", "tensor_tensor", ea_tok[:], p1[:, 0:64], lwtok[:], ALU.subtract, r=[k1, "lwtok"], w=["ea_tok"])
            P.I("act", "activation", ea_tok[:], ea_tok[:], AF.Exp, r=["ea_tok"], w=["ea_tok"])
            P.I("dve", "tensor_copy", te_tok[:], p1[:, 0:64], r=[k1], w=["te_tok"])
            P.I("dve", "tensor_tensor", te_tok[:], p2[:, 0:64], te_tok[:], ALU.subtract, r=[k2, "te_tok"], w=["te_tok"])
            P.I("act", "activation", te_tok[:], te_tok[:], AF.Exp, r=["te_tok"], w=["te_tok"])
            P.I("act", "activation", ep[:], p3[0:64, 0:128], AF.Exp, r=[k3], w=["ep"])
            P.I("act", "activation", em[:], p3[0:64, 0:128], AF.Exp, scale=-1.0, r=[k3], w=["em"])
            P.I("dve", "tensor_tensor", eaT[:], p3[0:64, 0:128], lw[:, sl], ALU.subtract, r=[k3, lwk], w=["eaT"])
            P.I("act", "activation", eaT[:], eaT[:], AF.Exp, r=["eaT"], w=["eaT"])
            cA, cB = (63, 127) if d == 0 else (0, 64)
            P.I("act", "activation", pc2[:, 0:1], p3[0:64, cA:cA + 1], AF.Exp, r=[k3], w=["pc2"])
            P.I("act", "activation", pc2[:, 1:2], p3[0:64, cB:cB + 1], AF.Exp, r=[k3], w=["pc2"])
            P.I("dve", "tensor_tensor", atl[:], avT[0:64, sl], eaT[:], ALU.mult, r=["mix", "eaT"], w=["atl"])
            P.I("dve", "tensor_tensor", btl[:], bT[:, sl], em[:], ALU.mult, r=["bT", "em"], w=["btl"])
            P.I("pool", "tensor_tensor", ktl[:], kT[:, sl], em[:], ALU.mult, r=["kT", "em"], w=["ktl"])
            P.I("pool", "tensor_tensor", rtl[:], rT[:, sl], ep[:], ALU.mult, r=["rT", "ep"], w=["rtl"])
            P.I("pe", "matmul", p4[:, 0:128], atl[:], btl[:], start=True, stop=True, r=["atl", "btl"], w=[k4])
            P.I("pe", "matmul", p5[:, 0:128], ktl[:], atl[:], start=True, stop=True, r=["ktl", "atl"], w=[k5])
            P.I("pe", "matmul", p6[:, 0:128], btl[:], rtl[:], start=True, stop=True, r=["btl", "rtl"], w=[k6])
            P.I("pe", "matmul", p7[:, 0:128], ktl[:], rtl[:], start=True, stop=True, r=["ktl", "rtl"], w=[k7])
            P.I("dve", "scalar_tensor_tensor", Lm[:], p4[:, 0:128], -1.0, m_strict_ts[:], ALU.mult, ALU.mult, r=[k4, mk_ts], w=["rwL"])
            P.I("dve", "tensor_tensor", AakT[:], p5[:, 0:128], m_strict_st[:], ALU.mult, r=[k5, mk_st], w=["AakT"])
            P.I("dve", "tensor_tensor", ArbT[:], p6[:, 0:128], m_incl_st[:], ALU.mult, r=[k6, mk_in], w=["ArbT"])
            P.I("dve", "tensor_tensor", ArkT[:], p7[:, 0:128], m_incl_st[:], ALU.mult, r=[k7, mk_in], w=["ArkT"])
            P.I("dve", "tensor_tensor", X[:, 0:64], tk["av"][:], ea_tok[:], ALU.mult, r=["av_t", "ea_tok"], w=["rwX"])
            P.I("pe", "matmul", p4[:, 0:64], AakT[:], tk["v"][:], start=True, stop=True, r=["AakT", "v_t"], w=[k4])
            P.I("act", "activation", X[:, 64:128], p4[:, 0:64], AF.Copy, r=[k4], w=["rwX"])
            P.I("pool", "tensor_tensor", Bh[:], tk["b"][:], te_tok[:], ALU.mult, r=["b_t", "te_tok"], w=["Bh"])
            P.I("pool", "tensor_tensor", Kh[:], tk["k"][:], te_tok[:], ALU.mult, r=["k_t", "te_tok"], w=["Kh"])
            tri_inverse_apply(P, C, Lm, X, 128, bkinv, "rw")
            P.I("pe", "transpose", p2[0:64, 0:128], X[:, 0:64], C["ident"][:], r=["rwX", "c_ident"], w=[k2])
            P.I("act", "activation", W1T[:], p2[0:64, 0:128], AF.Copy, r=[k2], w=["W1T"])
            for half in ((0, 1) if d == 0 else (1, 0)):
                rows = slice(half * 64, (half + 1) * 64)
                P.I("pe", "matmul", p5[:, 0:64], W1T[:], H[:], start=True, stop=True, r=["W1T", "H"], w=[k5])
                P.I("dve", "tensor_tensor", U[rows, :], X[rows, 64:128], p5[rows, 0:64], ALU.add, r=["rwX", k5], w=["U"])
                P.I("pe", "matmul", p6[:, 0:64], rtl[:], H[:], start=True, stop=False, r=["rtl", "H"], w=[k6])
                P.I("pe", "matmul", p6[:, 0:64], ArbT[rows, :], U[rows, :], start=False, stop=False, r=["ArbT", "U"], w=[k6])
                P.I("pe", "matmul", p6[:, 0:64], ArkT[rows, :], tk["v"][rows, :], start=False, stop=True, r=["ArkT", "v_t"], w=[k6])
                if d == 0:
                    P.I("act", "activation", oacc[rows, c, :], p6[rows, 0:64], AF.Copy, r=[k6], w=[f"oacc{c}"])
                else:
                    P.I("dve", "tensor_tensor", oacc[rows, c, :], oacc[rows, c, :], p6[rows, 0:64], ALU.add, r=[k6, f"oacc{c}"], w=[f"oacc{c}"])
                P.I("pe", "matmul", p7[0:64, 0:64], Bh[rows, :], U[rows, :], start=True, stop=False, r=["Bh", "U"], w=[k7])
                P.I("pe", "matmul", p7[0:64, 0:64], Kh[rows, :], tk["v"][rows, :], start=False, stop=True, r=["Kh", "v_t"], w=[k7])
                P.I("dve", "scalar_tensor_tensor", H[:], H[:], pc2[:, half:half + 1], p7[0:64, 0:64], ALU.mult, ALU.add, r=["H", "pc2", k7], w=["H"])
    allo = [f"oacc{c}" for c in range(NPK)]
    yT = raw; t1 = aT; t2 = bT
    for c in range(NPK):
        (pa, pk) = BK(c % 4)
        P.I("pe", "transpose", pa[0:64, 0:128], oacc[:, c, :], C["ident"][:], r=allo + ["c_ident"], w=[pk])
        P.I("act" if c % 2 else "dve", "activation" if c % 2 else "tensor_copy", yT[0:64, c * 128:(c + 1) * 128], pa[0:64, 0:128], *([AF.Copy] if c % 2 else []),
            r=[pk], w=["raw"])
    on64 = C["ones"][0:64, 0:64]
    for i, t0 in enumerate(range(0, TSEQ, NB)):
        ts = slice(t0, t0 + NB); b = i % 2
        (pm, km), (pvv, kvv), (pb, kb) = BK(b * 3), BK(b * 3 + 1), BK(b * 3 + 2)
        P.I("pe", "matmul", pm[0:64, 0:NB], on64, yT[0:64, ts], start=True, stop=True, r=["raw", "c_ones"], w=[km])
        P.I("dve", "scalar_tensor_tensor", yT[0:64, ts], pm[0:64, 0:NB], -1.0 / 64, yT[0:64, ts], ALU.mult, ALU.add, r=[km, "raw"], w=["raw"])
        P.I("act", "activation", t1[:, ts], yT[0:64, ts], AF.Square, r=["raw"], w=["aT"])
        P.I("pe", "matmul", pvv[0:64, 0:NB], on64, t1[:, ts], start=True, stop=True, r=["aT", "c_ones"], w=[kvv])
        P.I("dve", "tensor_scalar", rs[b][:], pvv[0:64, 0:NB], 1.0 / 64, 64e-5, ALU.mult, ALU.add, r=[kvv], w=[f"rs{b}"])
        P.I("dve", "reciprocal", rs[b][:], rs[b][:], r=[f"rs{b}"], w=[f"rs{b}"])
        P.I("act", "activation", rs[b][:], rs[b][:], AF.Sqrt, r=[f"rs{b}"], w=[f"rs{b}"])
        P.I("dve", "scalar_tensor_tensor", yT[0:64, ts], yT[0:64, ts], pv[:, 5:6], rs[b][:], ALU.mult, ALU.mult, r=["raw", "pv", f"rs{b}"], w=["raw"])
        P.I("dve", "scalar_tensor_tensor", t2[:, ts], rT[:, ts], pv[:, 7:8], kT[:, ts], ALU.mult, ALU.mult, r=["rT", "pv", "kT"], w=["bT"])
        P.I("pe", "matmul", pb[0:64, 0:NB], on64, t2[:, ts], start=True, stop=True, r=["bT", "c_ones"], w=[kb])
        P.I("dve", "tensor_tensor", t2[:, ts], pb[0:64, 0:NB], vT[:, ts], ALU.mult, r=[kb, "vT"], w=["bT"])
        P.I("dve", "scalar_tensor_tensor", yT[0:64, ts], yT[0:64, ts], pv[:, 6:7], t2[:, ts], ALU.add, ALU.add, r=["raw", "pv", "bT"], w=["raw"])
        P.I("dve", "tensor_tensor", yT[0:64, ts], yT[0:64, ts], gT[:, ts], ALU.mult, r=["raw", "gT"], w=["raw"])
    P.dma("sp", yd, yT[0:64, :], reads=["raw"], is_output=True)
    P.finish()
    return nc


def host_B3_inputs(pc_, L, b, head, prm):
    p = pc_[b]
    hc = slice(head * 64, (head + 1) * 64)
    mu = prm['rw_mu'][L]
    def seg(o, n):
        cols = np.arange(o, o + n); m = mu[cols]; cl = cols % 4
        tab = np.zeros((n, 8), np.float32)
        tab[:, 0] = m
        for j in range(4):
            tab[:, 1 + j] = np.where(cl == j, m, 0.0)
        tab[:, 5] = np.where(cl % 2 == 0, m, 0.0); tab[:, 6] = np.where(cl % 2 == 1, m, 0.0)
        return p[:, cols].T, tab
    s64 = [seg(head * 64, 64), seg(256 + head * 64, 64), seg(512 + head * 64, 64), seg(896, 64)]
    s128 = [seg(768, 128), seg(960, 128)]
    pv = np.zeros((64, 8), np.float32)
    pv[:, 0] = prm['rw_a0'][L][hc]; pv[:, 1] = prm['rw_k_k'][L][hc]; pv[:, 2] = prm['rw_k_a'][L][hc]
    pv[:, 3] = prm['rw_w0'][L][0][hc]; pv[:, 4] = prm['rw_w0'][L][1][hc]
    pv[:, 5] = prm['rw_ln_w'][L][hc]; pv[:, 6] = prm['rw_ln_b'][L][hc]; pv[:, 7] = prm['rw_r_k'][L][head]
    w2pad = np.zeros((2, 128, 64), np.float32)
    for j in range(2):
        w2pad[j, j * 64:(j + 1) * 64] = prm['rw_w2'][L][j][:, hc]
    A = lambda a: np.ascontiguousarray(a, dtype=np.float32)
    return {"p64": A(np.stack([s[0] for s in s64])), "p128": A(np.stack([s[0] for s in s128])),
            "mu64": A(np.stack([s[1] for s in s64])), "mu128": A(np.stack([s[1] for s in s128])),
            "pv": pv, "a2h": A(prm['rw_a2'][L][:, hc]), "g2h": A(prm['rw_g2'][L][:, hc]), "w2pad": w2pad,
            "consts": host_consts64()}


NTT = 17


def build_C1():
    nc = bass.Bass("TRN2", target_bir_lowering=False)
    D = lambda n, s: nc.dram_tensor(n, s, F32, kind="ExternalInput").ap()
    xTd = D("xT", [128, 8, NTOK]); yTd = D("yT", [128, 8, NTOK]); woutd = D("wout", [128, 8, 1024])
    modd = D("mod", [128, 8, 6]); nwd = D("nw", [128, 8]); wrd = D("wr", [128, 8, 32]); brd = D("br", [128, 32])
    O = lambda n, s: nc.dram_tensor(n, s, F32, kind="ExternalOutput").ap()
    xmd = O("xmT", [128, 8, NTOK]); h2d = O("h2T", [128, 8, NTOK]); Gd = O("G", [128, NTT, 32])
    P = Prog(nc)
    xT = P.sb("xT", [128, 8, NTOK]); ybf = P.sb("ybf", [128, 8, NTOK], BF16)
    hT32 = xT
    mod = P.sb("mod", [128, 8, 6]); nw = P.sb("nw", [128, 8]); wbf = P.sb("wbf", [128, 8, 1024], BF16)
    wr = P.sb("wr", [128, 8, 32]); br = P.sb("br", [128, 32])
    ones_bf = P.sb("ones_bf", [128, 128], BF16)
    P.I("pool", "memset", ones_bf[:], 1.0, w=["ones_bf"])
    for k in range(8):
        P.dma("sp", xT[:, k, :], xTd[:, k, :], writes=["xT"])
        P.dma("pool", ybf[:, k, :], yTd[:, k, :], writes=["ybf"])
        P.dma("pool", wbf[:, k, :], woutd[:, k, :], writes=["wbf"])
    for t, d, kk in ((mod, modd, "mod"), (nw, nwd, "nw"), (wr, wrd, "wr"), (br, brd, "br")):
        P.dma("sp", t[:], d, writes=[kk])
    pp = [P.ps(f"bank{i}", [128, 512]) for i in range(8)]
    i = 0
    for m in range(8):
        for it, (t0, n) in enumerate(TILES):
            b = i % 4; i += 1
            for k in range(8):
                P.I("pe", "matmul", pp[b][:, 0:n], wbf[:, k, m * 128:(m + 1) * 128], ybf[:, k, t0:t0 + n], start=(k == 0), stop=(k == 7),
                    r=["wbf", "ybf"], w=[f"bk{b}"])
            gcol = 3 if it == 0 else 0
            P.I("dve", "scalar_tensor_tensor", xT[:, m, t0:t0 + n], pp[b][:, 0:n], mod[:, m, gcol:gcol + 1], xT[:, m, t0:t0 + n], ALU.mult, ALU.add,
                r=[f"bk{b}", "mod", "xT"], w=["xT"])
    for k in range(8):
        P.dma("sp", xmd[:, k, :], xT[:, k, :], reads=["xT"], is_output=True)
    rms_modulate(P, xT, xT, mod, nw, ones_bf, shift_i=(1, 4), scale_i=(2, 5), tagp="n2", psb=(pp[4], pp[5]), hkey="xT")
    for k in range(8):
        P.dma("sp", h2d[:, k, :], hT32[:, k, :], reads=["xT"], is_output=True)
    G = P.sb("G", [128, NTT, 32]); lg = P.sb("lg", [128, NTT, 32]); m8 = P.sb("m8", [128, NTT, 8]); nmx = P.sb("nmx", [128, NTT])
    msk = P.sb("msk", [128, NTT, 32]); ssum = P.sb("ssum", [128, NTT])
    for tt in range(NTT):
        b = 6 + tt % 2
        for k in range(8):
            P.I("pe", "matmul", pp[b][:, 0:32], hT32[:, k, tt * 128:(tt + 1) * 128], wr[:, k, :], start=(k == 0), stop=(k == 7),
                r=["xT", "wr"], w=[f"bk{b}"])
        P.I("dve", "tensor_tensor", lg[:, tt, :], pp[b][:, 0:32], br[:], ALU.add, r=[f"bk{b}", "br"], w=["lg"])
        P.I("dve", "max", m8[:, tt, :], lg[:, tt, :], r=["lg"], w=["m8"])
        P.I("dve", "tensor_scalar", msk[:, tt, :], lg[:, tt, :], m8[:, tt, 3:4], None, ALU.is_ge, r=["lg", "m8"], w=["msk"])
        P.I("dve", "tensor_scalar", nmx[:, tt:tt + 1], m8[:, tt, 0:1], -1.0, None, ALU.mult, r=["m8"], w=["nmx"])
        P.I("act", "activation", G[:, tt, :], lg[:, tt, :], AF.Exp, bias=nmx[:, tt:tt + 1], r=["lg", "nmx"], w=["G"])
        P.I("dve", "tensor_tensor", G[:, tt, :], G[:, tt, :], msk[:, tt, :], ALU.mult, r=["G", "msk"], w=["G"])
        P.I("dve", "tensor_reduce", ssum[:, tt:tt + 1], G[:, tt, :], AX.X, ALU.add, r=["G"], w=["ssum"])
        P.I("dve", "reciprocal", ssum[:, tt:tt + 1], ssum[:, tt:tt + 1], r=["ssum"], w=["ssum"])
        P.I("dve", "tensor_scalar", G[:, tt, :], G[:, tt, :], ssum[:, tt:tt + 1], None, ALU.mult, r=["G", "ssum"], w=["G"])
    P.dma("sp", Gd, G[:], reads=["G"], is_output=True)
    P.finish()
    return nc


NT2 = 1088
T2 = [(0, 512), (512, 512), (1024, 64)]
ST2 = [(i * 128, 128) for i in range(8)] + [(1024, 64)]


def build_C2(NB=16, NE=4):
    nc = bass.Bass("TRN2", target_bir_lowering=False)
    D = lambda n, s: nc.dram_tensor(n, s, F32, kind="ExternalInput").ap()
    h2d = D("h2T", [128, 8, NB * NT2]); Gd = D("G", [128, NB, 9, NE]); wgud = D("wgu", [NE, 128, 8, 2048]); wdd = D("wd", [NE, 128, 8, 1024])
    bgud = D("bgu", [128, NE, 16]); bdd = D("bd", [NE, 1024]); idd = D("ident", [128, 128])
    fd = nc.dram_tensor("f", [NB, 128, 9, 1024], F32, kind="ExternalOutput").ap()
    P = Prog(nc)
    hbf = [P.sb(f"hbf{i}", [128, 8, NT2], BF16) for i in range(2)]
    G = P.sb("G", [128, NB, 9, NE]); bgu = P.sb("bgu", [128, NE, 16]); bd = P.sb("bd", [NE, 1024]); ident = P.sb("ident", [128, 128])
    P.dma("sp", G[:], Gd, writes=["G"]); P.dma("sp", bgu[:], bgud, writes=["bgu"]); P.dma("sp", bd[:], bdd, writes=["bd"]); P.dma("sp", ident[:], idd, writes=["ident"])
    wgu = [P.sb(f"wgu{i}", [128, 8, 2048], BF16) for i in range(2)]; wd = [P.sb(f"wd{i}", [128, 8, 1024], BF16) for i in range(2)]
    act = P.sb("act", [128, 8, NT2], BF16); acc = P.sb("acc", [128, 9, 1024])
    gc_ = [P.sb(f"gc{i}", [128, 512]) for i in range(2)]; sg = [P.sb(f"sg{i}", [128, 512]) for i in range(2)]
    uc = [P.sb(f"uc{i}", [128, 512]) for i in range(2)]
    GT = P.sb("GT", [NE, 128])
    pp = [P.ps(f"bank{i}", [128, 512]) for i in range(8)]

    wgus = [nc.dram_tensor(f"wgu_bf{e}", [128, 8, 2048], BF16).ap() for e in range(NE)]
    wds = [nc.dram_tensor(f"wd_bf{e}", [128, 8, 1024], BF16).ap() for e in range(NE)]
    for e in range(NE):
        for k in range(8):
            P.dma("pool", wgus[e][:, k, :], wgud[e, :, k, :], writes=[f"wgus{e}"])
            P.dma("pool", wds[e][:, k, :], wdd[e, :, k, :], writes=[f"wds{e}"])

    def load_w(j):
        e = j % NE; b = j % 2
        for k in range(0, 8, 2):
            P.dma("sp", wgu[b][:, k:k + 2, :], wgus[e][:, k:k + 2, :], reads=[f"wgus{e}"], writes=[f"wgu{b}"])
        for k in range(0, 8, 4):
            P.dma("act", wd[b][:, k:k + 4, :], wds[e][:, k:k + 4, :], reads=[f"wds{e}"], writes=[f"wd{b}"])

    def load_h(blk):
        for k in range(8):
            P.dma("pool", hbf[blk % 2][:, k, :], h2d[:, k, blk * NT2:(blk + 1) * NT2], writes=[f"hbf{blk%2}"])
    load_h(0); load_w(0)
    it = 0; jt = 0; j = 0
    for blk in range(NB):
        hb = hbf[blk % 2]; hk = f"hbf{blk%2}"
        if blk + 1 < NB:
            load_h(blk + 1)
        for e in range(NE):
            b = j % 2
            if j + 1 < NB * NE:
                load_w(j + 1)
            j += 1
            for fc in range(8):
                for (t0, n) in T2:
                    s = it % 2; it += 1
                    pg, pu = pp[2 * s], pp[2 * s + 1]; kg, ku = f"bk{2*s}", f"bk{2*s+1}"
                    for k in range(8):
                        P.I("pe", "matmul", pg[:, 0:n], wgu[b][:, k, fc * 128:(fc + 1) * 128], hb[:, k, t0:t0 + n], start=(k == 0), stop=(k == 7),
                            r=[f"wgu{b}", hk], w=[kg])
                    for k in range(8):
                        P.I("pe", "matmul", pu[:, 0:n], wgu[b][:, k, 1024 + fc * 128:1024 + (fc + 1) * 128], hb[:, k, t0:t0 + n], start=(k == 0), stop=(k == 7),
                            r=[f"wgu{b}", hk], w=[ku])
                    P.I("dve", "tensor_scalar", gc_[s][:, 0:n], pg[:, 0:n], bgu[:, e, fc:fc + 1], 7.0, ALU.add, ALU.min, r=[kg, "bgu"], w=[f"gc{s}"])
                    P.I("act", "activation", sg[s][:, 0:n], gc_[s][:, 0:n], AF.Sigmoid, scale=1.702, r=[f"gc{s}"], w=[f"sg{s}"])
                    P.I("dve", "tensor_scalar", uc[s][:, 0:n], pu[:, 0:n], bgu[:, e, 8 + fc:9 + fc], 7.0, ALU.add, ALU.min, r=[ku, "bgu"], w=[f"uc{s}"])
                    P.I("dve", "tensor_scalar", uc[s][:, 0:n], uc[s][:, 0:n], -7.0, 1.0, ALU.max, ALU.add, r=[f"uc{s}"], w=[f"uc{s}"])
                    P.I("dve", "tensor_tensor", gc_[s][:, 0:n], gc_[s][:, 0:n], sg[s][:, 0:n], ALU.mult, r=[f"gc{s}", f"sg{s}"], w=[f"gc{s}"])
                    P.I("dve", "tensor_tensor", act[:, fc, t0:t0 + n], gc_[s][:, 0:n], uc[s][:, 0:n], ALU.mult, r=[f"gc{s}", f"uc{s}"], w=["act"])
            for si, (s0, sn) in enumerate(ST2):
                for half in range(2):
                    pb = 4 + jt % 4; jt += 1
                    hs = slice(half * 512, (half + 1) * 512)
                    for fc in range(8):
                        P.I("pe", "matmul", pp[pb][0:sn, 0:512], act[:, fc, s0:s0 + sn], wd[b][:, fc, hs], start=(fc == 0), stop=(fc == 7),
                            r=["act", f"wd{b}"], w=[f"bk{pb}"])
                    if e == 0:
                        P.I("dve", "tensor_scalar", acc[0:sn, si, hs], pp[pb][0:sn, 0:512], G[0:sn, blk, si, e:e + 1], None, ALU.mult,
                            r=[f"bk{pb}", "G"], w=["acc"])
                    else:
                        P.I("dve", "scalar_tensor_tensor", acc[0:sn, si, hs], pp[pb][0:sn, 0:512], G[0:sn, blk, si, e:e + 1], acc[0:sn, si, hs],
                            ALU.mult, ALU.add, r=[f"bk{pb}", "G", "acc"], w=["acc"])
        for si, (s0, sn) in enumerate(ST2):
            P.I("pe", "transpose", pp[0][0:NE, 0:sn], G[0:sn, blk, si, :], ident[0:sn, 0:sn], r=["G", "ident"], w=["bk0"])
            P.I("dve", "tensor_copy", GT[:, 0:sn], pp[0][0:NE, 0:sn], r=["bk0"], w=["GT"])
            for half in range(2):
                pb = 1 + half; hs = slice(half * 512, (half + 1) * 512)
                P.I("pe", "matmul", pp[pb][0:sn, 0:512], GT[:, 0:sn], bd[:, hs], start=True, stop=True, r=["GT", "bd"], w=[f"bk{pb}"])
                P.I("dve", "tensor_tensor", acc[0:sn, si, hs], acc[0:sn, si, hs], pp[pb][0:sn, 0:512], ALU.add, r=[f"bk{pb}", "acc"], w=["acc"])
        P.I("pool", "memset", acc[64:128, 8, :], 0.0, r=["acc"], w=["acc"]) if blk == 0 else None
        P.dma("sp", fd[blk], acc[:], reads=["acc"], is_output=True)
    P.finish()
    return nc


def build_D():
    nc = bass.Bass("TRN2", target_bir_lowering=False)
    D = lambda n, s: nc.dram_tensor(n, s, F32, kind="ExternalInput").ap()
    xmd = D("xmT", [128, 8, NTOK]); fTd = D("fT", [8, 128, 8, NTOK]); modd = D("mod", [128, 8, 2]); nwd = D("nw", [128, 8])
    od = nc.dram_tensor("oT", [128, 8, NTOK], F32, kind="ExternalOutput").ap()
    P = Prog(nc)
    xT = P.sb("xT", [128, 8, NTOK]); fT = P.sb("fT", [128, 8, NTOK]); mod = P.sb("mod", [128, 8, 2]); nw = P.sb("nw", [128, 8])
    ones_bf = P.sb("ones_bf", [128, 128], BF16)
    P.I("pool", "memset", ones_bf[:], 1.0, w=["ones_bf"])
    for k in range(8):
        P.dma("sp", xT[:, k, :], xmd[:, k, :], writes=["xT"])
    P.dma("sp", mod[:], modd, writes=["mod"]); P.dma("sp", nw[:], nwd, writes=["nw"])
    for c in range(8):
        for k in range(8):
            P.dma("sp", fT[:, k, :], fTd[c, :, k, :], writes=["fT"])
        add_gated(P, xT, fT, mod, 0, 1)
    sq = [P.sb(f"sq{i}", [128, 8, 512], BF16) for i in range(2)]; rs = [P.sb(f"rs{i}", [128, 512]) for i in range(2)]
    ss = [P.ps(f"bank{i}", [128, 512]) for i in range(2)]
    for it, (t0, n) in enumerate(TILES):
        b = it % 2
        for k in range(8):
            P.I("act", "activation", sq[b][:, k, 0:n], xT[:, k, t0:t0 + n], AF.Square, r=["xT"], w=[f"sq{b}"])
        for k in range(8):
            P.I("pe", "matmul", ss[b][:, 0:n], ones_bf[:], sq[b][:, k, 0:n], start=(k == 0), stop=(k == 7), r=[f"sq{b}", "ones_bf"], w=[f"bk{b}"])
        P.I("dve", "tensor_scalar", rs[b][:, 0:n], ss[b][:, 0:n], 1.0 / 1024, 1e-6, ALU.mult, ALU.add, r=[f"bk{b}"], w=[f"rs{b}"])
        P.I("dve", "reciprocal", rs[b][:, 0:n], rs[b][:, 0:n], r=[f"rs{b}"], w=[f"rs{b}"])
        P.I("act", "activation", rs[b][:, 0:n], rs[b][:, 0:n], AF.Sqrt, r=[f"rs{b}"], w=[f"rs{b}"])
        for k in range(8):
            P.I("dve", "scalar_tensor_tensor", fT[:, k, t0:t0 + n], xT[:, k, t0:t0 + n], nw[:, k:k + 1], rs[b][:, 0:n], ALU.mult, ALU.mult,
                r=["xT", "nw", f"rs{b}"], w=["fT"])
    for k in range(8):
        P.dma("sp", od[:, k, :], fT[:, k, :], reads=["fT"], is_output=True)
    P.finish()
    return nc


def add_gated(P, xT, fT, mod, col_l, col_c):
    for k in range(8):
        P.I("dve", "scalar_tensor_tensor", xT[:, k, 0:128], fT[:, k, 0:128], mod[:, k, col_c:col_c + 1], xT[:, k, 0:128], ALU.mult, ALU.add,
            r=["fT", "mod", "xT"], w=["xT"])
        P.I("dve", "scalar_tensor_tensor", xT[:, k, 128:NTOK], fT[:, k, 128:NTOK], mod[:, k, col_l:col_l + 1], xT[:, k, 128:NTOK], ALU.mult, ALU.add,
            r=["fT", "mod", "xT"], w=["xT"])


I32 = mybir.dt.int32


def build_C2s(NTILE=136, NE=4, CAP=4608, NPASS=4):
    NTOKA = NTILE * 128; NJ = CAP // 128; NJH = NJ // NPASS; HALF = CAP // NPASS
    T3 = [(t0, min(512, HALF - t0)) for t0 in range(0, HALF, 512)]
    nc = bass.Bass("TRN2", target_bir_lowering=False)
    D = lambda n, s: nc.dram_tensor(n, s, F32, kind="ExternalInput").ap()
    h2d = D("h2tok", [NTOKA + 128, 1024]); Gd = D("Gm", [128, NTILE, NE]); tokd = D("tokid", [128, NTILE]); Ld = D("lst", [128, 128])
    padd = D("padtab", [128, NJ, 2]); idd = D("ident", [128, 128]); dumpd = D("dump", [128, 1])
    wgud = D("wgu", [NE, 128, 8, 2048]); wdd = D("wd", [NE, 128, 8, 1024]); bgud = D("bgu", [128, NE, 16]); bdbd = D("bdb", [NE, 128, 1024])
    fd = nc.dram_tensor("fpart", [NTOKA + 128, 1024], F32, kind="ExternalOutput").ap()
    tab = [nc.dram_tensor(f"slot_tab{e}", [CAP + 128, 2], F32).ap() for e in range(NE)]
    P = Prog(nc)
    pp = [P.ps(f"bank{i}", [128, 512]) for i in range(8)]
    z = P.sb("z", [128, 1024]); ones = P.sb("ones", [128, 128]); lst = P.sb("lst", [128, 128]); ident = P.sb("ident", [128, 128])
    P.I("pool", "memset", z[:], 0.0, w=["z"]); P.I("pool", "memset", ones[:], 1.0, w=["ones"])
    P.dma("sp", lst[:], Ld, writes=["lst"]); P.dma("sp", ident[:], idd, writes=["ident"])
    for r in range(NTILE + 1):
        P.dma("sp", fd[r * 128:(r + 1) * 128, :], z[:], reads=["z"], writes=["fpart"])
    Gm = P.sb("Gm", [128, NTILE, NE]); tokid = P.sb("tokid", [128, NTILE]); padt = P.sb("padt", [128, NJ, 2]); bgu = P.sb("bgu", [128, NE, 16])
    P.dma("act", Gm[:], Gd, writes=["Gm"]); P.dma("act", tokid[:], tokd, writes=["tokid"]); P.dma("act", padt[:], padd, writes=["padt"])
    P.dma("act", bgu[:], bgud, writes=["bgu"])
    dump = P.sb("dump", [128, 1]); P.dma("act", dump[:], dumpd, writes=["dump"])
    wgu = [P.sb(f"wgu{i}", [128, 8, 2048], BF16) for i in range(2)]; wd = [P.sb(f"wd{i}", [128, 8, 1024], BF16) for i in range(2)]
    bdb = [P.sb(f"bdb{i}", [128, 1024]) for i in range(2)]
    hsel = P.sb("hsel", [128, 8, HALF], BF16); act = P.sb("act", [128, 8, HALF], BF16)
    hg = [P.sb(f"hg{i}", [128, 1024]) for i in range(3)]; yst = [P.sb(f"yst{i}", [128, 1024]) for i in range(4)]
    gc_ = [P.sb(f"gc{i}", [128, 512]) for i in range(2)]; sg = [P.sb(f"sg{i}", [128, 512]) for i in range(2)]
    uc = [P.sb(f"uc{i}", [128, 512]) for i in range(2)]

    def load_w(e):
        b = e % 2
        for k in range(8):
            P.dma("pool", wgu[b][:, k, :], wgud[e, :, k, :], writes=[f"wgu{b}"])
        for k in range(8):
            P.dma("pool", wd[b][:, k, :], wdd[e, :, k, :], writes=[f"wd{b}"])
        P.dma("act", bdb[b][:], bdbd[e], writes=[f"bdb{b}"])
    load_w(0)
    m = P.sb("m", [128, NE, NTILE]); cs = P.sb("cs", [128, NE, NTILE]); inc = P.sb("inc", [128, NE, NTILE]); rk = P.sb("rk", [128, NE, NTILE])
    idx = P.sb("idx", [128, NE, NTILE], I32); pr = P.sb("pr", [128, NTILE, NE, 2]); onesw = P.sb("onesw", [128, NTILE])
    P.I("pool", "memset", onesw[:], 1.0, w=["onesw"])
    for e in range(NE):
        P.I("dve", "tensor_scalar", m[:, e, :], Gm[:, :, e], 0.0, None, ALU.is_gt, r=["Gm"], w=["m"])
        P.I("pe", "matmul", pp[0][:, 0:NTILE], lst[:], m[:, e, :], start=True, stop=True, r=["lst", "m"], w=["bk0"])
        P.I("pe", "matmul", pp[1][:, 0:NTILE], ones[:], m[:, e, :], start=True, stop=True, r=["ones", "m"], w=["bk1"])
        P.I("act", "activation", cs[:, e, :], pp[1][:, 0:NTILE], AF.Copy, r=["bk1"], w=["cs"])
        P.I("dve", "tensor_tensor_scan", inc[:, e, :], onesw[:], cs[:, e, :], 0.0, ALU.mult, ALU.add, r=["onesw", "cs"], w=["inc"])
        P.I("dve", "tensor_tensor", rk[:, e, :], pp[0][:, 0:NTILE], inc[:, e, :], ALU.add, r=["bk0", "inc"], w=["rk"])
        P.I("dve", "tensor_tensor", rk[:, e, :], rk[:, e, :], cs[:, e, :], ALU.subtract, r=["rk", "cs"], w=["rk"])
        P.I("dve", "tensor_scalar", cs[:, e, :], rk[:, e, :], float(CAP), None, ALU.is_lt, r=["rk", "cs"], w=["cs"])
        P.I("dve", "tensor_tensor", cs[:, e, :], cs[:, e, :], m[:, e, :], ALU.mult, r=["cs", "m"], w=["cs"])
        P.I("dve", "tensor_scalar", rk[:, e, :], rk[:, e, :], dump[:, 0:1], None, ALU.subtract, r=["rk", "dump"], w=["rk"])
        P.I("dve", "tensor_tensor", rk[:, e, :], rk[:, e, :], cs[:, e, :], ALU.mult, r=["rk", "cs"], w=["rk"])
        P.I("dve", "tensor_scalar", rk[:, e, :], rk[:, e, :], dump[:, 0:1], None, ALU.add, r=["rk", "dump"], w=["rk"])
        P.I("dve", "tensor_copy", idx[:, e, :], rk[:, e, :], r=["rk"], w=["idx"])
        P.I("pool", "tensor_copy", pr[:, :, e, 0], tokid[:], r=["tokid"], w=["pr"])
        P.I("pool", "tensor_copy", pr[:, :, e, 1], Gm[:, :, e], r=["Gm"], w=["pr"])
    for e in range(NE):
        P.dma("act", tab[e][0:CAP, :].rearrange("(p j) c -> p j c", j=NJ), padt[:], reads=["padt"], writes=[f"tabinit{e}"])
    for t in range(NTILE):
        for e in range(NE):
            P.idma(tab[e], pr[:, t, e, :], out_idx=idx[:, e, t:t + 1], reads=["pr", "idx", f"tabinit{e}"], writes=[f"sc{e}_{t}"])
    tabsb = P.sb("tabsb", [128, NE, NJ, 2]); tok_i = P.sb("tok_i", [128, NE, NJ], I32); gate = P.sb("gate", [128, NE, NJ])
    for e in range(NE):
        P.dma("act", tabsb[:, e, :, :], tab[e][0:CAP, :].rearrange("(p j) c -> p j c", j=NJ), reads=[f"sc{e}_{t}" for t in range(NTILE)], writes=["tabsb"])
    P.I("dve", "tensor_copy", tok_i[:], tabsb[:, :, :, 0], r=["tabsb"], w=["tok_i"])
    P.I("dve", "tensor_copy", gate[:], tabsb[:, :, :, 1], r=["tabsb"], w=["gate"])
    it = 0; jt = 0; gi = 0; ti = 0
    for e in range(NE):
        b = e % 2
        if e + 1 < NE:
            load_w(e + 1)
        for half in range(NPASS):
            for jj in range(NJH):
                j = half * NJH + jj; g = gi % 3; gi += 1
                P.idma(hg[g][:], h2d, in_idx=tok_i[:, e, j:j + 1], reads=["tok_i"], writes=[f"hg{g}"])
                for k in range(8):
                    pb = 4 + ti % 4; ti += 1
                    P.I("pe", "transpose", pp[pb][:, 0:128], hg[g][:, k * 128:(k + 1) * 128], ident[:], r=[f"hg{g}", "ident"], w=[f"bk{pb}"])
                    if ti % 2:
                        P.I("act", "activation", hsel[:, k, jj * 128:(jj + 1) * 128], pp[pb][:, 0:128], AF.Copy, r=[f"bk{pb}"], w=["hsel"])
                    else:
                        P.I("dve", "tensor_copy", hsel[:, k, jj * 128:(jj + 1) * 128], pp[pb][:, 0:128], r=[f"bk{pb}"], w=["hsel"])
            for fc in range(8):
                for (t0, n) in T3:
                    s = it % 2; it += 1
                    pg, pu = pp[2 * s], pp[2 * s + 1]; kg, ku = f"bk{2*s}", f"bk{2*s+1}"
                    for k in range(8):
                        P.I("pe", "matmul", pg[:, 0:n], wgu[b][:, k, fc * 128:(fc + 1) * 128], hsel[:, k, t0:t0 + n], start=(k == 0), stop=(k == 7),
                            r=[f"wgu{b}", "hsel"], w=[kg])
                    for k in range(8):
                        P.I("pe", "matmul", pu[:, 0:n], wgu[b][:, k, 1024 + fc * 128:1024 + (fc + 1) * 128], hsel[:, k, t0:t0 + n], start=(k == 0), stop=(k == 7),
                            r=[f"wgu{b}", "hsel"], w=[ku])
                    P.I("dve", "tensor_scalar", gc_[s][:, 0:n], pg[:, 0:n], bgu[:, e, fc:fc + 1], 7.0, ALU.add, ALU.min, r=[kg, "bgu"], w=[f"gc{s}"])
                    P.I("act", "activation", sg[s][:, 0:n], gc_[s][:, 0:n], AF.Sigmoid, scale=1.702, r=[f"gc{s}"], w=[f"sg{s}"])
                    P.I("dve", "tensor_scalar", uc[s][:, 0:n], pu[:, 0:n], bgu[:, e, 8 + fc:9 + fc], 7.0, ALU.add, ALU.min, r=[ku, "bgu"], w=[f"uc{s}"])
                    P.I("dve", "tensor_scalar", uc[s][:, 0:n], uc[s][:, 0:n], -7.0, 1.0, ALU.max, ALU.add, r=[f"uc{s}"], w=[f"uc{s}"])
                    P.I("dve", "tensor_tensor", gc_[s][:, 0:n], gc_[s][:, 0:n], sg[s][:, 0:n], ALU.mult, r=[f"gc{s}", f"sg{s}"], w=[f"gc{s}"])
                    P.I("dve", "tensor_tensor", act[:, fc, t0:t0 + n], gc_[s][:, 0:n], uc[s][:, 0:n], ALU.mult, r=[f"gc{s}", f"uc{s}"], w=["act"])
            for jj in range(NJH):
                j = half * NJH + jj; y = jt % 2
                for hh in range(2):
                    pb = 4 + jt % 4; jt += 1
                    hs = slice(hh * 512, (hh + 1) * 512)
                    for fc in range(8):
                        P.I("pe", "matmul", pp[pb][:, 0:512], act[:, fc, jj * 128:(jj + 1) * 128], wd[b][:, fc, hs], start=(fc == 0), stop=(fc == 7),
                            r=["act", f"wd{b}"], w=[f"bk{pb}"])
                    P.I("dve", "tensor_tensor", yst[jj % 4][:, hs], pp[pb][:, 0:512], bdb[b][:, hs], ALU.add, r=[f"bk{pb}", f"bdb{b}"], w=[f"yst{jj%4}"])
                P.I("pool", "tensor_scalar", yst[jj % 4][:], yst[jj % 4][:], gate[:, e, j:j + 1], None, ALU.mult, r=[f"yst{jj%4}", "gate"], w=[f"yst{jj%4}"])
                prev = [f"fp{e-1}_{q}" for q in range(NJ)] if e > 0 else []
                P.idma(fd, yst[jj % 4][:], out_idx=tok_i[:, e, j:j + 1], reads=[f"yst{jj%4}", "tok_i", "fpart"] + prev, writes=[f"fp{e}_{j}"], is_output=True, compute_op=ALU.add)
    P.finish()
    return nc


def host_C2s_consts(NTILE=136, CAP=4608):
    NJ = CAP // 128
    tokid = (np.arange(NTILE)[None, :] * 128 + np.arange(128)[:, None]).astype(np.float32)
    p_ = np.arange(128)[:, None]; q_ = np.arange(128)[None, :]
    lst = (p_ < q_).astype(np.float32)
    padtab = np.zeros((128, NJ, 2), np.float32); padtab[:, :, 0] = NTILE * 128 + np.arange(128)[:, None]
    return {"tokid": tokid, "lst": lst, "padtab": padtab, "ident": np.eye(128, dtype=np.float32), "dump": (CAP + np.arange(128, dtype=np.float32))[:, None]}


def _fm(tok):
    return np.ascontiguousarray(tok.T.reshape(8, 128, -1).transpose(1, 0, 2))


def _tok(fm):
    return fm.transpose(2, 1, 0).reshape(fm.shape[2], -1)


def _vec(v):
    return np.ascontiguousarray(np.asarray(v, np.float32).reshape(8, 128).T)


_PROGS = {}
C2S_CAP = 4608
MOE_SPARSE = True


def _prog(name, builder, *a):
    key = (name,) + a
    if key not in _PROGS:
        _PROGS[key] = builder(*a)
    return _PROGS[key]


def _run(nc, maps):
    res = run_bass_kernel_spmd(nc, maps, core_ids=list(range(len(maps))))
    return res.results


def kernel(**inp):
    prm = {k: np.asarray(v, dtype=np.float32) for k, v in inp.items()}
    x, c, ctx, c_ctx = prm['x'], prm['c'], prm['ctx'], prm['c_ctx']
    NC = 8
    A_ = lambda a: np.ascontiguousarray(a, dtype=np.float32)
    cs = np.zeros((128, 8, 5), np.float32)
    for v in range(4):
        cs[:, :, v] = _vec(c[v])
    cs[:, :, 4] = _vec(c_ctx)
    items = [(l, fc) for l in range(2) for fc in range(48)]
    maps = []
    for i in range(NC):
        its = items[i * 12:(i + 1) * 12]
        wm = np.stack([prm['w_mod'][l][:, fc * 128:(fc + 1) * 128].reshape(8, 128, 128).transpose(1, 0, 2) for l, fc in its])
        bm = np.stack([prm['b_mod'][l][fc * 128:(fc + 1) * 128] for l, fc in its], 1)
        maps.append({"cs": cs, "wm": A_(wm), "bm": A_(bm)})
    res = _run(_prog("M", build_M), maps)
    modv = {}
    for i in range(NC):
        for j, (l, fc) in enumerate(items[i * 12:(i + 1) * 12]):
            modv[(l, fc)] = res[i]["modT"][:, j, :]
    mod6 = [[np.stack([modv[(l, i6 * 8 + k)] for k in range(8)], 1) for i6 in range(6)] for l in range(2)]

    def core_tokens(arr_c, arr_l, b, half):
        return np.concatenate([arr_c[b, half * 128:(half + 1) * 128], arr_l[b, half * 2048:(half + 1) * 2048]], 0)

    xm = [_fm(core_tokens(ctx, x, j // 2, j % 2)) for j in range(NC)]
    fparts = None
    consts64 = host_consts64()
    for l in range(2):
        win = np.zeros((1024, NCT * 128), np.float32); win[:, :4184] = prm['w_in'][l]
        win = A_(win.reshape(8, 128, NCT * 128).transpose(1, 0, 2)); nw1 = _vec(prm['norm1_w'][l])
        maps = []
        for j in range(NC):
            b = j // 2
            g5l = mod6[l - 1][5][:, :, b] if l > 0 else np.zeros((128, 8), np.float32)
            g5c = mod6[l - 1][5][:, :, 4] if l > 0 else np.zeros((128, 8), np.float32)
            mod = np.stack([mod6[l][0][:, :, b], mod6[l][1][:, :, b], mod6[l][0][:, :, 4], mod6[l][1][:, :, 4], g5l, g5c], -1)
            m = {"xT": xm[j], "mod": A_(mod), "nw": nw1, "win": win}
            if l > 0:
                m["fT"] = A_(np.stack([_fm(fparts[cc][j * NTOK:(j + 1) * NTOK]) for cc in range(NC)]))
            maps.append(m)
        res = _run(_prog("A", build_A, l == 0), maps)
        xcur = [res[j]["xout"] for j in range(NC)]
        ptok = [res[j]["pT"].reshape(NCT * 128, NTOK).T[:, :4184] for j in range(NC)]
        del res
        pfull = np.stack([np.concatenate([ptok[2 * b][:128], ptok[2 * b + 1][:128], ptok[2 * b][128:], ptok[2 * b + 1][128:]], 0) for b in range(4)])
        del ptok
        yall = np.zeros((4, TSEQ, 1024), np.float32)
        pa = pfull[:, :, 0:1032]
        res = _run(_prog("B1", build_B1), [host_B1_inputs(pa, l, j // 2, j % 2, prm) for j in range(NC)])
        for j in range(NC):
            yall[j // 2, :, (j % 2) * 128:(j % 2 + 1) * 128] = res[j]["yT"].T
        pb = pfull[:, :, 1032:3096]
        for rnd in range(2):
            its = [(i // 4, i % 4) for i in range(rnd * 8, rnd * 8 + 8)]
            maps = [host_B2_inputs(pb, l, b, h, prm) for b, h in its]
            for m in maps:
                m["consts"] = consts64
            res = _run(_prog("B2", build_B2), maps)
            for (b, h), r in zip(its, res):
                yall[b, :, 256 + h * 128:256 + (h + 1) * 128] = host_B2_output(r["y"])
        pc = pfull[:, :, 3096:4184]
        for rnd in range(2):
            its = [(i // 4, i % 4) for i in range(rnd * 8, rnd * 8 + 8)]
            maps = [host_B3_inputs(pc, l, b, h, prm) for b, h in its]
            for m in maps:
                m["consts"] = consts64
            res = _run(_prog("B3", build_B3), maps)
            for (b, h), r in zip(its, res):
                yall[b, :, 768 + h * 64:768 + (h + 1) * 64] = r["y"].T
        del pfull
        wout = A_(prm['w_out'][l].reshape(8, 128, 1024).transpose(1, 0, 2)); nw2 = _vec(prm['norm2_w'][l])
        wr = A_(prm['w_router'][l].reshape(8, 128, 32).transpose(1, 0, 2)); br = A_(np.broadcast_to(prm['b_router'][l][None], (128, 32)))
        maps = []
        for j in range(NC):
            b, half = j // 2, j % 2
            ytok = np.concatenate([yall[b, half * 128:(half + 1) * 128], yall[b, 256 + half * 2048:256 + (half + 1) * 2048]], 0)
            mod = np.stack([mod6[l][2][:, :, b], mod6[l][3][:, :, b], mod6[l][4][:, :, b], mod6[l][2][:, :, 4], mod6[l][3][:, :, 4], mod6[l][4][:, :, 4]], -1)
            maps.append({"xT": xcur[j], "yT": _fm(ytok), "wout": wout, "mod": A_(mod), "nw": nw2, "wr": wr, "br": br})
        res = _run(_prog("C1", build_C1), maps)
        xm = [res[j]["xmT"] for j in range(NC)]
        Gall = np.concatenate([res[j]["G"].transpose(1, 0, 2).reshape(NTOK, 32) for j in range(NC)], 0)
        loads = np.count_nonzero(Gall, axis=0)
        use_sparse = MOE_SPARSE and int(loads.max()) <= C2S_CAP
        print(f"[moe] layer {l}: max expert load {int(loads.max())} (mean {float(loads.mean()):.0f}) -> {'dispatch' if use_sparse else 'dense'}", flush=True)
        if use_sparse:
            h2tok = np.concatenate([_tok(res[j]["h2T"]) for j in range(NC)] + [np.zeros((128, 1024), np.float32)], 0)
        else:
            h2all = np.ascontiguousarray(np.concatenate([res[j]["h2T"] for j in range(NC)], 2))
        del res, yall
        if use_sparse:
            maps = []
            c2c = host_C2s_consts()
            for cc in range(NC):
                es = slice(4 * cc, 4 * cc + 4)
                m_ = {"h2tok": h2tok, "Gm": A_(Gall[:, es].reshape(136, 128, 4).transpose(1, 0, 2)),
                      "wgu": A_(prm['w_gate_up'][l][es].reshape(4, 8, 128, 2048).transpose(0, 2, 1, 3)),
                      "wd": A_(prm['w_down'][l][es].reshape(4, 8, 128, 1024).transpose(0, 2, 1, 3)),
                      "bgu": A_(prm['b_gate_up'][l][es].reshape(4, 16, 128).transpose(2, 0, 1)),
                      "bdb": A_(np.broadcast_to(prm['b_down'][l][es][:, None, :], (4, 128, 1024)))}
                m_.update(c2c)
                maps.append(m_)
            res = _run(_prog("C2s", build_C2s), maps)
            del maps, h2tok
            fparts = [res[cc]["fpart"][:16 * NT2] for cc in range(NC)]
            del res
        else:
            maps = []
            ident = np.eye(128, dtype=np.float32)
            for cc in range(NC):
                es = slice(4 * cc, 4 * cc + 4)
                Gp = np.zeros((16, 1152, 4), np.float32); Gp[:, :NT2] = Gall[:, es].reshape(16, NT2, 4)
                maps.append({"h2T": h2all, "G": A_(Gp.reshape(16, 9, 128, 4).transpose(2, 0, 1, 3)),
                             "wgu": A_(prm['w_gate_up'][l][es].reshape(4, 8, 128, 2048).transpose(0, 2, 1, 3)),
                             "wd": A_(prm['w_down'][l][es].reshape(4, 8, 128, 1024).transpose(0, 2, 1, 3)),
                             "bgu": A_(prm['b_gate_up'][l][es].reshape(4, 16, 128).transpose(2, 0, 1)), "bd": A_(prm['b_down'][l][es]), "ident": ident})
            res = _run(_prog("C2", build_C2), maps)
            del maps, h2all
            fparts = [res[cc]["f"].transpose(0, 2, 1, 3).reshape(16, 1152, 1024)[:, :NT2].reshape(16 * NT2, 1024) for cc in range(NC)]
            del res
    nwf = _vec(prm['norm_f_w'])
    maps = []
    for j in range(NC):
        b = j // 2
        mod = np.stack([mod6[1][5][:, :, b], mod6[1][5][:, :, 4]], -1)
        maps.append({"xmT": xm[j], "fT": A_(np.stack([_fm(fparts[cc][j * NTOK:(j + 1) * NTOK]) for cc in range(NC)])), "mod": A_(mod), "nw": nwf})
    res = _run(_prog("D", build_D), maps)
    out = np.zeros((4, 4096, 1024), np.float32)
    for j in range(NC):
        b, half = j // 2, j % 2
        out[b, half * 2048:(half + 1) * 2048] = _tok(res[j]["oT"])[128:]
    return out
```

```python
import numpy as np
from contextlib import ExitStack
import concourse.bass as bass
import concourse.mybir as mybir
from concourse.bass_utils import run_bass_kernel_spmd

F32 = mybir.dt.float32
BF16 = mybir.dt.bfloat16
AF = mybir.ActivationFunctionType
ALU = mybir.AluOpType
AX = mybir.AxisListType

ENGS = ("pe", "dve", "act", "pool", "sp")
N_DMA_SEMS = 12


class Prog:
    def __init__(self, nc, same_engine_sync=None):
        import os
        if same_engine_sync is None:
            same_engine_sync = os.environ.get('SAMESYNC', '1') == '1'
        self.nc = nc
        self.es = ExitStack()
        self.ops = {e: [] for e in ENGS}
        self.cnt = {e: 0 for e in ENGS}
        self.sem = {}
        for e in ENGS:
            self.sem[e] = self.es.enter_context(nc.semaphore("s_" + e))
        self.dsem = {q: [self.es.enter_context(nc.semaphore(f"d_{q}{i}")) for i in range(N_DMA_SEMS)]
                     for q in ("sp", "pool", "act")}
        self.dsem_uses = {q: [0] * N_DMA_SEMS for q in ("sp", "pool", "act")}
        self.dsem_next = {q: 0 for q in ("sp", "pool", "act")}
        self.waited = {e: {} for e in ENGS}
        self.lastw = {}
        self.readers = {}
        self.same = same_engine_sync
        self.semobj = {}
        self.out_tokens = []
        self.nops = 0

    def sb(self, name, shape, dt=F32):
        return self.es.enter_context(self.nc.sbuf_tensor("sb_" + name, list(shape), dt))

    def ps(self, name, shape, dt=F32):
        return self.es.enter_context(self.nc.psum_tensor("ps_" + name, list(shape), dt))

    def _need(self, eng, tok, waits):
        if tok is None:
            return
        semkey, val, src = tok
        if src == eng and (not self.same or eng == "pe"):
            return
        if self.waited[eng].get(semkey, 0) >= val:
            return
        waits[semkey] = max(waits.get(semkey, 0), val)

    def _deps(self, eng, reads, writes):
        waits = {}
        for k in reads:
            self._need(eng, self.lastw.get(k), waits)
        for k in writes:
            self._need(eng, self.lastw.get(k), waits)
            for t in self.readers.get(k, ()):
                self._need(eng, t, waits)
        for semkey, val in waits.items():
            self.waited[eng][semkey] = val
            self.ops[eng].append(("wait", semkey, val))

    def _commit(self, tok, reads, writes):
        for k in reads:
            self.readers.setdefault(k, []).append(tok)
        for k in writes:
            self.lastw[k] = tok
            self.readers[k] = []

    def op(self, eng, fn, reads=(), writes=()):
        import os
        lim = os.environ.get("OPLIMIT")
        if lim is not None and self.nops >= int(lim):
            return None
        writes = list(writes) + [k for k in reads if isinstance(k, str) and k.startswith("bk")]
        reads = [k for k in reads if not (isinstance(k, str) and k.startswith("bk"))]
        self._deps(eng, reads, writes)
        self.cnt[eng] += 1
        tok = (("e", eng), self.cnt[eng], eng)
        self.ops[eng].append(("op", fn, ("e", eng), 1))
        self._commit(tok, reads, writes)
        self.nops += 1
        self._pass_turn()
        return tok

    def interleave(self, fns):
        import threading
        n = len(fns)
        st = {"turn": 0, "alive": [True] * n, "err": None}
        cv = threading.Condition()
        self._il = (st, cv, {})

        def nxt(i):
            for k in range(1, n + 1):
                j = (i + k) % n
                if st["alive"][j]:
                    return j
            return -1

        def runner(i):
            self._il[2][threading.get_ident()] = i
            with cv:
                while st["turn"] != i:
                    cv.wait()
            try:
                fns[i]()
            except BaseException as ex:
                st["err"] = ex
            finally:
                with cv:
                    st["alive"][i] = False
                    st["turn"] = nxt(i)
                    cv.notify_all()
        ths = [threading.Thread(target=runner, args=(i,)) for i in range(n)]
        for t in ths:
            t.start()
        for t in ths:
            t.join()
        self._il = None
        if st["err"] is not None:
            raise st["err"]

    def _pass_turn(self):
        il = getattr(self, "_il", None)
        if not il:
            return
        import threading
        st, cv, ids = il
        me = ids.get(threading.get_ident())
        if me is None:
            return
        n = len(st["alive"])
        with cv:
            j = me
            for k in range(1, n + 1):
                c = (me + k) % n
                if st["alive"][c]:
                    j = c
                    break
            if j != me:
                st["turn"] = j
                cv.notify_all()
                while st["turn"] != me:
                    cv.wait()

    def I(self, eng, meth, *args, r=(), w=(), **kw):
        return self.op(eng, lambda e: getattr(e, meth)(*args, **kw), reads=r, writes=w)

    def dma(self, q, out, in_, reads=(), writes=(), is_output=False, **kw):
        import os
        lim = os.environ.get("OPLIMIT")
        if lim is not None and self.nops >= int(lim) and not is_output:
            return None
        i = self.dsem_next[q]
        self.dsem_next[q] = (i + 1) % N_DMA_SEMS
        uses = self.dsem_uses[q][i]
        semkey = ("d", q, i)
        if uses > 0 and self.waited[q].get(semkey, 0) < 16 * uses:
            self.waited[q][semkey] = 16 * uses
            self.ops[q].append(("wait", semkey, 16 * uses))
        self._deps(q, reads, writes)
        self.dsem_uses[q][i] = uses + 1
        tok = (semkey, 16 * (uses + 1), None)
        self.ops[q].append(("op", lambda e: e.dma_start(out=out, in_=in_, **kw), semkey, 16))
        self._commit(tok, reads, writes)
        if is_output:
            self.out_tokens.append(tok)
        self.nops += 1
        return tok

    def idma(self, out, in_, out_idx=None, in_idx=None, reads=(), writes=(), is_output=False, **kw):
        q = "pool"
        i = self.dsem_next[q]
        self.dsem_next[q] = (i + 1) % N_DMA_SEMS
        uses = self.dsem_uses[q][i]
        semkey = ("d", q, i)
        if uses > 0 and self.waited[q].get(semkey, 0) < 16 * uses:
            self.waited[q][semkey] = 16 * uses
            self.ops[q].append(("wait", semkey, 16 * uses))
        self._deps(q, reads, writes)
        self.dsem_uses[q][i] = uses + 1
        tok = (semkey, 16 * (uses + 1), None)
        oo = bass.IndirectOffsetOnAxis(ap=out_idx, axis=0) if out_idx is not None else None
        io = bass.IndirectOffsetOnAxis(ap=in_idx, axis=0) if in_idx is not None else None
        self.ops[q].append(("op", lambda e: e.indirect_dma_start(out=out, out_offset=oo, in_=in_, in_offset=io, **kw), semkey, 16))
        self._commit(tok, reads, writes)
        if is_output:
            self.out_tokens.append(tok)
        self.nops += 1
        return tok

    def _semh(self, semkey):
        if semkey[0] == "e":
            return self.sem[semkey[1]]
        return self.dsem[semkey[1]][semkey[2]]

    def finish(self):
        for tok in self.out_tokens:
            semkey, val, _ = tok
            if self.waited["sp"].get(semkey, 0) < val:
                self.waited["sp"][semkey] = val
                self.ops["sp"].append(("wait", semkey, val))
        for e in ENGS:
            if self.cnt[e] > 0 and e != "sp":
                self.ops["sp"].append(("wait", ("e", e), self.cnt[e]))
        for q in ("sp", "pool", "act"):
            for i in range(N_DMA_SEMS):
                if self.dsem_uses[q][i] > 0:
                    self.ops["sp"].append(("wait", ("d", q, i), 16 * self.dsem_uses[q][i]))
        nc = self.nc
        with nc.Block() as block:
            def mk(ename):
                def body(e):
                    for item in self.ops[ename]:
                        if item[0] == "wait":
                            e.wait_ge(self._semh(item[1]), item[2])
                        else:
                            ins = item[1](e)
                            ins.then_inc(self._semh(item[2]), item[3])
                return body
            block.tensor(mk("pe"))
            block.vector(mk("dve"))
            block.scalar(mk("act"))
            block.gpsimd(mk("pool"))
            block.sync(mk("sp"))
        self.es.close()


def build_M():
    nc = bass.Bass("TRN2", target_bir_lowering=False)
    cs = nc.dram_tensor("cs", [128, 8, 5], F32, kind="ExternalInput").ap()
    wm = nc.dram_tensor("wm", [12, 128, 8, 128], F32, kind="ExternalInput").ap()
    bm = nc.dram_tensor("bm", [128, 12], F32, kind="ExternalInput").ap()
    out = nc.dram_tensor("modT", [128, 12, 5], F32, kind="ExternalOutput").ap()
    P = Prog(nc)
    cst = P.sb("cst", [128, 8, 5]); sg = P.sb("sg", [128, 8, 5]); sc = P.sb("sc", [128, 8, 5])
    bmt = P.sb("bmt", [128, 12]); ot = P.sb("ot", [128, 12, 5])
    wt = [P.sb(f"wt{i}", [128, 8, 128]) for i in range(2)]
    pp = [P.ps(f"pp{i}", [128, 8]) for i in range(2)]
    P.dma("sp", cst[:], cs, writes=["cst"])
    P.dma("sp", bmt[:], bm, writes=["bmt"])
    P.op("act", lambda e: e.activation(sg[:], cst[:], AF.Sigmoid), reads=["cst"], writes=["sg"])
    P.op("dve", lambda e: e.tensor_tensor(sc[:], cst[:], sg[:], ALU.mult), reads=["cst", "sg"], writes=["sc"])
    for j in range(12):
        w = wt[j % 2]; wk = f"wt{j%2}"; pk = f"pp{j%2}"; p_ = pp[j % 2]
        P.dma("sp", w[:], wm[j], writes=[wk])
        for k in range(8):
            P.op("pe", lambda e, w=w, k=k, p_=p_: e.matmul(p_[:, 0:5], w[:, k, :], sc[:, k, :], start=(k == 0), stop=(k == 7)),
                 reads=[wk, "sc"], writes=[pk])
        P.op("dve", lambda e, j=j, p_=p_: e.tensor_scalar(ot[:, j, :], p_[:, 0:5], bmt[:, j:j + 1], None, ALU.add),
             reads=[pk, "bmt"], writes=["ot"])
    P.dma("sp", out, ot[:], reads=["ot"], is_output=True)
    P.finish()
    return nc


NTOK = 2176
TILES = [(0, 128)] + [(128 + 512 * i, 512) for i in range(4)]
NCT = 33


def rms_modulate(P, xT, hT, mod, nw, ones_bf, shift_i, scale_i, hT32=None, tagp="n", psb=None, hkey="hT"):
    g = P.sb(tagp + "_g", [128, 8, 2]);
    for v, (sh, sci) in enumerate(zip(shift_i, scale_i)):
        P.op("dve", lambda e, v=v, sci=sci: e.scalar_tensor_tensor(g[:, :, v], mod[:, :, sci], 1.0, nw[:, :], ALU.add, ALU.mult),
             reads=["mod", "nw"], writes=[tagp + "_g"])
    sq = [P.sb(f"{tagp}_sq{i}", [128, 8, 512], BF16) for i in range(2)]
    ss = list(psb); ssk = [f"bk_{tagp}0", f"bk_{tagp}1"]
    rs = [P.sb(f"{tagp}_rs{i}", [128, 512]) for i in range(2)]
    tmp = [P.sb(f"{tagp}_tmp{i}", [128, 512]) for i in range(2)]
    ti = 0
    for it, (t0, n) in enumerate(TILES):
        b = it % 2
        v = 1 if it == 0 else 0
        for k in range(8):
            P.op("act", lambda e, k=k, b=b, t0=t0, n=n: e.activation(sq[b][:, k, 0:n], xT[:, k, t0:t0 + n], AF.Square),
                 reads=["xT"], writes=[f"{tagp}_sq{b}"])
        for k in range(8):
            P.op("pe", lambda e, k=k, b=b, n=n: e.matmul(ss[b][:, 0:n], ones_bf[:], sq[b][:, k, 0:n], start=(k == 0), stop=(k == 7)),
                 reads=[f"{tagp}_sq{b}", "ones_bf"], writes=[ssk[b]])
        P.op("dve", lambda e, b=b, n=n: e.tensor_scalar(rs[b][:, 0:n], ss[b][:, 0:n], 1.0 / 1024, 1e-6, ALU.mult, ALU.add),
             reads=[ssk[b]], writes=[f"{tagp}_rs{b}"])
        P.op("dve", lambda e, b=b, n=n: e.reciprocal(rs[b][:, 0:n], rs[b][:, 0:n]),
             reads=[f"{tagp}_rs{b}"], writes=[f"{tagp}_rs{b}"])
        P.op("act", lambda e, b=b, n=n: e.activation(rs[b][:, 0:n], rs[b][:, 0:n], AF.Sqrt),
             reads=[f"{tagp}_rs{b}"], writes=[f"{tagp}_rs{b}"])
        for k in range(8):
            tb = ti % 2; ti += 1
            P.op("dve", lambda e, k=k, b=b, tb=tb, t0=t0, n=n, v=v: e.scalar_tensor_tensor(
                tmp[tb][:, 0:n], xT[:, k, t0:t0 + n], g[:, k, v:v + 1], rs[b][:, 0:n], ALU.mult, ALU.mult),
                 reads=["xT", tagp + "_g", f"{tagp}_rs{b}"], writes=[f"{tagp}_tmp{tb}"])
            sh = shift_i[v]
            P.op("act", lambda e, k=k, tb=tb, t0=t0, n=n, sh=sh: e.activation(
                hT[:, k, t0:t0 + n], tmp[tb][:, 0:n], AF.Identity, bias=mod[:, k, sh:sh + 1]),
                 reads=[f"{tagp}_tmp{tb}", "mod"], writes=[hkey])
            if hT32 is not None:
                P.op("pool", lambda e, k=k, tb=tb, t0=t0, n=n, sh=sh: e.tensor_scalar(
                    hT32[:, k, t0:t0 + n], tmp[tb][:, 0:n], mod[:, k, sh:sh + 1], None, ALU.add),
                     reads=[f"{tagp}_tmp{tb}", "mod"], writes=["hT32"])


def build_A(first=False):
    nc = bass.Bass("TRN2", target_bir_lowering=False)
    xTd = nc.dram_tensor("xT", [128, 8, NTOK], F32, kind="ExternalInput").ap()
    modd = nc.dram_tensor("mod", [128, 8, 6], F32, kind="ExternalInput").ap()
    fTd = nc.dram_tensor("fT", [8, 128, 8, NTOK], F32, kind="ExternalInput").ap() if not first else None
    xoutd = nc.dram_tensor("xout", [128, 8, NTOK], F32, kind="ExternalOutput").ap()
    nwd = nc.dram_tensor("nw", [128, 8], F32, kind="ExternalInput").ap()
    wind = nc.dram_tensor("win", [128, 8, NCT * 128], F32, kind="ExternalInput").ap()
    pTd = nc.dram_tensor("pT", [NCT, 128, NTOK], F32, kind="ExternalOutput").ap()
    P = Prog(nc)
    xT = P.sb("xT", [128, 8, NTOK]); hT = P.sb("hT", [128, 8, NTOK], BF16)
    mod = P.sb("mod", [128, 8, 6]); nw = P.sb("nw", [128, 8])
    wbf = P.sb("wbf", [128, 8, NCT * 128], BF16)
    ones_bf = P.sb("ones_bf", [128, 128], BF16)
    P.op("pool", lambda e: e.memset(ones_bf[:], 1.0), writes=["ones_bf"])
    for k in range(8):
        P.dma("sp", xT[:, k, :], xTd[:, k, :], writes=["xT"])
    P.dma("sp", mod[:], modd, writes=["mod"])
    P.dma("sp", nw[:], nwd, writes=["nw"])
    for k in range(8):
        P.dma("pool", wbf[:, k, :], wind[:, k, :], writes=["wbf"])
    fT = P.sb("fT", [128, 512])
    for c, k in [(c, k) for c in range(0 if first else 8) for k in range(8)]:
        for (t0, n) in TILES:
            P.dma("sp", fT[:, 0:n], fTd[c, :, k, t0:t0 + n], writes=["fT"])
            gcol = 5 if t0 == 0 else 4
            P.I("dve", "scalar_tensor_tensor", xT[:, k, t0:t0 + n], fT[:, 0:n], mod[:, k, gcol:gcol + 1], xT[:, k, t0:t0 + n], ALU.mult, ALU.add,
                r=["fT", "mod", "xT"], w=["xT"])
    for k in range(8):
        P.dma("sp", xoutd[:, k, :], xT[:, k, :], reads=["xT"], is_output=True)
    pp = [P.ps(f"bank{i}", [128, 512]) for i in range(6)]
    rms_modulate(P, xT, hT, mod, nw, ones_bf, shift_i=(0, 2), scale_i=(1, 3), tagp="n1", psb=(pp[4], pp[5]))
    st = [P.sb(f"st{i}", [128, 512]) for i in range(4)]
    i = 0
    for ct in range(NCT):
        for (t0, n) in TILES:
            b = i % 4; i += 1
            for k in range(8):
                P.op("pe", lambda e, k=k, b=b, ct=ct, t0=t0, n=n: e.matmul(
                    pp[b][:, 0:n], wbf[:, k, ct * 128:(ct + 1) * 128], hT[:, k, t0:t0 + n], start=(k == 0), stop=(k == 7)),
                     reads=["wbf", "hT"], writes=[f"bk{b}"])
            if b % 2 == 0:
                P.op("dve", lambda e, b=b, n=n: e.tensor_copy(st[b][:, 0:n], pp[b][:, 0:n]), reads=[f"bk{b}"], writes=[f"st{b}"])
            else:
                P.op("act", lambda e, b=b, n=n: e.activation(st[b][:, 0:n], pp[b][:, 0:n], AF.Copy), reads=[f"bk{b}"], writes=[f"st{b}"])
            P.dma("sp", pTd[ct, :, t0:t0 + n], st[b][:, 0:n], reads=[f"st{b}"], is_output=True)
    P.finish()
    return nc


TSEQ = 4352
QS = 64
NCH = 34
SEGS = [(0, 256), (256, 4352)]


def conv_silu(P, dst, src, cw, cb, ti, key_dst, key_src, tmp, key_tmp, out_dt_tile=None):
    for (s, e_) in SEGS:
        if cb is not None:
            P.op("act", lambda e, s=s, e_=e_: e.activation(tmp[:, s:e_], src[:, s:e_], AF.Identity, bias=cb[:, ti:ti + 1], scale=cw[:, ti, 1:2]),
                 reads=[key_src, "cw", "cb"], writes=[key_tmp])
        else:
            P.op("act", lambda e, s=s, e_=e_: e.activation(tmp[:, s:e_], src[:, s:e_], AF.Copy, scale=cw[:, ti, 1:2]),
                 reads=[key_src, "cw"], writes=[key_tmp])
        P.op("dve", lambda e, s=s, e_=e_: e.scalar_tensor_tensor(tmp[:, s + 1:e_], src[:, s:e_ - 1], cw[:, ti, 0:1], tmp[:, s + 1:e_], ALU.mult, ALU.add),
             reads=[key_src, "cw", key_tmp], writes=[key_tmp])
        P.op("dve", lambda e, s=s, e_=e_: e.scalar_tensor_tensor(tmp[:, s:e_ - 1], src[:, s + 1:e_], cw[:, ti, 2:3], tmp[:, s:e_ - 1], ALU.mult, ALU.add),
             reads=[key_src, "cw", key_tmp], writes=[key_tmp])
    P.op("act", lambda e: e.activation(dst[:, :], tmp[:, :], AF.Silu), reads=[key_tmp], writes=[key_dst])


def load_consts(P, cd):
    c = {}
    for i, nm in enumerate(["tri_f", "tri_b", "nm_f", "nm_b", "ident"]):
        t = P.sb("c_" + nm, [128, 128]); P.dma("sp", t[:], cd[i], writes=["c_" + nm]); c[nm] = t
    ones = P.sb("c_ones", [128, 128]); P.op("pool", lambda e: e.memset(ones[:], 1.0), writes=["c_ones"]); c["ones"] = ones
    idb = P.sb("c_identb", [128, 128], BF16)
    P.op("dve", lambda e: e.tensor_copy(idb[:], c["ident"][:]), reads=["c_ident"], writes=["c_identb"]); c["identb"] = idb
    return c


def host_consts():
    k = np.arange(128)[:, None]; i = np.arange(128)[None, :]
    tri_f = (k <= i).astype(np.float32); tri_b = (k >= i).astype(np.float32)
    nm_f = np.where(i >= k, 0.0, -30000.0).astype(np.float32); nm_b = np.where(i <= k, 0.0, -30000.0).astype(np.float32)
    return np.stack([tri_f, tri_b, nm_f, nm_b, np.eye(128, dtype=np.float32)])


def build_B1(stage=99):
    nc = bass.Bass("TRN2", target_bir_lowering=False)
    D = lambda n, s: nc.dram_tensor(n, s, F32, kind="ExternalInput").ap()
    zTd = D("zT", [128, TSEQ]); xbcd = D("xbcT", [3, 128, TSEQ]); dtrd = D("dtr", [128, 4 * QS])
    dtbd = D("dtb", [128, 4 * QS]); alogd = D("alog", [128, 4 * QS]); cwd = D("cw", [128, 3, 3]); cbd = D("cb", [128, 3])
    dvd = D("dvec", [128, 1]); nwd = D("normw", [128, 1]); cd = D("consts", [5, 128, 128])
    yTd = nc.dram_tensor("yT", [128, TSEQ], F32, kind="ExternalOutput").ap()
    P = Prog(nc)
    C = load_consts(P, cd)
    raw = P.sb("raw", [128, TSEQ]); tmp = P.sb("tmp", [128, TSEQ])
    xT = P.sb("xT", [128, TSEQ]); B32 = P.sb("B32", [128, TSEQ]); C32 = P.sb("C32", [128, TSEQ])
    Bb = P.sb("Bb", [128, TSEQ], BF16); Cb = P.sb("Cb", [128, TSEQ], BF16)
    cw = P.sb("cw", [128, 3, 3]); cb = P.sb("cb", [128, 3]); dvec = P.sb("dvec", [128, 1]); normw = P.sb("normw", [128, 1])
    for t, d, k in ((cw, cwd, "cw"), (cb, cbd, "cb"), (dvec, dvd, "dvec"), (normw, nwd, "normw")):
        P.dma("sp", t[:], d, writes=[k])
    if stage == 0:
        P.op("dve", lambda e: e.tensor_copy(xT[:, 0:128], C["ident"][:]), reads=["c_ident"], writes=["xT"])
        P.op("dve", lambda e: e.tensor_scalar(xT[:, 128:256], C["tri_f"][:], cw[:, 0, 0:1], dvec[:, 0:1], ALU.mult, ALU.add), reads=["c_tri_f", "cw", "dvec"], writes=["xT"])
        P.dma("sp", yTd, xT[:], reads=["xT"], is_output=True); P.finish(); return nc
    import os
    NT = int(os.environ.get("NT", "3"))
    for ti, (dst, kd) in enumerate(((xT, "xT"), (B32, "B32"), (C32, "C32"))[:NT]):
        P.dma("sp", raw[:], xbcd[ti], writes=["raw"])
        conv_silu(P, dst, raw, cw, cb, ti, kd, "raw", tmp, "tmp")
    if NT == 3 and os.environ.get("NOCAST") is None:
        P.op("pool", lambda e: e.tensor_copy(Bb[:], B32[:]), reads=["B32"], writes=["Bb"])
        P.op("pool", lambda e: e.tensor_copy(Cb[:], C32[:]), reads=["C32"], writes=["Cb"])
    if stage == 1:
        P.dma("sp", yTd, xT[:], reads=["xT"], is_output=True); P.finish(); return nc
    dtr = P.sb("dtr", [128, 4 * QS]); dtb = P.sb("dtb", [128, 4 * QS]); alog = P.sb("alog", [128, 4 * QS])
    dt = P.sb("dt", [128, 4 * QS]); la = P.sb("la", [128, 4 * QS]); ncum = P.sb("ncum", [128, 4 * QS])
    wgt = P.sb("wgt", [128, 4 * QS]); dec = P.sb("dec", [128, 4 * QS])
    P.dma("sp", dtr[:], dtrd, writes=["dtr"]); P.dma("sp", dtb[:], dtbd, writes=["dtb"]); P.dma("sp", alog[:], alogd, writes=["alog"])
    P.op("dve", lambda e: e.tensor_tensor(dtr[:], dtr[:], dtb[:], ALU.add), reads=["dtr", "dtb"], writes=["dtr"])
    P.op("dve", lambda e: e.tensor_scalar(dtr[:], dtr[:], 60.0, None, ALU.min), reads=["dtr"], writes=["dtr"])
    P.op("act", lambda e: e.activation(dtr[:], dtr[:], AF.Exp), reads=["dtr"], writes=["dtr"])
    P.op("act", lambda e: e.activation(dt[:], dtr[:], AF.Ln, bias=1.0), reads=["dtr"], writes=["dt"])
    P.op("act", lambda e: e.activation(alog[:], alog[:], AF.Exp), reads=["alog"], writes=["alog"])
    P.op("dve", lambda e: e.scalar_tensor_tensor(la[:], dt[:], -1.0, alog[:], ALU.mult, ALU.mult), reads=["dt", "alog"], writes=["la"])
    bk = [P.ps(f"bank{i}", [128, 512]) for i in range(8)]
    pc = bk[0][:, 0:4 * QS]; pt = bk[1][:, 0:4 * QS]
    P.op("pe", lambda e: e.matmul(pc[:, 0:2 * QS], C["tri_f"][:], la[:, 0:2 * QS], start=True, stop=True), reads=["la", "c_tri_f"], writes=["bk0"])
    P.op("pe", lambda e: e.matmul(pc[:, 2 * QS:4 * QS], C["tri_b"][:], la[:, 2 * QS:4 * QS], start=True, stop=True), reads=["la", "c_tri_b"], writes=["bk0"])
    P.op("pe", lambda e: e.matmul(pt, C["ones"][:], la[:], start=True, stop=True), reads=["la", "c_ones"], writes=["bk1"])
    P.op("dve", lambda e: e.tensor_scalar(ncum[:], pc, -1.0, None, ALU.mult), reads=["bk0"], writes=["ncum"])
    P.op("dve", lambda e: e.tensor_tensor(wgt[:], pt, ncum[:], ALU.add), reads=["bk1", "ncum"], writes=["wgt"])
    P.op("act", lambda e: e.activation(wgt[:], wgt[:], AF.Exp), reads=["wgt"], writes=["wgt"])
    P.op("dve", lambda e: e.tensor_tensor(wgt[:], wgt[:], dt[:], ALU.mult), reads=["wgt", "dt"], writes=["wgt"])
    P.op("act", lambda e: e.activation(dec[:], pt, AF.Exp), reads=["bk1"], writes=["dec"])
    if stage == 2:
        for i_, (t_, k_) in enumerate(((dt, "dt"), (la, "la"), (ncum, "ncum"), (wgt, "wgt"), (dec, "dec"))):
            P.dma("sp", yTd[:, i_ * 256:(i_ + 1) * 256], t_[:], reads=[k_], is_output=True)
        for i_, (t_, k_) in enumerate(((B32, "B32"), (C32, "C32"), (xT, "xT"))):
            P.dma("sp", yTd[:, 1280 + i_ * 1024:1280 + (i_ + 1) * 1024], t_[:, 0:1024], reads=[k_], is_output=True)
        P.finish(); return nc
    xpad = [P.sb(f"xpad{h}", [128, NCH, 128], BF16) for h in range(2)]
    Btok = P.sb("Btok", [128, NCH, 128], BF16); xw = P.sb("xw", [128, NCH, 4, 64], BF16)
    for h in range(2):
        P.op("pool", lambda e, h=h: e.memset(xpad[h][:], 0.0), writes=[f"xpad{h}"])
    ptr = [bk[2][:, 0:128], bk[3][:, 0:128]]
    for c in range(NCH):
        sl = slice(c * 128, (c + 1) * 128)
        P.op("pe", lambda e, sl=sl: e.transpose(ptr[0], xT[:, sl], C["ident"][:]), reads=["xT", "c_ident"], writes=["bk2"])
        P.op("pe", lambda e, sl=sl: e.transpose(ptr[1], B32[:, sl], C["ident"][:]), reads=["B32", "c_ident"], writes=["bk3"])
        for h in range(2):
            P.op("act", lambda e, h=h, c=c: e.activation(xpad[h][:, c, h * 64:(h + 1) * 64], ptr[0][:, h * 64:(h + 1) * 64], AF.Copy),
                 reads=["bk2"], writes=[f"xpad{h}"])
        for q in range(4):
            h = q % 2
            P.op("dve", lambda e, q=q, h=h, c=c: e.tensor_scalar(xw[:, c, q, :], ptr[0][:, h * 64:(h + 1) * 64], wgt[:, q * QS + c:q * QS + c + 1], None, ALU.mult),
                 reads=["bk2", "wgt"], writes=["xw"])
        P.op("act", lambda e, c=c: e.activation(Btok[:, c, :], ptr[1], AF.Copy), reads=["bk3"], writes=["Btok"])
    if stage == 3:
        P.dma("sp", yTd, xT[:], reads=["xT"], is_output=True); P.finish(); return nc
    yacc = P.sb("yacc", [128, TSEQ])
    Hpad = [P.sb(f"Hpad{q}", [128, 128]) for q in range(4)]
    larep = [P.sb(f"larep{i}", [128, 128]) for i in range(2)]
    seg = [P.sb(f"seg{i}", [128, 128]) for i in range(2)]; Et = [P.sb(f"Et{i}", [128, 128]) for i in range(2)]
    STp = [P.sb(f"STp{i}", [128, 128], BF16) for i in range(2)]; CTs = [P.sb(f"CTs{i}", [128, 128]) for i in range(2)]
    psA = bk[0][:, 0:128]; psE = bk[1][:, 0:128]; psS = bk[4][:, 0:128]; psY = bk[5][:, 0:128]
    psH = [bk[6][:, 0:64], bk[7][:, 0:64]]
    import os
    for d in range(int(os.environ.get('ND', '2'))):
        tri = C["tri_f"] if d == 0 else C["tri_b"]; nm = C["nm_f"] if d == 0 else C["nm_b"]
        trik = "c_tri_f" if d == 0 else "c_tri_b"; nmk = "c_nm_f" if d == 0 else "c_nm_b"
        order = list(range(NCH)) if d == 0 else [1, 0] + list(range(NCH - 1, 1, -1))
        for hh in range(2):
            P.op("pool", lambda e, q=d * 2 + hh: e.memset(Hpad[q][:], 0.0), writes=[f"Hpad{d*2+hh}"])
        for c in order:
            sl = slice(c * 128, (c + 1) * 128)
            P.op("pe", lambda e, sl=sl: e.matmul(psS, Bb[:, sl], Cb[:, sl], start=True, stop=True), reads=["Bb", "Cb"], writes=["bk4"])
            for hh in range(2):
                q = d * 2 + hh
                P.op("pool", lambda e, q=q, c=c, hh=hh: e.tensor_scalar(larep[hh][:], C["ones"][:], la[:, q * QS + c:q * QS + c + 1], None, ALU.mult),
                     reads=["c_ones", "la"], writes=[f"larep{hh}"])
                P.op("pe", lambda e, hh=hh, tri=tri: e.matmul(psA, larep[hh][:], tri[:], start=True, stop=False), reads=[f"larep{hh}", trik], writes=["bk0"])
                P.op("pe", lambda e, nm=nm: e.matmul(psA, C["ident"][:], nm[:], start=False, stop=True), reads=["c_ident", nmk], writes=["bk0"])
                P.op("pe", lambda e, hh=hh, tri=tri: e.matmul(psE, larep[hh][:], tri[:], start=True, stop=True), reads=[f"larep{hh}", trik], writes=["bk1"])
                P.op("act", lambda e, q=q, c=c, hh=hh: e.activation(seg[hh][:], psA, AF.Exp, bias=ncum[:, q * QS + c:q * QS + c + 1]), reads=["bk0", "ncum"], writes=[f"seg{hh}"])
                P.op("act", lambda e, hh=hh: e.activation(Et[hh][:], psE, AF.Exp), reads=["bk1"], writes=[f"Et{hh}"])
                P.op("dve", lambda e, q=q, c=c, hh=hh: e.scalar_tensor_tensor(STp[hh][:], psS, dt[:, q * QS + c:q * QS + c + 1], seg[hh][:], ALU.mult, ALU.mult),
                     reads=["bk4", "dt", f"seg{hh}"], writes=[f"STp{hh}"])
                P.op("dve", lambda e, sl=sl, hh=hh: e.tensor_tensor(CTs[hh][:], C32[:, sl], Et[hh][:], ALU.mult), reads=["C32", f"Et{hh}"], writes=[f"CTs{hh}"])
            for hh in range(2):
                q = d * 2 + hh
                P.op("pe", lambda e, hh=hh, c=c: e.matmul(psY, xpad[hh][:, c, :], STp[hh][:], start=(hh == 0), stop=False),
                     reads=[f"xpad{hh}", f"STp{hh}"], writes=["bk5"])
            for hh in range(2):
                q = d * 2 + hh
                P.op("pe", lambda e, hh=hh, q=q: e.matmul(psY, Hpad[q][:], CTs[hh][:], start=False, stop=(hh == 1)),
                     reads=[f"Hpad{q}", f"CTs{hh}"], writes=["bk5"])
            for hh in range(2):
                q = d * 2 + hh
                P.op("pe", lambda e, hh=hh, q=q, c=c: e.matmul(psH[hh], Btok[:, c, :], xw[:, c, q, :], start=True, stop=True),
                     reads=["Btok", "xw"], writes=[f"bk{6+hh}"])
                P.op("dve", lambda e, hh=hh, q=q, c=c: e.scalar_tensor_tensor(
                    Hpad[q][:, hh * 64:(hh + 1) * 64], Hpad[q][:, hh * 64:(hh + 1) * 64], dec[:, q * QS + c:q * QS + c + 1], psH[hh], ALU.mult, ALU.add),
                     reads=[f"Hpad{q}", "dec", f"bk{6+hh}"], writes=[f"Hpad{q}"])
            if d == 0:
                P.op("dve", lambda e, sl=sl: e.scalar_tensor_tensor(yacc[:, sl], xT[:, sl], dvec[:, 0:1], psY, ALU.mult, ALU.add),
                     reads=["xT", "dvec", "bk5"], writes=[f"yacc{c}"])
            else:
                P.op("dve", lambda e, sl=sl: e.tensor_tensor(yacc[:, sl], yacc[:, sl], psY, ALU.add), reads=[f"yacc{c}", "bk5"], writes=[f"yacc{c}"])
    if stage == 4:
        P.dma("sp", yTd, yacc[:], reads=[f"yacc{c}" for c in range(NCH)], is_output=True); P.finish(); return nc
    zT = raw
    P.dma("sp", zT[:], zTd, writes=["raw"])
    P.op("act", lambda e: e.activation(tmp[:], zT[:], AF.Silu), reads=["raw"], writes=["tmp"])
    allc = [f"yacc{c}" for c in range(NCH)]
    P.op("dve", lambda e: e.tensor_tensor(yacc[:], yacc[:], tmp[:], ALU.mult), reads=allc + ["tmp"], writes=allc)
    P.op("act", lambda e: e.activation(tmp[:], yacc[:], AF.Square), reads=allc, writes=["tmp"])
    pss = [bk[2][:, 0:256], bk[3][:, 0:256]]; rs = [P.sb(f"rs{i}", [128, 256]) for i in range(2)]
    for i, t0 in enumerate(range(0, TSEQ, 256)):
        n = min(256, TSEQ - t0); b = i % 2
        P.op("pe", lambda e, b=b, t0=t0, n=n: e.matmul(pss[b][:, 0:n], C["ones"][:], tmp[:, t0:t0 + n], start=True, stop=True), reads=["tmp", "c_ones"], writes=[f"bk{2+b}"])
        P.op("dve", lambda e, b=b, n=n: e.tensor_scalar(rs[b][:, 0:n], pss[b][:, 0:n], 1.0 / 128, 1e-5, ALU.mult, ALU.add), reads=[f"bk{2+b}"], writes=[f"rs{b}"])
        P.op("dve", lambda e, b=b, n=n: e.reciprocal(rs[b][:, 0:n], rs[b][:, 0:n]), reads=[f"rs{b}"], writes=[f"rs{b}"])
        P.op("act", lambda e, b=b, n=n: e.activation(rs[b][:, 0:n], rs[b][:, 0:n], AF.Sqrt), reads=[f"rs{b}"], writes=[f"rs{b}"])
        P.op("dve", lambda e, b=b, t0=t0, n=n: e.scalar_tensor_tensor(xT[:, t0:t0 + n], yacc[:, t0:t0 + n], normw[:, 0:1], rs[b][:, 0:n], ALU.mult, ALU.mult),
             reads=allc + ["normw", f"rs{b}"], writes=["xT"])
    P.dma("sp", yTd, xT[:], reads=["xT"], is_output=True)
    P.finish()
    return nc


def host_B1_inputs(pa, L, b, hp, prm):
    p = pa[b]
    z = p[:, hp * 128:(hp + 1) * 128].T
    x = p[:, 256 + hp * 128:256 + (hp + 1) * 128].T
    Bm = p[:, 512 + hp * 128:512 + (hp + 1) * 128].T
    Cm = p[:, 768 + hp * 128:768 + (hp + 1) * 128].T
    cols = [1024 + d * 4 + 2 * hp + hh for d in range(2) for hh in range(2)]
    dtr = np.zeros((128, 4, QS), np.float32); dtr[:, :, :NCH] = p[:, cols].reshape(NCH, 128, 4).transpose(1, 2, 0); dtr = dtr.reshape(128, 4 * QS)
    bc = lambda v: np.broadcast_to(np.asarray(v, np.float32)[None, :, None], (128, 4, QS)).reshape(128, 4 * QS)
    dtb = bc([prm['m_dt_bias'][L, d, 2 * hp + hh] for d in range(2) for hh in range(2)])
    alog = bc([prm['m_a_log'][L, d, 2 * hp + hh] for d in range(2) for hh in range(2)])
    cwfull = prm['m_conv_w'][L]; cbfull = prm['m_conv_b'][L]
    offs = [hp * 128, 256 + hp * 128, 512 + hp * 128]
    cw = np.stack([cwfull[:, o:o + 128].T for o in offs], 1)
    cb = np.stack([cbfull[o:o + 128] for o in offs], 1)
    dvec = np.repeat(prm['m_d'][L, 2 * hp:2 * hp + 2], 64)[:, None]
    normw = prm['m_norm_w'][L, hp * 128:(hp + 1) * 128][:, None]
    A = np.ascontiguousarray
    return {"zT": A(z), "xbcT": A(np.stack([x, Bm, Cm])), "dtr": A(dtr), "dtb": A(dtb), "alog": A(alog), "cw": A(cw), "cb": A(cb),
            "dvec": A(dvec), "normw": A(normw), "consts": host_consts()}


NPK = 34


def host_consts64():
    k = np.arange(128)[:, None]; i = np.arange(128)[None, :]
    same = (k // 64) == (i // 64)
    f = lambda m: m.astype(np.float32)
    tri_f = f(same & (k <= i)); tri_b = f(same & (k >= i))
    nm_f = np.where(same & (i >= k), 0.0, -30000.0); nm_b = np.where(same & (i <= k), 0.0, -30000.0)
    pms_f = np.where(same & (i < k), 0.0, 30000.0); pms_b = np.where(same & (i > k), 0.0, 30000.0)
    blk = f(same); selA = f(np.broadcast_to(k < 64, (128, 128))); selB = f(np.broadcast_to(k >= 64, (128, 128)))
    inc_f = f(same & (i < k)); inc_b = f(same & (i > k))
    return np.stack([np.eye(128), tri_f, tri_b, nm_f, nm_b, pms_f, pms_b, blk, selA, selB, inc_f, inc_b]).astype(np.float32)

C64_NAMES = ["ident", "tri_f", "tri_b", "nm_f", "nm_b", "pms_f", "pms_b", "blk", "selA", "selB", "sl", "su"]


def load_consts64(P, cd, only=None):
    c = {}
    for i, nm in enumerate(C64_NAMES):
        if only is not None and nm not in only:
            continue
        t = P.sb("c_" + nm, [128, 128]); P.dma("sp", t[:], cd[i], writes=["c_" + nm]); c[nm] = t
    ones = P.sb("c_ones", [128, 128]); P.I("pool", "memset", ones[:], 1.0, w=["c_ones"]); c["ones"] = ones
    return c


def tri_inverse_apply(P, C, Lm, X, ncolsX, bk, tg):
    Pt = [P.sb(f"{tg}P{i}", [128, 128]) for i in range(2)] if not hasattr(P, "_tri_" + tg) else getattr(P, "_tri_" + tg)[0]
    Qt = [P.sb(f"{tg}Q{i}", [128, 128]) for i in range(2)] if not hasattr(P, "_tri_" + tg) else getattr(P, "_tri_" + tg)[1]
    setattr(P, "_tri_" + tg, (Pt, Qt))
    (pP, kP), (pQ, kQ), (pT, kT), (pX, kX) = bk["P"], bk["Q"], bk["T"], bk["X"]
    ident = C["ident"]
    P.I("pe", "transpose", pT[:, 0:128], Lm[:], ident[:], r=[tg + "L", "c_ident"], w=[kT])
    P.I("act", "activation", Qt[0][:], pT[:, 0:128], AF.Copy, r=[kT], w=[f"{tg}Q0"])
    P.I("pe", "matmul", pX[:, 0:ncolsX], Qt[0][:], X[:], start=True, stop=True, r=[f"{tg}Q0", tg + "X"], w=[kX])
    P.I("dve", "tensor_tensor", X[:], X[:], pX[:, 0:ncolsX], ALU.subtract, r=[tg + "X", kX], w=[tg + "X"])
    Pc, Pk, Qc, Qk = Lm, tg + "L", Qt[0], f"{tg}Q0"
    for lvl in range(1, 6):
        a = lvl % 2
        P.I("pe", "matmul", pQ[:, 0:128], Pc[:], Qc[:], start=True, stop=True, r=[Pk, Qk], w=[kQ])
        if lvl < 5:
            P.I("pe", "matmul", pP[:, 0:128], Qc[:], Pc[:], start=True, stop=True, r=[Pk, Qk], w=[kP])
            P.I("dve", "tensor_copy", Pt[a][:], pP[:, 0:128], r=[kP], w=[f"{tg}P{a}"])
        P.I("act", "activation", Qt[a][:], pQ[:, 0:128], AF.Copy, r=[kQ], w=[f"{tg}Q{a}"])
        Pc, Pk, Qc, Qk = Pt[a], f"{tg}P{a}", Qt[a], f"{tg}Q{a}"
        P.I("pe", "matmul", pX[:, 0:ncolsX], Qc[:], X[:], start=True, stop=True, r=[Qk, tg + "X"], w=[kX])
        P.I("dve", "tensor_tensor", X[:], X[:], pX[:, 0:ncolsX], ALU.add, r=[tg + "X", kX], w=[tg + "X"])


def build_B2():
    nc = bass.Bass("TRN2", target_bir_lowering=False)
    D = lambda n, s: nc.dram_tensor(n, s, F32, kind="ExternalInput").ap()
    qkvd = D("qkvT", [3, 128, TSEQ]); gated = D("gate", [128, NPK, 128]); tabd = D("tab", [4, 128, 2 * QS])
    cwd = D("cw", [128, 3, 3]); nwd = D("normw", [128, 128]); cd = D("consts", [len(C64_NAMES), 128, 128])
    yd = nc.dram_tensor("y", [128, NPK, 128], F32, kind="ExternalOutput").ap()
    P = Prog(nc)
    C = load_consts64(P, cd)
    bkt = [P.ps(f"bank{i}", [128, 512]) for i in range(8)]
    BK = lambda i: (bkt[i], f"bk{i}")
    raw = P.sb("raw", [128, TSEQ]); tmp = P.sb("tmp", [128, TSEQ])
    qT = P.sb("qT", [128, TSEQ]); kT = P.sb("kT", [128, TSEQ])
    cw = P.sb("cw", [128, 3, 3]); P.dma("sp", cw[:], cwd, writes=["cw"])
    normw = P.sb("normw", [128, 128]); P.dma("sp", normw[:], nwd, writes=["normw"])
    ktok = P.sb("ktok", [128, NPK, 128]); vtok = P.sb("vtok", [128, NPK, 128]); oacc = P.sb("oacc", [128, NPK, 128])
    rs = [P.sb(f"rs{i}", [128, 256]) for i in range(2)]
    for ti, (dst, kd) in enumerate(((qT, "qT"), (kT, "kT"), (raw, "raw"))):
        P.dma("sp", raw[:], qkvd[ti], writes=["raw"])
        conv_silu(P, dst, raw, cw, None, ti, kd, "raw", tmp, "tmp")
        if ti < 2:
            P.I("act", "activation", tmp[:], dst[:], AF.Square, r=[kd], w=["tmp"])
            for i, t0 in enumerate(range(0, TSEQ, 256)):
                b = i % 2; (pa, pk) = BK(b)
                P.I("pe", "matmul", pa[:, 0:256], C["ones"][:], tmp[:, t0:t0 + 256], start=True, stop=True, r=["tmp", "c_ones"], w=[pk])
                P.I("dve", "tensor_scalar", rs[b][:], pa[:, 0:256], 1e-6, None, ALU.add, r=[pk], w=[f"rs{b}"])
                P.I("dve", "reciprocal", rs[b][:], rs[b][:], r=[f"rs{b}"], w=[f"rs{b}"])
                P.I("act", "activation", rs[b][:], rs[b][:], AF.Sqrt, r=[f"rs{b}"], w=[f"rs{b}"])
                sc = 128.0 ** -0.5 if ti == 0 else 1.0
                P.I("dve", "scalar_tensor_tensor", dst[:, t0:t0 + 256], dst[:, t0:t0 + 256], sc, rs[b][:], ALU.mult, ALU.mult,
                    r=[kd, f"rs{b}"], w=[kd])
    vT = raw
    for c in range(NPK):
        sl = slice(c * 128, (c + 1) * 128)
        for src, sk, dst, dk, bi in ((kT, "kT", ktok, "ktok", 0), (vT, "raw", vtok, "vtok", 1)):
            (pa, pk) = BK(bi)
            P.I("pe", "transpose", pa[:, 0:128], src[:, sl], C["ident"][:], r=[sk, "c_ident"], w=[pk])
            P.I("act" if bi else "dve", "activation" if bi else "tensor_copy", dst[:, c, :], pa[:, 0:128], *([AF.Copy] if bi else []), r=[pk], w=[dk])
    W2 = 2 * QS
    tb = {n: P.sb("t_" + n, [128, W2]) for n in ("braw", "araw", "dtb", "alog", "beta", "g", "gc", "ngc", "egc", "toend", "glA", "glB", "bw")}
    for i, n in enumerate(("braw", "araw", "dtb", "alog")):
        P.dma("sp", tb[n][:], tabd[i], writes=["t_" + n])
    P.I("act", "activation", tb["beta"][:], tb["braw"][:], AF.Sigmoid, r=["t_braw"], w=["t_beta"])
    P.I("dve", "tensor_tensor", tb["araw"][:], tb["araw"][:], tb["dtb"][:], ALU.add, r=["t_araw", "t_dtb"], w=["t_araw"])
    P.I("dve", "tensor_scalar", tb["araw"][:], tb["araw"][:], 60.0, None, ALU.min, r=["t_araw"], w=["t_araw"])
    P.I("act", "activation", tb["araw"][:], tb["araw"][:], AF.Exp, r=["t_araw"], w=["t_araw"])
    P.I("act", "activation", tb["araw"][:], tb["araw"][:], AF.Ln, bias=1.0, r=["t_araw"], w=["t_araw"])
    P.I("act", "activation", tb["alog"][:], tb["alog"][:], AF.Exp, r=["t_alog"], w=["t_alog"])
    P.I("dve", "scalar_tensor_tensor", tb["g"][:], tb["araw"][:], -1.0, tb["alog"][:], ALU.mult, ALU.mult, r=["t_araw", "t_alog"], w=["t_g"])
    (p0, k0), (p1, k1), (p2, k2), (p3, k3) = BK(0), BK(1), BK(2), BK(3)
    P.I("pe", "matmul", p0[:, 0:QS], C["tri_f"][:], tb["g"][:, 0:QS], start=True, stop=True, r=["t_g", "c_tri_f"], w=[k0])
    P.I("pe", "matmul", p0[:, QS:W2], C["tri_b"][:], tb["g"][:, QS:W2], start=True, stop=True, r=["t_g", "c_tri_b"], w=[k0])
    P.I("pe", "matmul", p1[:, 0:W2], C["blk"][:], tb["g"][:], start=True, stop=True, r=["t_g", "c_blk"], w=[k1])
    P.I("pe", "matmul", p2[:, 0:W2], C["selA"][:], tb["g"][:], start=True, stop=True, r=["t_g", "c_selA"], w=[k2])
    P.I("pe", "matmul", p3[:, 0:W2], C["selB"][:], tb["g"][:], start=True, stop=True, r=["t_g", "c_selB"], w=[k3])
    P.I("dve", "tensor_copy", tb["gc"][:], p0[:, 0:W2], r=[k0], w=["t_gc"])
    P.I("dve", "tensor_scalar", tb["ngc"][:], tb["gc"][:], -1.0, None, ALU.mult, r=["t_gc"], w=["t_ngc"])
    P.I("act", "activation", tb["egc"][:], tb["gc"][:], AF.Exp, r=["t_gc"], w=["t_egc"])
    P.I("dve", "tensor_tensor", tb["toend"][:], p1[:, 0:W2], tb["gc"][:], ALU.subtract, r=[k1, "t_gc"], w=["t_toend"])
    P.I("act", "activation", tb["toend"][:], tb["toend"][:], AF.Exp, r=["t_toend"], w=["t_toend"])
    P.I("act", "activation", tb["glA"][:], p2[:, 0:W2], AF.Exp, r=[k2], w=["t_glA"])
    P.I("act", "activation", tb["glB"][:], p3[:, 0:W2], AF.Exp, r=[k3], w=["t_glB"])
    P.I("dve", "tensor_tensor", tb["bw"][:], tb["beta"][:], tb["egc"][:], ALU.mult, r=["t_beta", "t_egc"], w=["t_bw"])
    P.I("pool", "memset", oacc[:], 0.0, w=[f"oacc{c}" for c in range(NPK)])
    names = (("grep", [128, 128]), ("DmT", [128, 128]), ("DmS", [128, 128]), ("Et", [128, 128]), ("attnT", [128, 128]), ("L", [128, 128]),
             ("qdT", [128, 128]), ("X", [128, 256]), ("kdec", [128, 128]), ("wT", [128, 128]), ("vnew", [128, 128]), ("S", [128, 128]))
    TL = [{n: P.sb(f"dn{d}{n}", shp) for n, shp in names} for d in range(2)]

    def run_dir(d):
        T = TL[d]; K = lambda n: f"dn{d}{n}"
        ba, bb, bc_, bd_ = [bkt[4 * d + i] for i in range(4)]; ka, kb, kc, kd = [f"bk{4*d+i}" for i in range(4)]
        pG, pA, pPq, pQq = ba[:, 0:128], ba[:, 128:256], ba[:, 256:384], ba[:, 384:512]
        pD1, pD2, pE, pT = bb[:, 0:128], bb[:, 128:256], bb[:, 256:384], bb[:, 384:512]
        pX, pV, pS = bc_[:, 0:256], bc_[:, 256:384], bc_[:, 384:512]
        pO = bd_[:, 0:128]
        bkinv = {"P": (pPq, ka), "Q": (pQq, ka), "T": (pT, kb), "X": (pX, kc)}
        sfx = "_f" if d == 0 else "_b"
        tri, nm, pms = C["tri" + sfx], C["nm" + sfx], C["pms" + sfx]
        order = list(range(NPK)) if d == 0 else [1, 0] + list(range(NPK - 1, 1, -1))
        S, grep, DmT, DmS, Et, attnT, Lm, qdT, X, kdec, wT, vnew = [T[n] for n in ("S", "grep", "DmT", "DmS", "Et", "attnT", "L", "qdT", "X", "kdec", "wT", "vnew")]
        P.I("pool", "memset", S[:], 0.0, w=[K("S")])
        for c in order:
            sl = slice(c * 128, (c + 1) * 128); col = d * QS + c; cs = slice(col, col + 1)
            P.I("pe", "matmul", pG, kT[:, sl], kT[:, sl], start=True, stop=True, r=["kT"], w=[ka])
            P.I("pe", "matmul", pA, kT[:, sl], qT[:, sl], start=True, stop=True, r=["kT", "qT"], w=[ka])
            P.I("pool", "tensor_scalar", grep[:], C["ones"][:], tb["g"][:, cs], None, ALU.mult, r=["c_ones", "t_g"], w=[K("grep")])
            P.I("pe", "matmul", pD1, grep[:], tri[:], start=True, stop=False, r=[K("grep"), "c_tri" + sfx], w=[kb])
            P.I("pe", "matmul", pD1, C["ident"][:], nm[:], start=False, stop=True, r=["c_ident", "c_nm" + sfx], w=[kb])
            P.I("act", "activation", DmT[:], pD1, AF.Exp, bias=tb["ngc"][:, cs], r=[kb, "t_ngc"], w=[K("DmT")])
            P.I("pe", "matmul", pD2, grep[:], tri[:], start=True, stop=False, r=[K("grep"), "c_tri" + sfx], w=[kb])
            P.I("pe", "matmul", pD2, C["ident"][:], pms[:], start=False, stop=True, r=["c_ident", "c_pms" + sfx], w=[kb])
            P.I("act", "activation", DmS[:], pD2, AF.Exp, bias=tb["gc"][:, cs], scale=-1.0, r=[kb, "t_gc"], w=[K("DmS")])
            P.I("pe", "matmul", pE, grep[:], tri[:], start=True, stop=True, r=[K("grep"), "c_tri" + sfx], w=[kb])
            P.I("act", "activation", Et[:], pE, AF.Exp, r=[kb], w=[K("Et")])
            P.I("dve", "tensor_tensor", attnT[:], pA, DmT[:], ALU.mult, r=[ka, K("DmT")], w=[K("attnT")])
            P.I("dve", "scalar_tensor_tensor", Lm[:], pG, tb["beta"][:, cs], DmS[:], ALU.mult, ALU.mult, r=[ka, "t_beta", K("DmS")], w=[K("L")])
            P.I("dve", "tensor_tensor", qdT[:], qT[:, sl], Et[:], ALU.mult, r=["qT", K("Et")], w=[K("qdT")])
            P.I("dve", "tensor_scalar", X[:, 0:128], vtok[:, c, :], tb["beta"][:, cs], None, ALU.mult, r=["vtok", "t_beta"], w=[K("X")])
            P.I("dve", "tensor_scalar", X[:, 128:256], ktok[:, c, :], tb["bw"][:, cs], None, ALU.mult, r=["ktok", "t_bw"], w=[K("X")])
            P.I("pool", "tensor_scalar", kdec[:], ktok[:, c, :], tb["toend"][:, cs], None, ALU.mult, r=["ktok", "t_toend"], w=[K("kdec")])
            tri_inverse_apply(P, C, Lm, X, 256, bkinv, f"dn{d}")
            P.I("pe", "transpose", pT, X[:, 128:256], C["ident"][:], r=[K("X"), "c_ident"], w=[kb])
            P.I("act", "activation", wT[:], pT, AF.Copy, r=[kb], w=[K("wT")])
            for half in ((0, 1) if d == 0 else (1, 0)):
                rows = slice(half * 64, (half + 1) * 64)
                gl = tb["glA"] if half == 0 else tb["glB"]; glk = "t_glA" if half == 0 else "t_glB"
                P.I("pe", "matmul", pV, wT[:], S[:], start=True, stop=True, r=[K("wT"), K("S")], w=[kc])
                P.I("dve", "tensor_tensor", vnew[rows, :], X[rows, 0:128], pV[rows, :], ALU.subtract, r=[K("X"), kc], w=[K("vnew")])
                P.I("pe", "matmul", pO, qdT[:], S[:], start=True, stop=False, r=[K("qdT"), K("S")], w=[kd])
                P.I("pe", "matmul", pO, attnT[rows, :], vnew[rows, :], start=False, stop=True, r=[K("attnT"), K("vnew")], w=[kd])
                P.I("dve", "tensor_tensor", oacc[rows, c, :], oacc[rows, c, :], pO[rows, :], ALU.add, r=[kd, f"oacc{c}"], w=[f"oacc{c}"])
                P.I("pe", "matmul", pS, kdec[rows, :], vnew[rows, :], start=True, stop=True, r=[K("kdec"), K("vnew")], w=[kc])
                P.I("dve", "scalar_tensor_tensor", S[:], S[:], gl[:, cs], pS, ALU.mult, ALU.add, r=[K("S"), glk, kc], w=[K("S")])

    P.interleave([lambda: run_dir(0), lambda: run_dir(1)])
    allo = [f"oacc{c}" for c in range(NPK)]
    gate = P.sb("gate", [128, NPK, 128]); sq = P.sb("sq", [128, NPK, 128]); ss = P.sb("ss", [128, NPK])
    P.dma("sp", gate[:], gated, writes=["gate"])
    P.I("act", "activation", gate[:], gate[:], AF.Silu, r=["gate"], w=["gate"])
    P.I("act", "activation", sq[:], oacc[:], AF.Square, r=allo, w=["sq"])
    P.I("dve", "tensor_reduce", ss[:], sq[:], AX.X, ALU.add, r=["sq"], w=["ss"])
    P.I("dve", "tensor_scalar", ss[:], ss[:], 1.0 / 128, 1e-6, ALU.mult, ALU.add, r=["ss"], w=["ss"])
    P.I("dve", "reciprocal", ss[:], ss[:], r=["ss"], w=["ss"])
    P.I("act", "activation", ss[:], ss[:], AF.Sqrt, r=["ss"], w=["ss"])
    for c in range(NPK):
        P.I("dve", "scalar_tensor_tensor", sq[:, c, :], oacc[:, c, :], ss[:, c:c + 1], normw[:], ALU.mult, ALU.mult, r=allo + ["ss", "normw"], w=["sq"])
    P.I("dve", "tensor_tensor", sq[:], sq[:], gate[:], ALU.mult, r=["sq", "gate"], w=["sq"])
    P.dma("sp", yd, sq[:], reads=["sq"], is_output=True)
    P.finish()
    return nc


def colmajor_perm():
    t = np.arange(4096).reshape(64, 64)
    return t.T.reshape(-1)


def host_B2_inputs(pb, L, b, head, prm):
    perm = np.concatenate([np.arange(256), 256 + colmajor_perm()])
    p = pb[b][perm]
    q = p[:, head * 128:(head + 1) * 128].T; k = p[:, 512 + head * 128:512 + (head + 1) * 128].T
    v = p[:, 1024 + head * 128:1024 + (head + 1) * 128].T
    gate = p[:, 1536 + head * 128:1536 + (head + 1) * 128].reshape(NPK, 128, 128).transpose(1, 0, 2)
    def tabl(cols):
        t = np.zeros((128, 2, QS), np.float32); t[:, :, :NPK] = p[:, cols].reshape(NPK, 128, 2).transpose(1, 2, 0); return t.reshape(128, 2 * QS)
    braw = tabl([2048 + d * 4 + head for d in range(2)]); araw = tabl([2056 + d * 4 + head for d in range(2)])
    bc = lambda v_: np.broadcast_to(np.asarray(v_, np.float32)[None, :, None], (128, 2, QS)).reshape(128, 2 * QS)
    dtb = bc(prm['dn_dt_bias'][L, :, head]); alog = bc(prm['dn_a_log'][L, :, head])
    cwf = prm['dn_conv_w'][L]
    cw = np.stack([cwf[:, o + head * 128:o + (head + 1) * 128].T for o in (0, 512, 1024)], 1)
    normw = np.broadcast_to(prm['dn_norm_w'][L][None, :], (128, 128))
    A = lambda a: np.ascontiguousarray(a, dtype=np.float32)
    return {"qkvT": A(np.stack([q, k, v])), "gate": A(gate), "tab": A(np.stack([braw, araw, dtb, alog])), "cw": A(cw), "normw": A(normw),
            "consts": host_consts64()}


def host_B2_output(y):
    yy = y.transpose(1, 0, 2).reshape(TSEQ, 128)
    out = np.empty_like(yy)
    perm = np.concatenate([np.arange(256), 256 + colmajor_perm()])
    out[perm] = yy
    return out


def build_B3():
    nc = bass.Bass("TRN2", target_bir_lowering=False)
    D = lambda n, s: nc.dram_tensor(n, s, F32, kind="ExternalInput").ap()
    p64d = D("p64", [4, 64, TSEQ]); p128d = D("p128", [2, 128, TSEQ]); mu64d = D("mu64", [4, 64, 8]); mu128d = D("mu128", [2, 128, 8])
    pvd = D("pv", [64, 8]); a2d = D("a2h", [64, 64]); g2d = D("g2h", [128, 64]); w2d = D("w2pad", [2, 128, 64])
    cd = D("consts", [len(C64_NAMES), 128, 128])
    yd = nc.dram_tensor("y", [64, TSEQ], F32, kind="ExternalOutput").ap()
    P = Prog(nc)
    C = load_consts64(P, cd)
    bkt = [P.ps(f"bank{i}", [128, 512]) for i in range(8)]
    BK = lambda i: (bkt[i], f"bk{i}")
    raw = P.sb("raw", [128, TSEQ]); mix = P.sb("mix", [128, TSEQ])
    mu64 = P.sb("mu64", [64, 4, 8]); mu128 = P.sb("mu128", [128, 2, 8]); pv = P.sb("pv", [64, 8])
    for i in range(4):
        P.dma("sp", mu64[:, i, :], mu64d[i], writes=["mu64"])
    for i in range(2):
        P.dma("sp", mu128[:, i, :], mu128d[i], writes=["mu128"])
    P.dma("sp", pv[:], pvd, writes=["pv"])
    a2h = P.sb("a2h", [64, 64]); g2h = P.sb("g2h", [128, 64]); w2p = P.sb("w2p", [128, 2, 64])
    P.dma("sp", a2h[:], a2d, writes=["a2h"]); P.dma("sp", g2h[:], g2d, writes=["g2h"])
    for j in range(2):
        P.dma("sp", w2p[:, j, :], w2d[j], writes=["w2p"])
    omm64 = P.sb("omm64", [64, 4]); omm128 = P.sb("omm128", [128, 2])
    P.I("dve", "tensor_scalar", omm64[:], mu64[:, :, 0], -1.0, 1.0, ALU.mult, ALU.add, r=["mu64"], w=["omm64"])
    P.I("dve", "tensor_scalar", omm128[:], mu128[:, :, 0], -1.0, 1.0, ALU.mult, ALU.add, r=["mu128"], w=["omm128"])

    def token_mix(dst, dk, src_d, npart, mu, muk, omm, ommk, ti):
        R = slice(0, npart)
        P.dma("sp", raw[R, :], src_d, writes=["raw"])
        P.I("dve", "tensor_scalar", dst[R, :], raw[R, :], omm[R, ti:ti + 1], None, ALU.mult, r=["raw", ommk], w=[dk])
        def acc(o0, o1, i0, i1, mcol, eng="dve"):
            P.I(eng, "scalar_tensor_tensor", dst[R, o0:o1], raw[R, i0:i1], mu[R, ti, mcol:mcol + 1], dst[R, o0:o1], ALU.mult, ALU.add,
                r=["raw", muk, dk], w=[dk])
        acc(1, 256, 0, 255, 5); acc(0, 255, 1, 256, 6)
        acc(256 + 64, TSEQ, 256, TSEQ - 64, 3); acc(256, TSEQ - 64, 256 + 64, TSEQ, 4)
        dl = dst[R, 256:TSEQ].rearrange("p (r c) -> p r c", c=64); rl = raw[R, 256:TSEQ].rearrange("p (r c) -> p r c", c=64)
        P.I("dve", "scalar_tensor_tensor", dl[:, :, 1:64], rl[:, :, 0:63], mu[R, ti, 1:2], dl[:, :, 1:64], ALU.mult, ALU.add, r=["raw", muk, dk], w=[dk])
        P.I("dve", "scalar_tensor_tensor", dl[:, :, 0:63], rl[:, :, 1:64], mu[R, ti, 2:3], dl[:, :, 0:63], ALU.mult, ALU.add, r=["raw", muk, dk], w=[dk])

    rT = P.sb("rT", [64, TSEQ]); kT = P.sb("kT", [64, TSEQ]); vT = P.sb("vT", [64, TSEQ]); aT = P.sb("aT", [64, TSEQ])
    gT = P.sb("gT", [64, TSEQ]); bT = P.sb("bT", [64, TSEQ]); lwT = [P.sb(f"lwT{j}", [64, TSEQ]) for j in range(2)]
    token_mix(rT, "rT", p64d[0], 64, mu64, "mu64", omm64, "omm64", 0)
    token_mix(kT, "kT", p64d[1], 64, mu64, "mu64", omm64, "omm64", 1)
    token_mix(vT, "vT", p64d[2], 64, mu64, "mu64", omm64, "omm64", 2)
    NB = 256
    token_mix(mix, "mix", p64d[3], 64, mu64, "mu64", omm64, "omm64", 3)
    for i, t0 in enumerate(range(0, TSEQ, NB)):
        (pa, pk) = BK(i % 2)
        P.I("pe", "matmul", pa[0:64, 0:NB], a2h[:], mix[0:64, t0:t0 + NB], start=True, stop=True, r=["a2h", "mix"], w=[pk])
        P.I("act", "activation", aT[:, t0:t0 + NB], pa[0:64, 0:NB], AF.Sigmoid, bias=pv[:, 0:1], r=[pk, "pv"], w=["aT"])
    token_mix(mix, "mix", p128d[0], 128, mu128, "mu128", omm128, "omm128", 0)
    P.I("act", "activation", mix[:], mix[:], AF.Tanh, r=["mix"], w=["mix"])
    for j in range(2):
        for i, t0 in enumerate(range(0, TSEQ, NB)):
            (pa, pk) = BK(i % 2)
            P.I("pe", "matmul", pa[0:64, 0:NB], w2p[:, j, :], mix[:, t0:t0 + NB], start=True, stop=True, r=["w2p", "mix"], w=[pk])
            P.I("act", "activation", lwT[j][:, t0:t0 + NB], pa[0:64, 0:NB], AF.Sigmoid, bias=pv[:, 3 + j:4 + j], r=[pk, "pv"], w=[f"lwT{j}"])
        P.I("dve", "tensor_scalar", lwT[j][:], lwT[j][:], -float(np.exp(-0.5)), None, ALU.mult, r=[f"lwT{j}"], w=[f"lwT{j}"])
    token_mix(mix, "mix", p128d[1], 128, mu128, "mu128", omm128, "omm128", 1)
    P.I("act", "activation", mix[:], mix[:], AF.Sigmoid, r=["mix"], w=["mix"])
    for i, t0 in enumerate(range(0, TSEQ, NB)):
        (pa, pk) = BK(i % 2)
        P.I("pe", "matmul", pa[0:64, 0:NB], g2h[:], mix[:, t0:t0 + NB], start=True, stop=True, r=["g2h", "mix"], w=[pk])
        P.I("act", "activation", gT[:, t0:t0 + NB], pa[0:64, 0:NB], AF.Copy, r=[pk], w=["gT"])
    kk = mix
    P.I("dve", "tensor_scalar", kk[0:64, :], kT[:], pv[:, 1:2], None, ALU.mult, r=["kT", "pv"], w=["mix"])
    P.I("act", "activation", raw[0:64, :], kk[0:64, :], AF.Square, r=["mix"], w=["raw"])
    rs = [P.sb(f"rs{i}", [64, NB]) for i in range(2)]
    for i, t0 in enumerate(range(0, TSEQ, NB)):
        b = i % 2; (pa, pk) = BK(b)
        P.I("pe", "matmul", pa[0:64, 0:NB], C["ones"][0:64, 0:64], raw[0:64, t0:t0 + NB], start=True, stop=True, r=["raw", "c_ones"], w=[pk])
        P.I("dve", "tensor_scalar", rs[b][:], pa[0:64, 0:NB], 1e-6, None, ALU.add, r=[pk], w=[f"rs{b}"])
        P.I("dve", "reciprocal", rs[b][:], rs[b][:], r=[f"rs{b}"], w=[f"rs{b}"])
        P.I("act", "activation", rs[b][:], rs[b][:], AF.Sqrt, r=[f"rs{b}"], w=[f"rs{b}"])
        P.I("dve", "tensor_tensor", kk[0:64, t0:t0 + NB], kk[0:64, t0:t0 + NB], rs[b][:], ALU.mult, r=["mix", f"rs{b}"], w=["mix"])
    P.I("dve", "tensor_tensor", bT[:], kk[0:64, :], aT[:], ALU.mult, r=["mix", "aT"], w=["bT"])
    P.I("dve", "tensor_scalar", kk[0:64, :], kk[0:64, :], -1.0, None, ALU.mult, r=["mix"], w=["mix"])
    P.I("dve", "tensor_scalar", aT[:], aT[:], -1.0, pv[:, 2:3], ALU.add, ALU.mult, r=["aT", "pv"], w=["aT"])
    P.I("dve", "scalar_tensor_tensor", kT[:], aT[:], 1.0, kT[:], ALU.add, ALU.mult, r=["aT", "kT"], w=["kT"])
    avT = kk
    oacc = P.sb("oacc", [128, NPK, 64])
    H = P.sb("H", [64, 64])
    T_ = lambda n, sh: P.sb(n, sh)
    lwtok = T_("lwtok", [128, 64]); ea_tok = T_("ea_tok", [128, 64]); te_tok = T_("te_tok", [128, 64])
    ep = T_("ep", [64, 128]); em = T_("em", [64, 128]); eaT = T_("eaT", [64, 128])
    atl = T_("atl", [64, 128]); btl = T_("btl", [64, 128]); ktl = T_("ktl", [64, 128]); rtl = T_("rtl", [64, 128])
    Lm = T_("rwL", [128, 128]); AakT = T_("AakT", [128, 128]); ArbT = T_("ArbT", [128, 128]); ArkT = T_("ArkT", [128, 128])
    X = T_("rwX", [128, 128]); Bh = T_("Bh", [128, 64]); Kh = T_("Kh", [128, 64]); W1T = T_("W1T", [64, 128]); U = T_("U", [128, 64])
    pc2 = T_("pc2", [64, 2])
    tk = {n: T_(n + "_t", [128, 64]) for n in ("av", "b", "k", "v")}
    bkinv = {"P": BK(0), "Q": BK(1), "T": BK(2), "X": BK(3)}
    for d in range(2):
        sfx = "_f" if d == 0 else "_b"
        tri = C["tri" + sfx]; trik = "c_tri" + sfx
        m_strict_ts = C["sl"] if d == 0 else C["su"]; mk_ts = "c_sl" if d == 0 else "c_su"
        m_strict_st = C["su"] if d == 0 else C["sl"]; mk_st = "c_su" if d == 0 else "c_sl"
        m_incl_st = C["tri_f"] if d == 0 else C["tri_b"]; mk_in = trik
        order = list(range(NPK)) if d == 0 else [1, 0] + list(range(NPK - 1, 1, -1))
        P.I("pool", "memset", H[:], 0.0, w=["H"])
        for c in order:
            sl = slice(c * 128, (c + 1) * 128)
            (p0, k0), (p1, k1), (p2, k2), (p3, k3), (p4, k4), (p5, k5), (p6, k6), (p7, k7) = [BK(i) for i in range(8)]
            lw = lwT[d]; lwk = f"lwT{d}"
            for ii, (n, src, sk) in enumerate((("av", avT, "mix"), ("b", bT, "bT"), ("k", kT, "kT"), ("v", vT, "vT"))):
                (pa, pk) = BK(4 + ii)
                P.I("pe", "transpose", pa[:, 0:64], src[0:64, sl], C["ident"][0:64, 0:64], r=[sk, "c_ident"], w=[pk])
                if ii % 2:
                    P.I("act", "activation", tk[n][:], pa[:, 0:64], AF.Copy, r=[pk], w=[n + "_t"])
                else:
                    P.I("dve", "tensor_copy", tk[n][:], pa[:, 0:64], r=[pk], w=[n + "_t"])
            P.I("pe", "transpose", p0[:, 0:64], lw[:, sl], C["ident"][0:64, 0:64], r=[lwk, "c_ident"], w=[k0])
            P.I("dve", "tensor_copy", lwtok[:], p0[:, 0:64], r=[k0], w=["lwtok"])
            P.I("pe", "matmul", p1[:, 0:64], tri[:], lwtok[:], start=True, stop=True, r=[trik, "lwtok"], w=[k1])
            P.I("pe", "matmul", p2[:, 0:64], C["blk"][:], lwtok[:], start=True, stop=True, r=["c_blk", "lwtok"], w=[k2])
            P.I("pe", "matmul", p3[0:64, 0:128], lwtok[:], tri[:], start=True, stop=True, r=[trik, "lwtok"], w=[k3])
            P.I("dve", "tensor_tensor", ea_tok[:], p1[:, 0:64], lwtok[:], ALU.subtract, r=[k1, "lwtok"], w=["ea_tok"])
            P.I("act", "activation", ea_tok[:], ea_tok[:], AF.Exp, r=["ea_tok"], w=["ea_tok"])
            P.I("dve", "tensor_copy", te_tok[:], p1[:, 0:64], r=[k1], w=["te_tok"])
            P.I("dve", "tensor_tensor", te_tok[:], p2[:, 0:64], te_tok[:], ALU.subtract, r=[k2, "te_tok"], w=["te_tok"])
            P.I("act", "activation", te_tok[:], te_tok[:], AF.Exp, r=["te_tok"], w=["te_tok"])
            P.I("act", "activation", ep[:], p3[0:64, 0:128], AF.Exp, r=[k3], w=["ep"])
            P.I("act", "activation", em[:], p3[0:64, 0:128], AF.Exp, scale=-1.0, r=[k3], w=["em"])
            P.I("dve", "tensor_tensor", eaT[:], p3[0:64, 0:128], lw[:, sl], ALU.subtract, r=[k3, lwk], w=["eaT"])
            P.I("act", "activation", eaT[:], eaT[:], AF.Exp, r=["eaT"], w=["eaT"])
            cA, cB = (63, 127) if d == 0 else (0, 64)
            P.I("act", "activation", pc2[:, 0:1], p3[0:64, cA:cA + 1], AF.Exp, r=[k3], w=["pc2"])
            P.I("act", "activation", pc2[:, 1:2], p3[0:64, cB:cB + 1], AF.Exp, r=[k3], w=["pc2"])
            P.I("dve", "tensor_tensor", atl[:], avT[0:64, sl], eaT[:], ALU.mult, r=["mix", "eaT"], w=["atl"])
            P.I("dve", "tensor_tensor", btl[:], bT[:, sl], em[:], ALU.mult, r=["bT", "em"], w=["btl"])
            P.I("pool", "tensor_tensor", ktl[:], kT[:, sl], em[:], ALU.mult, r=["kT", "em"], w=["ktl"])
            P.I("pool", "tensor_tensor", rtl[:], rT[:, sl], ep[:], ALU.mult, r=["rT", "ep"], w=["rtl"])
            P.I("pe", "matmul", p4[:, 0:128], atl[:], btl[:], start=True, stop=True, r=["atl", "btl"], w=[k4])
            P.I("pe", "matmul", p5[:, 0:128], ktl[:], atl[:], start=True, stop=True, r=["ktl", "atl"], w=[k5])
            P.I("pe", "matmul", p6[:, 0:128], btl[:], rtl[:], start=True, stop=True, r=["btl", "rtl"], w=[k6])
            P.I("pe", "matmul", p7[:, 0:128], ktl[:], rtl[:], start=True, stop=True, r=["ktl", "rtl"], w=[k7])
            P.I("dve", "scalar_tensor_tensor", Lm[:], p4[:, 0:128], -1.0, m_strict_ts[:], ALU.mult, ALU.mult, r=[k4, mk_ts], w=["rwL"])
            P.I("dve", "tensor_tensor", AakT[:], p5[:, 0:128], m_strict_st[:], ALU.mult, r=[k5, mk_st], w=["AakT"])
            P.I("dve", "tensor_tensor", ArbT[:], p6[:, 0:128], m_incl_st[:], ALU.mult, r=[k6, mk_in], w=["ArbT"])
            P.I("dve", "tensor_tensor", ArkT[:], p7[:, 0:128], m_incl_st[:], ALU.mult, r=[k7, mk_in], w=["ArkT"])
            P.I("dve", "tensor_tensor", X[:, 0:64], tk["av"][:], ea_tok[:], ALU.mult, r=["av_t", "ea_tok"], w=["rwX"])
            P.I("pe", "matmul", p4[:, 0:64], AakT[:], tk["v"][:], start=True, stop=True, r=["AakT", "v_t"], w=[k4])
            P.I("act", "activation", X[:, 64:128], p4[:, 0:64], AF.Copy, r=[k4], w=["rwX"])
            P.I("pool", "tensor_tensor", Bh[:], tk["b"][:], te_tok[:], ALU.mult, r=["b_t", "te_tok"], w=["Bh"])
            P.I("pool", "tensor_tensor", Kh[:], tk["k"][:], te_tok[:], ALU.mult, r=["k_t", "te_tok"], w=["Kh"])
            tri_inverse_apply(P, C, Lm, X, 128, bkinv, "rw")
            P.I("pe", "transpose", p2[0:64, 0:128], X[:, 0:64], C["ident"][:], r=["rwX", "c_ident"], w=[k2])
            P.I("act", "activation", W1T[:], p2[0:64, 0:128], AF.Copy, r=[k2], w=["W1T"])
            for half in ((0, 1) if d == 0 else (1, 0)):
                rows = slice(half * 64, (half + 1) * 64)
                P.I("pe", "matmul", p5[:, 0:64], W1T[:], H[:], start=True, stop=True, r=["W1T", "H"], w=[k5])
                P.I("dve", "tensor_tensor", U[rows, :], X[rows, 64:128], p5[rows, 0:64], ALU.add, r=["rwX", k5], w=["U"])
                P.I("pe", "matmul", p6[:, 0:64], rtl[:], H[:], start=True, stop=False, r=["rtl", "H"], w=[k6])
                P.I("pe", "matmul", p6[:, 0:64], ArbT[rows, :], U[rows, :], start=False, stop=False, r=["ArbT", "U"], w=[k6])
                P.I("pe", "matmul", p6[:, 0:64], ArkT[rows, :], tk["v"][rows, :], start=False, stop=True, r=["ArkT", "v_t"], w=[k6])
                if d == 0:
                    P.I("act", "activation", oacc[rows, c, :], p6[rows, 0:64], AF.Copy, r=[k6], w=[f"oacc{c}"])
                else:
                    P.I("dve", "tensor_tensor", oacc[rows, c, :], oacc[rows, c, :], p6[rows, 0:64], ALU.add, r=[k6, f"oacc{c}"], w=[f"oacc{c}"])
                P.I("pe", "matmul", p7[0:64, 0:64], Bh[rows, :], U[rows, :], start=True, stop=False, r=["Bh", "U"], w=[k7])
                P.I("pe", "matmul", p7[0:64, 0:64], Kh[rows, :], tk["v"][rows, :], start=False, stop=True, r=["Kh", "v_t"], w=[k7])
                P.I("dve", "scalar_tensor_tensor", H[:], H[:], pc2[:, half:half + 1], p7[0:64, 0:64], ALU.mult, ALU.add, r=["H", "pc2", k7], w=["H"])
    allo = [f"oacc{c}" for c in range(NPK)]
    yT = raw; t1 = aT; t2 = bT
    for c in range(NPK):
        (pa, pk) = BK(c % 4)
        P.I("pe", "transpose", pa[0:64, 0:128], oacc[:, c, :], C["ident"][:], r=allo + ["c_ident"], w=[pk])
        P.I("act" if c % 2 else "dve", "activation" if c % 2 else "tensor_copy", yT[0:64, c * 128:(c + 1) * 128], pa[0:64, 0:128], *([AF.Copy] if c % 2 else []),
            r=[pk], w=["raw"])
    on64 = C["ones"][0:64, 0:64]
    for i, t0 in enumerate(range(0, TSEQ, NB)):
        ts = slice(t0, t0 + NB); b = i % 2
        (pm, km), (pvv, kvv), (pb, kb) = BK(b * 3), BK(b * 3 + 1), BK(b * 3 + 2)
        P.I("pe", "matmul", pm[0:64, 0:NB], on64, yT[0:64, ts], start=True, stop=True, r=["raw", "c_ones"], w=[km])
        P.I("dve", "scalar_tensor_tensor", yT[0:64, ts], pm[0:64, 0:NB], -1.0 / 64, yT[0:64, ts], ALU.mult, ALU.add, r=[km, "raw"], w=["raw"])
        P.I("act", "activation", t1[:, ts], yT[0:64, ts], AF.Square, r=["raw"], w=["aT"])
        P.I("pe", "matmul", pvv[0:64, 0:NB], on64, t1[:, ts], start=True, stop=True, r=["aT", "c_ones"], w=[kvv])
        P.I("dve", "tensor_scalar", rs[b][:], pvv[0:64, 0:NB], 1.0 / 64, 64e-5, ALU.mult, ALU.add, r=[kvv], w=[f"rs{b}"])
        P.I("dve", "reciprocal", rs[b][:], rs[b][:], r=[f"rs{b}"], w=[f"rs{b}"])
        P.I("act", "activation", rs[b][:], rs[b][:], AF.Sqrt, r=[f"rs{b}"], w=[f"rs{b}"])
        P.I("dve", "scalar_tensor_tensor", yT[0:64, ts], yT[0:64, ts], pv[:, 5:6], rs[b][:], ALU.mult, ALU.mult, r=["raw", "pv", f"rs{b}"], w=["raw"])
        P.I("dve", "scalar_tensor_tensor", t2[:, ts], rT[:, ts], pv[:, 7:8], kT[:, ts], ALU.mult, ALU.mult, r=["rT", "pv", "kT"], w=["bT"])
        P.I("pe", "matmul", pb[0:64, 0:NB], on64, t2[:, ts], start=True, stop=True, r=["bT", "c_ones"], w=[kb])
        P.I("dve", "tensor_tensor", t2[:, ts], pb[0:64, 0:NB], vT[:, ts], ALU.mult, r=[kb, "vT"], w=["bT"])
        P.I("dve", "scalar_tensor_tensor", yT[0:64, ts], yT[0:64, ts], pv[:, 6:7], t2[:, ts], ALU.add, ALU.add, r=["raw", "pv", "bT"], w=["raw"])
        P.I("dve", "tensor_tensor", yT[0:64, ts], yT[0:64, ts], gT[:, ts], ALU.mult, r=["raw", "gT"], w=["raw"])
    P.dma("sp", yd, yT[0:64, :], reads=["raw"], is_output=True)
    P.finish()
    return nc


def host_B3_inputs(pc_, L, b, head, prm):
    p = pc_[b]
    hc = slice(head * 64, (head + 1) * 64)
    mu = prm['rw_mu'][L]
    def seg(o, n):
        cols = np.arange(o, o + n); m = mu[cols]; cl = cols % 4
        tab = np.zeros((n, 8), np.float32)
        tab[:, 0] = m
        for j in range(4):
            tab[:, 1 + j] = np.where(cl == j, m, 0.0)
        tab[:, 5] = np.where(cl % 2 == 0, m, 0.0); tab[:, 6] = np.where(cl % 2 == 1, m, 0.0)
        return p[:, cols].T, tab
    s64 = [seg(head * 64, 64), seg(256 + head * 64, 64), seg(512 + head * 64, 64), seg(896, 64)]
    s128 = [seg(768, 128), seg(960, 128)]
    pv = np.zeros((64, 8), np.float32)
    pv[:, 0] = prm['rw_a0'][L][hc]; pv[:, 1] = prm['rw_k_k'][L][hc]; pv[:, 2] = prm['rw_k_a'][L][hc]
    pv[:, 3] = prm['rw_w0'][L][0][hc]; pv[:, 4] = prm['rw_w0'][L][1][hc]
    pv[:, 5] = prm['rw_ln_w'][L][hc]; pv[:, 6] = prm['rw_ln_b'][L][hc]; pv[:, 7] = prm['rw_r_k'][L][head]
    w2pad = np.zeros((2, 128, 64), np.float32)
    for j in range(2):
        w2pad[j, j * 64:(j + 1) * 64] = prm['rw_w2'][L][j][:, hc]
    A = lambda a: np.ascontiguousarray(a, dtype=np.float32)
    return {"p64": A(np.stack([s[0] for s in s64])), "p128": A(np.stack([s[0] for s in s128])),
            "mu64": A(np.stack([s[1] for s in s64])), "mu128": A(np.stack([s[1] for s in s128])),
            "pv": pv, "a2h": A(prm['rw_a2'][L][:, hc]), "g2h": A(prm['rw_g2'][L][:, hc]), "w2pad": w2pad,
            "consts": host_consts64()}


NTT = 17


def build_C1():
    nc = bass.Bass("TRN2", target_bir_lowering=False)
    D = lambda n, s: nc.dram_tensor(n, s, F32, kind="ExternalInput").ap()
    xTd = D("xT", [128, 8, NTOK]); yTd = D("yT", [128, 8, NTOK]); woutd = D("wout", [128, 8, 1024])
    modd = D("mod", [128, 8, 6]); nwd = D("nw", [128, 8]); wrd = D("wr", [128, 8, 32]); brd = D("br", [128, 32])
    O = lambda n, s: nc.dram_tensor(n, s, F32, kind="ExternalOutput").ap()
    xmd = O("xmT", [128, 8, NTOK]); h2d = O("h2T", [128, 8, NTOK]); Gd = O("G", [128, NTT, 32])
    P = Prog(nc)
    xT = P.sb("xT", [128, 8, NTOK]); ybf = P.sb("ybf", [128, 8, NTOK], BF16)
    hT32 = xT
    mod = P.sb("mod", [128, 8, 6]); nw = P.sb("nw", [128, 8]); wbf = P.sb("wbf", [128, 8, 1024], BF16)
    wr = P.sb("wr", [128, 8, 32]); br = P.sb("br", [128, 32])
    ones_bf = P.sb("ones_bf", [128, 128], BF16)
    P.I("pool", "memset", ones_bf[:], 1.0, w=["ones_bf"])
    for k in range(8):
        P.dma("sp", xT[:, k, :], xTd[:, k, :], writes=["xT"])
        P.dma("pool", ybf[:, k, :], yTd[:, k, :], writes=["ybf"])
        P.dma("pool", wbf[:, k, :], woutd[:, k, :], writes=["wbf"])
    for t, d, kk in ((mod, modd, "mod"), (nw, nwd, "nw"), (wr, wrd, "wr"), (br, brd, "br")):
        P.dma("sp", t[:], d, writes=[kk])
    pp = [P.ps(f"bank{i}", [128, 512]) for i in range(8)]
    i = 0
    for m in range(8):
        for it, (t0, n) in enumerate(TILES):
            b = i % 4; i += 1
            for k in range(8):
                P.I("pe", "matmul", pp[b][:, 0:n], wbf[:, k, m * 128:(m + 1) * 128], ybf[:, k, t0:t0 + n], start=(k == 0), stop=(k == 7),
                    r=["wbf", "ybf"], w=[f"bk{b}"])
            gcol = 3 if it == 0 else 0
            P.I("dve", "scalar_tensor_tensor", xT[:, m, t0:t0 + n], pp[b][:, 0:n], mod[:, m, gcol:gcol + 1], xT[:, m, t0:t0 + n], ALU.mult, ALU.add,
                r=[f"bk{b}", "mod", "xT"], w=["xT"])
    for k in range(8):
        P.dma("sp", xmd[:, k, :], xT[:, k, :], reads=["xT"], is_output=True)
    rms_modulate(P, xT, xT, mod, nw, ones_bf, shift_i=(1, 4), scale_i=(2, 5), tagp="n2", psb=(pp[4], pp[5]), hkey="xT")
    for k in range(8):
        P.dma("sp", h2d[:, k, :], hT32[:, k, :], reads=["xT"], is_output=True)
    G = P.sb("G", [128, NTT, 32]); lg = P.sb("lg", [128, NTT, 32]); m8 = P.sb("m8", [128, NTT, 8]); nmx = P.sb("nmx", [128, NTT])
    msk = P.sb("msk", [128, NTT, 32]); ssum = P.sb("ssum", [128, NTT])
    for tt in range(NTT):
        b = 6 + tt % 2
        for k in range(8):
            P.I("pe", "matmul", pp[b][:, 0:32], hT32[:, k, tt * 128:(tt + 1) * 128], wr[:, k, :], start=(k == 0), stop=(k == 7),
                r=["xT", "wr"], w=[f"bk{b}"])
        P.I("dve", "tensor_tensor", lg[:, tt, :], pp[b][:, 0:32], br[:], ALU.add, r=[f"bk{b}", "br"], w=["lg"])
        P.I("dve", "max", m8[:, tt, :], lg[:, tt, :], r=["lg"], w=["m8"])
        P.I("dve", "tensor_scalar", msk[:, tt, :], lg[:, tt, :], m8[:, tt, 3:4], None, ALU.is_ge, r=["lg", "m8"], w=["msk"])
        P.I("dve", "tensor_scalar", nmx[:, tt:tt + 1], m8[:, tt, 0:1], -1.0, None, ALU.mult, r=["m8"], w=["nmx"])
        P.I("act", "activation", G[:, tt, :], lg[:, tt, :], AF.Exp, bias=nmx[:, tt:tt + 1], r=["lg", "nmx"], w=["G"])
        P.I("dve", "tensor_tensor", G[:, tt, :], G[:, tt, :], msk[:, tt, :], ALU.mult, r=["G", "msk"], w=["G"])
        P.I("dve", "tensor_reduce", ssum[:, tt:tt + 1], G[:, tt, :], AX.X, ALU.add, r=["G"], w=["ssum"])
        P.I("dve", "reciprocal", ssum[:, tt:tt + 1], ssum[:, tt:tt + 1], r=["ssum"], w=["ssum"])
        P.I("dve", "tensor_scalar", G[:, tt, :], G[:, tt, :], ssum[:, tt:tt + 1], None, ALU.mult, r=["G", "ssum"], w=["G"])
    P.dma("sp", Gd, G[:], reads=["G"], is_output=True)
    P.finish()
    return nc


NT2 = 1088
T2 = [(0, 512), (512, 512), (1024, 64)]
ST2 = [(i * 128, 128) for i in range(8)] + [(1024, 64)]


def build_C2(NB=16, NE=4):
    nc = bass.Bass("TRN2", target_bir_lowering=False)
    D = lambda n, s: nc.dram_tensor(n, s, F32, kind="ExternalInput").ap()
    h2d = D("h2T", [128, 8, NB * NT2]); Gd = D("G", [128, NB, 9, NE]); wgud = D("wgu", [NE, 128, 8, 2048]); wdd = D("wd", [NE, 128, 8, 1024])
    bgud = D("bgu", [128, NE, 16]); bdd = D("bd", [NE, 1024]); idd = D("ident", [128, 128])
    fd = nc.dram_tensor("f", [NB, 128, 9, 1024], F32, kind="ExternalOutput").ap()
    P = Prog(nc)
    hbf = [P.sb(f"hbf{i}", [128, 8, NT2], BF16) for i in range(2)]
    G = P.sb("G", [128, NB, 9, NE]); bgu = P.sb("bgu", [128, NE, 16]); bd = P.sb("bd", [NE, 1024]); ident = P.sb("ident", [128, 128])
    P.dma("sp", G[:], Gd, writes=["G"]); P.dma("sp", bgu[:], bgud, writes=["bgu"]); P.dma("sp", bd[:], bdd, writes=["bd"]); P.dma("sp", ident[:], idd, writes=["ident"])
    wgu = [P.sb(f"wgu{i}", [128, 8, 2048], BF16) for i in range(2)]; wd = [P.sb(f"wd{i}", [128, 8, 1024], BF16) for i in range(2)]
    act = P.sb("act", [128, 8, NT2], BF16); acc = P.sb("acc", [128, 9, 1024])
    gc_ = [P.sb(f"gc{i}", [128, 512]) for i in range(2)]; sg = [P.sb(f"sg{i}", [128, 512]) for i in range(2)]
    uc = [P.sb(f"uc{i}", [128, 512]) for i in range(2)]
    GT = P.sb("GT", [NE, 128])
    pp = [P.ps(f"bank{i}", [128, 512]) for i in range(8)]

    wgus = [nc.dram_tensor(f"wgu_bf{e}", [128, 8, 2048], BF16).ap() for e in range(NE)]
    wds = [nc.dram_tensor(f"wd_bf{e}", [128, 8, 1024], BF16).ap() for e in range(NE)]
    for e in range(NE):
        for k in range(8):
            P.dma("pool", wgus[e][:, k, :], wgud[e, :, k, :], writes=[f"wgus{e}"])
            P.dma("pool", wds[e][:, k, :], wdd[e, :, k, :], writes=[f"wds{e}"])

    def load_w(j):
        e = j % NE; b = j % 2
        for k in range(0, 8, 2):
            P.dma("sp", wgu[b][:, k:k + 2, :], wgus[e][:, k:k + 2, :], reads=[f"wgus{e}"], writes=[f"wgu{b}"])
        for k in range(0, 8, 4):
            P.dma("act", wd[b][:, k:k + 4, :], wds[e][:, k:k + 4, :], reads=[f"wds{e}"], writes=[f"wd{b}"])

    def load_h(blk):
        for k in range(8):
            P.dma("pool", hbf[blk % 2][:, k, :], h2d[:, k, blk * NT2:(blk + 1) * NT2], writes=[f"hbf{blk%2}"])
    load_h(0); load_w(0)
    it = 0; jt = 0; j = 0
    for blk in range(NB):
        hb = hbf[blk % 2]; hk = f"hbf{blk%2}"
        if blk + 1 < NB:
            load_h(blk + 1)
        for e in range(NE):
            b = j % 2
            if j + 1 < NB * NE:
                load_w(j + 1)
            j += 1
            for fc in range(8):
                for (t0, n) in T2:
                    s = it % 2; it += 1
                    pg, pu = pp[2 * s], pp[2 * s + 1]; kg, ku = f"bk{2*s}", f"bk{2*s+1}"
                    for k in range(8):
                        P.I("pe", "matmul", pg[:, 0:n], wgu[b][:, k, fc * 128:(fc + 1) * 128], hb[:, k, t0:t0 + n], start=(k == 0), stop=(k == 7),
                            r=[f"wgu{b}", hk], w=[kg])
                    for k in range(8):
                        P.I("pe", "matmul", pu[:, 0:n], wgu[b][:, k, 1024 + fc * 128:1024 + (fc + 1) * 128], hb[:, k, t0:t0 + n], start=(k == 0), stop=(k == 7),
                            r=[f"wgu{b}", hk], w=[ku])
                    P.I("dve", "tensor_scalar", gc_[s][:, 0:n], pg[:, 0:n], bgu[:, e, fc:fc + 1], 7.0, ALU.add, ALU.min, r=[kg, "bgu"], w=[f"gc{s}"])
                    P.I("act", "activation", sg[s][:, 0:n], gc_[s][:, 0:n], AF.Sigmoid, scale=1.702, r=[f"gc{s}"], w=[f"sg{s}"])
                    P.I("dve", "tensor_scalar", uc[s][:, 0:n], pu[:, 0:n], bgu[:, e, 8 + fc:9 + fc], 7.0, ALU.add, ALU.min, r=[ku, "bgu"], w=[f"uc{s}"])
                    P.I("dve", "tensor_scalar", uc[s][:, 0:n], uc[s][:, 0:n], -7.0, 1.0, ALU.max, ALU.add, r=[f"uc{s}"], w=[f"uc{s}"])
                    P.I("dve", "tensor_tensor", gc_[s][:, 0:n], gc_[s][:, 0:n], sg[s][:, 0:n], ALU.mult, r=[f"gc{s}", f"sg{s}"], w=[f"gc{s}"])
                    P.I("dve", "tensor_tensor", act[:, fc, t0:t0 + n], gc_[s][:, 0:n], uc[s][:, 0:n], ALU.mult, r=[f"gc{s}", f"uc{s}"], w=["act"])
            for si, (s0, sn) in enumerate(ST2):
                for half in range(2):
                    pb = 4 + jt % 4; jt += 1
                    hs = slice(half * 512, (half + 1) * 512)
                    for fc in range(8):
                        P.I("pe", "matmul", pp[pb][0:sn, 0:512], act[:, fc, s0:s0 + sn], wd[b][:, fc, hs], start=(fc == 0), stop=(fc == 7),
                            r=["act", f"wd{b}"], w=[f"bk{pb}"])
                    if e == 0:
                        P.I("dve", "tensor_scalar", acc[0:sn, si, hs], pp[pb][0:sn, 0:512], G[0:sn, blk, si, e:e + 1], None, ALU.mult,
                            r=[f"bk{pb}", "G"], w=["acc"])
                    else:
                        P.I("dve", "scalar_tensor_tensor", acc[0:sn, si, hs], pp[pb][0:sn, 0:512], G[0:sn, blk, si, e:e + 1], acc[0:sn, si, hs],
                            ALU.mult, ALU.add, r=[f"bk{pb}", "G", "acc"], w=["acc"])
        for si, (s0, sn) in enumerate(ST2):
            P.I("pe", "transpose", pp[0][0:NE, 0:sn], G[0:sn, blk, si, :], ident[0:sn, 0:sn], r=["G", "ident"], w=["bk0"])
            P.I("dve", "tensor_copy", GT[:, 0:sn], pp[0][0:NE, 0:sn], r=["bk0"], w=["GT"])
            for half in range(2):
                pb = 1 + half; hs = slice(half * 512, (half + 1) * 512)
                P.I("pe", "matmul", pp[pb][0:sn, 0:512], GT[:, 0:sn], bd[:, hs], start=True, stop=True, r=["GT", "bd"], w=[f"bk{pb}"])
                P.I("dve", "tensor_tensor", acc[0:sn, si, hs], acc[0:sn, si, hs], pp[pb][0:sn, 0:512], ALU.add, r=[f"bk{pb}", "acc"], w=["acc"])
        P.I("pool", "memset", acc[64:128, 8, :], 0.0, r=["acc"], w=["acc"]) if blk == 0 else None
        P.dma("sp", fd[blk], acc[:], reads=["acc"], is_output=True)
    P.finish()
    return nc


def build_D():
    nc = bass.Bass("TRN2", target_bir_lowering=False)
    D = lambda n, s: nc.dram_tensor(n, s, F32, kind="ExternalInput").ap()
    xmd = D("xmT", [128, 8, NTOK]); fTd = D("fT", [8, 128, 8, NTOK]); modd = D("mod", [128, 8, 2]); nwd = D("nw", [128, 8])
    od = nc.dram_tensor("oT", [128, 8, NTOK], F32, kind="ExternalOutput").ap()
    P = Prog(nc)
    xT = P.sb("xT", [128, 8, NTOK]); fT = P.sb("fT", [128, 8, NTOK]); mod = P.sb("mod", [128, 8, 2]); nw = P.sb("nw", [128, 8])
    ones_bf = P.sb("ones_bf", [128, 128], BF16)
    P.I("pool", "memset", ones_bf[:], 1.0, w=["ones_bf"])
    for k in range(8):
        P.dma("sp", xT[:, k, :], xmd[:, k, :], writes=["xT"])
    P.dma("sp", mod[:], modd, writes=["mod"]); P.dma("sp", nw[:], nwd, writes=["nw"])
    for c in range(8):
        for k in range(8):
            P.dma("sp", fT[:, k, :], fTd[c, :, k, :], writes=["fT"])
        add_gated(P, xT, fT, mod, 0, 1)
    sq = [P.sb(f"sq{i}", [128, 8, 512], BF16) for i in range(2)]; rs = [P.sb(f"rs{i}", [128, 512]) for i in range(2)]
    ss = [P.ps(f"bank{i}", [128, 512]) for i in range(2)]
    for it, (t0, n) in enumerate(TILES):
        b = it % 2
        for k in range(8):
            P.I("act", "activation", sq[b][:, k, 0:n], xT[:, k, t0:t0 + n], AF.Square, r=["xT"], w=[f"sq{b}"])
        for k in range(8):
            P.I("pe", "matmul", ss[b][:, 0:n], ones_bf[:], sq[b][:, k, 0:n], start=(k == 0), stop=(k == 7), r=[f"sq{b}", "ones_bf"], w=[f"bk{b}"])
        P.I("dve", "tensor_scalar", rs[b][:, 0:n], ss[b][:, 0:n], 1.0 / 1024, 1e-6, ALU.mult, ALU.add, r=[f"bk{b}"], w=[f"rs{b}"])
        P.I("dve", "reciprocal", rs[b][:, 0:n], rs[b][:, 0:n], r=[f"rs{b}"], w=[f"rs{b}"])
        P.I("act", "activation", rs[b][:, 0:n], rs[b][:, 0:n], AF.Sqrt, r=[f"rs{b}"], w=[f"rs{b}"])
        for k in range(8):
            P.I("dve", "scalar_tensor_tensor", fT[:, k, t0:t0 + n], xT[:, k, t0:t0 + n], nw[:, k:k + 1], rs[b][:, 0:n], ALU.mult, ALU.mult,
                r=["xT", "nw", f"rs{b}"], w=["fT"])
    for k in range(8):
        P.dma("sp", od[:, k, :], fT[:, k, :], reads=["fT"], is_output=True)
    P.finish()
    return nc


def add_gated(P, xT, fT, mod, col_l, col_c):
    for k in range(8):
        P.I("dve", "scalar_tensor_tensor", xT[:, k, 0:128], fT[:, k, 0:128], mod[:, k, col_c:col_c + 1], xT[:, k, 0:128], ALU.mult, ALU.add,
            r=["fT", "mod", "xT"], w=["xT"])
        P.I("dve", "scalar_tensor_tensor", xT[:, k, 128:NTOK], fT[:, k, 128:NTOK], mod[:, k, col_l:col_l + 1], xT[:, k, 128:NTOK], ALU.mult, ALU.add,
            r=["fT", "mod", "xT"], w=["xT"])


I32 = mybir.dt.int32


def build_C2s(NTILE=136, NE=4, CAP=4608, NPASS=4):
    NTOKA = NTILE * 128; NJ = CAP // 128; NJH = NJ // NPASS; HALF = CAP // NPASS
    T3 = [(t0, min(512, HALF - t0)) for t0 in range(0, HALF, 512)]
    nc = bass.Bass("TRN2", target_bir_lowering=False)
    D = lambda n, s: nc.dram_tensor(n, s, F32, kind="ExternalInput").ap()
    h2d = D("h2tok", [NTOKA + 128, 1024]); Gd = D("Gm", [128, NTILE, NE]); tokd = D("tokid", [128, NTILE]); Ld = D("lst", [128, 128])
    padd = D("padtab", [128, NJ, 2]); idd = D("ident", [128, 128]); dumpd = D("dump", [128, 1])
    wgud = D("wgu", [NE, 128, 8, 2048]); wdd = D("wd", [NE, 128, 8, 1024]); bgud = D("bgu", [128, NE, 16]); bdbd = D("bdb", [NE, 128, 1024])
    fd = nc.dram_tensor("fpart", [NTOKA + 128, 1024], F32, kind="ExternalOutput").ap()
    tab = [nc.dram_tensor(f"slot_tab{e}", [CAP + 128, 2], F32).ap() for e in range(NE)]
    P = Prog(nc)
    pp = [P.ps(f"bank{i}", [128, 512]) for i in range(8)]
    z = P.sb("z", [128, 1024]); ones = P.sb("ones", [128, 128]); lst = P.sb("lst", [128, 128]); ident = P.sb("ident", [128, 128])
    P.I("pool", "memset", z[:], 0.0, w=["z"]); P.I("pool", "memset", ones[:], 1.0, w=["ones"])
    P.dma("sp", lst[:], Ld, writes=["lst"]); P.dma("sp", ident[:], idd, writes=["ident"])
    for r in range(NTILE + 1):
        P.dma("sp", fd[r * 128:(r + 1) * 128, :], z[:], reads=["z"], writes=["fpart"])
    Gm = P.sb("Gm", [128, NTILE, NE]); tokid = P.sb("tokid", [128, NTILE]); padt = P.sb("padt", [128, NJ, 2]); bgu = P.sb("bgu", [128, NE, 16])
    P.dma("act", Gm[:], Gd, writes=["Gm"]); P.dma("act", tokid[:], tokd, writes=["tokid"]); P.dma("act", padt[:], padd, writes=["padt"])
    P.dma("act", bgu[:], bgud, writes=["bgu"])
    dump = P.sb("dump", [128, 1]); P.dma("act", dump[:], dumpd, writes=["dump"])
    wgu = [P.sb(f"wgu{i}", [128, 8, 2048], BF16) for i in range(2)]; wd = [P.sb(f"wd{i}", [128, 8, 1024], BF16) for i in range(2)]
    bdb = [P.sb(f"bdb{i}", [128, 1024]) for i in range(2)]
    hsel = P.sb("hsel", [128, 8, HALF], BF16); act = P.sb("act", [128, 8, HALF], BF16)
    hg = [P.sb(f"hg{i}", [128, 1024]) for i in range(2)]; yst = [P.sb(f"yst{i}", [128, 1024]) for i in range(2)]
    gc_ = [P.sb(f"gc{i}", [128, 512]) for i in range(2)]; sg = [P.sb(f"sg{i}", [128, 512]) for i in range(2)]
    uc = [P.sb(f"uc{i}", [128, 512]) for i in range(2)]

    def load_w(e):
        b = e % 2
        for k in range(8):
            P.dma("pool", wgu[b][:, k, :], wgud[e, :, k, :], writes=[f"wgu{b}"])
        for k in range(8):
            P.dma("pool", wd[b][:, k, :], wdd[e, :, k, :], writes=[f"wd{b}"])
        P.dma("act", bdb[b][:], bdbd[e], writes=[f"bdb{b}"])
    load_w(0)
    m = P.sb("m", [128, NE, NTILE]); cs = P.sb("cs", [128, NE, NTILE]); inc = P.sb("inc", [128, NE, NTILE]); rk = P.sb("rk", [128, NE, NTILE])
    idx = P.sb("idx", [128, NE, NTILE], I32); pr = P.sb("pr", [128, NTILE, NE, 2]); onesw = P.sb("onesw", [128, NTILE])
    P.I("pool", "memset", onesw[:], 1.0, w=["onesw"])
    for e in range(NE):
        P.I("dve", "tensor_scalar", m[:, e, :], Gm[:, :, e], 0.0, None, ALU.is_gt, r=["Gm"], w=["m"])
        P.I("pe", "matmul", pp[0][:, 0:NTILE], lst[:], m[:, e, :], start=True, stop=True, r=["lst", "m"], w=["bk0"])
        P.I("pe", "matmul", pp[1][:, 0:NTILE], ones[:], m[:, e, :], start=True, stop=True, r=["ones", "m"], w=["bk1"])
        P.I("act", "activation", cs[:, e, :], pp[1][:, 0:NTILE], AF.Copy, r=["bk1"], w=["cs"])
        P.I("dve", "tensor_tensor_scan", inc[:, e, :], onesw[:], cs[:, e, :], 0.0, ALU.mult, ALU.add, r=["onesw", "cs"], w=["inc"])
        P.I("dve", "tensor_tensor", rk[:, e, :], pp[0][:, 0:NTILE], inc[:, e, :], ALU.add, r=["bk0", "inc"], w=["rk"])
        P.I("dve", "tensor_tensor", rk[:, e, :], rk[:, e, :], cs[:, e, :], ALU.subtract, r=["rk", "cs"], w=["rk"])
        P.I("dve", "tensor_scalar", cs[:, e, :], rk[:, e, :], float(CAP), None, ALU.is_lt, r=["rk", "cs"], w=["cs"])
        P.I("dve", "tensor_tensor", cs[:, e, :], cs[:, e, :], m[:, e, :], ALU.mult, r=["cs", "m"], w=["cs"])
        P.I("dve", "tensor_scalar", rk[:, e, :], rk[:, e, :], dump[:, 0:1], None, ALU.subtract, r=["rk", "dump"], w=["rk"])
        P.I("dve", "tensor_tensor", rk[:, e, :], rk[:, e, :], cs[:, e, :], ALU.mult, r=["rk", "cs"], w=["rk"])
        P.I("dve", "tensor_scalar", rk[:, e, :], rk[:, e, :], dump[:, 0:1], None, ALU.add, r=["rk", "dump"], w=["rk"])
        P.I("dve", "tensor_copy", idx[:, e, :], rk[:, e, :], r=["rk"], w=["idx"])
        P.I("pool", "tensor_copy", pr[:, :, e, 0], tokid[:], r=["tokid"], w=["pr"])
        P.I("pool", "tensor_copy", pr[:, :, e, 1], Gm[:, :, e], r=["Gm"], w=["pr"])
    for e in range(NE):
        P.dma("act", tab[e][0:CAP, :].rearrange("(p j) c -> p j c", j=NJ), padt[:], reads=["padt"], writes=[f"tabinit{e}"])
    for t in range(NTILE):
        for e in range(NE):
            P.idma(tab[e], pr[:, t, e, :], out_idx=idx[:, e, t:t + 1], reads=["pr", "idx", f"tabinit{e}"], writes=[f"sc{e}_{t}"])
    tabsb = P.sb("tabsb", [128, NE, NJ, 2]); tok_i = P.sb("tok_i", [128, NE, NJ], I32); gate = P.sb("gate", [128, NE, NJ])
    for e in range(NE):
        P.dma("act", tabsb[:, e, :, :], tab[e][0:CAP, :].rearrange("(p j) c -> p j c", j=NJ), reads=[f"sc{e}_{t}" for t in range(NTILE)], writes=["tabsb"])
    P.I("dve", "tensor_copy", tok_i[:], tabsb[:, :, :, 0], r=["tabsb"], w=["tok_i"])
    P.I("dve", "tensor_copy", gate[:], tabsb[:, :, :, 1], r=["tabsb"], w=["gate"])
    it = 0; jt = 0; gi = 0; ti = 0
    for e in range(NE):
        b = e % 2
        if e + 1 < NE:
            load_w(e + 1)
        for half in range(NPASS):
            for jj in range(NJH):
                j = half * NJH + jj; g = gi % 2; gi += 1
                P.idma(hg[g][:], h2d, in_idx=tok_i[:, e, j:j + 1], reads=["tok_i"], writes=[f"hg{g}"])
                for k in range(8):
                    pb = 4 + ti % 4; ti += 1
                    P.I("pe", "transpose", pp[pb][:, 0:128], hg[g][:, k * 128:(k + 1) * 128], ident[:], r=[f"hg{g}", "ident"], w=[f"bk{pb}"])
                    if ti % 2:
                        P.I("act", "activation", hsel[:, k, jj * 128:(jj + 1) * 128], pp[pb][:, 0:128], AF.Copy, r=[f"bk{pb}"], w=["hsel"])
                    else:
                        P.I("dve", "tensor_copy", hsel[:, k, jj * 128:(jj + 1) * 128], pp[pb][:, 0:128], r=[f"bk{pb}"], w=["hsel"])
            for fc in range(8):
                for (t0, n) in T3:
                    s = it % 2; it += 1
                    pg, pu = pp[2 * s], pp[2 * s + 1]; kg, ku = f"bk{2*s}", f"bk{2*s+1}"
                    for k in range(8):
                        P.I("pe", "matmul", pg[:, 0:n], wgu[b][:, k, fc * 128:(fc + 1) * 128], hsel[:, k, t0:t0 + n], start=(k == 0), stop=(k == 7),
                            r=[f"wgu{b}", "hsel"], w=[kg])
                    for k in range(8):
                        P.I("pe", "matmul", pu[:, 0:n], wgu[b][:, k, 1024 + fc * 128:1024 + (fc + 1) * 128], hsel[:, k, t0:t0 + n], start=(k == 0), stop=(k == 7),
                            r=[f"wgu{b}", "hsel"], w=[ku])
                    P.I("dve", "tensor_scalar", gc_[s][:, 0:n], pg[:, 0:n], bgu[:, e, fc:fc + 1], 7.0, ALU.add, ALU.min, r=[kg, "bgu"], w=[f"gc{s}"])
                    P.I("act", "activation", sg[s][:, 0:n], gc_[s][:, 0:n], AF.Sigmoid, scale=1.702, r=[f"gc{s}"], w=[f"sg{s}"])
                    P.I("dve", "tensor_scalar", uc[s][:, 0:n], pu[:, 0:n], bgu[:, e, 8 + fc:9 + fc], 7.0, ALU.add, ALU.min, r=[ku, "bgu"], w=[f"uc{s}"])
                    P.I("dve", "tensor_scalar", uc[s][:, 0:n], uc[s][:, 0:n], -7.0, 1.0, ALU.max, ALU.add, r=[f"uc{s}"], w=[f"uc{s}"])
                    P.I("dve", "tensor_tensor", gc_[s][:, 0:n], gc_[s][:, 0:n], sg[s][:, 0:n], ALU.mult, r=[f"gc{s}", f"sg{s}"], w=[f"gc{s}"])
                    P.I("dve", "tensor_tensor", act[:, fc, t0:t0 + n], gc_[s][:, 0:n], uc[s][:, 0:n], ALU.mult, r=[f"gc{s}", f"uc{s}"], w=["act"])
            for jj in range(NJH):
                j = half * NJH + jj; y = jt % 2
                for hh in range(2):
                    pb = 4 + jt % 4; jt += 1
                    hs = slice(hh * 512, (hh + 1) * 512)
                    for fc in range(8):
                        P.I("pe", "matmul", pp[pb][:, 0:512], act[:, fc, jj * 128:(jj + 1) * 128], wd[b][:, fc, hs], start=(fc == 0), stop=(fc == 7),
                            r=["act", f"wd{b}"], w=[f"bk{pb}"])
                    P.I("dve", "tensor_tensor", yst[jj % 2][:, hs], pp[pb][:, 0:512], bdb[b][:, hs], ALU.add, r=[f"bk{pb}", f"bdb{b}"], w=[f"yst{jj%2}"])
                P.I("pool", "tensor_scalar", yst[jj % 2][:], yst[jj % 2][:], gate[:, e, j:j + 1], None, ALU.mult, r=[f"yst{jj%2}", "gate"], w=[f"yst{jj%2}"])
                P.idma(fd, yst[jj % 2][:], out_idx=tok_i[:, e, j:j + 1], reads=[f"yst{jj%2}", "tok_i", "fpart"], writes=["fpart"], is_output=True, compute_op=ALU.add)
    P.finish()
    return nc


def host_C2s_consts(NTILE=136, CAP=4608):
    NJ = CAP // 128
    tokid = (np.arange(NTILE)[None, :] * 128 + np.arange(128)[:, None]).astype(np.float32)
    p_ = np.arange(128)[:, None]; q_ = np.arange(128)[None, :]
    lst = (p_ < q_).astype(np.float32)
    padtab = np.zeros((128, NJ, 2), np.float32); padtab[:, :, 0] = NTILE * 128 + np.arange(128)[:, None]
    return {"tokid": tokid, "lst": lst, "padtab": padtab, "ident": np.eye(128, dtype=np.float32), "dump": (CAP + np.arange(128, dtype=np.float32))[:, None]}


def _fm(tok):
    return np.ascontiguousarray(tok.T.reshape(8, 128, -1).transpose(1, 0, 2))


def _tok(fm):
    return fm.transpose(2, 1, 0).reshape(fm.shape[2], -1)


def _vec(v):
    return np.ascontiguousarray(np.asarray(v, np.float32).reshape(8, 128).T)


_PROGS = {}
C2S_CAP = 4608
MOE_SPARSE = True


def _prog(name, builder, *a):
    key = (name,) + a
    if key not in _PROGS:
        _PROGS[key] = builder(*a)
    return _PROGS[key]


def _run(nc, maps):
    res = run_bass_kernel_spmd(nc, maps, core_ids=list(range(len(maps))))
    return res.results


def kernel(**inp):
    prm = {k: np.asarray(v, dtype=np.float32) for k, v in inp.items()}
    x, c, ctx, c_ctx = prm['x'], prm['c'], prm['ctx'], prm['c_ctx']
    NC = 8
    A_ = lambda a: np.ascontiguousarray(a, dtype=np.float32)
    cs = np.zeros((128, 8, 5), np.float32)
    for v in range(4):
        cs[:, :, v] = _vec(c[v])
    cs[:, :, 4] = _vec(c_ctx)
    items = [(l, fc) for l in range(2) for fc in range(48)]
    maps = []
    for i in range(NC):
        its = items[i * 12:(i + 1) * 12]
        wm = np.stack([prm['w_mod'][l][:, fc * 128:(fc + 1) * 128].reshape(8, 128, 128).transpose(1, 0, 2) for l, fc in its])
        bm = np.stack([prm['b_mod'][l][fc * 128:(fc + 1) * 128] for l, fc in its], 1)
        maps.append({"cs": cs, "wm": A_(wm), "bm": A_(bm)})
    res = _run(_prog("M", build_M), maps)
    modv = {}
    for i in range(NC):
        for j, (l, fc) in enumerate(items[i * 12:(i + 1) * 12]):
            modv[(l, fc)] = res[i]["modT"][:, j, :]
    mod6 = [[np.stack([modv[(l, i6 * 8 + k)] for k in range(8)], 1) for i6 in range(6)] for l in range(2)]

    def core_tokens(arr_c, arr_l, b, half):
        return np.concatenate([arr_c[b, half * 128:(half + 1) * 128], arr_l[b, half * 2048:(half + 1) * 2048]], 0)

    xm = [_fm(core_tokens(ctx, x, j // 2, j % 2)) for j in range(NC)]
    fparts = None
    consts64 = host_consts64()
    for l in range(2):
        win = np.zeros((1024, NCT * 128), np.float32); win[:, :4184] = prm['w_in'][l]
        win = A_(win.reshape(8, 128, NCT * 128).transpose(1, 0, 2)); nw1 = _vec(prm['norm1_w'][l])
        maps = []
        for j in range(NC):
            b = j // 2
            g5l = mod6[l - 1][5][:, :, b] if l > 0 else np.zeros((128, 8), np.float32)
            g5c = mod6[l - 1][5][:, :, 4] if l > 0 else np.zeros((128, 8), np.float32)
            mod = np.stack([mod6[l][0][:, :, b], mod6[l][1][:, :, b], mod6[l][0][:, :, 4], mod6[l][1][:, :, 4], g5l, g5c], -1)
            m = {"xT": xm[j], "mod": A_(mod), "nw": nw1, "win": win}
            if l > 0:
                m["fT"] = A_(np.stack([_fm(fparts[cc][j * NTOK:(j + 1) * NTOK]) for cc in range(NC)]))
            maps.append(m)
        res = _run(_prog("A", build_A, l == 0), maps)
        xcur = [res[j]["xout"] for j in range(NC)]
        ptok = [res[j]["pT"].reshape(NCT * 128, NTOK).T[:, :4184] for j in range(NC)]
        del res
        pfull = np.stack([np.concatenate([ptok[2 * b][:128], ptok[2 * b + 1][:128], ptok[2 * b][128:], ptok[2 * b + 1][128:]], 0) for b in range(4)])
        del ptok
        yall = np.zeros((4, TSEQ, 1024), np.float32)
        pa = pfull[:, :, 0:1032]
        res = _run(_prog("B1", build_B1), [host_B1_inputs(pa, l, j // 2, j % 2, prm) for j in range(NC)])
        for j in range(NC):
            yall[j // 2, :, (j % 2) * 128:(j % 2 + 1) * 128] = res[j]["yT"].T
        pb = pfull[:, :, 1032:3096]
        for rnd in range(2):
            its = [(i // 4, i % 4) for i in range(rnd * 8, rnd * 8 + 8)]
            maps = [host_B2_inputs(pb, l, b, h, prm) for b, h in its]
            for m in maps:
                m["consts"] = consts64
            res = _run(_prog("B2", build_B2), maps)
            for (b, h), r in zip(its, res):
                yall[b, :, 256 + h * 128:256 + (h + 1) * 128] = host_B2_output(r["y"])
        pc = pfull[:, :, 3096:4184]
        for rnd in range(2):
            its = [(i // 4, i % 4) for i in range(rnd * 8, rnd * 8 + 8)]
            maps = [host_B3_inputs(pc, l, b, h, prm) for b, h in its]
            for m in maps:
                m["consts"] = consts64
            res = _run(_prog("B3", build_B3), maps)
            for (b, h), r in zip(its, res):
                yall[b, :, 768 + h * 64:768 + (h + 1) * 64] = r["y"].T
        del pfull
        wout = A_(prm['w_out'][l].reshape(8, 128, 1024).transpose(1, 0, 2)); nw2 = _vec(prm['norm2_w'][l])
        wr = A_(prm['w_router'][l].reshape(8, 128, 32).transpose(1, 0, 2)); br = A_(np.broadcast_to(prm['b_router'][l][None], (128, 32)))
        maps = []
        for j in range(NC):
            b, half = j // 2, j % 2
            ytok = np.concatenate([yall[b, half * 128:(half + 1) * 128], yall[b, 256 + half * 2048:256 + (half + 1) * 2048]], 0)
            mod = np.stack([mod6[l][2][:, :, b], mod6[l][3][:, :, b], mod6[l][4][:, :, b], mod6[l][2][:, :, 4], mod6[l][3][:, :, 4], mod6[l][4][:, :, 4]], -1)
            maps.append({"xT": xcur[j], "yT": _fm(ytok), "wout": wout, "mod": A_(mod), "nw": nw2, "wr": wr, "br": br})
        res = _run(_prog("C1", build_C1), maps)
        xm = [res[j]["xmT"] for j in range(NC)]
        Gall = np.concatenate([res[j]["G"].transpose(1, 0, 2).reshape(NTOK, 32) for j in range(NC)], 0)
        loads = np.count_nonzero(Gall, axis=0)
        use_sparse = MOE_SPARSE and int(loads.max()) <= C2S_CAP
        print(f"[moe] layer {l}: max expert load {int(loads.max())} (mean {float(loads.mean()):.0f}) -> {'dispatch' if use_sparse else 'dense'}", flush=True)
        if use_sparse:
            h2tok = np.concatenate([_tok(res[j]["h2T"]) for j in range(NC)] + [np.zeros((128, 1024), np.float32)], 0)
        else:
            h2all = np.ascontiguousarray(np.concatenate([res[j]["h2T"] for j in range(NC)], 2))
        del res, yall
        if use_sparse:
            maps = []
            c2c = host_C2s_consts()
            for cc in range(NC):
                es = slice(4 * cc, 4 * cc + 4)
                m_ = {"h2tok": h2tok, "Gm": A_(Gall[:, es].reshape(136, 128, 4).transpose(1, 0, 2)),
                      "wgu": A_(prm['w_gate_up'][l][es].reshape(4, 8, 128, 2048).transpose(0, 2, 1, 3)),
                      "wd": A_(prm['w_down'][l][es].reshape(4, 8, 128, 1024).transpose(0, 2, 1, 3)),
                      "bgu": A_(prm['b_gate_up'][l][es].reshape(4, 16, 128).transpose(2, 0, 1)),
                      "bdb": A_(np.broadcast_to(prm['b_down'][l][es][:, None, :], (4, 128, 1024)))}
                m_.update(c2c)
                maps.append(m_)
            res = _run(_prog("C2s", build_C2s), maps)
            del maps, h2tok
            fparts = [res[cc]["fpart"][:16 * NT2] for cc in range(NC)]
            del res
        else:
            maps = []
            ident = np.eye(128, dtype=np.float32)
            for cc in range(NC):
                es = slice(4 * cc, 4 * cc + 4)
                Gp = np.zeros((16, 1152, 4), np.float32); Gp[:, :NT2] = Gall[:, es].reshape(16, NT2, 4)
                maps.append({"h2T": h2all, "G": A_(Gp.reshape(16, 9, 128, 4).transpose(2, 0, 1, 3)),
                             "wgu": A_(prm['w_gate_up'][l][es].reshape(4, 8, 128, 2048).transpose(0, 2, 1, 3)),
                             "wd": A_(prm['w_down'][l][es].reshape(4, 8, 128, 1024).transpose(0, 2, 1, 3)),
                             "bgu": A_(prm['b_gate_up'][l][es].reshape(4, 16, 128).transpose(2, 0, 1)), "bd": A_(prm['b_down'][l][es]), "ident": ident})
            res = _run(_prog("C2", build_C2), maps)
            del maps, h2all
            fparts = [res[cc]["f"].transpose(0, 2, 1, 3).reshape(16, 1152, 1024)[:, :NT2].reshape(16 * NT2, 1024) for cc in range(NC)]
            del res
    nwf = _vec(prm['norm_f_w'])
    maps = []
    for j in range(NC):
        b = j // 2
        mod = np.stack([mod6[1][5][:, :, b], mod6[1][5][:, :, 4]], -1)
        maps.append({"xmT": xm[j], "fT": A_(np.stack([_fm(fparts[cc][j * NTOK:(j + 1) * NTOK]) for cc in range(NC)])), "mod": A_(mod), "nw": nwf})
    res = _run(_prog("D", build_D), maps)
    out = np.zeros((4, 4096, 1024), np.float32)
    for j in range(NC):
        b, half = j // 2, j % 2
        out[b, half * 2048:(half + 1) * 2048] = _tok(res[j]["oT"])[128:]
    return out
```

```python
import numpy as np
from contextlib import ExitStack
import concourse.bass as bass
import concourse.mybir as mybir
from concourse.bass_utils import run_bass_kernel_spmd

F32 = mybir.dt.float32
BF16 = mybir.dt.bfloat16
AF = mybir.ActivationFunctionType
ALU = mybir.AluOpType
AX = mybir.AxisListType

ENGS = ("pe", "dve", "act", "pool", "sp")
N_DMA_SEMS = 12


class Prog:
    def __init__(self, nc, same_engine_sync=None):
        import os
        if same_engine_sync is None:
            same_engine_sync = os.environ.get('SAMESYNC', '1') == '1'
        self.nc = nc
        self.es = ExitStack()
        self.ops = {e: [] for e in ENGS}
        self.cnt = {e: 0 for e in ENGS}
        self.sem = {}
        for e in ENGS:
            self.sem[e] = self.es.enter_context(nc.semaphore("s_" + e))
        self.dsem = {q: [self.es.enter_context(nc.semaphore(f"d_{q}{i}")) for i in range(N_DMA_SEMS)]
                     for q in ("sp", "pool", "act")}
        self.dsem_uses = {q: [0] * N_DMA_SEMS for q in ("sp", "pool", "act")}
        self.dsem_next = {q: 0 for q in ("sp", "pool", "act")}
        self.waited = {e: {} for e in ENGS}
        self.lastw = {}
        self.readers = {}
        self.same = same_engine_sync
        self.semobj = {}
        self.out_tokens = []
        self.nops = 0

    def sb(self, name, shape, dt=F32):
        return self.es.enter_context(self.nc.sbuf_tensor("sb_" + name, list(shape), dt))

    def ps(self, name, shape, dt=F32):
        return self.es.enter_context(self.nc.psum_tensor("ps_" + name, list(shape), dt))

    def _need(self, eng, tok, waits):
        if tok is None:
            return
        semkey, val, src = tok
        if src == eng and (not self.same or eng == "pe"):
            return
        if self.waited[eng].get(semkey, 0) >= val:
            return
        waits[semkey] = max(waits.get(semkey, 0), val)

    def _deps(self, eng, reads, writes):
        waits = {}
        for k in reads:
            self._need(eng, self.lastw.get(k), waits)
        for k in writes:
            self._need(eng, self.lastw.get(k), waits)
            for t in self.readers.get(k, ()):
                self._need(eng, t, waits)
        for semkey, val in waits.items():
            self.waited[eng][semkey] = val
            self.ops[eng].append(("wait", semkey, val))

    def _commit(self, tok, reads, writes):
        for k in reads:
            self.readers.setdefault(k, []).append(tok)
        for k in writes:
            self.lastw[k] = tok
            self.readers[k] = []

    def op(self, eng, fn, reads=(), writes=()):
        import os
        lim = os.environ.get("OPLIMIT")
        if lim is not None and self.nops >= int(lim):
            return None
        writes = list(writes) + [k for k in reads if isinstance(k, str) and k.startswith("bk")]
        reads = [k for k in reads if not (isinstance(k, str) and k.startswith("bk"))]
        self._deps(eng, reads, writes)
        self.cnt[eng] += 1
        tok = (("e", eng), self.cnt[eng], eng)
        self.ops[eng].append(("op", fn, ("e", eng), 1))
        self._commit(tok, reads, writes)
        self.nops += 1
        self._pass_turn()
        return tok

    def interleave(self, fns):
        import threading
        n = len(fns)
        st = {"turn": 0, "alive": [True] * n, "err": None}
        cv = threading.Condition()
        self._il = (st, cv, {})

        def nxt(i):
            for k in range(1, n + 1):
                j = (i + k) % n
                if st["alive"][j]:
                    return j
            return -1

        def runner(i):
            self._il[2][threading.get_ident()] = i
            with cv:
                while st["turn"] != i:
                    cv.wait()
            try:
                fns[i]()
            except BaseException as ex:
                st["err"] = ex
            finally:
                with cv:
                    st["alive"][i] = False
                    st["turn"] = nxt(i)
                    cv.notify_all()
        ths = [threading.Thread(target=runner, args=(i,)) for i in range(n)]
        for t in ths:
            t.start()
        for t in ths:
            t.join()
        self._il = None
        if st["err"] is not None:
            raise st["err"]

    def _pass_turn(self):
        il = getattr(self, "_il", None)
        if not il:
            return
        import threading
        st, cv, ids = il
        me = ids.get(threading.get_ident())
        if me is None:
            return
        n = len(st["alive"])
        with cv:
            j = me
            for k in range(1, n + 1):
                c = (me + k) % n
                if st["alive"][c]:
                    j = c
                    break
            if j != me:
                st["turn"] = j
                cv.notify_all()
                while st["turn"] != me:
                    cv.wait()

    def I(self, eng, meth, *args, r=(), w=(), **kw):
        return self.op(eng, lambda e: getattr(e, meth)(*args, **kw), reads=r, writes=w)

    def dma(self, q, out, in_, reads=(), writes=(), is_output=False, **kw):
        import os
        lim = os.environ.get("OPLIMIT")
        if lim is not None and self.nops >= int(lim) and not is_output:
            return None
        i = self.dsem_next[q]
        self.dsem_next[q] = (i + 1) % N_DMA_SEMS
        uses = self.dsem_uses[q][i]
        semkey = ("d", q, i)
        if uses > 0 and self.waited[q].get(semkey, 0) < 16 * uses:
            self.waited[q][semkey] = 16 * uses
            self.ops[q].append(("wait", semkey, 16 * uses))
        self._deps(q, reads, writes)
        self.dsem_uses[q][i] = uses + 1
        tok = (semkey, 16 * (uses + 1), None)
        self.ops[q].append(("op", lambda e: e.dma_start(out=out, in_=in_, **kw), semkey, 16))
        self._commit(tok, reads, writes)
        if is_output:
            self.out_tokens.append(tok)
        self.nops += 1
        return tok

    def idma(self, out, in_, out_idx=None, in_idx=None, reads=(), writes=(), is_output=False, **kw):
        q = "pool"
        i = self.dsem_next[q]
        self.dsem_next[q] = (i + 1) % N_DMA_SEMS
        uses = self.dsem_uses[q][i]
        semkey = ("d", q, i)
        if uses > 0 and self.waited[q].get(semkey, 0) < 16 * uses:
            self.waited[q][semkey] = 16 * uses
            self.ops[q].append(("wait", semkey, 16 * uses))
        self._deps(q, reads, writes)
        self.dsem_uses[q][i] = uses + 1
        tok = (semkey, 16 * (uses + 1), None)
        oo = bass.IndirectOffsetOnAxis(ap=out_idx, axis=0) if out_idx is not None else None
        io = bass.IndirectOffsetOnAxis(ap=in_idx, axis=0) if in_idx is not None else None
        self.ops[q].append(("op", lambda e: e.indirect_dma_start(out=out, out_offset=oo, in_=in_, in_offset=io, **kw), semkey, 16))
        self._commit(tok, reads, writes)
        if is_output:
            self.out_tokens.append(tok)
        self.nops += 1
        return tok

    def _semh(self, semkey):
        if semkey[0] == "e":
            return self.sem[semkey[1]]
        return self.dsem[semkey[1]][semkey[2]]

    def finish(self):
        for tok in self.out_tokens:
            semkey, val, _ = tok
            if self.waited["sp"].get(semkey, 0) < val:
                self.waited["sp"][semkey] = val
                self.ops["sp"].append(("wait", semkey, val))
        for e in ENGS:
            if self.cnt[e] > 0 and e != "sp":
                self.ops["sp"].append(("wait", ("e", e), self.cnt[e]))
        for q in ("sp", "pool", "act"):
            for i in range(N_DMA_SEMS):
                if self.dsem_uses[q][i] > 0:
                    self.ops["sp"].append(("wait", ("d", q, i), 16 * self.dsem_uses[q][i]))
        nc = self.nc
        with nc.Block() as block:
            def mk(ename):
                def body(e):
                    for item in self.ops[ename]:
                        if item[0] == "wait":
                            e.wait_ge(self._semh(item[1]), item[2])
                        else:
                            ins = item[1](e)
                            ins.then_inc(self._semh(item[2]), item[3])
                return body
            block.tensor(mk("pe"))
            block.vector(mk("dve"))
            block.scalar(mk("act"))
            block.gpsimd(mk("pool"))
            block.sync(mk("sp"))
        self.es.close()


def build_M():
    nc = bass.Bass("TRN2", target_bir_lowering=False)
    cs = nc.dram_tensor("cs", [128, 8, 5], F32, kind="ExternalInput").ap()
    wm = nc.dram_tensor("wm", [12, 128, 8, 128], F32, kind="ExternalInput").ap()
    bm = nc.dram_tensor("bm", [128, 12], F32, kind="ExternalInput").ap()
    out = nc.dram_tensor("modT", [128, 12, 5], F32, kind="ExternalOutput").ap()
    P = Prog(nc)
    cst = P.sb("cst", [128, 8, 5]); sg = P.sb("sg", [128, 8, 5]); sc = P.sb("sc", [128, 8, 5])
    bmt = P.sb("bmt", [128, 12]); ot = P.sb("ot", [128, 12, 5])
    wt = [P.sb(f"wt{i}", [128, 8, 128]) for i in range(2)]
    pp = [P.ps(f"pp{i}", [128, 8]) for i in range(2)]
    P.dma("sp", cst[:], cs, writes=["cst"])
    P.dma("sp", bmt[:], bm, writes=["bmt"])
    P.op("act", lambda e: e.activation(sg[:], cst[:], AF.Sigmoid), reads=["cst"], writes=["sg"])
    P.op("dve", lambda e: e.tensor_tensor(sc[:], cst[:], sg[:], ALU.mult), reads=["cst", "sg"], writes=["sc"])
    for j in range(12):
        w = wt[j % 2]; wk = f"wt{j%2}"; pk = f"pp{j%2}"; p_ = pp[j % 2]
        P.dma("sp", w[:], wm[j], writes=[wk])
        for k in range(8):
            P.op("pe", lambda e, w=w, k=k, p_=p_: e.matmul(p_[:, 0:5], w[:, k, :], sc[:, k, :], start=(k == 0), stop=(k == 7)),
                 reads=[wk, "sc"], writes=[pk])
        P.op("dve", lambda e, j=j, p_=p_: e.tensor_scalar(ot[:, j, :], p_[:, 0:5], bmt[:, j:j + 1], None, ALU.add),
             reads=[pk, "bmt"], writes=["ot"])
    P.dma("sp", out, ot[:], reads=["ot"], is_output=True)
    P.finish()
    return nc


NTOK = 2176
TILES = [(0, 128)] + [(128 + 512 * i, 512) for i in range(4)]
NCT = 33


def rms_modulate(P, xT, hT, mod, nw, ones_bf, shift_i, scale_i, hT32=None, tagp="n", psb=None, hkey="hT"):
    g = P.sb(tagp + "_g", [128, 8, 2]);
    for v, (sh, sci) in enumerate(zip(shift_i, scale_i)):
        P.op("dve", lambda e, v=v, sci=sci: e.scalar_tensor_tensor(g[:, :, v], mod[:, :, sci], 1.0, nw[:, :], ALU.add, ALU.mult),
             reads=["mod", "nw"], writes=[tagp + "_g"])
    sq = [P.sb(f"{tagp}_sq{i}", [128, 8, 512], BF16) for i in range(2)]
    ss = list(psb); ssk = [f"bk_{tagp}0", f"bk_{tagp}1"]
    rs = [P.sb(f"{tagp}_rs{i}", [128, 512]) for i in range(2)]
    tmp = [P.sb(f"{tagp}_tmp{i}", [128, 512]) for i in range(2)]
    ti = 0
    for it, (t0, n) in enumerate(TILES):
        b = it % 2
        v = 1 if it == 0 else 0
        for k in range(8):
            P.op("act", lambda e, k=k, b=b, t0=t0, n=n: e.activation(sq[b][:, k, 0:n], xT[:, k, t0:t0 + n], AF.Square),
                 reads=["xT"], writes=[f"{tagp}_sq{b}"])
        for k in range(8):
            P.op("pe", lambda e, k=k, b=b, n=n: e.matmul(ss[b][:, 0:n], ones_bf[:], sq[b][:, k, 0:n], start=(k == 0), stop=(k == 7)),
                 reads=[f"{tagp}_sq{b}", "ones_bf"], writes=[ssk[b]])
        P.op("dve", lambda e, b=b, n=n: e.tensor_scalar(rs[b][:, 0:n], ss[b][:, 0:n], 1.0 / 1024, 1e-6, ALU.mult, ALU.add),
             reads=[ssk[b]], writes=[f"{tagp}_rs{b}"])
        P.op("dve", lambda e, b=b, n=n: e.reciprocal(rs[b][:, 0:n], rs[b][:, 0:n]),
             reads=[f"{tagp}_rs{b}"], writes=[f"{tagp}_rs{b}"])
        P.op("act", lambda e, b=b, n=n: e.activation(rs[b][:, 0:n], rs[b][:, 0:n], AF.Sqrt),
             reads=[f"{tagp}_rs{b}"], writes=[f"{tagp}_rs{b}"])
        for k in range(8):
            tb = ti % 2; ti += 1
            P.op("dve", lambda e, k=k, b=b, tb=tb, t0=t0, n=n, v=v: e.scalar_tensor_tensor(
                tmp[tb][:, 0:n], xT[:, k, t0:t0 + n], g[:, k, v:v + 1], rs[b][:, 0:n], ALU.mult, ALU.mult),
                 reads=["xT", tagp + "_g", f"{tagp}_rs{b}"], writes=[f"{tagp}_tmp{tb}"])
            sh = shift_i[v]
            P.op("act", lambda e, k=k, tb=tb, t0=t0, n=n, sh=sh: e.activation(
                hT[:, k, t0:t0 + n], tmp[tb][:, 0:n], AF.Identity, bias=mod[:, k, sh:sh + 1]),
                 reads=[f"{tagp}_tmp{tb}", "mod"], writes=[hkey])
            if hT32 is not None:
                P.op("pool", lambda e, k=k, tb=tb, t0=t0, n=n, sh=sh: e.tensor_scalar(
                    hT32[:, k, t0:t0 + n], tmp[tb][:, 0:n], mod[:, k, sh:sh + 1], None, ALU.add),
                     reads=[f"{tagp}_tmp{tb}", "mod"], writes=["hT32"])


def build_A(first=False):
    nc = bass.Bass("TRN2", target_bir_lowering=False)
    xTd = nc.dram_tensor("xT", [128, 8, NTOK], F32, kind="ExternalInput").ap()
    modd = nc.dram_tensor("mod", [128, 8, 6], F32, kind="ExternalInput").ap()
    fTd = nc.dram_tensor("fT", [8, 128, 8, NTOK], F32, kind="ExternalInput").ap() if not first else None
    xoutd = nc.dram_tensor("xout", [128, 8, NTOK], F32, kind="ExternalOutput").ap()
    nwd = nc.dram_tensor("nw", [128, 8], F32, kind="ExternalInput").ap()
    wind = nc.dram_tensor("win", [128, 8, NCT * 128], F32, kind="ExternalInput").ap()
    pTd = nc.dram_tensor("pT", [NCT, 128, NTOK], F32, kind="ExternalOutput").ap()
    P = Prog(nc)
    xT = P.sb("xT", [128, 8, NTOK]); hT = P.sb("hT", [128, 8, NTOK], BF16)
    mod = P.sb("mod", [128, 8, 6]); nw = P.sb("nw", [128, 8])
    wbf = P.sb("wbf", [128, 8, NCT * 128], BF16)
    ones_bf = P.sb("ones_bf", [128, 128], BF16)
    P.op("pool", lambda e: e.memset(ones_bf[:], 1.0), writes=["ones_bf"])
    for k in range(8):
        P.dma("sp", xT[:, k, :], xTd[:, k, :], writes=["xT"])
    P.dma("sp", mod[:], modd, writes=["mod"])
    P.dma("sp", nw[:], nwd, writes=["nw"])
    for k in range(8):
        P.dma("pool", wbf[:, k, :], wind[:, k, :], writes=["wbf"])
    fT = P.sb("fT", [128, 512])
    for c, k in [(c, k) for c in range(0 if first else 8) for k in range(8)]:
        for (t0, n) in TILES:
            P.dma("sp", fT[:, 0:n], fTd[c, :, k, t0:t0 + n], writes=["fT"])
            gcol = 5 if t0 == 0 else 4
            P.I("dve", "scalar_tensor_tensor", xT[:, k, t0:t0 + n], fT[:, 0:n], mod[:, k, gcol:gcol + 1], xT[:, k, t0:t0 + n], ALU.mult, ALU.add,
                r=["fT", "mod", "xT"], w=["xT"])
    for k in range(8):
        P.dma("sp", xoutd[:, k, :], xT[:, k, :], reads=["xT"], is_output=True)
    pp = [P.ps(f"bank{i}", [128, 512]) for i in range(6)]
    rms_modulate(P, xT, hT, mod, nw, ones_bf, shift_i=(0, 2), scale_i=(1, 3), tagp="n1", psb=(pp[4], pp[5]))
    st = [P.sb(f"st{i}", [128, 512]) for i in range(4)]
    i = 0
    for ct in range(NCT):
        for (t0, n) in TILES:
            b = i % 4; i += 1
            for k in range(8):
                P.op("pe", lambda e, k=k, b=b, ct=ct, t0=t0, n=n: e.matmul(
                    pp[b][:, 0:n], wbf[:, k, ct * 128:(ct + 1) * 128], hT[:, k, t0:t0 + n], start=(k == 0), stop=(k == 7)),
                     reads=["wbf", "hT"], writes=[f"bk{b}"])
            if b % 2 == 0:
                P.op("dve", lambda e, b=b, n=n: e.tensor_copy(st[b][:, 0:n], pp[b][:, 0:n]), reads=[f"bk{b}"], writes=[f"st{b}"])
            else:
                P.op("act", lambda e, b=b, n=n: e.activation(st[b][:, 0:n], pp[b][:, 0:n], AF.Copy), reads=[f"bk{b}"], writes=[f"st{b}"])
            P.dma("sp", pTd[ct, :, t0:t0 + n], st[b][:, 0:n], reads=[f"st{b}"], is_output=True)
    P.finish()
    return nc


TSEQ = 4352
QS = 64
NCH = 34
SEGS = [(0, 256), (256, 4352)]


def conv_silu(P, dst, src, cw, cb, ti, key_dst, key_src, tmp, key_tmp, out_dt_tile=None):
    for (s, e_) in SEGS:
        if cb is not None:
            P.op("act", lambda e, s=s, e_=e_: e.activation(tmp[:, s:e_], src[:, s:e_], AF.Identity, bias=cb[:, ti:ti + 1], scale=cw[:, ti, 1:2]),
                 reads=[key_src, "cw", "cb"], writes=[key_tmp])
        else:
            P.op("act", lambda e, s=s, e_=e_: e.activation(tmp[:, s:e_], src[:, s:e_], AF.Copy, scale=cw[:, ti, 1:2]),
                 reads=[key_src, "cw"], writes=[key_tmp])
        P.op("dve", lambda e, s=s, e_=e_: e.scalar_tensor_tensor(tmp[:, s + 1:e_], src[:, s:e_ - 1], cw[:, ti, 0:1], tmp[:, s + 1:e_], ALU.mult, ALU.add),
             reads=[key_src, "cw", key_tmp], writes=[key_tmp])
        P.op("dve", lambda e, s=s, e_=e_: e.scalar_tensor_tensor(tmp[:, s:e_ - 1], src[:, s + 1:e_], cw[:, ti, 2:3], tmp[:, s:e_ - 1], ALU.mult, ALU.add),
             reads=[key_src, "cw", key_tmp], writes=[key_tmp])
    P.op("act", lambda e: e.activation(dst[:, :], tmp[:, :], AF.Silu), reads=[key_tmp], writes=[key_dst])


def load_consts(P, cd):
    c = {}
    for i, nm in enumerate(["tri_f", "tri_b", "nm_f", "nm_b", "ident"]):
        t = P.sb("c_" + nm, [128, 128]); P.dma("sp", t[:], cd[i], writes=["c_" + nm]); c[nm] = t
    ones = P.sb("c_ones", [128, 128]); P.op("pool", lambda e: e.memset(ones[:], 1.0), writes=["c_ones"]); c["ones"] = ones
    idb = P.sb("c_identb", [128, 128], BF16)
    P.op("dve", lambda e: e.tensor_copy(idb[:], c["ident"][:]), reads=["c_ident"], writes=["c_identb"]); c["identb"] = idb
    return c


def host_consts():
    k = np.arange(128)[:, None]; i = np.arange(128)[None, :]
    tri_f = (k <= i).astype(np.float32); tri_b = (k >= i).astype(np.float32)
    nm_f = np.where(i >= k, 0.0, -30000.0).astype(np.float32); nm_b = np.where(i <= k, 0.0, -30000.0).astype(np.float32)
    return np.stack([tri_f, tri_b, nm_f, nm_b, np.eye(128, dtype=np.float32)])


def build_B1(stage=99):
    nc = bass.Bass("TRN2", target_bir_lowering=False)
    D = lambda n, s: nc.dram_tensor(n, s, F32, kind="ExternalInput").ap()
    zTd = D("zT", [128, TSEQ]); xbcd = D("xbcT", [3, 128, TSEQ]); dtrd = D("dtr", [128, 4 * QS])
    dtbd = D("dtb", [128, 4 * QS]); alogd = D("alog", [128, 4 * QS]); cwd = D("cw", [128, 3, 3]); cbd = D("cb", [128, 3])
    dvd = D("dvec", [128, 1]); nwd = D("normw", [128, 1]); cd = D("consts", [5, 128, 128])
    yTd = nc.dram_tensor("yT", [128, TSEQ], F32, kind="ExternalOutput").ap()
    P = Prog(nc)
    C = load_consts(P, cd)
    raw = P.sb("raw", [128, TSEQ]); tmp = P.sb("tmp", [128, TSEQ])
    xT = P.sb("xT", [128, TSEQ]); B32 = P.sb("B32", [128, TSEQ]); C32 = P.sb("C32", [128, TSEQ])
    Bb = P.sb("Bb", [128, TSEQ], BF16); Cb = P.sb("Cb", [128, TSEQ], BF16)
    cw = P.sb("cw", [128, 3, 3]); cb = P.sb("cb", [128, 3]); dvec = P.sb("dvec", [128, 1]); normw = P.sb("normw", [128, 1])
    for t, d, k in ((cw, cwd, "cw"), (cb, cbd, "cb"), (dvec, dvd, "dvec"), (normw, nwd, "normw")):
        P.dma("sp", t[:], d, writes=[k])
    if stage == 0:
        P.op("dve", lambda e: e.tensor_copy(xT[:, 0:128], C["ident"][:]), reads=["c_ident"], writes=["xT"])
        P.op("dve", lambda e: e.tensor_scalar(xT[:, 128:256], C["tri_f"][:], cw[:, 0, 0:1], dvec[:, 0:1], ALU.mult, ALU.add), reads=["c_tri_f", "cw", "dvec"], writes=["xT"])
        P.dma("sp", yTd, xT[:], reads=["xT"], is_output=True); P.finish(); return nc
    import os
    NT = int(os.environ.get("NT", "3"))
    for ti, (dst, kd) in enumerate(((xT, "xT"), (B32, "B32"), (C32, "C32"))[:NT]):
        P.dma("sp", raw[:], xbcd[ti], writes=["raw"])
        conv_silu(P, dst, raw, cw, cb, ti, kd, "raw", tmp, "tmp")
    if NT == 3 and os.environ.get("NOCAST") is None:
        P.op("pool", lambda e: e.tensor_copy(Bb[:], B32[:]), reads=["B32"], writes=["Bb"])
        P.op("pool", lambda e: e.tensor_copy(Cb[:], C32[:]), reads=["C32"], writes=["Cb"])
    if stage == 1:
        P.dma("sp", yTd, xT[:], reads=["xT"], is_output=True); P.finish(); return nc
    dtr = P.sb("dtr", [128, 4 * QS]); dtb = P.sb("dtb", [128, 4 * QS]); alog = P.sb("alog", [128, 4 * QS])
    dt = P.sb("dt", [128, 4 * QS]); la = P.sb("la", [128, 4 * QS]); ncum = P.sb("ncum", [128, 4 * QS])
    wgt = P.sb("wgt", [128, 4 * QS]); dec = P.sb("dec", [128, 4 * QS])
    P.dma("sp", dtr[:], dtrd, writes=["dtr"]); P.dma("sp", dtb[:], dtbd, writes=["dtb"]); P.dma("sp", alog[:], alogd, writes=["alog"])
    P.op("dve", lambda e: e.tensor_tensor(dtr[:], dtr[:], dtb[:], ALU.add), reads=["dtr", "dtb"], writes=["dtr"])
    P.op("dve", lambda e: e.tensor_scalar(dtr[:], dtr[:], 60.0, None, ALU.min), reads=["dtr"], writes=["dtr"])
    P.op("act", lambda e: e.activation(dtr[:], dtr[:], AF.Exp), reads=["dtr"], writes=["dtr"])
    P.op("act", lambda e: e.activation(dt[:], dtr[:], AF.Ln, bias=1.0), reads=["dtr"], writes=["dt"])
    P.op("act", lambda e: e.activation(alog[:], alog[:], AF.Exp), reads=["alog"], writes=["alog"])
    P.op("dve", lambda e: e.scalar_tensor_tensor(la[:], dt[:], -1.0, alog[:], ALU.mult, ALU.mult), reads=["dt", "alog"], writes=["la"])
    bk = [P.ps(f"bank{i}", [128, 512]) for i in range(8)]
    pc = bk[0][:, 0:4 * QS]; pt = bk[1][:, 0:4 * QS]
    P.op("pe", lambda e: e.matmul(pc[:, 0:2 * QS], C["tri_f"][:], la[:, 0:2 * QS], start=True, stop=True), reads=["la", "c_tri_f"], writes=["bk0"])
    P.op("pe", lambda e: e.matmul(pc[:, 2 * QS:4 * QS], C["tri_b"][:], la[:, 2 * QS:4 * QS], start=True, stop=True), reads=["la", "c_tri_b"], writes=["bk0"])
    P.op("pe", lambda e: e.matmul(pt, C["ones"][:], la[:], start=True, stop=True), reads=["la", "c_ones"], writes=["bk1"])
    P.op("dve", lambda e: e.tensor_scalar(ncum[:], pc, -1.0, None, ALU.mult), reads=["bk0"], writes=["ncum"])
    P.op("dve", lambda e: e.tensor_tensor(wgt[:], pt, ncum[:], ALU.add), reads=["bk1", "ncum"], writes=["wgt"])
    P.op("act", lambda e: e.activation(wgt[:], wgt[:], AF.Exp), reads=["wgt"], writes=["wgt"])
    P.op("dve", lambda e: e.tensor_tensor(wgt[:], wgt[:], dt[:], ALU.mult), reads=["wgt", "dt"], writes=["wgt"])
    P.op("act", lambda e: e.activation(dec[:], pt, AF.Exp), reads=["bk1"], writes=["dec"])
    if stage == 2:
        for i_, (t_, k_) in enumerate(((dt, "dt"), (la, "la"), (ncum, "ncum"), (wgt, "wgt"), (dec, "dec"))):
            P.dma("sp", yTd[:, i_ * 256:(i_ + 1) * 256], t_[:], reads=[k_], is_output=True)
        for i_, (t_, k_) in enumerate(((B32, "B32"), (C32, "C32"), (xT, "xT"))):
            P.dma("sp", yTd[:, 1280 + i_ * 1024:1280 + (i_ + 1) * 1024], t_[:, 0:1024], reads=[k_], is_output=True)
        P.finish(); return nc
    xpad = [P.sb(f"xpad{h}", [128, NCH, 128], BF16) for h in range(2)]
    Btok = P.sb("Btok", [128, NCH, 128], BF16); xw = P.sb("xw", [128, NCH, 4, 64], BF16)
    for h in range(2):
        P.op("pool", lambda e, h=h: e.memset(xpad[h][:], 0.0), writes=[f"xpad{h}"])
    ptr = [bk[2][:, 0:128], bk[3][:, 0:128]]
    for c in range(NCH):
        sl = slice(c * 128, (c + 1) * 128)
        P.op("pe", lambda e, sl=sl: e.transpose(ptr[0], xT[:, sl], C["ident"][:]), reads=["xT", "c_ident"], writes=["bk2"])
        P.op("pe", lambda e, sl=sl: e.transpose(ptr[1], B32[:, sl], C["ident"][:]), reads=["B32", "c_ident"], writes=["bk3"])
        for h in range(2):
            P.op("act", lambda e, h=h, c=c: e.activation(xpad[h][:, c, h * 64:(h + 1) * 64], ptr[0][:, h * 64:(h + 1) * 64], AF.Copy),
                 reads=["bk2"], writes=[f"xpad{h}"])
        for q in range(4):
            h = q % 2
            P.op("dve", lambda e, q=q, h=h, c=c: e.tensor_scalar(xw[:, c, q, :], ptr[0][:, h * 64:(h + 1) * 64], wgt[:, q * QS + c:q * QS + c + 1], None, ALU.mult),
                 reads=["bk2", "wgt"], writes=["xw"])
        P.op("act", lambda e, c=c: e.activation(Btok[:, c, :], ptr[1], AF.Copy), reads=["bk3"], writes=["Btok"])
    if stage == 3:
        P.dma("sp", yTd, xT[:], reads=["xT"], is_output=True); P.finish(); return nc
    yacc = P.sb("yacc", [128, TSEQ])
    Hpad = [P.sb(f"Hpad{q}", [128, 128]) for q in range(4)]
    larep = [P.sb(f"larep{i}", [128, 128]) for i in range(2)]
    seg = [P.sb(f"seg{i}", [128, 128]) for i in range(2)]; Et = [P.sb(f"Et{i}", [128, 128]) for i in range(2)]
    STp = [P.sb(f"STp{i}", [128, 128], BF16) for i in range(2)]; CTs = [P.sb(f"CTs{i}", [128, 128]) for i in range(2)]
    psA = bk[0][:, 0:128]; psE = bk[1][:, 0:128]; psS = bk[4][:, 0:128]; psY = bk[5][:, 0:128]
    psH = [bk[6][:, 0:64], bk[7][:, 0:64]]
    import os
    for d in range(int(os.environ.get('ND', '2'))):
        tri = C["tri_f"] if d == 0 else C["tri_b"]; nm = C["nm_f"] if d == 0 else C["nm_b"]
        trik = "c_tri_f" if d == 0 else "c_tri_b"; nmk = "c_nm_f" if d == 0 else "c_nm_b"
        order = list(range(NCH)) if d == 0 else [1, 0] + list(range(NCH - 1, 1, -1))
        for hh in range(2):
            P.op("pool", lambda e, q=d * 2 + hh: e.memset(Hpad[q][:], 0.0), writes=[f"Hpad{d*2+hh}"])
        for c in order:
            sl = slice(c * 128, (c + 1) * 128)
            P.op("pe", lambda e, sl=sl: e.matmul(psS, Bb[:, sl], Cb[:, sl], start=True, stop=True), reads=["Bb", "Cb"], writes=["bk4"])
            for hh in range(2):
                q = d * 2 + hh
                P.op("pool", lambda e, q=q, c=c, hh=hh: e.tensor_scalar(larep[hh][:], C["ones"][:], la[:, q * QS + c:q * QS + c + 1], None, ALU.mult),
                     reads=["c_ones", "la"], writes=[f"larep{hh}"])
                P.op("pe", lambda e, hh=hh, tri=tri: e.matmul(psA, larep[hh][:], tri[:], start=True, stop=False), reads=[f"larep{hh}", trik], writes=["bk0"])
                P.op("pe", lambda e, nm=nm: e.matmul(psA, C["ident"][:], nm[:], start=False, stop=True), reads=["c_ident", nmk], writes=["bk0"])
                P.op("pe", lambda e, hh=hh, tri=tri: e.matmul(psE, larep[hh][:], tri[:], start=True, stop=True), reads=[f"larep{hh}", trik], writes=["bk1"])
                P.op("act", lambda e, q=q, c=c, hh=hh: e.activation(seg[hh][:], psA, AF.Exp, bias=ncum[:, q * QS + c:q * QS + c + 1]), reads=["bk0", "ncum"], writes=[f"seg{hh}"])
                P.op("act", lambda e, hh=hh: e.activation(Et[hh][:], psE, AF.Exp), reads=["bk1"], writes=[f"Et{hh}"])
                P.op("dve", lambda e, q=q, c=c, hh=hh: e.scalar_tensor_tensor(STp[hh][:], psS, dt[:, q * QS + c:q * QS + c + 1], seg[hh][:], ALU.mult, ALU.mult),
                     reads=["bk4", "dt", f"seg{hh}"], writes=[f"STp{hh}"])
                P.op("dve", lambda e, sl=sl, hh=hh: e.tensor_tensor(CTs[hh][:], C32[:, sl], Et[hh][:], ALU.mult), reads=["C32", f"Et{hh}"], writes=[f"CTs{hh}"])
            for hh in range(2):
                q = d * 2 + hh
                P.op("pe", lambda e, hh=hh, c=c: e.matmul(psY, xpad[hh][:, c, :], STp[hh][:], start=(hh == 0), stop=False),
                     reads=[f"xpad{hh}", f"STp{hh}"], writes=["bk5"])
            for hh in range(2):
                q = d * 2 + hh
                P.op("pe", lambda e, hh=hh, q=q: e.matmul(psY, Hpad[q][:], CTs[hh][:], start=False, stop=(hh == 1)),
                     reads=[f"Hpad{q}", f"CTs{hh}"], writes=["bk5"])
            for hh in range(2):
                q = d * 2 + hh
                P.op("pe", lambda e, hh=hh, q=q, c=c: e.matmul(psH[hh], Btok[:, c, :], xw[:, c, q, :], start=True, stop=True),
                     reads=["Btok", "xw"], writes=[f"bk{6+hh}"])
                P.op("dve", lambda e, hh=hh, q=q, c=c: e.scalar_tensor_tensor(
                    Hpad[q][:, hh * 64:(hh + 1) * 64], Hpad[q][:, hh * 64:(hh + 1) * 64], dec[:, q * QS + c:q * QS + c + 1], psH[hh], ALU.mult, ALU.add),
                     reads=[f"Hpad{q}", "dec", f"bk{6+hh}"], writes=[f"Hpad{q}"])
            if d == 0:
                P.op("dve", lambda e, sl=sl: e.scalar_tensor_tensor(yacc[:, sl], xT[:, sl], dvec[:, 0:1], psY, ALU.mult, ALU.add),
                     reads=["xT", "dvec", "bk5"], writes=[f"yacc{c}"])
            else:
                P.op("dve", lambda e, sl=sl: e.tensor_tensor(yacc[:, sl], yacc[:, sl], psY, ALU.add), reads=[f"yacc{c}", "bk5"], writes=[f"yacc{c}"])
    if stage == 4:
        P.dma("sp", yTd, yacc[:], reads=[f"yacc{c}" for c in range(NCH)], is_output=True); P.finish(); return nc
    zT = raw
    P.dma("sp", zT[:], zTd, writes=["raw"])
    P.op("act", lambda e: e.activation(tmp[:], zT[:], AF.Silu), reads=["raw"], writes=["tmp"])
    allc = [f"yacc{c}" for c in range(NCH)]
    P.op("dve", lambda e: e.tensor_tensor(yacc[:], yacc[:], tmp[:], ALU.mult), reads=allc + ["tmp"], writes=allc)
    P.op("act", lambda e: e.activation(tmp[:], yacc[:], AF.Square), reads=allc, writes=["tmp"])
    pss = [bk[2][:, 0:256], bk[3][:, 0:256]]; rs = [P.sb(f"rs{i}", [128, 256]) for i in range(2)]
    for i, t0 in enumerate(range(0, TSEQ, 256)):
        n = min(256, TSEQ - t0); b = i % 2
        P.op("pe", lambda e, b=b, t0=t0, n=n: e.matmul(pss[b][:, 0:n], C["ones"][:], tmp[:, t0:t0 + n], start=True, stop=True), reads=["tmp", "c_ones"], writes=[f"bk{2+b}"])
        P.op("dve", lambda e, b=b, n=n: e.tensor_scalar(rs[b][:, 0:n], pss[b][:, 0:n], 1.0 / 128, 1e-5, ALU.mult, ALU.add), reads=[f"bk{2+b}"], writes=[f"rs{b}"])
        P.op("dve", lambda e, b=b, n=n: e.reciprocal(rs[b][:, 0:n], rs[b][:, 0:n]), reads=[f"rs{b}"], writes=[f"rs{b}"])
        P.op("act", lambda e, b=b, n=n: e.activation(rs[b][:, 0:n], rs[b][:, 0:n], AF.Sqrt), reads=[f"rs{b}"], writes=[f"rs{b}"])
        P.op("dve", lambda e, b=b, t0=t0, n=n: e.scalar_tensor_tensor(xT[:, t0:t0 + n], yacc[:, t0:t0 + n], normw[:, 0:1], rs[b][:, 0:n], ALU.mult, ALU.mult),
             reads=allc + ["normw", f"rs{b}"], writes=["xT"])
    P.dma("sp", yTd, xT[:], reads=["xT"], is_output=True)
    P.finish()
    return nc


def host_B1_inputs(pa, L, b, hp, prm):
    p = pa[b]
    z = p[:, hp * 128:(hp + 1) * 128].T
    x = p[:, 256 + hp * 128:256 + (hp + 1) * 128].T
    Bm = p[:, 512 + hp * 128:512 + (hp + 1) * 128].T
    Cm = p[:, 768 + hp * 128:768 + (hp + 1) * 128].T
    cols = [1024 + d * 4 + 2 * hp + hh for d in range(2) for hh in range(2)]
    dtr = np.zeros((128, 4, QS), np.float32); dtr[:, :, :NCH] = p[:, cols].reshape(NCH, 128, 4).transpose(1, 2, 0); dtr = dtr.reshape(128, 4 * QS)
    bc = lambda v: np.broadcast_to(np.asarray(v, np.float32)[None, :, None], (128, 4, QS)).reshape(128, 4 * QS)
    dtb = bc([prm['m_dt_bias'][L, d, 2 * hp + hh] for d in range(2) for hh in range(2)])
    alog = bc([prm['m_a_log'][L, d, 2 * hp + hh] for d in range(2) for hh in range(2)])
    cwfull = prm['m_conv_w'][L]; cbfull = prm['m_conv_b'][L]
    offs = [hp * 128, 256 + hp * 128, 512 + hp * 128]
    cw = np.stack([cwfull[:, o:o + 128].T for o in offs], 1)
    cb = np.stack([cbfull[o:o + 128] for o in offs], 1)
    dvec = np.repeat(prm['m_d'][L, 2 * hp:2 * hp + 2], 64)[:, None]
    normw = prm['m_norm_w'][L, hp * 128:(hp + 1) * 128][:, None]
    A = np.ascontiguousarray
    return {"zT": A(z), "xbcT": A(np.stack([x, Bm, Cm])), "dtr": A(dtr), "dtb": A(dtb), "alog": A(alog), "cw": A(cw), "cb": A(cb),
            "dvec": A(dvec), "normw": A(normw), "consts": host_consts()}


NPK = 34


def host_consts64():
    k = np.arange(128)[:, None]; i = np.arange(128)[None, :]
    same = (k // 64) == (i // 64)
    f = lambda m: m.astype(np.float32)
    tri_f = f(same & (k <= i)); tri_b = f(same & (k >= i))
    nm_f = np.where(same & (i >= k), 0.0, -30000.0); nm_b = np.where(same & (i <= k), 0.0, -30000.0)
    pms_f = np.where(same & (i < k), 0.0, 30000.0); pms_b = np.where(same & (i > k), 0.0, 30000.0)
    blk = f(same); selA = f(np.broadcast_to(k < 64, (128, 128))); selB = f(np.broadcast_to(k >= 64, (128, 128)))
    inc_f = f(same & (i < k)); inc_b = f(same & (i > k))
    return np.stack([np.eye(128), tri_f, tri_b, nm_f, nm_b, pms_f, pms_b, blk, selA, selB, inc_f, inc_b]).astype(np.float32)

C64_NAMES = ["ident", "tri_f", "tri_b", "nm_f", "nm_b", "pms_f", "pms_b", "blk", "selA", "selB", "sl", "su"]


def load_consts64(P, cd, only=None):
    c = {}
    for i, nm in enumerate(C64_NAMES):
        if only is not None and nm not in only:
            continue
        t = P.sb("c_" + nm, [128, 128]); P.dma("sp", t[:], cd[i], writes=["c_" + nm]); c[nm] = t
    ones = P.sb("c_ones", [128, 128]); P.I("pool", "memset", ones[:], 1.0, w=["c_ones"]); c["ones"] = ones
    return c


def tri_inverse_apply(P, C, Lm, X, ncolsX, bk, tg):
    Pt = [P.sb(f"{tg}P{i}", [128, 128]) for i in range(2)] if not hasattr(P, "_tri_" + tg) else getattr(P, "_tri_" + tg)[0]
    Qt = [P.sb(f"{tg}Q{i}", [128, 128]) for i in range(2)] if not hasattr(P, "_tri_" + tg) else getattr(P, "_tri_" + tg)[1]
    setattr(P, "_tri_" + tg, (Pt, Qt))
    (pP, kP), (pQ, kQ), (pT, kT), (pX, kX) = bk["P"], bk["Q"], bk["T"], bk["X"]
    ident = C["ident"]
    P.I("pe", "transpose", pT[:, 0:128], Lm[:], ident[:], r=[tg + "L", "c_ident"], w=[kT])
    P.I("act", "activation", Qt[0][:], pT[:, 0:128], AF.Copy, r=[kT], w=[f"{tg}Q0"])
    P.I("pe", "matmul", pX[:, 0:ncolsX], Qt[0][:], X[:], start=True, stop=True, r=[f"{tg}Q0", tg + "X"], w=[kX])
    P.I("dve", "tensor_tensor", X[:], X[:], pX[:, 0:ncolsX], ALU.subtract, r=[tg + "X", kX], w=[tg + "X"])
    Pc, Pk, Qc, Qk = Lm, tg + "L", Qt[0], f"{tg}Q0"
    for lvl in range(1, 6):
        a = lvl % 2
        P.I("pe", "matmul", pQ[:, 0:128], Pc[:], Qc[:], start=True, stop=True, r=[Pk, Qk], w=[kQ])
        if lvl < 5:
            P.I("pe", "matmul", pP[:, 0:128], Qc[:], Pc[:], start=True, stop=True, r=[Pk, Qk], w=[kP])
            P.I("dve", "tensor_copy", Pt[a][:], pP[:, 0:128], r=[kP], w=[f"{tg}P{a}"])
        P.I("act", "activation", Qt[a][:], pQ[:, 0:128], AF.Copy, r=[kQ], w=[f"{tg}Q{a}"])
        Pc, Pk, Qc, Qk = Pt[a], f"{tg}P{a}", Qt[a], f"{tg}Q{a}"
        P.I("pe", "matmul", pX[:, 0:ncolsX], Qc[:], X[:], start=True, stop=True, r=[Qk, tg + "X"], w=[kX])
        P.I("dve", "tensor_tensor", X[:], X[:], pX[:, 0:ncolsX], ALU.add, r=[tg + "X", kX], w=[tg + "X"])


def build_B2():
    nc = bass.Bass("TRN2", target_bir_lowering=False)
    D = lambda n, s: nc.dram_tensor(n, s, F32, kind="ExternalInput").ap()
    qkvd = D("qkvT", [3, 128, TSEQ]); gated = D("gate", [128, NPK, 128]); tabd = D("tab", [4, 128, 2 * QS])
    cwd = D("cw", [128, 3, 3]); nwd = D("normw", [128, 128]); cd = D("consts", [len(C64_NAMES), 128, 128])
    yd = nc.dram_tensor("y", [128, NPK, 128], F32, kind="ExternalOutput").ap()
    P = Prog(nc)
    C = load_consts64(P, cd)
    bkt = [P.ps(f"bank{i}", [128, 512]) for i in range(8)]
    BK = lambda i: (bkt[i], f"bk{i}")
    raw = P.sb("raw", [128, TSEQ]); tmp = P.sb("tmp", [128, TSEQ])
    qT = P.sb("qT", [128, TSEQ]); kT = P.sb("kT", [128, TSEQ])
    cw = P.sb("cw", [128, 3, 3]); P.dma("sp", cw[:], cwd, writes=["cw"])
    normw = P.sb("normw", [128, 128]); P.dma("sp", normw[:], nwd, writes=["normw"])
    ktok = P.sb("ktok", [128, NPK, 128]); vtok = P.sb("vtok", [128, NPK, 128]); oacc = P.sb("oacc", [128, NPK, 128])
    rs = [P.sb(f"rs{i}", [128, 256]) for i in range(2)]
    for ti, (dst, kd) in enumerate(((qT, "qT"), (kT, "kT"), (raw, "raw"))):
        P.dma("sp", raw[:], qkvd[ti], writes=["raw"])
        conv_silu(P, dst, raw, cw, None, ti, kd, "raw", tmp, "tmp")
        if ti < 2:
            P.I("act", "activation", tmp[:], dst[:], AF.Square, r=[kd], w=["tmp"])
            for i, t0 in enumerate(range(0, TSEQ, 256)):
                b = i % 2; (pa, pk) = BK(b)
                P.I("pe", "matmul", pa[:, 0:256], C["ones"][:], tmp[:, t0:t0 + 256], start=True, stop=True, r=["tmp", "c_ones"], w=[pk])
                P.I("dve", "tensor_scalar", rs[b][:], pa[:, 0:256], 1e-6, None, ALU.add, r=[pk], w=[f"rs{b}"])
                P.I("dve", "reciprocal", rs[b][:], rs[b][:], r=[f"rs{b}"], w=[f"rs{b}"])
                P.I("act", "activation", rs[b][:], rs[b][:], AF.Sqrt, r=[f"rs{b}"], w=[f"rs{b}"])
                sc = 128.0 ** -0.5 if ti == 0 else 1.0
                P.I("dve", "scalar_tensor_tensor", dst[:, t0:t0 + 256], dst[:, t0:t0 + 256], sc, rs[b][:], ALU.mult, ALU.mult,
                    r=[kd, f"rs{b}"], w=[kd])
    vT = raw
    for c in range(NPK):
        sl = slice(c * 128, (c + 1) * 128)
        for src, sk, dst, dk, bi in ((kT, "kT", ktok, "ktok", 0), (vT, "raw", vtok, "vtok", 1)):
            (pa, pk) = BK(bi)
            P.I("pe", "transpose", pa[:, 0:128], src[:, sl], C["ident"][:], r=[sk, "c_ident"], w=[pk])
            P.I("act" if bi else "dve", "activation" if bi else "tensor_copy", dst[:, c, :], pa[:, 0:128], *([AF.Copy] if bi else []), r=[pk], w=[dk])
    W2 = 2 * QS
    tb = {n: P.sb("t_" + n, [128, W2]) for n in ("braw", "araw", "dtb", "alog", "beta", "g", "gc", "ngc", "egc", "toend", "glA", "glB", "bw")}
    for i, n in enumerate(("braw", "araw", "dtb", "alog")):
        P.dma("sp", tb[n][:], tabd[i], writes=["t_" + n])
    P.I("act", "activation", tb["beta"][:], tb["braw"][:], AF.Sigmoid, r=["t_braw"], w=["t_beta"])
    P.I("dve", "tensor_tensor", tb["araw"][:], tb["araw"][:], tb["dtb"][:], ALU.add, r=["t_araw", "t_dtb"], w=["t_araw"])
    P.I("dve", "tensor_scalar", tb["araw"][:], tb["araw"][:], 60.0, None, ALU.min, r=["t_araw"], w=["t_araw"])
    P.I("act", "activation", tb["araw"][:], tb["araw"][:], AF.Exp, r=["t_araw"], w=["t_araw"])
    P.I("act", "activation", tb["araw"][:], tb["araw"][:], AF.Ln, bias=1.0, r=["t_araw"], w=["t_araw"])
    P.I("act", "activation", tb["alog"][:], tb["alog"][:], AF.Exp, r=["t_alog"], w=["t_alog"])
    P.I("dve", "scalar_tensor_tensor", tb["g"][:], tb["araw"][:], -1.0, tb["alog"][:], ALU.mult, ALU.mult, r=["t_araw", "t_alog"], w=["t_g"])
    (p0, k0), (p1, k1), (p2, k2), (p3, k3) = BK(0), BK(1), BK(2), BK(3)
    P.I("pe", "matmul", p0[:, 0:QS], C["tri_f"][:], tb["g"][:, 0:QS], start=True, stop=True, r=["t_g", "c_tri_f"], w=[k0])
    P.I("pe", "matmul", p0[:, QS:W2], C["tri_b"][:], tb["g"][:, QS:W2], start=True, stop=True, r=["t_g", "c_tri_b"], w=[k0])
    P.I("pe", "matmul", p1[:, 0:W2], C["blk"][:], tb["g"][:], start=True, stop=True, r=["t_g", "c_blk"], w=[k1])
    P.I("pe", "matmul", p2[:, 0:W2], C["selA"][:], tb["g"][:], start=True, stop=True, r=["t_g", "c_selA"], w=[k2])
    P.I("pe", "matmul", p3[:, 0:W2], C["selB"][:], tb["g"][:], start=True, stop=True, r=["t_g", "c_selB"], w=[k3])
    P.I("dve", "tensor_copy", tb["gc"][:], p0[:, 0:W2], r=[k0], w=["t_gc"])
    P.I("dve", "tensor_scalar", tb["ngc"][:], tb["gc"][:], -1.0, None, ALU.mult, r=["t_gc"], w=["t_ngc"])
    P.I("act", "activation", tb["egc"][:], tb["gc"][:], AF.Exp, r=["t_gc"], w=["t_egc"])
    P.I("dve", "tensor_tensor", tb["toend"][:], p1[:, 0:W2], tb["gc"][:], ALU.subtract, r=[k1, "t_gc"], w=["t_toend"])
    P.I("act", "activation", tb["toend"][:], tb["toend"][:], AF.Exp, r=["t_toend"], w=["t_toend"])
    P.I("act", "activation", tb["glA"][:], p2[:, 0:W2], AF.Exp, r=[k2], w=["t_glA"])
    P.I("act", "activation", tb["glB"][:], p3[:, 0:W2], AF.Exp, r=[k3], w=["t_glB"])
    P.I("dve", "tensor_tensor", tb["bw"][:], tb["beta"][:], tb["egc"][:], ALU.mult, r=["t_beta", "t_egc"], w=["t_bw"])
    P.I("pool", "memset", oacc[:], 0.0, w=[f"oacc{c}" for c in range(NPK)])
    names = (("grep", [128, 128]), ("DmT", [128, 128]), ("DmS", [128, 128]), ("Et", [128, 128]), ("attnT", [128, 128]), ("L", [128, 128]),
             ("qdT", [128, 128]), ("X", [128, 256]), ("kdec", [128, 128]), ("wT", [128, 128]), ("vnew", [128, 128]), ("S", [128, 128]))
    TL = [{n: P.sb(f"dn{d}{n}", shp) for n, shp in names} for d in range(2)]

    def run_dir(d):
        T = TL[d]; K = lambda n: f"dn{d}{n}"
        ba, bb, bc_, bd_ = [bkt[4 * d + i] for i in range(4)]; ka, kb, kc, kd = [f"bk{4*d+i}" for i in range(4)]
        pG, pA, pPq, pQq = ba[:, 0:128], ba[:, 128:256], ba[:, 256:384], ba[:, 384:512]
        pD1, pD2, pE, pT = bb[:, 0:128], bb[:, 128:256], bb[:, 256:384], bb[:, 384:512]
        pX, pV, pS = bc_[:, 0:256], bc_[:, 256:384], bc_[:, 384:512]
        pO = bd_[:, 0:128]
        bkinv = {"P": (pPq, ka), "Q": (pQq, ka), "T": (pT, kb), "X": (pX, kc)}
        sfx = "_f" if d == 0 else "_b"
        tri, nm, pms = C["tri" + sfx], C["nm" + sfx], C["pms" + sfx]
        order = list(range(NPK)) if d == 0 else [1, 0] + list(range(NPK - 1, 1, -1))
        S, grep, DmT, DmS, Et, attnT, Lm, qdT, X, kdec, wT, vnew = [T[n] for n in ("S", "grep", "DmT", "DmS", "Et", "attnT", "L", "qdT", "X", "kdec", "wT", "vnew")]
        P.I("pool", "memset", S[:], 0.0, w=[K("S")])
        for c in order:
            sl = slice(c * 128, (c + 1) * 128); col = d * QS + c; cs = slice(col, col + 1)
            P.I("pe", "matmul", pG, kT[:, sl], kT[:, sl], start=True, stop=True, r=["kT"], w=[ka])
            P.I("pe", "matmul", pA, kT[:, sl], qT[:, sl], start=True, stop=True, r=["kT", "qT"], w=[ka])
            P.I("pool", "tensor_scalar", grep[:], C["ones"][:], tb["g"][:, cs], None, ALU.mult, r=["c_ones", "t_g"], w=[K("grep")])
            P.I("pe", "matmul", pD1, grep[:], tri[:], start=True, stop=False, r=[K("grep"), "c_tri" + sfx], w=[kb])
            P.I("pe", "matmul", pD1, C["ident"][:], nm[:], start=False, stop=True, r=["c_ident", "c_nm" + sfx], w=[kb])
            P.I("act", "activation", DmT[:], pD1, AF.Exp, bias=tb["ngc"][:, cs], r=[kb, "t_ngc"], w=[K("DmT")])
            P.I("pe", "matmul", pD2, grep[:], tri[:], start=True, stop=False, r=[K("grep"), "c_tri" + sfx], w=[kb])
            P.I("pe", "matmul", pD2, C["ident"][:], pms[:], start=False, stop=True, r=["c_ident", "c_pms" + sfx], w=[kb])
            P.I("act", "activation", DmS[:], pD2, AF.Exp, bias=tb["gc"][:, cs], scale=-1.0, r=[kb, "t_gc"], w=[K("DmS")])
            P.I("pe", "matmul", pE, grep[:], tri[:], start=True, stop=True, r=[K("grep"), "c_tri" + sfx], w=[kb])
            P.I("act", "activation", Et[:], pE, AF.Exp, r=[kb], w=[K("Et")])
            P.I("dve", "tensor_tensor", attnT[:], pA, DmT[:], ALU.mult, r=[ka, K("DmT")], w=[K("attnT")])
            P.I("dve", "scalar_tensor_tensor", Lm[:], pG, tb["beta"][:, cs], DmS[:], ALU.mult, ALU.mult, r=[ka, "t_beta", K("DmS")], w=[K("L")])
            P.I("dve", "tensor_tensor", qdT[:], qT[:, sl], Et[:], ALU.mult, r=["qT", K("Et")], w=[K("qdT")])
            P.I("dve", "tensor_scalar", X[:, 0:128], vtok[:, c, :], tb["beta"][:, cs], None, ALU.mult, r=["vtok", "t_beta"], w=[K("X")])
            P.I("dve", "tensor_scalar", X[:, 128:256], ktok[:, c, :], tb["bw"][:, cs], None, ALU.mult, r=["ktok", "t_bw"], w=[K("X")])
            P.I("pool", "tensor_scalar", kdec[:], ktok[:, c, :], tb["toend"][:, cs], None, ALU.mult, r=["ktok", "t_toend"], w=[K("kdec")])
            tri_inverse_apply(P, C, Lm, X, 256, bkinv, f"dn{d}")
            P.I("pe", "transpose", pT, X[:, 128:256], C["ident"][:], r=[K("X"), "c_ident"], w=[kb])
            P.I("act", "activation", wT[:], pT, AF.Copy, r=[kb], w=[K("wT")])
            for half in ((0, 1) if d == 0 else (1, 0)):
                rows = slice(half * 64, (half + 1) * 64)
                gl = tb["glA"] if half == 0 else tb["glB"]; glk = "t_glA" if half == 0 else "t_glB"
                P.I("pe", "matmul", pV, wT[:], S[:], start=True, stop=True, r=[K("wT"), K("S")], w=[kc])
                P.I("dve", "tensor_tensor", vnew[rows, :], X[rows, 0:128], pV[rows, :], ALU.subtract, r=[K("X"), kc], w=[K("vnew")])
                P.I("pe", "matmul", pO, qdT[:], S[:], start=True, stop=False, r=[K("qdT"), K("S")], w=[kd])
                P.I("pe", "matmul", pO, attnT[rows, :], vnew[rows, :], start=False, stop=True, r=[K("attnT"), K("vnew")], w=[kd])
                P.I("dve", "tensor_tensor", oacc[rows, c, :], oacc[rows, c, :], pO[rows, :], ALU.add, r=[kd, f"oacc{c}"], w=[f"oacc{c}"])
                P.I("pe", "matmul", pS, kdec[rows, :], vnew[rows, :], start=True, stop=True, r=[K("kdec"), K("vnew")], w=[kc])
                P.I("dve", "scalar_tensor_tensor", S[:], S[:], gl[:, cs], pS, ALU.mult, ALU.add, r=[K("S"), glk, kc], w=[K("S")])

    P.interleave([lambda: run_dir(0), lambda: run_dir(1)])
    allo = [f"oacc{c}" for c in range(NPK)]
    gate = P.sb("gate", [128, NPK, 128]); sq = P.sb("sq", [128, NPK, 128]); ss = P.sb("ss", [128, NPK])
    P.dma("sp", gate[:], gated, writes=["gate"])
    P.I("act", "activation", gate[:], gate[:], AF.Silu, r=["gate"], w=["gate"])
    P.I("act", "activation", sq[:], oacc[:], AF.Square, r=allo, w=["sq"])
    P.I("dve", "tensor_reduce", ss[:], sq[:], AX.X, ALU.add, r=["sq"], w=["ss"])
    P.I("dve", "tensor_scalar", ss[:], ss[:], 1.0 / 128, 1e-6, ALU.mult, ALU.add, r=["ss"], w=["ss"])
    P.I("dve", "reciprocal", ss[:], ss[:], r=["ss"], w=["ss"])
    P.I("act", "activation", ss[:], ss[:], AF.Sqrt, r=["ss"], w=["ss"])
    for c in range(NPK):
        P.I("dve", "scalar_tensor_tensor", sq[:, c, :], oacc[:, c, :], ss[:, c:c + 1], normw[:], ALU.mult, ALU.mult, r=allo + ["ss", "normw"], w=["sq"])
    P.I("dve", "tensor_tensor", sq[:], sq[:], gate[:], ALU.mult, r=["sq", "gate"], w=["sq"])
    P.dma("sp", yd, sq[:], reads=["sq"], is_output=True)
    P.finish()
    return nc


def colmajor_perm():
    t = np.arange(4096).reshape(64, 64)
    return t.T.reshape(-1)


def host_B2_inputs(pb, L, b, head, prm):
    perm = np.concatenate([np.arange(256), 256 + colmajor_perm()])
    p = pb[b][perm]
    q = p[:, head * 128:(head + 1) * 128].T; k = p[:, 512 + head * 128:512 + (head + 1) * 128].T
    v = p[:, 1024 + head * 128:1024 + (head + 1) * 128].T
    gate = p[:, 1536 + head * 128:1536 + (head + 1) * 128].reshape(NPK, 128, 128).transpose(1, 0, 2)
    def tabl(cols):
        t = np.zeros((128, 2, QS), np.float32); t[:, :, :NPK] = p[:, cols].reshape(NPK, 128, 2).transpose(1, 2, 0); return t.reshape(128, 2 * QS)
    braw = tabl([2048 + d * 4 + head for d in range(2)]); araw = tabl([2056 + d * 4 + head for d in range(2)])
    bc = lambda v_: np.broadcast_to(np.asarray(v_, np.float32)[None, :, None], (128, 2, QS)).reshape(128, 2 * QS)
    dtb = bc(prm['dn_dt_bias'][L, :, head]); alog = bc(prm['dn_a_log'][L, :, head])
    cwf = prm['dn_conv_w'][L]
    cw = np.stack([cwf[:, o + head * 128:o + (head + 1) * 128].T for o in (0, 512, 1024)], 1)
    normw = np.broadcast_to(prm['dn_norm_w'][L][None, :], (128, 128))
    A = lambda a: np.ascontiguousarray(a, dtype=np.float32)
    return {"qkvT": A(np.stack([q, k, v])), "gate": A(gate), "tab": A(np.stack([braw, araw, dtb, alog])), "cw": A(cw), "normw": A(normw),
            "consts": host_consts64()}


def host_B2_output(y):
    yy = y.transpose(1, 0, 2).reshape(TSEQ, 128)
    out = np.empty_like(yy)
    perm = np.concatenate([np.arange(256), 256 + colmajor_perm()])
    out[perm] = yy
    return out


def build_B3():
    nc = bass.Bass("TRN2", target_bir_lowering=False)
    D = lambda n, s: nc.dram_tensor(n, s, F32, kind="ExternalInput").ap()
    p64d = D("p64", [4, 64, TSEQ]); p128d = D("p128", [2, 128, TSEQ]); mu64d = D("mu64", [4, 64, 8]); mu128d = D("mu128", [2, 128, 8])
    pvd = D("pv", [64, 8]); a2d = D("a2h", [64, 64]); g2d = D("g2h", [128, 64]); w2d = D("w2pad", [2, 128, 64])
    cd = D("consts", [len(C64_NAMES), 128, 128])
    yd = nc.dram_tensor("y", [64, TSEQ], F32, kind="ExternalOutput").ap()
    P = Prog(nc)
    C = load_consts64(P, cd)
    bkt = [P.ps(f"bank{i}", [128, 512]) for i in range(8)]
    BK = lambda i: (bkt[i], f"bk{i}")
    raw = P.sb("raw", [128, TSEQ]); mix = P.sb("mix", [128, TSEQ])
    mu64 = P.sb("mu64", [64, 4, 8]); mu128 = P.sb("mu128", [128, 2, 8]); pv = P.sb("pv", [64, 8])
    for i in range(4):
        P.dma("sp", mu64[:, i, :], mu64d[i], writes=["mu64"])
    for i in range(2):
        P.dma("sp", mu128[:, i, :], mu128d[i], writes=["mu128"])
    P.dma("sp", pv[:], pvd, writes=["pv"])
    a2h = P.sb("a2h", [64, 64]); g2h = P.sb("g2h", [128, 64]); w2p = P.sb("w2p", [128, 2, 64])
    P.dma("sp", a2h[:], a2d, writes=["a2h"]); P.dma("sp", g2h[:], g2d, writes=["g2h"])
    for j in range(2):
        P.dma("sp", w2p[:, j, :], w2d[j], writes=["w2p"])
    omm64 = P.sb("omm64", [64, 4]); omm128 = P.sb("omm128", [128, 2])
    P.I("dve", "tensor_scalar", omm64[:], mu64[:, :, 0], -1.0, 1.0, ALU.mult, ALU.add, r=["mu64"], w=["omm64"])
    P.I("dve", "tensor_scalar", omm128[:], mu128[:, :, 0], -1.0, 1.0, ALU.mult, ALU.add, r=["mu128"], w=["omm128"])

    def token_mix(dst, dk, src_d, npart, mu, muk, omm, ommk, ti):
        R = slice(0, npart)
        P.dma("sp", raw[R, :], src_d, writes=["raw"])
        P.I("dve", "tensor_scalar", dst[R, :], raw[R, :], omm[R, ti:ti + 1], None, ALU.mult, r=["raw", ommk], w=[dk])
        def acc(o0, o1, i0, i1, mcol, eng="dve"):
            P.I(eng, "scalar_tensor_tensor", dst[R, o0:o1], raw[R, i0:i1], mu[R, ti, mcol:mcol + 1], dst[R, o0:o1], ALU.mult, ALU.add,
                r=["raw", muk, dk], w=[dk])
        acc(1, 256, 0, 255, 5); acc(0, 255, 1, 256, 6)
        acc(256 + 64, TSEQ, 256, TSEQ - 64, 3); acc(256, TSEQ - 64, 256 + 64, TSEQ, 4)
        dl = dst[R, 256:TSEQ].rearrange("p (r c) -> p r c", c=64); rl = raw[R, 256:TSEQ].rearrange("p (r c) -> p r c", c=64)
        P.I("dve", "scalar_tensor_tensor", dl[:, :, 1:64], rl[:, :, 0:63], mu[R, ti, 1:2], dl[:, :, 1:64], ALU.mult, ALU.add, r=["raw", muk, dk], w=[dk])
        P.I("dve", "scalar_tensor_tensor", dl[:, :, 0:63], rl[:, :, 1:64], mu[R, ti, 2:3], dl[:, :, 0:63], ALU.mult, ALU.add, r=["raw", muk, dk], w=[dk])

    rT = P.sb("rT", [64, TSEQ]); kT = P.sb("kT", [64, TSEQ]); vT = P.sb("vT", [64, TSEQ]); aT = P.sb("aT", [64, TSEQ])
    gT = P.sb("gT", [64, TSEQ]); bT = P.sb("bT", [64, TSEQ]); lwT = [P.sb(f"lwT{j}", [64, TSEQ]) for j in range(2)]
    token_mix(rT, "rT", p64d[0], 64, mu64, "mu64", omm64, "omm64", 0)
    token_mix(kT, "kT", p64d[1], 64, mu64, "mu64", omm64, "omm64", 1)
    token_mix(vT, "vT", p64d[2], 64, mu64, "mu64", omm64, "omm64", 2)
    NB = 256
    token_mix(mix, "mix", p64d[3], 64, mu64, "mu64", omm64, "omm64", 3)
    for i, t0 in enumerate(range(0, TSEQ, NB)):
        (pa, pk) = BK(i % 2)
        P.I("pe", "matmul", pa[0:64, 0:NB], a2h[:], mix[0:64, t0:t0 + NB], start=True, stop=True, r=["a2h", "mix"], w=[pk])
        P.I("act", "activation", aT[:, t0:t0 + NB], pa[0:64, 0:NB], AF.Sigmoid, bias=pv[:, 0:1], r=[pk, "pv"], w=["aT"])
    token_mix(mix, "mix", p128d[0], 128, mu128, "mu128", omm128, "omm128", 0)
    P.I("act", "activation", mix[:], mix[:], AF.Tanh, r=["mix"], w=["mix"])
    for j in range(2):
        for i, t0 in enumerate(range(0, TSEQ, NB)):
            (pa, pk) = BK(i % 2)
            P.I("pe", "matmul", pa[0:64, 0:NB], w2p[:, j, :], mix[:, t0:t0 + NB], start=True, stop=True, r=["w2p", "mix"], w=[pk])
            P.I("act", "activation", lwT[j][:, t0:t0 + NB], pa[0:64, 0:NB], AF.Sigmoid, bias=pv[:, 3 + j:4 + j], r=[pk, "pv"], w=[f"lwT{j}"])
        P.I("dve", "tensor_scalar", lwT[j][:], lwT[j][:], -float(np.exp(-0.5)), None, ALU.mult, r=[f"lwT{j}"], w=[f"lwT{j}"])
    token_mix(mix, "mix", p128d[1], 128, mu128, "mu128", omm128, "omm128", 1)
    P.I("act", "activation", mix[:], mix[:], AF.Sigmoid, r=["mix"], w=["mix"])
    for i, t0 in enumerate(range(0, TSEQ, NB)):
        (pa, pk) = BK(i % 2)
        P.I("pe", "matmul", pa[0:64, 0:NB], g2h[:], mix[:, t0:t0 + NB], start=True, stop=True, r=["g2h", "mix"], w=[pk])
        P.I("act", "activation", gT[:, t0:t0 + NB], pa[0:64, 0:NB], AF.Copy, r=[pk], w=["gT"])
    kk = mix
    P.I("dve", "tensor_scalar", kk[0:64, :], kT[:], pv[:, 1:2], None, ALU.mult, r=["kT", "pv"], w=["mix"])
    P.I("act", "activation", raw[0:64, :], kk[0:64, :], AF.Square, r=["mix"], w=["raw"])
    rs = [P.sb(f"rs{i}", [64, NB]) for i in range(2)]
    for i, t0 in enumerate(range(0, TSEQ, NB)):
        b = i % 2; (pa, pk) = BK(b)
        P.I("pe", "matmul", pa[0:64, 0:NB], C["ones"][0:64, 0:64], raw[0:64, t0:t0 + NB], start=True, stop=True, r=["raw", "c_ones"], w=[pk])
        P.I("dve", "tensor_scalar", rs[b][:], pa[0:64, 0:NB], 1e-6, None, ALU.add, r=[pk], w=[f"rs{b}"])
        P.I("dve", "reciprocal", rs[b][:], rs[b][:], r=[f"rs{b}"], w=[f"rs{b}"])
        P.I("act", "activation", rs[b][:], rs[b][:], AF.Sqrt, r=[f"rs{b}"], w=[f"rs{b}"])
        P.I("dve", "tensor_tensor", kk[0:64, t0:t0 + NB], kk[0:64, t0:t0 + NB], rs[b][:], ALU.mult, r=["mix", f"rs{b}"], w=["mix"])
    P.I("dve", "tensor_tensor", bT[:], kk[0:64, :], aT[:], ALU.mult, r=["mix", "aT"], w=["bT"])
    P.I("dve", "tensor_scalar", kk[0:64, :], kk[0:64, :], -1.0, None, ALU.mult, r=["mix"], w=["mix"])
    P.I("dve", "tensor_scalar", aT[:], aT[:], -1.0, pv[:, 2:3], ALU.add, ALU.mult, r=["aT", "pv"], w=["aT"])
    P.I("dve", "scalar_tensor_tensor", kT[:], aT[:], 1.0, kT[:], ALU.add, ALU.mult, r=["aT", "kT"], w=["kT"])
    avT = kk
    oacc = P.sb("oacc", [128, NPK, 64])
    H = P.sb("H", [64, 64])
    T_ = lambda n, sh: P.sb(n, sh)
    lwtok = T_("lwtok", [128, 64]); ea_tok = T_("ea_tok", [128, 64]); te_tok = T_("te_tok", [128, 64])
    ep = T_("ep", [64, 128]); em = T_("em", [64, 128]); eaT = T_("eaT", [64, 128])
    atl = T_("atl", [64, 128]); btl = T_("btl", [64, 128]); ktl = T_("ktl", [64, 128]); rtl = T_("rtl", [64, 128])
    Lm = T_("rwL", [128, 128]); AakT = T_("AakT", [128, 128]); ArbT = T_("ArbT", [128, 128]); ArkT = T_("ArkT", [128, 128])
    X = T_("rwX", [128, 128]); Bh = T_("Bh", [128, 64]); Kh = T_("Kh", [128, 64]); W1T = T_("W1T", [64, 128]); U = T_("U", [128, 64])
    pc2 = T_("pc2", [64, 2])
    tk = {n: T_(n + "_t", [128, 64]) for n in ("av", "b", "k", "v")}
    bkinv = {"P": BK(0), "Q": BK(1), "T": BK(2), "X": BK(3)}
    for d in range(2):
        sfx = "_f" if d == 0 else "_b"
        tri = C["tri" + sfx]; trik = "c_tri" + sfx
        m_strict_ts = C["sl"] if d == 0 else C["su"]; mk_ts = "c_sl" if d == 0 else "c_su"
        m_strict_st = C["su"] if d == 0 else C["sl"]; mk_st = "c_su" if d == 0 else "c_sl"
        m_incl_st = C["tri_f"] if d == 0 else C["tri_b"]; mk_in = trik
        order = list(range(NPK)) if d == 0 else [1, 0] + list(range(NPK - 1, 1, -1))
        P.I("pool", "memset", H[:], 0.0, w=["H"])
        for c in order:
            sl = slice(c * 128, (c + 1) * 128)
            (p0, k0), (p1, k1), (p2, k2), (p3, k3), (p4, k4), (p5, k5), (p6, k6), (p7, k7) = [BK(i) for i in range(8)]
            lw = lwT[d]; lwk = f"lwT{d}"
            for ii, (n, src, sk) in enumerate((("av", avT, "mix"), ("b", bT, "bT"), ("k", kT, "kT"), ("v", vT, "vT"))):
                (pa, pk) = BK(4 + ii)
                P.I("pe", "transpose", pa[:, 0:64], src[0:64, sl], C["ident"][0:64, 0:64], r=[sk, "c_ident"], w=[pk])
                if ii % 2:
                    P.I("act", "activation", tk[n][:], pa[:, 0:64], AF.Copy, r=[pk], w=[n + "_t"])
                else:
                    P.I("dve", "tensor_copy", tk[n][:], pa[:, 0:64], r=[pk], w=[n + "_t"])
            P.I("pe", "transpose", p0[:, 0:64], lw[:, sl], C["ident"][0:64, 0:64], r=[lwk, "c_ident"], w=[k0])
            P.I("dve", "tensor_copy", lwtok[:], p0[:, 0:64], r=[k0], w=["lwtok"])
            P.I("pe", "matmul", p1[:, 0:64], tri[:], lwtok[:], start=True, stop=True, r=[trik, "lwtok"], w=[k1])
            P.I("pe", "matmul", p2[:, 0:64], C["blk"][:], lwtok[:], start=True, stop=True, r=["c_blk", "lwtok"], w=[k2])
            P.I("pe", "matmul", p3[0:64, 0:128], lwtok[:], tri[:], start=True, stop=True, r=[trik, "lwtok"], w=[k3])
            P.I("dve", "tensor_tensor", ea_tok[:], p1[:, 0:64], lwtok[:], ALU.subtract, r=[k1, "lwtok"], w=["ea_tok"])
            P.I("act", "activation", ea_tok[:], ea_tok[:], AF.Exp, r=["ea_tok"], w=["ea_tok"])
            P.I("dve", "tensor_copy", te_tok[:], p1[:, 0:64], r=[k1], w=["te_tok"])
            P.I("dve", "tensor_tensor", te_tok[:], p2[:, 0:64], te_tok[:], ALU.subtract, r=[k2, "te_tok"], w=["te_tok"])
            P.I("act", "activation", te_tok[:], te_tok[:], AF.Exp, r=["te_tok"], w=["te_tok"])
            P.I("act", "activation", ep[:], p3[0:64, 0:128], AF.Exp, r=[k3], w=["ep"])
            P.I("act", "activation", em[:], p3[0:64, 0:128], AF.Exp, scale=-1.0, r=[k3], w=["em"])
            P.I("dve", "tensor_tensor", eaT[:], p3[0:64, 0:128], lw[:, sl], ALU.subtract, r=[k3, lwk], w=["eaT"])
            P.I("act", "activation", eaT[:], eaT[:], AF.Exp, r=["eaT"], w=["eaT"])
            cA, cB = (63, 127) if d == 0 else (0, 64)
            P.I("act", "activation", pc2[:, 0:1], p3[0:64, cA:cA + 1], AF.Exp, r=[k3], w=["pc2"])
            P.I("act", "activation", pc2[:, 1:2], p3[0:64, cB:cB + 1], AF.Exp, r=[k3], w=["pc2"])
            P.I("dve", "tensor_tensor", atl[:], avT[0:64, sl], eaT[:], ALU.mult, r=["mix", "eaT"], w=["atl"])
            P.I("dve", "tensor_tensor", btl[:], bT[:, sl], em[:], ALU.mult, r=["bT", "em"], w=["btl"])
            P.I("pool", "tensor_tensor", ktl[:], kT[:, sl], em[:], ALU.mult, r=["kT", "em"], w=["ktl"])
            P.I("pool", "tensor_tensor", rtl[:], rT[:, sl], ep[:], ALU.mult, r=["rT", "ep"], w=["rtl"])
            P.I("pe", "matmul", p4[:, 0:128], atl[:], btl[:], start=True, stop=True, r=["atl", "btl"], w=[k4])
            P.I("pe", "matmul", p5[:, 0:128], ktl[:], atl[:], start=True, stop=True, r=["ktl", "atl"], w=[k5])
            P.I("pe", "matmul", p6[:, 0:128], btl[:], rtl[:], start=True, stop=True, r=["btl", "rtl"], w=[k6])
            P.I("pe", "matmul", p7[:, 0:128], ktl[:], rtl[:], start=True, stop=True, r=["ktl", "rtl"], w=[k7])
            P.I("dve", "scalar_tensor_tensor", Lm[:], p4[:, 0:128], -1.0, m_strict_ts[:], ALU.mult, ALU.mult, r=[k4, mk_ts], w=["rwL"])
            P.I("dve", "tensor_tensor", AakT[:], p5[:, 0:128], m_strict_st[:], ALU.mult, r=[k5, mk_st], w=["AakT"])
            P.I("dve", "tensor_tensor", ArbT[:], p6[:, 0:128], m_incl_st[:], ALU.mult, r=[k6, mk_in], w=["ArbT"])
            P.I("dve", "tensor_tensor", ArkT[:], p7[:, 0:128], m_incl_st[:], ALU.mult, r=[k7, mk_in], w=["ArkT"])
            P.I("dve", "tensor_tensor", X[:, 0:64], tk["av"][:], ea_tok[:], ALU.mult, r=["av_t", "ea_tok"], w=["rwX"])
            P.I("pe", "matmul", p4[:, 0:64], AakT[:], tk["v"][:], start=True, stop=True, r=["AakT", "v_t"], w=[k4])
            P.I("act", "activation", X[:, 64:128], p4[:, 0:64], AF.Copy, r=[k4], w=["rwX"])
            P.I("pool", "tensor_tensor", Bh[:], tk["b"][:], te_tok[:], ALU.mult, r=["b_t", "te_tok"], w=["Bh"])
            P.I("pool", "tensor_tensor", Kh[:], tk["k"][:], te_tok[:], ALU.mult, r=["k_t", "te_tok"], w=["Kh"])
            tri_inverse_apply(P, C, Lm, X, 128, bkinv, "rw")
            P.I("pe", "transpose", p2[0:64, 0:128], X[:, 0:64], C["ident"][:], r=["rwX", "c_ident"], w=[k2])
            P.I("act", "activation", W1T[:], p2[0:64, 0:128], AF.Copy, r=[k2], w=["W1T"])
            for half in ((0, 1) if d == 0 else (1, 0)):
                rows = slice(half * 64, (half + 1) * 64)
                P.I("pe", "matmul", p5[:, 0:64], W1T[:], H[:], start=True, stop=True, r=["W1T", "H"], w=[k5])
                P.I("dve", "tensor_tensor", U[rows, :], X[rows, 64:128], p5[rows, 0:64], ALU.add, r=["rwX", k5], w=["U"])
                P.I("pe", "matmul", p6[:, 0:64], rtl[:], H[:], start=True, stop=False, r=["rtl", "H"], w=[k6])
                P.I("pe", "matmul", p6[:, 0:64], ArbT[rows, :], U[rows, :], start=False, stop=False, r=["ArbT", "U"], w=[k6])
                P.I("pe", "matmul", p6[:, 0:64], ArkT[rows, :], tk["v"][rows, :], start=False, stop=True, r=["ArkT", "v_t"], w=[k6])
                if d == 0:
                    P.I("act", "activation", oacc[rows, c, :], p6[rows, 0:64], AF.Copy, r=[k6], w=[f"oacc{c}"])
                else:
                    P.I("dve", "tensor_tensor", oacc[rows, c, :], oacc[rows, c, :], p6[rows, 0:64], ALU.add, r=[k6, f"oacc{c}"], w=[f"oacc{c}"])
                P.I("pe", "matmul", p7[0:64, 0:64], Bh[rows, :], U[rows, :], start=True, stop=False, r=["Bh", "U"], w=[k7])
                P.I("pe", "matmul", p7[0:64, 0:64], Kh[rows, :], tk["v"][rows, :], start=False, stop=True, r=["Kh", "v_t"], w=[k7])
                P.I("dve", "scalar_tensor_tensor", H[:], H[:], pc2[:, half:half + 1], p7[0:64, 0:64], ALU.mult, ALU.add, r=["H", "pc2", k7], w=["H"])
    allo = [f"oacc{c}" for c in range(NPK)]
    yT = raw; t1 = aT; t2 = bT
    for c in range(NPK):
        (pa, pk) = BK(c % 4)
        P.I("pe", "transpose", pa[0:64, 0:128], oacc[:, c, :], C["ident"][:], r=allo + ["c_ident"], w=[pk])
        P.I("act" if c % 2 else "dve", "activation" if c % 2 else "tensor_copy", yT[0:64, c * 128:(c + 1) * 128], pa[0:64, 0:128], *([AF.Copy] if c % 2 else []),
            r=[pk], w=["raw"])
    on64 = C["ones"][0:64, 0:64]
    for i, t0 in enumerate(range(0, TSEQ, NB)):
        ts = slice(t0, t0 + NB); b = i % 2
        (pm, km), (pvv, kvv), (pb, kb) = BK(b * 3), BK(b * 3 + 1), BK(b * 3 + 2)
        P.I("pe", "matmul", pm[0:64, 0:NB], on64, yT[0:64, ts], start=True, stop=True, r=["raw", "c_ones"], w=[km])
        P.I("dve", "scalar_tensor_tensor", yT[0:64, ts], pm[0:64, 0:NB], -1.0 / 64, yT[0:64, ts], ALU.mult, ALU.add, r=[km, "raw"], w=["raw"])
        P.I("act", "activation", t1[:, ts], yT[0:64, ts], AF.Square, r=["raw"], w=["aT"])
        P.I("pe", "matmul", pvv[0:64, 0:NB], on64, t1[:, ts], start=True, stop=True, r=["aT", "c_ones"], w=[kvv])
        P.I("dve", "tensor_scalar", rs[b][:], pvv[0:64, 0:NB], 1.0 / 64, 64e-5, ALU.mult, ALU.add, r=[kvv], w=[f"rs{b}"])
        P.I("dve", "reciprocal", rs[b][:], rs[b][:], r=[f"rs{b}"], w=[f"rs{b}"])
        P.I("act", "activation", rs[b][:], rs[b][:], AF.Sqrt, r=[f"rs{b}"], w=[f"rs{b}"])
        P.I("dve", "scalar_tensor_tensor", yT[0:64, ts], yT[0:64, ts], pv[:, 5:6], rs[b][:], ALU.mult, ALU.mult, r=["raw", "pv", f"rs{b}"], w=["raw"])
        P.I("dve", "scalar_tensor_tensor", t2[:, ts], rT[:, ts], pv[:, 7:8], kT[:, ts], ALU.mult, ALU.mult, r=["rT", "pv", "kT"], w=["bT"])
        P.I("pe", "matmul", pb[0:64, 0:NB], on64, t2[:, ts], start=True, stop=True, r=["bT", "c_ones"], w=[kb])
        P.I("dve", "tensor_tensor", t2[:, ts], pb[0:64, 0:NB], vT[:, ts], ALU.mult, r=[kb, "vT"], w=["bT"])
        P.I("dve", "scalar_tensor_tensor", yT[0:64, ts], yT[0:64, ts], pv[:, 6:7], t2[:, ts], ALU.add, ALU.add, r=["raw", "pv", "bT"], w=["raw"])
        P.I("dve", "tensor_tensor", yT[0:64, ts], yT[0:64, ts], gT[:, ts], ALU.mult, r=["raw", "gT"], w=["raw"])
    P.dma("sp", yd, yT[0:64, :], reads=["raw"], is_output=True)
    P.finish()
    return nc


def host_B3_inputs(pc_, L, b, head, prm):
    p = pc_[b]
    hc = slice(head * 64, (head + 1) * 64)
    mu = prm['rw_mu'][L]
    def seg(o, n):
        cols = np.arange(o, o + n); m = mu[cols]; cl = cols % 4
        tab = np.zeros((n, 8), np.float32)
        tab[:, 0] = m
        for j in range(4):
            tab[:, 1 + j] = np.where(cl == j, m, 0.0)
        tab[:, 5] = np.where(cl % 2 == 0, m, 0.0); tab[:, 6] = np.where(cl % 2 == 1, m, 0.0)
        return p[:, cols].T, tab
    s64 = [seg(head * 64, 64), seg(256 + head * 64, 64), seg(512 + head * 64, 64), seg(896, 64)]
    s128 = [seg(768, 128), seg(960, 128)]
    pv = np.zeros((64, 8), np.float32)
    pv[:, 0] = prm['rw_a0'][L][hc]; pv[:, 1] = prm['rw_k_k'][L][hc]; pv[:, 2] = prm['rw_k_a'][L][hc]
    pv[:, 3] = prm['rw_w0'][L][0][hc]; pv[:, 4] = prm['rw_w0'][L][1][hc]
    pv[:, 5] = prm['rw_ln_w'][L][hc]; pv[:, 6] = prm['rw_ln_b'][L][hc]; pv[:, 7] = prm['rw_r_k'][L][head]
    w2pad = np.zeros((2, 128, 64), np.float32)
    for j in range(2):
        w2pad[j, j * 64:(j + 1) * 64] = prm['rw_w2'][L][j][:, hc]
    A = lambda a: np.ascontiguousarray(a, dtype=np.float32)
    return {"p64": A(np.stack([s[0] for s in s64])), "p128": A(np.stack([s[0] for s in s128])),
            "mu64": A(np.stack([s[1] for s in s64])), "mu128": A(np.stack([s[1] for s in s128])),
            "pv": pv, "a2h": A(prm['rw_a2'][L][:, hc]), "g2h": A(prm['rw_g2'][L][:, hc]), "w2pad": w2pad,
            "consts": host_consts64()}


NTT = 17


def build_C1():
    nc = bass.Bass("TRN2", target_bir_lowering=False)
    D = lambda n, s: nc.dram_tensor(n, s, F32, kind="ExternalInput").ap()
    xTd = D("xT", [128, 8, NTOK]); yTd = D("yT", [128, 8, NTOK]); woutd = D("wout", [128, 8, 1024])
    modd = D("mod", [128, 8, 6]); nwd = D("nw", [128, 8]); wrd = D("wr", [128, 8, 32]); brd = D("br", [128, 32])
    O = lambda n, s: nc.dram_tensor(n, s, F32, kind="ExternalOutput").ap()
    xmd = O("xmT", [128, 8, NTOK]); h2d = O("h2T", [128, 8, NTOK]); Gd = O("G", [128, NTT, 32])
    P = Prog(nc)
    xT = P.sb("xT", [128, 8, NTOK]); ybf = P.sb("ybf", [128, 8, NTOK], BF16)
    hT32 = xT
    mod = P.sb("mod", [128, 8, 6]); nw = P.sb("nw", [128, 8]); wbf = P.sb("wbf", [128, 8, 1024], BF16)
    wr = P.sb("wr", [128, 8, 32]); br = P.sb("br", [128, 32])
    ones_bf = P.sb("ones_bf", [128, 128], BF16)
    P.I("pool", "memset", ones_bf[:], 1.0, w=["ones_bf"])
    for k in range(8):
        P.dma("sp", xT[:, k, :], xTd[:, k, :], writes=["xT"])
        P.dma("pool", ybf[:, k, :], yTd[:, k, :], writes=["ybf"])
        P.dma("pool", wbf[:, k, :], woutd[:, k, :], writes=["wbf"])
    for t, d, kk in ((mod, modd, "mod"), (nw, nwd, "nw"), (wr, wrd, "wr"), (br, brd, "br")):
        P.dma("sp", t[:], d, writes=[kk])
    pp = [P.ps(f"bank{i}", [128, 512]) for i in range(8)]
    i = 0
    for m in range(8):
        for it, (t0, n) in enumerate(TILES):
            b = i % 4; i += 1
            for k in range(8):
                P.I("pe", "matmul", pp[b][:, 0:n], wbf[:, k, m * 128:(m + 1) * 128], ybf[:, k, t0:t0 + n], start=(k == 0), stop=(k == 7),
                    r=["wbf", "ybf"], w=[f"bk{b}"])
            gcol = 3 if it == 0 else 0
            P.I("dve", "scalar_tensor_tensor", xT[:, m, t0:t0 + n], pp[b][:, 0:n], mod[:, m, gcol:gcol + 1], xT[:, m, t0:t0 + n], ALU.mult, ALU.add,
                r=[f"bk{b}", "mod", "xT"], w=["xT"])
    for k in range(8):
        P.dma("sp", xmd[:, k, :], xT[:, k, :], reads=["xT"], is_output=True)
    rms_modulate(P, xT, xT, mod, nw, ones_bf, shift_i=(1, 4), scale_i=(2, 5), tagp="n2", psb=(pp[4], pp[5]), hkey="xT")
    for k in range(8):
        P.dma("sp", h2d[:, k, :], hT32[:, k, :], reads=["xT"], is_output=True)
    G = P.sb("G", [128, NTT, 32]); lg = P.sb("lg", [128, NTT, 32]); m8 = P.sb("m8", [128, NTT, 8]); nmx = P.sb("nmx", [128, NTT])
    msk = P.sb("msk", [128, NTT, 32]); ssum = P.sb("ssum", [128, NTT])
    for tt in range(NTT):
        b = 6 + tt % 2
        for k in range(8):
            P.I("pe", "matmul", pp[b][:, 0:32], hT32[:, k, tt * 128:(tt + 1) * 128], wr[:, k, :], start=(k == 0), stop=(k == 7),
                r=["xT", "wr"], w=[f"bk{b}"])
        P.I("dve", "tensor_tensor", lg[:, tt, :], pp[b][:, 0:32], br[:], ALU.add, r=[f"bk{b}", "br"], w=["lg"])
        P.I("dve", "max", m8[:, tt, :], lg[:, tt, :], r=["lg"], w=["m8"])
        P.I("dve", "tensor_scalar", msk[:, tt, :], lg[:, tt, :], m8[:, tt, 3:4], None, ALU.is_ge, r=["lg", "m8"], w=["msk"])
        P.I("dve", "tensor_scalar", nmx[:, tt:tt + 1], m8[:, tt, 0:1], -1.0, None, ALU.mult, r=["m8"], w=["nmx"])
        P.I("act", "activation", G[:, tt, :], lg[:, tt, :], AF.Exp, bias=nmx[:, tt:tt + 1], r=["lg", "nmx"], w=["G"])
        P.I("dve", "tensor_tensor", G[:, tt, :], G[:, tt, :], msk[:, tt, :], ALU.mult, r=["G", "msk"], w=["G"])
        P.I("dve", "tensor_reduce", ssum[:, tt:tt + 1], G[:, tt, :], AX.X, ALU.add, r=["G"], w=["ssum"])
        P.I("dve", "reciprocal", ssum[:, tt:tt + 1], ssum[:, tt:tt + 1], r=["ssum"], w=["ssum"])
        P.I("dve", "tensor_scalar", G[:, tt, :], G[:, tt, :], ssum[:, tt:tt + 1], None, ALU.mult, r=["G", "ssum"], w=["G"])
    P.dma("sp", Gd, G[:], reads=["G"], is_output=True)
    P.finish()
    return nc


NT2 = 1088
T2 = [(0, 512), (512, 512), (1024, 64)]
ST2 = [(i * 128, 128) for i in range(8)] + [(1024, 64)]


def build_C2(NB=16, NE=4):
    nc = bass.Bass("TRN2", target_bir_lowering=False)
    D = lambda n, s: nc.dram_tensor(n, s, F32, kind="ExternalInput").ap()
    h2d = D("h2T", [128, 8, NB * NT2]); Gd = D("G", [128, NB, 9, NE]); wgud = D("wgu", [NE, 128, 8, 2048]); wdd = D("wd", [NE, 128, 8, 1024])
    bgud = D("bgu", [128, NE, 16]); bdd = D("bd", [NE, 1024]); idd = D("ident", [128, 128])
    fd = nc.dram_tensor("f", [NB, 128, 9, 1024], F32, kind="ExternalOutput").ap()
    P = Prog(nc)
    hbf = [P.sb(f"hbf{i}", [128, 8, NT2], BF16) for i in range(2)]
    G = P.sb("G", [128, NB, 9, NE]); bgu = P.sb("bgu", [128, NE, 16]); bd = P.sb("bd", [NE, 1024]); ident = P.sb("ident", [128, 128])
    P.dma("sp", G[:], Gd, writes=["G"]); P.dma("sp", bgu[:], bgud, writes=["bgu"]); P.dma("sp", bd[:], bdd, writes=["bd"]); P.dma("sp", ident[:], idd, writes=["ident"])
    wgu = [P.sb(f"wgu{i}", [128, 8, 2048], BF16) for i in range(2)]; wd = [P.sb(f"wd{i}", [128, 8, 1024], BF16) for i in range(2)]
    act = P.sb("act", [128, 8, NT2], BF16); acc = P.sb("acc", [128, 9, 1024])
    gc_ = [P.sb(f"gc{i}", [128, 512]) for i in range(2)]; sg = [P.sb(f"sg{i}", [128, 512]) for i in range(2)]
    uc = [P.sb(f"uc{i}", [128, 512]) for i in range(2)]
    GT = P.sb("GT", [NE, 128])
    pp = [P.ps(f"bank{i}", [128, 512]) for i in range(8)]

    wgus = [nc.dram_tensor(f"wgu_bf{e}", [128, 8, 2048], BF16).ap() for e in range(NE)]
    wds = [nc.dram_tensor(f"wd_bf{e}", [128, 8, 1024], BF16).ap() for e in range(NE)]
    for e in range(NE):
        for k in range(8):
            P.dma("pool", wgus[e][:, k, :], wgud[e, :, k, :], writes=[f"wgus{e}"])
            P.dma("pool", wds[e][:, k, :], wdd[e, :, k, :], writes=[f"wds{e}"])

    def load_w(j):
        e = j % NE; b = j % 2
        for k in range(0, 8, 2):
            P.dma("sp", wgu[b][:, k:k + 2, :], wgus[e][:, k:k + 2, :], reads=[f"wgus{e}"], writes=[f"wgu{b}"])
        for k in range(0, 8, 4):
            P.dma("act", wd[b][:, k:k + 4, :], wds[e][:, k:k + 4, :], reads=[f"wds{e}"], writes=[f"wd{b}"])

    def load_h(blk):
        for k in range(8):
            P.dma("pool", hbf[blk % 2][:, k, :], h2d[:, k, blk * NT2:(blk + 1) * NT2], writes=[f"hbf{blk%2}"])
    load_h(0); load_w(0)
    it = 0; jt = 0; j = 0
    for blk in range(NB):
        hb = hbf[blk % 2]; hk = f"hbf{blk%2}"
        if blk + 1 < NB:
            load_h(blk + 1)
        for e in range(NE):
            b = j % 2
            if j + 1 < NB * NE:
                load_w(j + 1)
            j += 1
            for fc in range(8):
                for (t0, n) in T2:
                    s = it % 2; it += 1
                    pg, pu = pp[2 * s], pp[2 * s + 1]; kg, ku = f"bk{2*s}", f"bk{2*s+1}"
                    for k in range(8):
                        P.I("pe", "matmul", pg[:, 0:n], wgu[b][:, k, fc * 128:(fc + 1) * 128], hb[:, k, t0:t0 + n], start=(k == 0), stop=(k == 7),
                            r=[f"wgu{b}", hk], w=[kg])
                    for k in range(8):
                        P.I("pe", "matmul", pu[:, 0:n], wgu[b][:, k, 1024 + fc * 128:1024 + (fc + 1) * 128], hb[:, k, t0:t0 + n], start=(k == 0), stop=(k == 7),
                            r=[f"wgu{b}", hk], w=[ku])
                    P.I("dve", "tensor_scalar", gc_[s][:, 0:n], pg[:, 0:n], bgu[:, e, fc:fc + 1], 7.0, ALU.add, ALU.min, r=[kg, "bgu"], w=[f"gc{s}"])
                    P.I("act", "activation", sg[s][:, 0:n], gc_[s][:, 0:n], AF.Sigmoid, scale=1.702, r=[f"gc{s}"], w=[f"sg{s}"])
                    P.I("dve", "tensor_scalar", uc[s][:, 0:n], pu[:, 0:n], bgu[:, e, 8 + fc:9 + fc], 7.0, ALU.add, ALU.min, r=[ku, "bgu"], w=[f"uc{s}"])
                    P.I("dve", "tensor_scalar", uc[s][:, 0:n], uc[s][:, 0:n], -7.0, 1.0, ALU.max, ALU.add, r=[f"uc{s}"], w=[f"uc{s}"])
                    P.I("dve", "tensor_tensor", gc_[s][:, 0:n], gc_[s][:, 0:n], sg[s][:, 0:n], ALU.mult, r=[f"gc{s}", f"sg{s}"], w=[f"gc{s}"])
                    P.I("dve", "tensor_tensor", act[:, fc, t0:t0 + n], gc_[s][:, 0:n], uc[s][:, 0:n], ALU.mult, r=[f"gc{s}", f"uc{s}"], w=["act"])
            for si, (s0, sn) in enumerate(ST2):
                for half in range(2):
                    pb = 4 + jt % 4; jt += 1
                    hs = slice(half * 512, (half + 1) * 512)
                    for fc in range(8):
                        P.I("pe", "matmul", pp[pb][0:sn, 0:512], act[:, fc, s0:s0 + sn], wd[b][:, fc, hs], start=(fc == 0), stop=(fc == 7),
                            r=["act", f"wd{b}"], w=[f"bk{pb}"])
                    if e == 0:
                        P.I("dve", "tensor_scalar", acc[0:sn, si, hs], pp[pb][0:sn, 0:512], G[0:sn, blk, si, e:e + 1], None, ALU.mult,
                            r=[f"bk{pb}", "G"], w=["acc"])
                    else:
                        P.I("dve", "scalar_tensor_tensor", acc[0:sn, si, hs], pp[pb][0:sn, 0:512], G[0:sn, blk, si, e:e + 1], acc[0:sn, si, hs],
                            ALU.mult, ALU.add, r=[f"bk{pb}", "G", "acc"], w=["acc"])
        for si, (s0, sn) in enumerate(ST2):
            P.I("pe", "transpose", pp[0][0:NE, 0:sn], G[0:sn, blk, si, :], ident[0:sn, 0:sn], r=["G", "ident"], w=["bk0"])
            P.I("dve", "tensor_copy", GT[:, 0:sn], pp[0][0:NE, 0:sn], r=["bk0"], w=["GT"])
            for half in range(2):
                pb = 1 + half; hs = slice(half * 512, (half + 1) * 512)
                P.I("pe", "matmul", pp[pb][0:sn, 0:512], GT[:, 0:sn], bd[:, hs], start=True, stop=True, r=["GT", "bd"], w=[f"bk{pb}"])
                P.I("dve", "tensor_tensor", acc[0:sn, si, hs], acc[0:sn, si, hs], pp[pb][0:sn, 0:512], ALU.add, r=[f"bk{pb}", "acc"], w=["acc"])
        P.I("pool", "memset", acc[64:128, 8, :], 0.0, r=["acc"], w=["acc"]) if blk == 0 else None
        P.dma("sp", fd[blk], acc[:], reads=["acc"], is_output=True)
    P.finish()
    return nc


def build_D():
    nc = bass.Bass("TRN2", target_bir_lowering=False)
    D = lambda n, s: nc.dram_tensor(n, s, F32, kind="ExternalInput").ap()
    xmd = D("xmT", [128, 8, NTOK]); fTd = D("fT", [8, 128, 8, NTOK]); modd = D("mod", [128, 8, 2]); nwd = D("nw", [128, 8])
    od = nc.dram_tensor("oT", [128, 8, NTOK], F32, kind="ExternalOutput").ap()
    P = Prog(nc)
    xT = P.sb("xT", [128, 8, NTOK]); fT = P.sb("fT", [128, 8, NTOK]); mod = P.sb("mod", [128, 8, 2]); nw = P.sb("nw", [128, 8])
    ones_bf = P.sb("ones_bf", [128, 128], BF16)
    P.I("pool", "memset", ones_bf[:], 1.0, w=["ones_bf"])
    for k in range(8):
        P.dma("sp", xT[:, k, :], xmd[:, k, :], writes=["xT"])
    P.dma("sp", mod[:], modd, writes=["mod"]); P.dma("sp", nw[:], nwd, writes=["nw"])
    for c in range(8):
        for k in range(8):
            P.dma("sp", fT[:, k, :], fTd[c, :, k, :], writes=["fT"])
        add_gated(P, xT, fT, mod, 0, 1)
    sq = [P.sb(f"sq{i}", [128, 8, 512], BF16) for i in range(2)]; rs = [P.sb(f"rs{i}", [128, 512]) for i in range(2)]
    ss = [P.ps(f"bank{i}", [128, 512]) for i in range(2)]
    for it, (t0, n) in enumerate(TILES):
        b = it % 2
        for k in range(8):
            P.I("act", "activation", sq[b][:, k, 0:n], xT[:, k, t0:t0 + n], AF.Square, r=["xT"], w=[f"sq{b}"])
        for k in range(8):
            P.I("pe", "matmul", ss[b][:, 0:n], ones_bf[:], sq[b][:, k, 0:n], start=(k == 0), stop=(k == 7), r=[f"sq{b}", "ones_bf"], w=[f"bk{b}"])
        P.I("dve", "tensor_scalar", rs[b][:, 0:n], ss[b][:, 0:n], 1.0 / 1024, 1e-6, ALU.mult, ALU.add, r=[f"bk{b}"], w=[f"rs{b}"])
        P.I("dve", "reciprocal", rs[b][:, 0:n], rs[b][:, 0:n], r=[f"rs{b}"], w=[f"rs{b}"])
        P.I("act", "activation", rs[b][:, 0:n], rs[b][:, 0:n], AF.Sqrt, r=[f"rs{b}"], w=[f"rs{b}"])
        for k in range(8):
            P.I("dve", "scalar_tensor_tensor", fT[:, k, t0:t0 + n], xT[:, k, t0:t0 + n], nw[:, k:k + 1], rs[b][:, 0:n], ALU.mult, ALU.mult,
                r=["xT", "nw", f"rs{b}"], w=["fT"])
    for k in range(8):
        P.dma("sp", od[:, k, :], fT[:, k, :], reads=["fT"], is_output=True)
    P.finish()
    return nc


def add_gated(P, xT, fT, mod, col_l, col_c):
    for k in range(8):
        P.I("dve", "scalar_tensor_tensor", xT[:, k, 0:128], fT[:, k, 0:128], mod[:, k, col_c:col_c + 1], xT[:, k, 0:128], ALU.mult, ALU.add,
            r=["fT", "mod", "xT"], w=["xT"])
        P.I("dve", "scalar_tensor_tensor", xT[:, k, 128:NTOK], fT[:, k, 128:NTOK], mod[:, k, col_l:col_l + 1], xT[:, k, 128:NTOK], ALU.mult, ALU.add,
            r=["fT", "mod", "xT"], w=["xT"])


I32 = mybir.dt.int32


def build_C2s(NTILE=136, NE=4, caps=(4608, 4608, 4608, 4608)):
    NTOKA = NTILE * 128; NJs = [c_ // 128 for c_ in caps]; NJ = max(NJs); HALF = 9 * 128
    nc = bass.Bass("TRN2", target_bir_lowering=False)
    D = lambda n, s: nc.dram_tensor(n, s, F32, kind="ExternalInput").ap()
    h2d = D("h2tok", [NTOKA + 128, 1024]); Gd = D("Gm", [128, NTILE, NE]); tokd = D("tokid", [128, NTILE]); Ld = D("lst", [128, 128])
    padd = D("padtab", [128, NJ, 2]); idd = D("ident", [128, 128]); dumpd = D("dump", [128, NE])
    wgud = D("wgu", [NE, 128, 8, 2048]); wdd = D("wd", [NE, 128, 8, 1024]); bgud = D("bgu", [128, NE, 16]); bdbd = D("bdb", [NE, 128, 1024])
    fd = nc.dram_tensor("fpart", [NTOKA + 128, 1024], F32, kind="ExternalOutput").ap()
    tab = [nc.dram_tensor(f"slot_tab{e}", [caps[e] + 128, 2], F32).ap() for e in range(NE)]
    P = Prog(nc)
    pp = [P.ps(f"bank{i}", [128, 512]) for i in range(8)]
    z = P.sb("z", [128, 1024]); ones = P.sb("ones", [128, 128]); lst = P.sb("lst", [128, 128]); ident = P.sb("ident", [128, 128])
    P.I("pool", "memset", z[:], 0.0, w=["z"]); P.I("pool", "memset", ones[:], 1.0, w=["ones"])
    P.dma("sp", lst[:], Ld, writes=["lst"]); P.dma("sp", ident[:], idd, writes=["ident"])
    for r in range(NTILE + 1):
        P.dma("sp", fd[r * 128:(r + 1) * 128, :], z[:], reads=["z"], writes=["fpart"])
    Gm = P.sb("Gm", [128, NTILE, NE]); tokid = P.sb("tokid", [128, NTILE]); padt = P.sb("padt", [128, NJ, 2]); bgu = P.sb("bgu", [128, NE, 16])
    P.dma("act", Gm[:], Gd, writes=["Gm"]); P.dma("act", tokid[:], tokd, writes=["tokid"]); P.dma("act", padt[:], padd, writes=["padt"])
    P.dma("act", bgu[:], bgud, writes=["bgu"])
    dump = P.sb("dump", [128, NE]); P.dma("act", dump[:], dumpd, writes=["dump"])
    wgu = [P.sb(f"wgu{i}", [128, 8, 2048], BF16) for i in range(2)]; wd = [P.sb(f"wd{i}", [128, 8, 1024], BF16) for i in range(2)]
    bdb = [P.sb(f"bdb{i}", [128, 1024]) for i in range(2)]
    hsel = P.sb("hsel", [128, 8, HALF], BF16); act = P.sb("act", [128, 8, HALF], BF16)
    hg = [P.sb(f"hg{i}", [128, 1024]) for i in range(2)]; yst = [P.sb(f"yst{i}", [128, 1024]) for i in range(2)]
    gc_ = [P.sb(f"gc{i}", [128, 512]) for i in range(2)]; sg = [P.sb(f"sg{i}", [128, 512]) for i in range(2)]
    uc = [P.sb(f"uc{i}", [128, 512]) for i in range(2)]

    def load_w(e):
        b = e % 2
        for k in range(8):
            P.dma("pool", wgu[b][:, k, :], wgud[e, :, k, :], writes=[f"wgu{b}"])
        for k in range(8):
            P.dma("pool", wd[b][:, k, :], wdd[e, :, k, :], writes=[f"wd{b}"])
        P.dma("act", bdb[b][:], bdbd[e], writes=[f"bdb{b}"])
    load_w(0)
    m = P.sb("m", [128, NE, NTILE]); cs = P.sb("cs", [128, NE, NTILE]); inc = P.sb("inc", [128, NE, NTILE]); rk = P.sb("rk", [128, NE, NTILE])
    idx = P.sb("idx", [128, NE, NTILE], I32); pr = P.sb("pr", [128, NTILE, NE, 2]); onesw = P.sb("onesw", [128, NTILE])
    P.I("pool", "memset", onesw[:], 1.0, w=["onesw"])
    for e in range(NE):
        P.I("dve", "tensor_scalar", m[:, e, :], Gm[:, :, e], 0.0, None, ALU.is_gt, r=["Gm"], w=["m"])
        P.I("pe", "matmul", pp[0][:, 0:NTILE], lst[:], m[:, e, :], start=True, stop=True, r=["lst", "m"], w=["bk0"])
        P.I("pe", "matmul", pp[1][:, 0:NTILE], ones[:], m[:, e, :], start=True, stop=True, r=["ones", "m"], w=["bk1"])
        P.I("act", "activation", cs[:, e, :], pp[1][:, 0:NTILE], AF.Copy, r=["bk1"], w=["cs"])
        P.I("dve", "tensor_tensor_scan", inc[:, e, :], onesw[:], cs[:, e, :], 0.0, ALU.mult, ALU.add, r=["onesw", "cs"], w=["inc"])
        P.I("dve", "tensor_tensor", rk[:, e, :], pp[0][:, 0:NTILE], inc[:, e, :], ALU.add, r=["bk0", "inc"], w=["rk"])
        P.I("dve", "tensor_tensor", rk[:, e, :], rk[:, e, :], cs[:, e, :], ALU.subtract, r=["rk", "cs"], w=["rk"])
        P.I("dve", "tensor_scalar", cs[:, e, :], rk[:, e, :], float(caps[e]), None, ALU.is_lt, r=["rk", "cs"], w=["cs"])
        P.I("dve", "tensor_tensor", cs[:, e, :], cs[:, e, :], m[:, e, :], ALU.mult, r=["cs", "m"], w=["cs"])
        P.I("dve", "tensor_scalar", rk[:, e, :], rk[:, e, :], dump[:, e:e + 1], None, ALU.subtract, r=["rk", "dump"], w=["rk"])
        P.I("dve", "tensor_tensor", rk[:, e, :], rk[:, e, :], cs[:, e, :], ALU.mult, r=["rk", "cs"], w=["rk"])
        P.I("dve", "tensor_scalar", rk[:, e, :], rk[:, e, :], dump[:, e:e + 1], None, ALU.add, r=["rk", "dump"], w=["rk"])
        P.I("dve", "tensor_copy", idx[:, e, :], rk[:, e, :], r=["rk"], w=["idx"])
        P.I("pool", "tensor_copy", pr[:, :, e, 0], tokid[:], r=["tokid"], w=["pr"])
        P.I("pool", "tensor_copy", pr[:, :, e, 1], Gm[:, :, e], r=["Gm"], w=["pr"])
    for e in range(NE):
        P.dma("act", tab[e][0:caps[e], :].rearrange("(p j) c -> p j c", j=NJs[e]), padt[:, 0:NJs[e], :], reads=["padt"], writes=[f"tabinit{e}"])
    for t in range(NTILE):
        for e in range(NE):
            P.idma(tab[e], pr[:, t, e, :], out_idx=idx[:, e, t:t + 1], reads=["pr", "idx", f"tabinit{e}"], writes=[f"sc{e}_{t}"])
    tabsb = P.sb("tabsb", [128, NE, NJ, 2]); tok_i = P.sb("tok_i", [128, NE, NJ], I32); gate = P.sb("gate", [128, NE, NJ])
    for e in range(NE):
        P.dma("act", tabsb[:, e, 0:NJs[e], :], tab[e][0:caps[e], :].rearrange("(p j) c -> p j c", j=NJs[e]), reads=[f"sc{e}_{t}" for t in range(NTILE)], writes=["tabsb"])
    P.I("pool", "memset", tabsb[:], 0.0, w=["tabsb"]) if False else None
    P.I("dve", "tensor_copy", tok_i[:], tabsb[:, :, :, 0], r=["tabsb"], w=["tok_i"])
    P.I("dve", "tensor_copy", gate[:], tabsb[:, :, :, 1], r=["tabsb"], w=["gate"])
    it = 0; jt = 0; gi = 0; ti = 0
    for e in range(NE):
        b = e % 2
        if e + 1 < NE:
            load_w(e + 1)
        for j0 in range(0, NJs[e], 9):
            NJH = min(9, NJs[e] - j0); SPN = NJH * 128
            T3 = [(t0, min(512, SPN - t0)) for t0 in range(0, SPN, 512)]
            for jj in range(NJH):
                j = j0 + jj; g = gi % 2; gi += 1
                P.idma(hg[g][:], h2d, in_idx=tok_i[:, e, j:j + 1], reads=["tok_i"], writes=[f"hg{g}"])
                for k in range(8):
                    pb = 4 + ti % 4; ti += 1
                    P.I("pe", "transpose", pp[pb][:, 0:128], hg[g][:, k * 128:(k + 1) * 128], ident[:], r=[f"hg{g}", "ident"], w=[f"bk{pb}"])
                    if ti % 2:
                        P.I("act", "activation", hsel[:, k, jj * 128:(jj + 1) * 128], pp[pb][:, 0:128], AF.Copy, r=[f"bk{pb}"], w=["hsel"])
                    else:
                        P.I("dve", "tensor_copy", hsel[:, k, jj * 128:(jj + 1) * 128], pp[pb][:, 0:128], r=[f"bk{pb}"], w=["hsel"])
            for fc in range(8):
                for (t0, n) in T3:
                    s = it % 2; it += 1
                    pg, pu = pp[2 * s], pp[2 * s + 1]; kg, ku = f"bk{2*s}", f"bk{2*s+1}"
                    for k in range(8):
                        P.I("pe", "matmul", pg[:, 0:n], wgu[b][:, k, fc * 128:(fc + 1) * 128], hsel[:, k, t0:t0 + n], start=(k == 0), stop=(k == 7),
                            r=[f"wgu{b}", "hsel"], w=[kg])
                    for k in range(8):
                        P.I("pe", "matmul", pu[:, 0:n], wgu[b][:, k, 1024 + fc * 128:1024 + (fc + 1) * 128], hsel[:, k, t0:t0 + n], start=(k == 0), stop=(k == 7),
                            r=[f"wgu{b}", "hsel"], w=[ku])
                    P.I("dve", "tensor_scalar", gc_[s][:, 0:n], pg[:, 0:n], bgu[:, e, fc:fc + 1], 7.0, ALU.add, ALU.min, r=[kg, "bgu"], w=[f"gc{s}"])
                    P.I("act", "activation", sg[s][:, 0:n], gc_[s][:, 0:n], AF.Sigmoid, scale=1.702, r=[f"gc{s}"], w=[f"sg{s}"])
                    P.I("dve", "tensor_scalar", uc[s][:, 0:n], pu[:, 0:n], bgu[:, e, 8 + fc:9 + fc], 7.0, ALU.add, ALU.min, r=[ku, "bgu"], w=[f"uc{s}"])
                    P.I("dve", "tensor_scalar", uc[s][:, 0:n], uc[s][:, 0:n], -7.0, 1.0, ALU.max, ALU.add, r=[f"uc{s}"], w=[f"uc{s}"])
                    P.I("dve", "tensor_tensor", gc_[s][:, 0:n], gc_[s][:, 0:n], sg[s][:, 0:n], ALU.mult, r=[f"gc{s}", f"sg{s}"], w=[f"gc{s}"])
                    P.I("dve", "tensor_tensor", act[:, fc, t0:t0 + n], gc_[s][:, 0:n], uc[s][:, 0:n], ALU.mult, r=[f"gc{s}", f"uc{s}"], w=["act"])
            for jj in range(NJH):
                j = j0 + jj; y = jt % 2
                for hh in range(2):
                    pb = 4 + jt % 4; jt += 1
                    hs = slice(hh * 512, (hh + 1) * 512)
                    for fc in range(8):
                        P.I("pe", "matmul", pp[pb][:, 0:512], act[:, fc, jj * 128:(jj + 1) * 128], wd[b][:, fc, hs], start=(fc == 0), stop=(fc == 7),
                            r=["act", f"wd{b}"], w=[f"bk{pb}"])
                    P.I("dve", "tensor_tensor", yst[jj % 2][:, hs], pp[pb][:, 0:512], bdb[b][:, hs], ALU.add, r=[f"bk{pb}", f"bdb{b}"], w=[f"yst{jj%2}"])
                P.I("pool", "tensor_scalar", yst[jj % 2][:], yst[jj % 2][:], gate[:, e, j:j + 1], None, ALU.mult, r=[f"yst{jj%2}", "gate"], w=[f"yst{jj%2}"])
                P.idma(fd, yst[jj % 2][:], out_idx=tok_i[:, e, j:j + 1], reads=[f"yst{jj%2}", "tok_i", "fpart"], writes=["fpart"], is_output=True, compute_op=ALU.add)
    P.finish()
    return nc


def host_C2s_consts(NTILE=136, caps=(4608, 4608, 4608, 4608)):
    NJ = max(caps) // 128
    tokid = (np.arange(NTILE)[None, :] * 128 + np.arange(128)[:, None]).astype(np.float32)
    p_ = np.arange(128)[:, None]; q_ = np.arange(128)[None, :]
    lst = (p_ < q_).astype(np.float32)
    padtab = np.zeros((128, NJ, 2), np.float32); padtab[:, :, 0] = NTILE * 128 + np.arange(128)[:, None]
    return {"tokid": tokid, "lst": lst, "padtab": padtab, "ident": np.eye(128, dtype=np.float32), "dump": np.stack([c_ + np.arange(128, dtype=np.float32) for c_ in caps], 1)}


def _fm(tok):
    return np.ascontiguousarray(tok.T.reshape(8, 128, -1).transpose(1, 0, 2))


def _tok(fm):
    return fm.transpose(2, 1, 0).reshape(fm.shape[2], -1)


def _vec(v):
    return np.ascontiguousarray(np.asarray(v, np.float32).reshape(8, 128).T)


_PROGS = {}
C2S_CAP = 6144
MOE_SPARSE = True


def _prog(name, builder, *a):
    key = (name,) + a
    if key not in _PROGS:
        _PROGS[key] = builder(*a)
    return _PROGS[key]


def _run(nc, maps):
    res = run_bass_kernel_spmd(nc, maps, core_ids=list(range(len(maps))))
    return res.results


def kernel(**inp):
    prm = {k: np.asarray(v, dtype=np.float32) for k, v in inp.items()}
    x, c, ctx, c_ctx = prm['x'], prm['c'], prm['ctx'], prm['c_ctx']
    NC = 8
    A_ = lambda a: np.ascontiguousarray(a, dtype=np.float32)
    cs = np.zeros((128, 8, 5), np.float32)
    for v in range(4):
        cs[:, :, v] = _vec(c[v])
    cs[:, :, 4] = _vec(c_ctx)
    items = [(l, fc) for l in range(2) for fc in range(48)]
    maps = []
    for i in range(NC):
        its = items[i * 12:(i + 1) * 12]
        wm = np.stack([prm['w_mod'][l][:, fc * 128:(fc + 1) * 128].reshape(8, 128, 128).transpose(1, 0, 2) for l, fc in its])
        bm = np.stack([prm['b_mod'][l][fc * 128:(fc + 1) * 128] for l, fc in its], 1)
        maps.append({"cs": cs, "wm": A_(wm), "bm": A_(bm)})
    res = _run(_prog("M", build_M), maps)
    modv = {}
    for i in range(NC):
        for j, (l, fc) in enumerate(items[i * 12:(i + 1) * 12]):
            modv[(l, fc)] = res[i]["modT"][:, j, :]
    mod6 = [[np.stack([modv[(l, i6 * 8 + k)] for k in range(8)], 1) for i6 in range(6)] for l in range(2)]

    def core_tokens(arr_c, arr_l, b, half):
        return np.concatenate([arr_c[b, half * 128:(half + 1) * 128], arr_l[b, half * 2048:(half + 1) * 2048]], 0)

    xm = [_fm(core_tokens(ctx, x, j // 2, j % 2)) for j in range(NC)]
    fparts = None
    consts64 = host_consts64()
    for l in range(2):
        win = np.zeros((1024, NCT * 128), np.float32); win[:, :4184] = prm['w_in'][l]
        win = A_(win.reshape(8, 128, NCT * 128).transpose(1, 0, 2)); nw1 = _vec(prm['norm1_w'][l])
        maps = []
        for j in range(NC):
            b = j // 2
            g5l = mod6[l - 1][5][:, :, b] if l > 0 else np.zeros((128, 8), np.float32)
            g5c = mod6[l - 1][5][:, :, 4] if l > 0 else np.zeros((128, 8), np.float32)
            mod = np.stack([mod6[l][0][:, :, b], mod6[l][1][:, :, b], mod6[l][0][:, :, 4], mod6[l][1][:, :, 4], g5l, g5c], -1)
            m = {"xT": xm[j], "mod": A_(mod), "nw": nw1, "win": win}
            if l > 0:
                m["fT"] = A_(np.stack([_fm(fparts[cc][j * NTOK:(j + 1) * NTOK]) for cc in range(NC)]))
            maps.append(m)
        res = _run(_prog("A", build_A, l == 0), maps)
        xcur = [res[j]["xout"] for j in range(NC)]
        ptok = [res[j]["pT"].reshape(NCT * 128, NTOK).T[:, :4184] for j in range(NC)]
        del res
        pfull = np.stack([np.concatenate([ptok[2 * b][:128], ptok[2 * b + 1][:128], ptok[2 * b][128:], ptok[2 * b + 1][128:]], 0) for b in range(4)])
        del ptok
        yall = np.zeros((4, TSEQ, 1024), np.float32)
        pa = pfull[:, :, 0:1032]
        res = _run(_prog("B1", build_B1), [host_B1_inputs(pa, l, j // 2, j % 2, prm) for j in range(NC)])
        for j in range(NC):
            yall[j // 2, :, (j % 2) * 128:(j % 2 + 1) * 128] = res[j]["yT"].T
        pb = pfull[:, :, 1032:3096]
        for rnd in range(2):
            its = [(i // 4, i % 4) for i in range(rnd * 8, rnd * 8 + 8)]
            maps = [host_B2_inputs(pb, l, b, h, prm) for b, h in its]
            for m in maps:
                m["consts"] = consts64
            res = _run(_prog("B2", build_B2), maps)
            for (b, h), r in zip(its, res):
                yall[b, :, 256 + h * 128:256 + (h + 1) * 128] = host_B2_output(r["y"])
        pc = pfull[:, :, 3096:4184]
        for rnd in range(2):
            its = [(i // 4, i % 4) for i in range(rnd * 8, rnd * 8 + 8)]
            maps = [host_B3_inputs(pc, l, b, h, prm) for b, h in its]
            for m in maps:
                m["consts"] = consts64
            res = _run(_prog("B3", build_B3), maps)
            for (b, h), r in zip(its, res):
                yall[b, :, 768 + h * 64:768 + (h + 1) * 64] = r["y"].T
        del pfull
        wout = A_(prm['w_out'][l].reshape(8, 128, 1024).transpose(1, 0, 2)); nw2 = _vec(prm['norm2_w'][l])
        wr = A_(prm['w_router'][l].reshape(8, 128, 32).transpose(1, 0, 2)); br = A_(np.broadcast_to(prm['b_router'][l][None], (128, 32)))
        maps = []
        for j in range(NC):
            b, half = j // 2, j % 2
            ytok = np.concatenate([yall[b, half * 128:(half + 1) * 128], yall[b, 256 + half * 2048:256 + (half + 1) * 2048]], 0)
            mod = np.stack([mod6[l][2][:, :, b], mod6[l][3][:, :, b], mod6[l][4][:, :, b], mod6[l][2][:, :, 4], mod6[l][3][:, :, 4], mod6[l][4][:, :, 4]], -1)
            maps.append({"xT": xcur[j], "yT": _fm(ytok), "wout": wout, "mod": A_(mod), "nw": nw2, "wr": wr, "br": br})
        res = _run(_prog("C1", build_C1), maps)
        xm = [res[j]["xmT"] for j in range(NC)]
        Gall = np.concatenate([res[j]["G"].transpose(1, 0, 2).reshape(NTOK, 32) for j in range(NC)], 0)
        loads = np.count_nonzero(Gall, axis=0)
        caps = tuple(int(-(-max(int(loads[4 * cc + e]) for cc in range(NC)) // 128) * 128) for e in range(4))
        use_sparse = MOE_SPARSE and max(caps) <= C2S_CAP
        print(f"[moe] layer {l}: max expert load {int(loads.max())} (mean {float(loads.mean()):.0f}) -> {'dispatch' if use_sparse else 'dense'} caps {caps}", flush=True)
        if use_sparse:
            h2tok = np.concatenate([_tok(res[j]["h2T"]) for j in range(NC)] + [np.zeros((128, 1024), np.float32)], 0)
        else:
            h2all = np.ascontiguousarray(np.concatenate([res[j]["h2T"] for j in range(NC)], 2))
        del res, yall
        if use_sparse:
            maps = []
            c2c = host_C2s_consts(136, caps)
            for cc in range(NC):
                es = slice(4 * cc, 4 * cc + 4)
                m_ = {"h2tok": h2tok, "Gm": A_(Gall[:, es].reshape(136, 128, 4).transpose(1, 0, 2)),
                      "wgu": A_(prm['w_gate_up'][l][es].reshape(4, 8, 128, 2048).transpose(0, 2, 1, 3)),
                      "wd": A_(prm['w_down'][l][es].reshape(4, 8, 128, 1024).transpose(0, 2, 1, 3)),
                      "bgu": A_(prm['b_gate_up'][l][es].reshape(4, 16, 128).transpose(2, 0, 1)),
                      "bdb": A_(np.broadcast_to(prm['b_down'][l][es][:, None, :], (4, 128, 1024)))}
                m_.update(c2c)
                maps.append(m_)
            res = _run(_prog("C2s", build_C2s, 136, 4, caps), maps)
            del maps, h2tok
            fparts = [res[cc]["fpart"][:16 * NT2] for cc in range(NC)]
            del res
        else:
            maps = []
            ident = np.eye(128, dtype=np.float32)
            for cc in range(NC):
                es = slice(4 * cc, 4 * cc + 4)
                Gp = np.zeros((16, 1152, 4), np.float32); Gp[:, :NT2] = Gall[:, es].reshape(16, NT2, 4)
                maps.append({"h2T": h2all, "G": A_(Gp.reshape(16, 9, 128, 4).transpose(2, 0, 1, 3)),
                             "wgu": A_(prm['w_gate_up'][l][es].reshape(4, 8, 128, 2048).transpose(0, 2, 1, 3)),
                             "wd": A_(prm['w_down'][l][es].reshape(4, 8, 128, 1024).transpose(0, 2, 1, 3)),
                             "bgu": A_(prm['b_gate_up'][l][es].reshape(4, 16, 128).transpose(2, 0, 1)), "bd": A_(prm['b_down'][l][es]), "ident": ident})
            res = _run(_prog("C2", build_C2), maps)
            del maps, h2all
            fparts = [res[cc]["f"].transpose(0, 2, 1, 3).reshape(16, 1152, 1024)[:, :NT2].reshape(16 * NT2, 1024) for cc in range(NC)]
            del res
    nwf = _vec(prm['norm_f_w'])
    maps = []
    for j in range(NC):
        b = j // 2
        mod = np.stack([mod6[1][5][:, :, b], mod6[1][5][:, :, 4]], -1)
        maps.append({"xmT": xm[j], "fT": A_(np.stack([_fm(fparts[cc][j * NTOK:(j + 1) * NTOK]) for cc in range(NC)])), "mod": A_(mod), "nw": nwf})
    res = _run(_prog("D", build_D), maps)
    out = np.zeros((4, 4096, 1024), np.float32)
    for j in range(NC):
        b, half = j // 2, j % 2
        out[b, half * 2048:(half + 1) * 2048] = _tok(res[j]["oT"])[128:]
    return out
```

```python
import numpy as np
from contextlib import ExitStack
import concourse.bass as bass
import concourse.mybir as mybir
from concourse.bass_utils import run_bass_kernel_spmd

F32 = mybir.dt.float32
BF16 = mybir.dt.bfloat16
AF = mybir.ActivationFunctionType
ALU = mybir.AluOpType
AX = mybir.AxisListType

ENGS = ("pe", "dve", "act", "pool", "sp")
N_DMA_SEMS = 12


class Prog:
    def __init__(self, nc, same_engine_sync=None):
        import os
        if same_engine_sync is None:
            same_engine_sync = os.environ.get('SAMESYNC', '1') == '1'
        self.nc = nc
        self.es = ExitStack()
        self.ops = {e: [] for e in ENGS}
        self.cnt = {e: 0 for e in ENGS}
        self.sem = {}
        for e in ENGS:
            self.sem[e] = self.es.enter_context(nc.semaphore("s_" + e))
        self.dsem = {q: [self.es.enter_context(nc.semaphore(f"d_{q}{i}")) for i in range(N_DMA_SEMS)]
                     for q in ("sp", "pool", "act")}
        self.dsem_uses = {q: [0] * N_DMA_SEMS for q in ("sp", "pool", "act")}
        self.dsem_next = {q: 0 for q in ("sp", "pool", "act")}
        self.waited = {e: {} for e in ENGS}
        self.lastw = {}
        self.readers = {}
        self.same = same_engine_sync
        self.semobj = {}
        self.out_tokens = []
        self.nops = 0

    def sb(self, name, shape, dt=F32):
        return self.es.enter_context(self.nc.sbuf_tensor("sb_" + name, list(shape), dt))

    def ps(self, name, shape, dt=F32):
        return self.es.enter_context(self.nc.psum_tensor("ps_" + name, list(shape), dt))

    def _need(self, eng, tok, waits):
        if tok is None:
            return
        semkey, val, src = tok
        if src == eng and (not self.same or eng == "pe") and not getattr(self, "_force_same", False):
            return
        if self.waited[eng].get(semkey, 0) >= val:
            return
        waits[semkey] = max(waits.get(semkey, 0), val)

    def _deps(self, eng, reads, writes):
        waits = {}
        for k in reads:
            self._need(eng, self.lastw.get(k), waits)
        for k in writes:
            self._need(eng, self.lastw.get(k), waits)
            for t in self.readers.get(k, ()):
                self._need(eng, t, waits)
        for semkey, val in waits.items():
            self.waited[eng][semkey] = val
            self.ops[eng].append(("wait", semkey, val))

    def _commit(self, tok, reads, writes):
        for k in reads:
            self.readers.setdefault(k, []).append(tok)
        for k in writes:
            self.lastw[k] = tok
            self.readers[k] = []

    def op(self, eng, fn, reads=(), writes=()):
        import os
        lim = os.environ.get("OPLIMIT")
        if lim is not None and self.nops >= int(lim):
            return None
        writes = list(writes) + [k for k in reads if isinstance(k, str) and k.startswith("bk")]
        reads = [k for k in reads if not (isinstance(k, str) and k.startswith("bk"))]
        self._deps(eng, reads, writes)
        self.cnt[eng] += 1
        tok = (("e", eng), self.cnt[eng], eng)
        self.ops[eng].append(("op", fn, ("e", eng), 1))
        self._commit(tok, reads, writes)
        self.nops += 1
        self._pass_turn()
        return tok

    def interleave(self, fns):
        import threading
        n = len(fns)
        st = {"turn": 0, "alive": [True] * n, "err": None}
        cv = threading.Condition()
        self._il = (st, cv, {})

        def nxt(i):
            for k in range(1, n + 1):
                j = (i + k) % n
                if st["alive"][j]:
                    return j
            return -1

        def runner(i):
            self._il[2][threading.get_ident()] = i
            with cv:
                while st["turn"] != i:
                    cv.wait()
            try:
                fns[i]()
            except BaseException as ex:
                st["err"] = ex
            finally:
                with cv:
                    st["alive"][i] = False
                    st["turn"] = nxt(i)
                    cv.notify_all()
        ths = [threading.Thread(target=runner, args=(i,)) for i in range(n)]
        for t in ths:
            t.start()
        for t in ths:
            t.join()
        self._il = None
        if st["err"] is not None:
            raise st["err"]

    def _pass_turn(self):
        il = getattr(self, "_il", None)
        if not il:
            return
        import threading
        st, cv, ids = il
        me = ids.get(threading.get_ident())
        if me is None:
            return
        n = len(st["alive"])
        with cv:
            j = me
            for k in range(1, n + 1):
                c = (me + k) % n
                if st["alive"][c]:
                    j = c
                    break
            if j != me:
                st["turn"] = j
                cv.notify_all()
                while st["turn"] != me:
                    cv.wait()

    def I(self, eng, meth, *args, r=(), w=(), force_same=False, **kw):
        self._force_same = force_same
        try:
            return self.op(eng, lambda e: getattr(e, meth)(*args, **kw), reads=r, writes=w)
        finally:
            self._force_same = False

    def dma(self, q, out, in_, reads=(), writes=(), is_output=False, **kw):
        import os
        lim = os.environ.get("OPLIMIT")
        if lim is not None and self.nops >= int(lim) and not is_output:
            return None
        i = self.dsem_next[q]
        self.dsem_next[q] = (i + 1) % N_DMA_SEMS
        uses = self.dsem_uses[q][i]
        semkey = ("d", q, i)
        if uses > 0 and self.waited[q].get(semkey, 0) < 16 * uses:
            self.waited[q][semkey] = 16 * uses
            self.ops[q].append(("wait", semkey, 16 * uses))
        self._deps(q, reads, writes)
        self.dsem_uses[q][i] = uses + 1
        tok = (semkey, 16 * (uses + 1), None)
        self.ops[q].append(("op", lambda e: e.dma_start(out=out, in_=in_, **kw), semkey, 16))
        self._commit(tok, reads, writes)
        if is_output:
            self.out_tokens.append(tok)
        self.nops += 1
        return tok

    def idma(self, out, in_, out_idx=None, in_idx=None, reads=(), writes=(), is_output=False, **kw):
        q = "pool"
        i = self.dsem_next[q]
        self.dsem_next[q] = (i + 1) % N_DMA_SEMS
        uses = self.dsem_uses[q][i]
        semkey = ("d", q, i)
        if uses > 0 and self.waited[q].get(semkey, 0) < 16 * uses:
            self.waited[q][semkey] = 16 * uses
            self.ops[q].append(("wait", semkey, 16 * uses))
        self._deps(q, reads, writes)
        self.dsem_uses[q][i] = uses + 1
        tok = (semkey, 16 * (uses + 1), None)
        oo = bass.IndirectOffsetOnAxis(ap=out_idx, axis=0) if out_idx is not None else None
        io = bass.IndirectOffsetOnAxis(ap=in_idx, axis=0) if in_idx is not None else None
        self.ops[q].append(("op", lambda e: e.indirect_dma_start(out=out, out_offset=oo, in_=in_, in_offset=io, **kw), semkey, 16))
        self._commit(tok, reads, writes)
        if is_output:
            self.out_tokens.append(tok)
        self.nops += 1
        return tok

    def _semh(self, semkey):
        if semkey[0] == "e":
            return self.sem[semkey[1]]
        return self.dsem[semkey[1]][semkey[2]]

    def finish(self):
        for tok in self.out_tokens:
            semkey, val, _ = tok
            if self.waited["sp"].get(semkey, 0) < val:
                self.waited["sp"][semkey] = val
                self.ops["sp"].append(("wait", semkey, val))
        for e in ENGS:
            if self.cnt[e] > 0 and e != "sp":
                self.ops["sp"].append(("wait", ("e", e), self.cnt[e]))
        for q in ("sp", "pool", "act"):
            for i in range(N_DMA_SEMS):
                if self.dsem_uses[q][i] > 0:
                    self.ops["sp"].append(("wait", ("d", q, i), 16 * self.dsem_uses[q][i]))
        nc = self.nc
        with nc.Block() as block:
            def mk(ename):
                def body(e):
                    for item in self.ops[ename]:
                        if item[0] == "wait":
                            e.wait_ge(self._semh(item[1]), item[2])
                        else:
                            ins = item[1](e)
                            ins.then_inc(self._semh(item[2]), item[3])
                return body
            block.tensor(mk("pe"))
            block.vector(mk("dve"))
            block.scalar(mk("act"))
            block.gpsimd(mk("pool"))
            block.sync(mk("sp"))
        self.es.close()


def build_M():
    nc = bass.Bass("TRN2", target_bir_lowering=False)
    cs = nc.dram_tensor("cs", [128, 8, 5], F32, kind="ExternalInput").ap()
    wm = nc.dram_tensor("wm", [12, 128, 8, 128], F32, kind="ExternalInput").ap()
    bm = nc.dram_tensor("bm", [128, 12], F32, kind="ExternalInput").ap()
    out = nc.dram_tensor("modT", [128, 12, 5], F32, kind="ExternalOutput").ap()
    P = Prog(nc)
    cst = P.sb("cst", [128, 8, 5]); sg = P.sb("sg", [128, 8, 5]); sc = P.sb("sc", [128, 8, 5])
    bmt = P.sb("bmt", [128, 12]); ot = P.sb("ot", [128, 12, 5])
    wt = [P.sb(f"wt{i}", [128, 8, 128]) for i in range(2)]
    pp = [P.ps(f"pp{i}", [128, 8]) for i in range(2)]
    P.dma("sp", cst[:], cs, writes=["cst"])
    P.dma("sp", bmt[:], bm, writes=["bmt"])
    P.op("act", lambda e: e.activation(sg[:], cst[:], AF.Sigmoid), reads=["cst"], writes=["sg"])
    P.op("dve", lambda e: e.tensor_tensor(sc[:], cst[:], sg[:], ALU.mult), reads=["cst", "sg"], writes=["sc"])
    for j in range(12):
        w = wt[j % 2]; wk = f"wt{j%2}"; pk = f"pp{j%2}"; p_ = pp[j % 2]
        P.dma("sp", w[:], wm[j], writes=[wk])
        for k in range(8):
            P.op("pe", lambda e, w=w, k=k, p_=p_: e.matmul(p_[:, 0:5], w[:, k, :], sc[:, k, :], start=(k == 0), stop=(k == 7)),
                 reads=[wk, "sc"], writes=[pk])
        P.op("dve", lambda e, j=j, p_=p_: e.tensor_scalar(ot[:, j, :], p_[:, 0:5], bmt[:, j:j + 1], None, ALU.add),
             reads=[pk, "bmt"], writes=["ot"])
    P.dma("sp", out, ot[:], reads=["ot"], is_output=True)
    P.finish()
    return nc


NTOK = 2176
TILES = [(0, 128)] + [(128 + 512 * i, 512) for i in range(4)]
NCT = 33


def rms_modulate(P, xT, hT, mod, nw, ones_bf, shift_i, scale_i, hT32=None, tagp="n", psb=None, hkey="hT"):
    g = P.sb(tagp + "_g", [128, 8, 2]);
    for v, (sh, sci) in enumerate(zip(shift_i, scale_i)):
        P.op("dve", lambda e, v=v, sci=sci: e.scalar_tensor_tensor(g[:, :, v], mod[:, :, sci], 1.0, nw[:, :], ALU.add, ALU.mult),
             reads=["mod", "nw"], writes=[tagp + "_g"])
    sq = [P.sb(f"{tagp}_sq{i}", [128, 8, 512], BF16) for i in range(2)]
    ss = list(psb); ssk = [f"bk_{tagp}0", f"bk_{tagp}1"]
    rs = [P.sb(f"{tagp}_rs{i}", [128, 512]) for i in range(2)]
    tmp = [P.sb(f"{tagp}_tmp{i}", [128, 512]) for i in range(2)]
    ti = 0
    for it, (t0, n) in enumerate(TILES):
        b = it % 2
        v = 1 if it == 0 else 0
        for k in range(8):
            P.op("act", lambda e, k=k, b=b, t0=t0, n=n: e.activation(sq[b][:, k, 0:n], xT[:, k, t0:t0 + n], AF.Square),
                 reads=["xT"], writes=[f"{tagp}_sq{b}"])
        for k in range(8):
            P.op("pe", lambda e, k=k, b=b, n=n: e.matmul(ss[b][:, 0:n], ones_bf[:], sq[b][:, k, 0:n], start=(k == 0), stop=(k == 7)),
                 reads=[f"{tagp}_sq{b}", "ones_bf"], writes=[ssk[b]])
        P.op("dve", lambda e, b=b, n=n: e.tensor_scalar(rs[b][:, 0:n], ss[b][:, 0:n], 1.0 / 1024, 1e-6, ALU.mult, ALU.add),
             reads=[ssk[b]], writes=[f"{tagp}_rs{b}"])
        P.op("dve", lambda e, b=b, n=n: e.reciprocal(rs[b][:, 0:n], rs[b][:, 0:n]),
             reads=[f"{tagp}_rs{b}"], writes=[f"{tagp}_rs{b}"])
        P.op("act", lambda e, b=b, n=n: e.activation(rs[b][:, 0:n], rs[b][:, 0:n], AF.Sqrt),
             reads=[f"{tagp}_rs{b}"], writes=[f"{tagp}_rs{b}"])
        for k in range(8):
            tb = ti % 2; ti += 1
            P.op("dve", lambda e, k=k, b=b, tb=tb, t0=t0, n=n, v=v: e.scalar_tensor_tensor(
                tmp[tb][:, 0:n], xT[:, k, t0:t0 + n], g[:, k, v:v + 1], rs[b][:, 0:n], ALU.mult, ALU.mult),
                 reads=["xT", tagp + "_g", f"{tagp}_rs{b}"], writes=[f"{tagp}_tmp{tb}"])
            sh = shift_i[v]
            P.op("act", lambda e, k=k, tb=tb, t0=t0, n=n, sh=sh: e.activation(
                hT[:, k, t0:t0 + n], tmp[tb][:, 0:n], AF.Identity, bias=mod[:, k, sh:sh + 1]),
                 reads=[f"{tagp}_tmp{tb}", "mod"], writes=[hkey])
            if hT32 is not None:
                P.op("pool", lambda e, k=k, tb=tb, t0=t0, n=n, sh=sh: e.tensor_scalar(
                    hT32[:, k, t0:t0 + n], tmp[tb][:, 0:n], mod[:, k, sh:sh + 1], None, ALU.add),
                     reads=[f"{tagp}_tmp{tb}", "mod"], writes=["hT32"])


def build_A(first=False):
    nc = bass.Bass("TRN2", target_bir_lowering=False)
    xTd = nc.dram_tensor("xT", [128, 8, NTOK], F32, kind="ExternalInput").ap()
    modd = nc.dram_tensor("mod", [128, 8, 6], F32, kind="ExternalInput").ap()
    fTd = nc.dram_tensor("fT", [8, 128, 8, NTOK], F32, kind="ExternalInput").ap() if not first else None
    xoutd = nc.dram_tensor("xout", [128, 8, NTOK], F32, kind="ExternalOutput").ap()
    nwd = nc.dram_tensor("nw", [128, 8], F32, kind="ExternalInput").ap()
    wind = nc.dram_tensor("win", [128, 8, NCT * 128], F32, kind="ExternalInput").ap()
    pTd = nc.dram_tensor("pT", [NCT, 128, NTOK], F32, kind="ExternalOutput").ap()
    P = Prog(nc)
    xT = P.sb("xT", [128, 8, NTOK]); hT = P.sb("hT", [128, 8, NTOK], BF16)
    mod = P.sb("mod", [128, 8, 6]); nw = P.sb("nw", [128, 8])
    wbf = P.sb("wbf", [128, 8, NCT * 128], BF16)
    ones_bf = P.sb("ones_bf", [128, 128], BF16)
    P.op("pool", lambda e: e.memset(ones_bf[:], 1.0), writes=["ones_bf"])
    for k in range(8):
        P.dma("sp", xT[:, k, :], xTd[:, k, :], writes=["xT"])
    P.dma("sp", mod[:], modd, writes=["mod"])
    P.dma("sp", nw[:], nwd, writes=["nw"])
    for k in range(8):
        P.dma("pool", wbf[:, k, :], wind[:, k, :], writes=["wbf"])
    fT = P.sb("fT", [128, 512])
    for c, k in [(c, k) for c in range(0 if first else 8) for k in range(8)]:
        for (t0, n) in TILES:
            P.dma("sp", fT[:, 0:n], fTd[c, :, k, t0:t0 + n], writes=["fT"])
            gcol = 5 if t0 == 0 else 4
            P.I("dve", "scalar_tensor_tensor", xT[:, k, t0:t0 + n], fT[:, 0:n], mod[:, k, gcol:gcol + 1], xT[:, k, t0:t0 + n], ALU.mult, ALU.add,
                r=["fT", "mod", "xT"], w=["xT"])
    for k in range(8):
        P.dma("sp", xoutd[:, k, :], xT[:, k, :], reads=["xT"], is_output=True)
    pp = [P.ps(f"bank{i}", [128, 512]) for i in range(6)]
    rms_modulate(P, xT, hT, mod, nw, ones_bf, shift_i=(0, 2), scale_i=(1, 3), tagp="n1", psb=(pp[4], pp[5]))
    st = [P.sb(f"st{i}", [128, 512]) for i in range(4)]
    i = 0
    for ct in range(NCT):
        for (t0, n) in TILES:
            b = i % 4; i += 1
            for k in range(8):
                P.op("pe", lambda e, k=k, b=b, ct=ct, t0=t0, n=n: e.matmul(
                    pp[b][:, 0:n], wbf[:, k, ct * 128:(ct + 1) * 128], hT[:, k, t0:t0 + n], start=(k == 0), stop=(k == 7)),
                     reads=["wbf", "hT"], writes=[f"bk{b}"])
            if b % 2 == 0:
                P.op("dve", lambda e, b=b, n=n: e.tensor_copy(st[b][:, 0:n], pp[b][:, 0:n]), reads=[f"bk{b}"], writes=[f"st{b}"])
            else:
                P.op("act", lambda e, b=b, n=n: e.activation(st[b][:, 0:n], pp[b][:, 0:n], AF.Copy), reads=[f"bk{b}"], writes=[f"st{b}"])
            P.dma("sp", pTd[ct, :, t0:t0 + n], st[b][:, 0:n], reads=[f"st{b}"], is_output=True)
    P.finish()
    return nc


TSEQ = 4352
QS = 64
NCH = 34
SEGS = [(0, 256), (256, 4352)]


def conv_silu(P, dst, src, cw, cb, ti, key_dst, key_src, tmp, key_tmp, out_dt_tile=None):
    for (s, e_) in SEGS:
        if cb is not None:
            P.op("act", lambda e, s=s, e_=e_: e.activation(tmp[:, s:e_], src[:, s:e_], AF.Identity, bias=cb[:, ti:ti + 1], scale=cw[:, ti, 1:2]),
                 reads=[key_src, "cw", "cb"], writes=[key_tmp])
        else:
            P.op("act", lambda e, s=s, e_=e_: e.activation(tmp[:, s:e_], src[:, s:e_], AF.Copy, scale=cw[:, ti, 1:2]),
                 reads=[key_src, "cw"], writes=[key_tmp])
        P.op("dve", lambda e, s=s, e_=e_: e.scalar_tensor_tensor(tmp[:, s + 1:e_], src[:, s:e_ - 1], cw[:, ti, 0:1], tmp[:, s + 1:e_], ALU.mult, ALU.add),
             reads=[key_src, "cw", key_tmp], writes=[key_tmp])
        P.op("dve", lambda e, s=s, e_=e_: e.scalar_tensor_tensor(tmp[:, s:e_ - 1], src[:, s + 1:e_], cw[:, ti, 2:3], tmp[:, s:e_ - 1], ALU.mult, ALU.add),
             reads=[key_src, "cw", key_tmp], writes=[key_tmp])
    P.op("act", lambda e: e.activation(dst[:, :], tmp[:, :], AF.Silu), reads=[key_tmp], writes=[key_dst])


def load_consts(P, cd):
    c = {}
    for i, nm in enumerate(["tri_f", "tri_b", "nm_f", "nm_b", "ident"]):
        t = P.sb("c_" + nm, [128, 128]); P.dma("sp", t[:], cd[i], writes=["c_" + nm]); c[nm] = t
    ones = P.sb("c_ones", [128, 128]); P.op("pool", lambda e: e.memset(ones[:], 1.0), writes=["c_ones"]); c["ones"] = ones
    idb = P.sb("c_identb", [128, 128], BF16)
    P.op("dve", lambda e: e.tensor_copy(idb[:], c["ident"][:]), reads=["c_ident"], writes=["c_identb"]); c["identb"] = idb
    return c


def host_consts():
    k = np.arange(128)[:, None]; i = np.arange(128)[None, :]
    tri_f = (k <= i).astype(np.float32); tri_b = (k >= i).astype(np.float32)
    nm_f = np.where(i >= k, 0.0, -30000.0).astype(np.float32); nm_b = np.where(i <= k, 0.0, -30000.0).astype(np.float32)
    return np.stack([tri_f, tri_b, nm_f, nm_b, np.eye(128, dtype=np.float32)])


def build_B1(stage=99):
    nc = bass.Bass("TRN2", target_bir_lowering=False)
    D = lambda n, s: nc.dram_tensor(n, s, F32, kind="ExternalInput").ap()
    zTd = D("zT", [128, TSEQ]); xbcd = D("xbcT", [3, 128, TSEQ]); dtrd = D("dtr", [128, 4 * QS])
    dtbd = D("dtb", [128, 4 * QS]); alogd = D("alog", [128, 4 * QS]); cwd = D("cw", [128, 3, 3]); cbd = D("cb", [128, 3])
    dvd = D("dvec", [128, 1]); nwd = D("normw", [128, 1]); cd = D("consts", [5, 128, 128])
    yTd = nc.dram_tensor("yT", [128, TSEQ], F32, kind="ExternalOutput").ap()
    P = Prog(nc)
    C = load_consts(P, cd)
    raw = P.sb("raw", [128, TSEQ]); tmp = P.sb("tmp", [128, TSEQ])
    xT = P.sb("xT", [128, TSEQ]); B32 = P.sb("B32", [128, TSEQ]); C32 = P.sb("C32", [128, TSEQ])
    Bb = P.sb("Bb", [128, TSEQ], BF16); Cb = P.sb("Cb", [128, TSEQ], BF16)
    cw = P.sb("cw", [128, 3, 3]); cb = P.sb("cb", [128, 3]); dvec = P.sb("dvec", [128, 1]); normw = P.sb("normw", [128, 1])
    for t, d, k in ((cw, cwd, "cw"), (cb, cbd, "cb"), (dvec, dvd, "dvec"), (normw, nwd, "normw")):
        P.dma("sp", t[:], d, writes=[k])
    if stage == 0:
        P.op("dve", lambda e: e.tensor_copy(xT[:, 0:128], C["ident"][:]), reads=["c_ident"], writes=["xT"])
        P.op("dve", lambda e: e.tensor_scalar(xT[:, 128:256], C["tri_f"][:], cw[:, 0, 0:1], dvec[:, 0:1], ALU.mult, ALU.add), reads=["c_tri_f", "cw", "dvec"], writes=["xT"])
        P.dma("sp", yTd, xT[:], reads=["xT"], is_output=True); P.finish(); return nc
    import os
    NT = int(os.environ.get("NT", "3"))
    for ti, (dst, kd) in enumerate(((xT, "xT"), (B32, "B32"), (C32, "C32"))[:NT]):
        P.dma("sp", raw[:], xbcd[ti], writes=["raw"])
        conv_silu(P, dst, raw, cw, cb, ti, kd, "raw", tmp, "tmp")
    if NT == 3 and os.environ.get("NOCAST") is None:
        P.op("pool", lambda e: e.tensor_copy(Bb[:], B32[:]), reads=["B32"], writes=["Bb"])
        P.op("pool", lambda e: e.tensor_copy(Cb[:], C32[:]), reads=["C32"], writes=["Cb"])
    if stage == 1:
        P.dma("sp", yTd, xT[:], reads=["xT"], is_output=True); P.finish(); return nc
    dtr = P.sb("dtr", [128, 4 * QS]); dtb = P.sb("dtb", [128, 4 * QS]); alog = P.sb("alog", [128, 4 * QS])
    dt = P.sb("dt", [128, 4 * QS]); la = P.sb("la", [128, 4 * QS]); ncum = P.sb("ncum", [128, 4 * QS])
    wgt = P.sb("wgt", [128, 4 * QS]); dec = P.sb("dec", [128, 4 * QS])
    P.dma("sp", dtr[:], dtrd, writes=["dtr"]); P.dma("sp", dtb[:], dtbd, writes=["dtb"]); P.dma("sp", alog[:], alogd, writes=["alog"])
    P.op("dve", lambda e: e.tensor_tensor(dtr[:], dtr[:], dtb[:], ALU.add), reads=["dtr", "dtb"], writes=["dtr"])
    P.op("dve", lambda e: e.tensor_scalar(dtr[:], dtr[:], 60.0, None, ALU.min), reads=["dtr"], writes=["dtr"])
    P.op("act", lambda e: e.activation(dtr[:], dtr[:], AF.Exp), reads=["dtr"], writes=["dtr"])
    P.op("act", lambda e: e.activation(dt[:], dtr[:], AF.Ln, bias=1.0), reads=["dtr"], writes=["dt"])
    P.op("act", lambda e: e.activation(alog[:], alog[:], AF.Exp), reads=["alog"], writes=["alog"])
    P.op("dve", lambda e: e.scalar_tensor_tensor(la[:], dt[:], -1.0, alog[:], ALU.mult, ALU.mult), reads=["dt", "alog"], writes=["la"])
    bk = [P.ps(f"bank{i}", [128, 512]) for i in range(8)]
    pc = bk[0][:, 0:4 * QS]; pt = bk[1][:, 0:4 * QS]
    P.op("pe", lambda e: e.matmul(pc[:, 0:2 * QS], C["tri_f"][:], la[:, 0:2 * QS], start=True, stop=True), reads=["la", "c_tri_f"], writes=["bk0"])
    P.op("pe", lambda e: e.matmul(pc[:, 2 * QS:4 * QS], C["tri_b"][:], la[:, 2 * QS:4 * QS], start=True, stop=True), reads=["la", "c_tri_b"], writes=["bk0"])
    P.op("pe", lambda e: e.matmul(pt, C["ones"][:], la[:], start=True, stop=True), reads=["la", "c_ones"], writes=["bk1"])
    P.op("dve", lambda e: e.tensor_scalar(ncum[:], pc, -1.0, None, ALU.mult), reads=["bk0"], writes=["ncum"])
    P.op("dve", lambda e: e.tensor_tensor(wgt[:], pt, ncum[:], ALU.add), reads=["bk1", "ncum"], writes=["wgt"])
    P.op("act", lambda e: e.activation(wgt[:], wgt[:], AF.Exp), reads=["wgt"], writes=["wgt"])
    P.op("dve", lambda e: e.tensor_tensor(wgt[:], wgt[:], dt[:], ALU.mult), reads=["wgt", "dt"], writes=["wgt"])
    P.op("act", lambda e: e.activation(dec[:], pt, AF.Exp), reads=["bk1"], writes=["dec"])
    if stage == 2:
        for i_, (t_, k_) in enumerate(((dt, "dt"), (la, "la"), (ncum, "ncum"), (wgt, "wgt"), (dec, "dec"))):
            P.dma("sp", yTd[:, i_ * 256:(i_ + 1) * 256], t_[:], reads=[k_], is_output=True)
        for i_, (t_, k_) in enumerate(((B32, "B32"), (C32, "C32"), (xT, "xT"))):
            P.dma("sp", yTd[:, 1280 + i_ * 1024:1280 + (i_ + 1) * 1024], t_[:, 0:1024], reads=[k_], is_output=True)
        P.finish(); return nc
    xpad = [P.sb(f"xpad{h}", [128, NCH, 128], BF16) for h in range(2)]
    Btok = P.sb("Btok", [128, NCH, 128], BF16); xw = P.sb("xw", [128, NCH, 4, 64], BF16)
    for h in range(2):
        P.op("pool", lambda e, h=h: e.memset(xpad[h][:], 0.0), writes=[f"xpad{h}"])
    ptr = [bk[2][:, 0:128], bk[3][:, 0:128]]
    for c in range(NCH):
        sl = slice(c * 128, (c + 1) * 128)
        P.op("pe", lambda e, sl=sl: e.transpose(ptr[0], xT[:, sl], C["ident"][:]), reads=["xT", "c_ident"], writes=["bk2"])
        P.op("pe", lambda e, sl=sl: e.transpose(ptr[1], B32[:, sl], C["ident"][:]), reads=["B32", "c_ident"], writes=["bk3"])
        for h in range(2):
            P.op("act", lambda e, h=h, c=c: e.activation(xpad[h][:, c, h * 64:(h + 1) * 64], ptr[0][:, h * 64:(h + 1) * 64], AF.Copy),
                 reads=["bk2"], writes=[f"xpad{h}"])
        for q in range(4):
            h = q % 2
            P.op("dve", lambda e, q=q, h=h, c=c: e.tensor_scalar(xw[:, c, q, :], ptr[0][:, h * 64:(h + 1) * 64], wgt[:, q * QS + c:q * QS + c + 1], None, ALU.mult),
                 reads=["bk2", "wgt"], writes=["xw"])
        P.op("act", lambda e, c=c: e.activation(Btok[:, c, :], ptr[1], AF.Copy), reads=["bk3"], writes=["Btok"])
    if stage == 3:
        P.dma("sp", yTd, xT[:], reads=["xT"], is_output=True); P.finish(); return nc
    yacc = P.sb("yacc", [128, TSEQ])
    Hpad = [P.sb(f"Hpad{q}", [128, 128]) for q in range(4)]
    larep = [P.sb(f"larep{i}", [128, 128]) for i in range(2)]
    seg = [P.sb(f"seg{i}", [128, 128]) for i in range(2)]; Et = [P.sb(f"Et{i}", [128, 128]) for i in range(2)]
    STp = [P.sb(f"STp{i}", [128, 128], BF16) for i in range(2)]; CTs = [P.sb(f"CTs{i}", [128, 128]) for i in range(2)]
    psA = bk[0][:, 0:128]; psE = bk[1][:, 0:128]; psS = bk[4][:, 0:128]; psY = bk[5][:, 0:128]
    psH = [bk[6][:, 0:64], bk[7][:, 0:64]]
    import os
    for d in range(int(os.environ.get('ND', '2'))):
        tri = C["tri_f"] if d == 0 else C["tri_b"]; nm = C["nm_f"] if d == 0 else C["nm_b"]
        trik = "c_tri_f" if d == 0 else "c_tri_b"; nmk = "c_nm_f" if d == 0 else "c_nm_b"
        order = list(range(NCH)) if d == 0 else [1, 0] + list(range(NCH - 1, 1, -1))
        for hh in range(2):
            P.op("pool", lambda e, q=d * 2 + hh: e.memset(Hpad[q][:], 0.0), writes=[f"Hpad{d*2+hh}"])
        for c in order:
            sl = slice(c * 128, (c + 1) * 128)
            P.op("pe", lambda e, sl=sl: e.matmul(psS, Bb[:, sl], Cb[:, sl], start=True, stop=True), reads=["Bb", "Cb"], writes=["bk4"])
            for hh in range(2):
                q = d * 2 + hh
                P.op("pool", lambda e, q=q, c=c, hh=hh: e.tensor_scalar(larep[hh][:], C["ones"][:], la[:, q * QS + c:q * QS + c + 1], None, ALU.mult),
                     reads=["c_ones", "la"], writes=[f"larep{hh}"])
                P.op("pe", lambda e, hh=hh, tri=tri: e.matmul(psA, larep[hh][:], tri[:], start=True, stop=False), reads=[f"larep{hh}", trik], writes=["bk0"])
                P.op("pe", lambda e, nm=nm: e.matmul(psA, C["ident"][:], nm[:], start=False, stop=True), reads=["c_ident", nmk], writes=["bk0"])
                P.op("pe", lambda e, hh=hh, tri=tri: e.matmul(psE, larep[hh][:], tri[:], start=True, stop=True), reads=[f"larep{hh}", trik], writes=["bk1"])
                P.op("act", lambda e, q=q, c=c, hh=hh: e.activation(seg[hh][:], psA, AF.Exp, bias=ncum[:, q * QS + c:q * QS + c + 1]), reads=["bk0", "ncum"], writes=[f"seg{hh}"])
                P.op("act", lambda e, hh=hh: e.activation(Et[hh][:], psE, AF.Exp), reads=["bk1"], writes=[f"Et{hh}"])
                P.op("dve", lambda e, q=q, c=c, hh=hh: e.scalar_tensor_tensor(STp[hh][:], psS, dt[:, q * QS + c:q * QS + c + 1], seg[hh][:], ALU.mult, ALU.mult),
                     reads=["bk4", "dt", f"seg{hh}"], writes=[f"STp{hh}"])
                P.op("dve", lambda e, sl=sl, hh=hh: e.tensor_tensor(CTs[hh][:], C32[:, sl], Et[hh][:], ALU.mult), reads=["C32", f"Et{hh}"], writes=[f"CTs{hh}"])
            for hh in range(2):
                q = d * 2 + hh
                P.op("pe", lambda e, hh=hh, c=c: e.matmul(psY, xpad[hh][:, c, :], STp[hh][:], start=(hh == 0), stop=False),
                     reads=[f"xpad{hh}", f"STp{hh}"], writes=["bk5"])
            for hh in range(2):
                q = d * 2 + hh
                P.op("pe", lambda e, hh=hh, q=q: e.matmul(psY, Hpad[q][:], CTs[hh][:], start=False, stop=(hh == 1)),
                     reads=[f"Hpad{q}", f"CTs{hh}"], writes=["bk5"])
            for hh in range(2):
                q = d * 2 + hh
                P.op("pe", lambda e, hh=hh, q=q, c=c: e.matmul(psH[hh], Btok[:, c, :], xw[:, c, q, :], start=True, stop=True),
                     reads=["Btok", "xw"], writes=[f"bk{6+hh}"])
                P.op("dve", lambda e, hh=hh, q=q, c=c: e.scalar_tensor_tensor(
                    Hpad[q][:, hh * 64:(hh + 1) * 64], Hpad[q][:, hh * 64:(hh + 1) * 64], dec[:, q * QS + c:q * QS + c + 1], psH[hh], ALU.mult, ALU.add),
                     reads=[f"Hpad{q}", "dec", f"bk{6+hh}"], writes=[f"Hpad{q}"])
            if d == 0:
                P.op("dve", lambda e, sl=sl: e.scalar_tensor_tensor(yacc[:, sl], xT[:, sl], dvec[:, 0:1], psY, ALU.mult, ALU.add),
                     reads=["xT", "dvec", "bk5"], writes=[f"yacc{c}"])
            else:
                P.op("dve", lambda e, sl=sl: e.tensor_tensor(yacc[:, sl], yacc[:, sl], psY, ALU.add), reads=[f"yacc{c}", "bk5"], writes=[f"yacc{c}"])
    if stage == 4:
        P.dma("sp", yTd, yacc[:], reads=[f"yacc{c}" for c in range(NCH)], is_output=True); P.finish(); return nc
    zT = raw
    P.dma("sp", zT[:], zTd, writes=["raw"])
    P.op("act", lambda e: e.activation(tmp[:], zT[:], AF.Silu), reads=["raw"], writes=["tmp"])
    allc = [f"yacc{c}" for c in range(NCH)]
    P.op("dve", lambda e: e.tensor_tensor(yacc[:], yacc[:], tmp[:], ALU.mult), reads=allc + ["tmp"], writes=allc)
    P.op("act", lambda e: e.activation(tmp[:], yacc[:], AF.Square), reads=allc, writes=["tmp"])
    pss = [bk[2][:, 0:256], bk[3][:, 0:256]]; rs = [P.sb(f"rs{i}", [128, 256]) for i in range(2)]
    for i, t0 in enumerate(range(0, TSEQ, 256)):
        n = min(256, TSEQ - t0); b = i % 2
        P.op("pe", lambda e, b=b, t0=t0, n=n: e.matmul(pss[b][:, 0:n], C["ones"][:], tmp[:, t0:t0 + n], start=True, stop=True), reads=["tmp", "c_ones"], writes=[f"bk{2+b}"])
        P.op("dve", lambda e, b=b, n=n: e.tensor_scalar(rs[b][:, 0:n], pss[b][:, 0:n], 1.0 / 128, 1e-5, ALU.mult, ALU.add), reads=[f"bk{2+b}"], writes=[f"rs{b}"])
        P.op("dve", lambda e, b=b, n=n: e.reciprocal(rs[b][:, 0:n], rs[b][:, 0:n]), reads=[f"rs{b}"], writes=[f"rs{b}"])
        P.op("act", lambda e, b=b, n=n: e.activation(rs[b][:, 0:n], rs[b][:, 0:n], AF.Sqrt), reads=[f"rs{b}"], writes=[f"rs{b}"])
        P.op("dve", lambda e, b=b, t0=t0, n=n: e.scalar_tensor_tensor(xT[:, t0:t0 + n], yacc[:, t0:t0 + n], normw[:, 0:1], rs[b][:, 0:n], ALU.mult, ALU.mult),
             reads=allc + ["normw", f"rs{b}"], writes=["xT"])
    P.dma("sp", yTd, xT[:], reads=["xT"], is_output=True)
    P.finish()
    return nc


def host_B1_inputs(pa, L, b, hp, prm):
    p = pa[b]
    z = p[:, hp * 128:(hp + 1) * 128].T
    x = p[:, 256 + hp * 128:256 + (hp + 1) * 128].T
    Bm = p[:, 512 + hp * 128:512 + (hp + 1) * 128].T
    Cm = p[:, 768 + hp * 128:768 + (hp + 1) * 128].T
    cols = [1024 + d * 4 + 2 * hp + hh for d in range(2) for hh in range(2)]
    dtr = np.zeros((128, 4, QS), np.float32); dtr[:, :, :NCH] = p[:, cols].reshape(NCH, 128, 4).transpose(1, 2, 0); dtr = dtr.reshape(128, 4 * QS)
    bc = lambda v: np.broadcast_to(np.asarray(v, np.float32)[None, :, None], (128, 4, QS)).reshape(128, 4 * QS)
    dtb = bc([prm['m_dt_bias'][L, d, 2 * hp + hh] for d in range(2) for hh in range(2)])
    alog = bc([prm['m_a_log'][L, d, 2 * hp + hh] for d in range(2) for hh in range(2)])
    cwfull = prm['m_conv_w'][L]; cbfull = prm['m_conv_b'][L]
    offs = [hp * 128, 256 + hp * 128, 512 + hp * 128]
    cw = np.stack([cwfull[:, o:o + 128].T for o in offs], 1)
    cb = np.stack([cbfull[o:o + 128] for o in offs], 1)
    dvec = np.repeat(prm['m_d'][L, 2 * hp:2 * hp + 2], 64)[:, None]
    normw = prm['m_norm_w'][L, hp * 128:(hp + 1) * 128][:, None]
    A = np.ascontiguousarray
    return {"zT": A(z), "xbcT": A(np.stack([x, Bm, Cm])), "dtr": A(dtr), "dtb": A(dtb), "alog": A(alog), "cw": A(cw), "cb": A(cb),
            "dvec": A(dvec), "normw": A(normw), "consts": host_consts()}


NPK = 34


def host_consts64():
    k = np.arange(128)[:, None]; i = np.arange(128)[None, :]
    same = (k // 64) == (i // 64)
    f = lambda m: m.astype(np.float32)
    tri_f = f(same & (k <= i)); tri_b = f(same & (k >= i))
    nm_f = np.where(same & (i >= k), 0.0, -30000.0); nm_b = np.where(same & (i <= k), 0.0, -30000.0)
    pms_f = np.where(same & (i < k), 0.0, 30000.0); pms_b = np.where(same & (i > k), 0.0, 30000.0)
    blk = f(same); selA = f(np.broadcast_to(k < 64, (128, 128))); selB = f(np.broadcast_to(k >= 64, (128, 128)))
    inc_f = f(same & (i < k)); inc_b = f(same & (i > k))
    return np.stack([np.eye(128), tri_f, tri_b, nm_f, nm_b, pms_f, pms_b, blk, selA, selB, inc_f, inc_b]).astype(np.float32)

C64_NAMES = ["ident", "tri_f", "tri_b", "nm_f", "nm_b", "pms_f", "pms_b", "blk", "selA", "selB", "sl", "su"]


def load_consts64(P, cd, only=None):
    c = {}
    for i, nm in enumerate(C64_NAMES):
        if only is not None and nm not in only:
            continue
        t = P.sb("c_" + nm, [128, 128]); P.dma("sp", t[:], cd[i], writes=["c_" + nm]); c[nm] = t
    ones = P.sb("c_ones", [128, 128]); P.I("pool", "memset", ones[:], 1.0, w=["c_ones"]); c["ones"] = ones
    return c


def tri_inverse_apply(P, C, Lm, X, ncolsX, bk, tg):
    Pt = [P.sb(f"{tg}P{i}", [128, 128]) for i in range(2)] if not hasattr(P, "_tri_" + tg) else getattr(P, "_tri_" + tg)[0]
    Qt = [P.sb(f"{tg}Q{i}", [128, 128]) for i in range(2)] if not hasattr(P, "_tri_" + tg) else getattr(P, "_tri_" + tg)[1]
    setattr(P, "_tri_" + tg, (Pt, Qt))
    (pP, kP), (pQ, kQ), (pT, kT), (pX, kX) = bk["P"], bk["Q"], bk["T"], bk["X"]
    ident = C["ident"]
    P.I("pe", "transpose", pT[:, 0:128], Lm[:], ident[:], r=[tg + "L", "c_ident"], w=[kT])
    P.I("act", "activation", Qt[0][:], pT[:, 0:128], AF.Copy, r=[kT], w=[f"{tg}Q0"])
    P.I("pe", "matmul", pX[:, 0:ncolsX], Qt[0][:], X[:], start=True, stop=True, r=[f"{tg}Q0", tg + "X"], w=[kX])
    P.I("dve", "tensor_tensor", X[:], X[:], pX[:, 0:ncolsX], ALU.subtract, r=[tg + "X", kX], w=[tg + "X"])
    Pc, Pk, Qc, Qk = Lm, tg + "L", Qt[0], f"{tg}Q0"
    for lvl in range(1, 6):
        a = lvl % 2
        P.I("pe", "matmul", pQ[:, 0:128], Pc[:], Qc[:], start=True, stop=True, r=[Pk, Qk], w=[kQ])
        if lvl < 5:
            P.I("pe", "matmul", pP[:, 0:128], Qc[:], Pc[:], start=True, stop=True, r=[Pk, Qk], w=[kP])
            P.I("dve", "tensor_copy", Pt[a][:], pP[:, 0:128], r=[kP], w=[f"{tg}P{a}"])
        P.I("act", "activation", Qt[a][:], pQ[:, 0:128], AF.Copy, r=[kQ], w=[f"{tg}Q{a}"])
        Pc, Pk, Qc, Qk = Pt[a], f"{tg}P{a}", Qt[a], f"{tg}Q{a}"
        P.I("pe", "matmul", pX[:, 0:ncolsX], Qc[:], X[:], start=True, stop=True, r=[Qk, tg + "X"], w=[kX])
        P.I("dve", "tensor_tensor", X[:], X[:], pX[:, 0:ncolsX], ALU.add, r=[tg + "X", kX], w=[tg + "X"])


def build_B2():
    nc = bass.Bass("TRN2", target_bir_lowering=False)
    D = lambda n, s: nc.dram_tensor(n, s, F32, kind="ExternalInput").ap()
    qkvd = D("qkvT", [3, 128, TSEQ]); gated = D("gate", [128, NPK, 128]); tabd = D("tab", [4, 128, 2 * QS])
    cwd = D("cw", [128, 3, 3]); nwd = D("normw", [128, 128]); cd = D("consts", [len(C64_NAMES), 128, 128])
    yd = nc.dram_tensor("y", [128, NPK, 128], F32, kind="ExternalOutput").ap()
    P = Prog(nc)
    C = load_consts64(P, cd)
    bkt = [P.ps(f"bank{i}", [128, 512]) for i in range(8)]
    BK = lambda i: (bkt[i], f"bk{i}")
    raw = P.sb("raw", [128, TSEQ]); tmp = P.sb("tmp", [128, TSEQ])
    qT = P.sb("qT", [128, TSEQ]); kT = P.sb("kT", [128, TSEQ])
    cw = P.sb("cw", [128, 3, 3]); P.dma("sp", cw[:], cwd, writes=["cw"])
    normw = P.sb("normw", [128, 128]); P.dma("sp", normw[:], nwd, writes=["normw"])
    ktok = P.sb("ktok", [128, NPK, 128]); vtok = P.sb("vtok", [128, NPK, 128]); oacc = P.sb("oacc", [128, NPK, 128])
    rs = [P.sb(f"rs{i}", [128, 256]) for i in range(2)]
    for ti, (dst, kd) in enumerate(((qT, "qT"), (kT, "kT"), (raw, "raw"))):
        P.dma("sp", raw[:], qkvd[ti], writes=["raw"])
        conv_silu(P, dst, raw, cw, None, ti, kd, "raw", tmp, "tmp")
        if ti < 2:
            P.I("act", "activation", tmp[:], dst[:], AF.Square, r=[kd], w=["tmp"])
            for i, t0 in enumerate(range(0, TSEQ, 256)):
                b = i % 2; (pa, pk) = BK(b)
                P.I("pe", "matmul", pa[:, 0:256], C["ones"][:], tmp[:, t0:t0 + 256], start=True, stop=True, r=["tmp", "c_ones"], w=[pk])
                P.I("dve", "tensor_scalar", rs[b][:], pa[:, 0:256], 1e-6, None, ALU.add, r=[pk], w=[f"rs{b}"])
                P.I("dve", "reciprocal", rs[b][:], rs[b][:], r=[f"rs{b}"], w=[f"rs{b}"])
                P.I("act", "activation", rs[b][:], rs[b][:], AF.Sqrt, r=[f"rs{b}"], w=[f"rs{b}"])
                sc = 128.0 ** -0.5 if ti == 0 else 1.0
                P.I("dve", "scalar_tensor_tensor", dst[:, t0:t0 + 256], dst[:, t0:t0 + 256], sc, rs[b][:], ALU.mult, ALU.mult,
                    r=[kd, f"rs{b}"], w=[kd])
    vT = raw
    for c in range(NPK):
        sl = slice(c * 128, (c + 1) * 128)
        for src, sk, dst, dk, bi in ((kT, "kT", ktok, "ktok", 0), (vT, "raw", vtok, "vtok", 1)):
            (pa, pk) = BK(bi)
            P.I("pe", "transpose", pa[:, 0:128], src[:, sl], C["ident"][:], r=[sk, "c_ident"], w=[pk])
            P.I("act" if bi else "dve", "activation" if bi else "tensor_copy", dst[:, c, :], pa[:, 0:128], *([AF.Copy] if bi else []), r=[pk], w=[dk])
    W2 = 2 * QS
    tb = {n: P.sb("t_" + n, [128, W2]) for n in ("braw", "araw", "dtb", "alog", "beta", "g", "gc", "ngc", "egc", "toend", "glA", "glB", "bw")}
    for i, n in enumerate(("braw", "araw", "dtb", "alog")):
        P.dma("sp", tb[n][:], tabd[i], writes=["t_" + n])
    P.I("act", "activation", tb["beta"][:], tb["braw"][:], AF.Sigmoid, r=["t_braw"], w=["t_beta"])
    P.I("dve", "tensor_tensor", tb["araw"][:], tb["araw"][:], tb["dtb"][:], ALU.add, r=["t_araw", "t_dtb"], w=["t_araw"])
    P.I("dve", "tensor_scalar", tb["araw"][:], tb["araw"][:], 60.0, None, ALU.min, r=["t_araw"], w=["t_araw"])
    P.I("act", "activation", tb["araw"][:], tb["araw"][:], AF.Exp, r=["t_araw"], w=["t_araw"])
    P.I("act", "activation", tb["araw"][:], tb["araw"][:], AF.Ln, bias=1.0, r=["t_araw"], w=["t_araw"])
    P.I("act", "activation", tb["alog"][:], tb["alog"][:], AF.Exp, r=["t_alog"], w=["t_alog"])
    P.I("dve", "scalar_tensor_tensor", tb["g"][:], tb["araw"][:], -1.0, tb["alog"][:], ALU.mult, ALU.mult, r=["t_araw", "t_alog"], w=["t_g"])
    (p0, k0), (p1, k1), (p2, k2), (p3, k3) = BK(0), BK(1), BK(2), BK(3)
    P.I("pe", "matmul", p0[:, 0:QS], C["tri_f"][:], tb["g"][:, 0:QS], start=True, stop=True, r=["t_g", "c_tri_f"], w=[k0])
    P.I("pe", "matmul", p0[:, QS:W2], C["tri_b"][:], tb["g"][:, QS:W2], start=True, stop=True, r=["t_g", "c_tri_b"], w=[k0])
    P.I("pe", "matmul", p1[:, 0:W2], C["blk"][:], tb["g"][:], start=True, stop=True, r=["t_g", "c_blk"], w=[k1])
    P.I("pe", "matmul", p2[:, 0:W2], C["selA"][:], tb["g"][:], start=True, stop=True, r=["t_g", "c_selA"], w=[k2])
    P.I("pe", "matmul", p3[:, 0:W2], C["selB"][:], tb["g"][:], start=True, stop=True, r=["t_g", "c_selB"], w=[k3])
    P.I("dve", "tensor_copy", tb["gc"][:], p0[:, 0:W2], r=[k0], w=["t_gc"])
    P.I("dve", "tensor_scalar", tb["ngc"][:], tb["gc"][:], -1.0, None, ALU.mult, r=["t_gc"], w=["t_ngc"])
    P.I("act", "activation", tb["egc"][:], tb["gc"][:], AF.Exp, r=["t_gc"], w=["t_egc"])
    P.I("dve", "tensor_tensor", tb["toend"][:], p1[:, 0:W2], tb["gc"][:], ALU.subtract, r=[k1, "t_gc"], w=["t_toend"])
    P.I("act", "activation", tb["toend"][:], tb["toend"][:], AF.Exp, r=["t_toend"], w=["t_toend"])
    P.I("act", "activation", tb["glA"][:], p2[:, 0:W2], AF.Exp, r=[k2], w=["t_glA"])
    P.I("act", "activation", tb["glB"][:], p3[:, 0:W2], AF.Exp, r=[k3], w=["t_glB"])
    P.I("dve", "tensor_tensor", tb["bw"][:], tb["beta"][:], tb["egc"][:], ALU.mult, r=["t_beta", "t_egc"], w=["t_bw"])
    P.I("pool", "memset", oacc[:], 0.0, w=[f"oacc{c}" for c in range(NPK)])
    names = (("grep", [128, 128]), ("DmT", [128, 128]), ("DmS", [128, 128]), ("Et", [128, 128]), ("attnT", [128, 128]), ("L", [128, 128]),
             ("qdT", [128, 128]), ("X", [128, 256]), ("kdec", [128, 128]), ("wT", [128, 128]), ("vnew", [128, 128]), ("S", [128, 128]))
    TL = [{n: P.sb(f"dn{d}{n}", shp) for n, shp in names} for d in range(2)]

    def run_dir(d):
        T = TL[d]; K = lambda n: f"dn{d}{n}"
        ba, bb, bc_, bd_ = [bkt[4 * d + i] for i in range(4)]; ka, kb, kc, kd = [f"bk{4*d+i}" for i in range(4)]
        pG, pA, pPq, pQq = ba[:, 0:128], ba[:, 128:256], ba[:, 256:384], ba[:, 384:512]
        pD1, pD2, pE, pT = bb[:, 0:128], bb[:, 128:256], bb[:, 256:384], bb[:, 384:512]
        pX, pV, pS = bc_[:, 0:256], bc_[:, 256:384], bc_[:, 384:512]
        pO = bd_[:, 0:128]
        bkinv = {"P": (pPq, ka), "Q": (pQq, ka), "T": (pT, kb), "X": (pX, kc)}
        sfx = "_f" if d == 0 else "_b"
        tri, nm, pms = C["tri" + sfx], C["nm" + sfx], C["pms" + sfx]
        order = list(range(NPK)) if d == 0 else [1, 0] + list(range(NPK - 1, 1, -1))
        S, grep, DmT, DmS, Et, attnT, Lm, qdT, X, kdec, wT, vnew = [T[n] for n in ("S", "grep", "DmT", "DmS", "Et", "attnT", "L", "qdT", "X", "kdec", "wT", "vnew")]
        P.I("pool", "memset", S[:], 0.0, w=[K("S")])
        for c in order:
            sl = slice(c * 128, (c + 1) * 128); col = d * QS + c; cs = slice(col, col + 1)
            P.I("pe", "matmul", pG, kT[:, sl], kT[:, sl], start=True, stop=True, r=["kT"], w=[ka])
            P.I("pe", "matmul", pA, kT[:, sl], qT[:, sl], start=True, stop=True, r=["kT", "qT"], w=[ka])
            P.I("pool", "tensor_scalar", grep[:], C["ones"][:], tb["g"][:, cs], None, ALU.mult, r=["c_ones", "t_g"], w=[K("grep")])
            P.I("pe", "matmul", pD1, grep[:], tri[:], start=True, stop=False, r=[K("grep"), "c_tri" + sfx], w=[kb])
            P.I("pe", "matmul", pD1, C["ident"][:], nm[:], start=False, stop=True, r=["c_ident", "c_nm" + sfx], w=[kb])
            P.I("act", "activation", DmT[:], pD1, AF.Exp, bias=tb["ngc"][:, cs], r=[kb, "t_ngc"], w=[K("DmT")])
            P.I("pe", "matmul", pD2, grep[:], tri[:], start=True, stop=False, r=[K("grep"), "c_tri" + sfx], w=[kb])
            P.I("pe", "matmul", pD2, C["ident"][:], pms[:], start=False, stop=True, r=["c_ident", "c_pms" + sfx], w=[kb])
            P.I("act", "activation", DmS[:], pD2, AF.Exp, bias=tb["gc"][:, cs], scale=-1.0, r=[kb, "t_gc"], w=[K("DmS")])
            P.I("pe", "matmul", pE, grep[:], tri[:], start=True, stop=True, r=[K("grep"), "c_tri" + sfx], w=[kb])
            P.I("act", "activation", Et[:], pE, AF.Exp, r=[kb], w=[K("Et")])
            P.I("dve", "tensor_tensor", attnT[:], pA, DmT[:], ALU.mult, r=[ka, K("DmT")], w=[K("attnT")])
            P.I("dve", "scalar_tensor_tensor", Lm[:], pG, tb["beta"][:, cs], DmS[:], ALU.mult, ALU.mult, r=[ka, "t_beta", K("DmS")], w=[K("L")])
            P.I("dve", "tensor_tensor", qdT[:], qT[:, sl], Et[:], ALU.mult, r=["qT", K("Et")], w=[K("qdT")])
            P.I("dve", "tensor_scalar", X[:, 0:128], vtok[:, c, :], tb["beta"][:, cs], None, ALU.mult, r=["vtok", "t_beta"], w=[K("X")])
            P.I("dve", "tensor_scalar", X[:, 128:256], ktok[:, c, :], tb["bw"][:, cs], None, ALU.mult, r=["ktok", "t_bw"], w=[K("X")])
            P.I("pool", "tensor_scalar", kdec[:], ktok[:, c, :], tb["toend"][:, cs], None, ALU.mult, r=["ktok", "t_toend"], w=[K("kdec")])
            tri_inverse_apply(P, C, Lm, X, 256, bkinv, f"dn{d}")
            P.I("pe", "transpose", pT, X[:, 128:256], C["ident"][:], r=[K("X"), "c_ident"], w=[kb])
            P.I("act", "activation", wT[:], pT, AF.Copy, r=[kb], w=[K("wT")])
            for half in ((0, 1) if d == 0 else (1, 0)):
                rows = slice(half * 64, (half + 1) * 64)
                gl = tb["glA"] if half == 0 else tb["glB"]; glk = "t_glA" if half == 0 else "t_glB"
                P.I("pe", "matmul", pV, wT[:], S[:], start=True, stop=True, r=[K("wT"), K("S")], w=[kc])
                P.I("dve", "tensor_tensor", vnew[rows, :], X[rows, 0:128], pV[rows, :], ALU.subtract, r=[K("X"), kc], w=[K("vnew")])
                P.I("pe", "matmul", pO, qdT[:], S[:], start=True, stop=False, r=[K("qdT"), K("S")], w=[kd])
                P.I("pe", "matmul", pO, attnT[rows, :], vnew[rows, :], start=False, stop=True, r=[K("attnT"), K("vnew")], w=[kd])
                P.I("dve", "tensor_tensor", oacc[rows, c, :], oacc[rows, c, :], pO[rows, :], ALU.add, r=[kd, f"oacc{c}"], w=[f"oacc{c}"])
                P.I("pe", "matmul", pS, kdec[rows, :], vnew[rows, :], start=True, stop=True, r=[K("kdec"), K("vnew")], w=[kc])
                P.I("dve", "scalar_tensor_tensor", S[:], S[:], gl[:, cs], pS, ALU.mult, ALU.add, r=[K("S"), glk, kc], w=[K("S")])

    P.interleave([lambda: run_dir(0), lambda: run_dir(1)])
    allo = [f"oacc{c}" for c in range(NPK)]
    gate = P.sb("gate", [128, NPK, 128]); sq = P.sb("sq", [128, NPK, 128]); ss = P.sb("ss", [128, NPK])
    P.dma("sp", gate[:], gated, writes=["gate"])
    P.I("act", "activation", gate[:], gate[:], AF.Silu, r=["gate"], w=["gate"])
    P.I("act", "activation", sq[:], oacc[:], AF.Square, r=allo, w=["sq"])
    P.I("dve", "tensor_reduce", ss[:], sq[:], AX.X, ALU.add, r=["sq"], w=["ss"])
    P.I("dve", "tensor_scalar", ss[:], ss[:], 1.0 / 128, 1e-6, ALU.mult, ALU.add, r=["ss"], w=["ss"])
    P.I("dve", "reciprocal", ss[:], ss[:], r=["ss"], w=["ss"])
    P.I("act", "activation", ss[:], ss[:], AF.Sqrt, r=["ss"], w=["ss"])
    for c in range(NPK):
        P.I("dve", "scalar_tensor_tensor", sq[:, c, :], oacc[:, c, :], ss[:, c:c + 1], normw[:], ALU.mult, ALU.mult, r=allo + ["ss", "normw"], w=["sq"])
    P.I("dve", "tensor_tensor", sq[:], sq[:], gate[:], ALU.mult, r=["sq", "gate"], w=["sq"])
    P.dma("sp", yd, sq[:], reads=["sq"], is_output=True)
    P.finish()
    return nc


def colmajor_perm():
    t = np.arange(4096).reshape(64, 64)
    return t.T.reshape(-1)


def host_B2_inputs(pb, L, b, head, prm):
    perm = np.concatenate([np.arange(256), 256 + colmajor_perm()])
    p = pb[b][perm]
    q = p[:, head * 128:(head + 1) * 128].T; k = p[:, 512 + head * 128:512 + (head + 1) * 128].T
    v = p[:, 1024 + head * 128:1024 + (head + 1) * 128].T
    gate = p[:, 1536 + head * 128:1536 + (head + 1) * 128].reshape(NPK, 128, 128).transpose(1, 0, 2)
    def tabl(cols):
        t = np.zeros((128, 2, QS), np.float32); t[:, :, :NPK] = p[:, cols].reshape(NPK, 128, 2).transpose(1, 2, 0); return t.reshape(128, 2 * QS)
    braw = tabl([2048 + d * 4 + head for d in range(2)]); araw = tabl([2056 + d * 4 + head for d in range(2)])
    bc = lambda v_: np.broadcast_to(np.asarray(v_, np.float32)[None, :, None], (128, 2, QS)).reshape(128, 2 * QS)
    dtb = bc(prm['dn_dt_bias'][L, :, head]); alog = bc(prm['dn_a_log'][L, :, head])
    cwf = prm['dn_conv_w'][L]
    cw = np.stack([cwf[:, o + head * 128:o + (head + 1) * 128].T for o in (0, 512, 1024)], 1)
    normw = np.broadcast_to(prm['dn_norm_w'][L][None, :], (128, 128))
    A = lambda a: np.ascontiguousarray(a, dtype=np.float32)
    return {"qkvT": A(np.stack([q, k, v])), "gate": A(gate), "tab": A(np.stack([braw, araw, dtb, alog])), "cw": A(cw), "normw": A(normw),
            "consts": host_consts64()}


def host_B2_output(y):
    yy = y.transpose(1, 0, 2).reshape(TSEQ, 128)
    out = np.empty_like(yy)
    perm = np.concatenate([np.arange(256), 256 + colmajor_perm()])
    out[perm] = yy
    return out


def build_B3():
    nc = bass.Bass("TRN2", target_bir_lowering=False)
    D = lambda n, s: nc.dram_tensor(n, s, F32, kind="ExternalInput").ap()
    p64d = D("p64", [4, 64, TSEQ]); p128d = D("p128", [2, 128, TSEQ]); mu64d = D("mu64", [4, 64, 8]); mu128d = D("mu128", [2, 128, 8])
    pvd = D("pv", [64, 8]); a2d = D("a2h", [64, 64]); g2d = D("g2h", [128, 64]); w2d = D("w2pad", [2, 128, 64])
    cd = D("consts", [len(C64_NAMES), 128, 128])
    yd = nc.dram_tensor("y", [64, TSEQ], F32, kind="ExternalOutput").ap()
    P = Prog(nc)
    C = load_consts64(P, cd, only=("ident", "tri_f", "tri_b", "blk", "sl", "su"))
    bkt = [P.ps(f"bank{i}", [128, 512]) for i in range(8)]
    BK = lambda i: (bkt[i], f"bk{i}")
    raw = P.sb("raw", [128, TSEQ]); mix = P.sb("mix", [128, TSEQ])
    mu64 = P.sb("mu64", [64, 4, 8]); mu128 = P.sb("mu128", [128, 2, 8]); pv = P.sb("pv", [64, 8])
    for i in range(4):
        P.dma("sp", mu64[:, i, :], mu64d[i], writes=["mu64"])
    for i in range(2):
        P.dma("sp", mu128[:, i, :], mu128d[i], writes=["mu128"])
    P.dma("sp", pv[:], pvd, writes=["pv"])
    a2h = P.sb("a2h", [64, 64]); g2h = P.sb("g2h", [128, 64]); w2p = P.sb("w2p", [128, 2, 64])
    P.dma("sp", a2h[:], a2d, writes=["a2h"]); P.dma("sp", g2h[:], g2d, writes=["g2h"])
    for j in range(2):
        P.dma("sp", w2p[:, j, :], w2d[j], writes=["w2p"])
    omm64 = P.sb("omm64", [64, 4]); omm128 = P.sb("omm128", [128, 2])
    P.I("dve", "tensor_scalar", omm64[:], mu64[:, :, 0], -1.0, 1.0, ALU.mult, ALU.add, r=["mu64"], w=["omm64"])
    P.I("dve", "tensor_scalar", omm128[:], mu128[:, :, 0], -1.0, 1.0, ALU.mult, ALU.add, r=["mu128"], w=["omm128"])

    def token_mix(dst, dk, src_d, npart, mu, muk, omm, ommk, ti):
        R = slice(0, npart)
        P.dma("sp", raw[R, :], src_d, writes=["raw"])
        P.I("dve", "tensor_scalar", dst[R, :], raw[R, :], omm[R, ti:ti + 1], None, ALU.mult, r=["raw", ommk], w=[dk])
        def acc(o0, o1, i0, i1, mcol, eng="dve"):
            P.I(eng, "scalar_tensor_tensor", dst[R, o0:o1], raw[R, i0:i1], mu[R, ti, mcol:mcol + 1], dst[R, o0:o1], ALU.mult, ALU.add,
                r=["raw", muk, dk], w=[dk])
        acc(1, 256, 0, 255, 5); acc(0, 255, 1, 256, 6)
        acc(256 + 64, TSEQ, 256, TSEQ - 64, 3); acc(256, TSEQ - 64, 256 + 64, TSEQ, 4)
        dl = dst[R, 256:TSEQ].rearrange("p (r c) -> p r c", c=64); rl = raw[R, 256:TSEQ].rearrange("p (r c) -> p r c", c=64)
        P.I("dve", "scalar_tensor_tensor", dl[:, :, 1:64], rl[:, :, 0:63], mu[R, ti, 1:2], dl[:, :, 1:64], ALU.mult, ALU.add, r=["raw", muk, dk], w=[dk])
        P.I("dve", "scalar_tensor_tensor", dl[:, :, 0:63], rl[:, :, 1:64], mu[R, ti, 2:3], dl[:, :, 0:63], ALU.mult, ALU.add, r=["raw", muk, dk], w=[dk])

    rT = P.sb("rT", [64, TSEQ]); kT = P.sb("kT", [64, TSEQ]); vT = P.sb("vT", [64, TSEQ]); aT = P.sb("aT", [64, TSEQ])
    gT = P.sb("gT", [64, TSEQ]); bT = P.sb("bT", [64, TSEQ]); lwT = [P.sb(f"lwT{j}", [64, TSEQ]) for j in range(2)]
    token_mix(rT, "rT", p64d[0], 64, mu64, "mu64", omm64, "omm64", 0)
    token_mix(kT, "kT", p64d[1], 64, mu64, "mu64", omm64, "omm64", 1)
    token_mix(vT, "vT", p64d[2], 64, mu64, "mu64", omm64, "omm64", 2)
    NB = 256
    token_mix(mix, "mix", p64d[3], 64, mu64, "mu64", omm64, "omm64", 3)
    for i, t0 in enumerate(range(0, TSEQ, NB)):
        (pa, pk) = BK(i % 2)
        P.I("pe", "matmul", pa[0:64, 0:NB], a2h[:], mix[0:64, t0:t0 + NB], start=True, stop=True, r=["a2h", "mix"], w=[pk])
        P.I("act", "activation", aT[:, t0:t0 + NB], pa[0:64, 0:NB], AF.Sigmoid, bias=pv[:, 0:1], r=[pk, "pv"], w=["aT"])
    token_mix(mix, "mix", p128d[0], 128, mu128, "mu128", omm128, "omm128", 0)
    P.I("act", "activation", mix[:], mix[:], AF.Tanh, r=["mix"], w=["mix"])
    for j in range(2):
        for i, t0 in enumerate(range(0, TSEQ, NB)):
            (pa, pk) = BK(i % 2)
            P.I("pe", "matmul", pa[0:64, 0:NB], w2p[:, j, :], mix[:, t0:t0 + NB], start=True, stop=True, r=["w2p", "mix"], w=[pk])
            P.I("act", "activation", lwT[j][:, t0:t0 + NB], pa[0:64, 0:NB], AF.Sigmoid, bias=pv[:, 3 + j:4 + j], r=[pk, "pv"], w=[f"lwT{j}"])
        P.I("dve", "tensor_scalar", lwT[j][:], lwT[j][:], -float(np.exp(-0.5)), None, ALU.mult, r=[f"lwT{j}"], w=[f"lwT{j}"])
    token_mix(mix, "mix", p128d[1], 128, mu128, "mu128", omm128, "omm128", 1)
    P.I("act", "activation", mix[:], mix[:], AF.Sigmoid, r=["mix"], w=["mix"])
    for i, t0 in enumerate(range(0, TSEQ, NB)):
        (pa, pk) = BK(i % 2)
        P.I("pe", "matmul", pa[0:64, 0:NB], g2h[:], mix[:, t0:t0 + NB], start=True, stop=True, r=["g2h", "mix"], w=[pk])
        P.I("act", "activation", gT[:, t0:t0 + NB], pa[0:64, 0:NB], AF.Copy, r=[pk], w=["gT"])
    kk = mix
    P.I("dve", "tensor_scalar", kk[0:64, :], kT[:], pv[:, 1:2], None, ALU.mult, r=["kT", "pv"], w=["mix"])
    P.I("act", "activation", raw[0:64, :], kk[0:64, :], AF.Square, r=["mix"], w=["raw"])
    rs = [P.sb(f"rs{i}", [64, NB]) for i in range(2)]
    for i, t0 in enumerate(range(0, TSEQ, NB)):
        b = i % 2; (pa, pk) = BK(b)
        P.I("pe", "matmul", pa[0:64, 0:NB], C["ones"][0:64, 0:64], raw[0:64, t0:t0 + NB], start=True, stop=True, r=["raw", "c_ones"], w=[pk])
        P.I("dve", "tensor_scalar", rs[b][:], pa[0:64, 0:NB], 1e-6, None, ALU.add, r=[pk], w=[f"rs{b}"])
        P.I("dve", "reciprocal", rs[b][:], rs[b][:], r=[f"rs{b}"], w=[f"rs{b}"])
        P.I("act", "activation", rs[b][:], rs[b][:], AF.Sqrt, r=[f"rs{b}"], w=[f"rs{b}"])
        P.I("dve", "tensor_tensor", kk[0:64, t0:t0 + NB], kk[0:64, t0:t0 + NB], rs[b][:], ALU.mult, r=["mix", f"rs{b}"], w=["mix"])
    P.I("dve", "tensor_tensor", bT[:], kk[0:64, :], aT[:], ALU.mult, r=["mix", "aT"], w=["bT"])
    P.I("dve", "tensor_scalar", kk[0:64, :], kk[0:64, :], -1.0, None, ALU.mult, r=["mix"], w=["mix"])
    P.I("dve", "tensor_scalar", aT[:], aT[:], -1.0, pv[:, 2:3], ALU.add, ALU.mult, r=["aT", "pv"], w=["aT"])
    P.I("dve", "scalar_tensor_tensor", kT[:], aT[:], 1.0, kT[:], ALU.add, ALU.mult, r=["aT", "kT"], w=["kT"])
    avT = kk
    oacc = P.sb("oacc", [128, NPK, 64])
    P.I("pool", "memset", oacc[:], 0.0, w=[f"oacc{c}" for c in range(NPK)])
    tnames = (("H", [64, 64]), ("lwtok", [128, 64]), ("ea_tok", [128, 64]), ("te_tok", [128, 64]), ("ep", [64, 128]), ("em", [64, 128]), ("eaT", [64, 128]),
              ("atl", [64, 128]), ("btl", [64, 128]), ("ktl", [64, 128]), ("rtl", [64, 128]), ("L", [128, 128]), ("AakT", [128, 128]), ("ArbT", [128, 128]),
              ("ArkT", [128, 128]), ("X", [128, 128]), ("Bh", [128, 64]), ("Kh", [128, 64]), ("W1T", [64, 128]), ("U", [128, 64]), ("pc2", [64, 2]),
              ("av_t", [128, 64]), ("b_t", [128, 64]), ("k_t", [128, 64]), ("v_t", [128, 64]))
    TL = [{n: P.sb(f"rw{d}{n}", shp) for n, shp in tnames} for d in range(2)]

    def run_dir(d):
        T = TL[d]; K = lambda n: f"rw{d}{n}"
        ba, bb, bc_, bd_ = [bkt[4 * d + i] for i in range(4)]; ka, kb, kc, kd = [f"bk{4*d+i}" for i in range(4)]
        ptr = {"av": ba[:, 0:64], "b": ba[:, 64:128], "k": ba[:, 128:192], "v": ba[:, 192:256]}
        p_lw, p_lp, p_tot = ba[:, 256:320], ba[:, 320:384], ba[:, 384:448]
        p_lpT, p_ab, p_ak, p_rb = bb[0:64, 0:128], bb[:, 128:256], bb[:, 256:384], bb[:, 384:512]
        p_rk, p_akv, p_T, p_w1 = bc_[:, 0:128], bc_[:, 128:192], bc_[:, 192:320], bc_[0:64, 320:448]
        p_H = bc_[0:64, 448:512]
        p_P, p_Q, p_X, p_U, p_Y = bd_[:, 0:128], bd_[:, 128:256], bd_[:, 256:384], bd_[:, 384:448], bd_[:, 448:512]
        bkinv = {"P": (p_P, kd), "Q": (p_Q, kd), "T": (p_T, kc), "X": (p_X, kd)}
        sfx = "_f" if d == 0 else "_b"
        tri = C["tri" + sfx]; trik = "c_tri" + sfx
        m_strict_ts = C["sl"] if d == 0 else C["su"]; mk_ts = "c_sl" if d == 0 else "c_su"
        m_strict_st = C["su"] if d == 0 else C["sl"]; mk_st = "c_su" if d == 0 else "c_sl"
        m_incl_st = C["tri_f"] if d == 0 else C["tri_b"]; mk_in = trik
        order = list(range(NPK)) if d == 0 else [1, 0] + list(range(NPK - 1, 1, -1))
        H, lwtok, ea_tok, te_tok, ep, em, eaT, atl, btl, ktl, rtl, Lm, AakT, ArbT, ArkT, X, Bh, Kh, W1T, U, pc2 = [T[n] for n in (
            "H", "lwtok", "ea_tok", "te_tok", "ep", "em", "eaT", "atl", "btl", "ktl", "rtl", "L", "AakT", "ArbT", "ArkT", "X", "Bh", "Kh", "W1T", "U", "pc2")]
        tk = {n: T[n + "_t"] for n in ("av", "b", "k", "v")}
        lw = lwT[d]; lwk = f"lwT{d}"
        P.I("pool", "memset", H[:], 0.0, w=[K("H")])
        for c in order:
            sl = slice(c * 128, (c + 1) * 128)
            for ii, (n, src, sk) in enumerate((("av", avT, "mix"), ("b", bT, "bT"), ("k", kT, "kT"), ("v", vT, "vT"))):
                P.I("pe", "transpose", ptr[n], src[0:64, sl], C["ident"][0:64, 0:64], r=[sk, "c_ident"], w=[ka])
                if ii % 2:
                    P.I("act", "activation", tk[n][:], ptr[n], AF.Copy, r=[ka], w=[K(n + "_t")])
                else:
                    P.I("dve", "tensor_copy", tk[n][:], ptr[n], r=[ka], w=[K(n + "_t")])
            P.I("pe", "transpose", p_lw, lw[:, sl], C["ident"][0:64, 0:64], r=[lwk, "c_ident"], w=[ka])
            P.I("dve", "tensor_copy", lwtok[:], p_lw, r=[ka], w=[K("lwtok")])
            P.I("pe", "matmul", p_lp, tri[:], lwtok[:], start=True, stop=True, r=[trik, K("lwtok")], w=[ka])
            P.I("pe", "matmul", p_tot, C["blk"][:], lwtok[:], start=True, stop=True, r=["c_blk", K("lwtok")], w=[ka])
            P.I("pe", "matmul", p_lpT, lwtok[:], tri[:], start=True, stop=True, r=[trik, K("lwtok")], w=[kb])
            P.I("dve", "tensor_tensor", ea_tok[:], p_lp, lwtok[:], ALU.subtract, r=[ka, K("lwtok")], w=[K("ea_tok")])
            P.I("act", "activation", ea_tok[:], ea_tok[:], AF.Exp, r=[K("ea_tok")], w=[K("ea_tok")])
            P.I("dve", "tensor_copy", te_tok[:], p_lp, r=[ka], w=[K("te_tok")])
            P.I("dve", "tensor_tensor", te_tok[:], p_tot, te_tok[:], ALU.subtract, r=[ka, K("te_tok")], w=[K("te_tok")])
            P.I("act", "activation", te_tok[:], te_tok[:], AF.Exp, r=[K("te_tok")], w=[K("te_tok")])
            P.I("act", "activation", ep[:], p_lpT, AF.Exp, r=[kb], w=[K("ep")])
            P.I("act", "activation", em[:], p_lpT, AF.Exp, scale=-1.0, r=[kb], w=[K("em")])
            P.I("dve", "tensor_tensor", eaT[:], p_lpT, lw[:, sl], ALU.subtract, r=[kb, lwk], w=[K("eaT")])
            P.I("act", "activation", eaT[:], eaT[:], AF.Exp, r=[K("eaT")], w=[K("eaT")])
            cA, cB = (63, 127) if d == 0 else (0, 64)
            P.I("act", "activation", pc2[:, 0:1], p_lpT[:, cA:cA + 1], AF.Exp, r=[kb], w=[K("pc2")])
            P.I("act", "activation", pc2[:, 1:2], p_lpT[:, cB:cB + 1], AF.Exp, r=[kb], w=[K("pc2")])
            P.I("dve", "tensor_tensor", atl[:], avT[0:64, sl], eaT[:], ALU.mult, r=["mix", K("eaT")], w=[K("atl")])
            P.I("dve", "tensor_tensor", btl[:], bT[:, sl], em[:], ALU.mult, r=["bT", K("em")], w=[K("btl")])
            P.I("pool", "tensor_tensor", ktl[:], kT[:, sl], em[:], ALU.mult, r=["kT", K("em")], w=[K("ktl")])
            P.I("pool", "tensor_tensor", rtl[:], rT[:, sl], ep[:], ALU.mult, r=["rT", K("ep")], w=[K("rtl")])
            P.I("pe", "matmul", p_ab, atl[:], btl[:], start=True, stop=True, r=[K("atl"), K("btl")], w=[kb])
            P.I("dve", "scalar_tensor_tensor", Lm[:], p_ab, -1.0, m_strict_ts[:], ALU.mult, ALU.mult, r=[kb, mk_ts], w=[K("L")])
            P.I("pe", "matmul", p_ak, ktl[:], atl[:], start=True, stop=True, r=[K("ktl"), K("atl")], w=[kb])
            P.I("dve", "tensor_tensor", AakT[:], p_ak, m_strict_st[:], ALU.mult, r=[kb, mk_st], w=[K("AakT")])
            P.I("pe", "matmul", p_rb, btl[:], rtl[:], start=True, stop=True, r=[K("btl"), K("rtl")], w=[kb])
            P.I("dve", "tensor_tensor", ArbT[:], p_rb, m_incl_st[:], ALU.mult, r=[kb, mk_in], w=[K("ArbT")])
            P.I("pe", "matmul", p_rk, ktl[:], rtl[:], start=True, stop=True, r=[K("ktl"), K("rtl")], w=[kc])
            P.I("dve", "tensor_tensor", ArkT[:], p_rk, m_incl_st[:], ALU.mult, r=[kc, mk_in], w=[K("ArkT")])
            P.I("dve", "tensor_tensor", X[:, 0:64], tk["av"][:], ea_tok[:], ALU.mult, r=[K("av_t"), K("ea_tok")], w=[K("X")])
            P.I("pe", "matmul", p_akv, AakT[:], tk["v"][:], start=True, stop=True, r=[K("AakT"), K("v_t")], w=[kc])
            P.I("act", "activation", X[:, 64:128], p_akv, AF.Copy, r=[kc], w=[K("X")])
            P.I("pool", "tensor_tensor", Bh[:], tk["b"][:], te_tok[:], ALU.mult, r=[K("b_t"), K("te_tok")], w=[K("Bh")])
            P.I("pool", "tensor_tensor", Kh[:], tk["k"][:], te_tok[:], ALU.mult, r=[K("k_t"), K("te_tok")], w=[K("Kh")])
            tri_inverse_apply(P, C, Lm, X, 128, bkinv, f"rw{d}")
            P.I("pe", "transpose", p_w1, X[:, 0:64], C["ident"][:], r=[K("X"), "c_ident"], w=[kc])
            P.I("act", "activation", W1T[:], p_w1, AF.Copy, r=[kc], w=[K("W1T")])
            for half in ((0, 1) if d == 0 else (1, 0)):
                rows = slice(half * 64, (half + 1) * 64)
                P.I("pe", "matmul", p_U, W1T[:], H[:], start=True, stop=True, r=[K("W1T"), K("H")], w=[kd], force_same=True)
                P.I("dve", "tensor_tensor", U[rows, :], X[rows, 64:128], p_U[rows, :], ALU.add, r=[K("X"), kd], w=[K("U")])
                P.I("pe", "matmul", p_Y, rtl[:], H[:], start=True, stop=False, r=[K("rtl"), K("H")], w=[kd])
                P.I("pe", "matmul", p_Y, ArbT[rows, :], U[rows, :], start=False, stop=False, r=[K("ArbT"), K("U")], w=[kd], force_same=True)
                P.I("pe", "matmul", p_Y, ArkT[rows, :], tk["v"][rows, :], start=False, stop=True, r=[K("ArkT"), K("v_t")], w=[kd])
                P.I("dve", "tensor_tensor", oacc[rows, c, :], oacc[rows, c, :], p_Y[rows, :], ALU.add, r=[kd, f"oacc{c}"], w=[f"oacc{c}"])
                P.I("pe", "matmul", p_H, Bh[rows, :], U[rows, :], start=True, stop=False, r=[K("Bh"), K("U")], w=[kc], force_same=True)
                P.I("pe", "matmul", p_H, Kh[rows, :], tk["v"][rows, :], start=False, stop=True, r=[K("Kh"), K("v_t")], w=[kc])
                P.I("dve", "scalar_tensor_tensor", H[:], H[:], pc2[:, half:half + 1], p_H, ALU.mult, ALU.add, r=[K("H"), K("pc2"), kc], w=[K("H")])

    P.interleave([lambda: run_dir(0), lambda: run_dir(1)])
    allo = [f"oacc{c}" for c in range(NPK)]
    yT = raw; t1 = aT; t2 = bT
    for c in range(NPK):
        (pa, pk) = BK(c % 4)
        P.I("pe", "transpose", pa[0:64, 0:128], oacc[:, c, :], C["ident"][:], r=allo + ["c_ident"], w=[pk])
        P.I("act" if c % 2 else "dve", "activation" if c % 2 else "tensor_copy", yT[0:64, c * 128:(c + 1) * 128], pa[0:64, 0:128], *([AF.Copy] if c % 2 else []),
            r=[pk], w=["raw"])
    on64 = C["ones"][0:64, 0:64]
    for i, t0 in enumerate(range(0, TSEQ, NB)):
        ts = slice(t0, t0 + NB); b = i % 2
        (pm, km), (pvv, kvv), (pb, kb) = BK(b * 3), BK(b * 3 + 1), BK(b * 3 + 2)
        P.I("pe", "matmul", pm[0:64, 0:NB], on64, yT[0:64, ts], start=True, stop=True, r=["raw", "c_ones"], w=[km])
        P.I("dve", "scalar_tensor_tensor", yT[0:64, ts], pm[0:64, 0:NB], -1.0 / 64, yT[0:64, ts], ALU.mult, ALU.add, r=[km, "raw"], w=["raw"])
        P.I("act", "activation", t1[:, ts], yT[0:64, ts], AF.Square, r=["raw"], w=["aT"])
        P.I("pe", "matmul", pvv[0:64, 0:NB], on64, t1[:, ts], start=True, stop=True, r=["aT", "c_ones"], w=[kvv])
        P.I("dve", "tensor_scalar", rs[b][:], pvv[0:64, 0:NB], 1.0 / 64, 64e-5, ALU.mult, ALU.add, r=[kvv], w=[f"rs{b}"])
        P.I("dve", "reciprocal", rs[b][:], rs[b][:], r=[f"rs{b}"], w=[f"rs{b}"])
        P.I("act", "activation", rs[b][:], rs[b][:], AF.Sqrt, r=[f"rs{b}"], w=[f"rs{b}"])
        P.I("dve", "scalar_tensor_tensor", yT[0:64, ts], yT[0:64, ts], pv[:, 5:6], rs[b][:], ALU.mult, ALU.mult, r=["raw", "pv", f"rs{b}"], w=["raw"])
        P.I("dve", "scalar_tensor_tensor", t2[:, ts], rT[:, ts], pv[:, 7:8], kT[:, ts], ALU.mult, ALU.mult, r=["rT", "pv", "kT"], w=["bT"])
        P.I("pe", "matmul", pb[0:64, 0:NB], on64, t2[:, ts], start=True, stop=True, r=["bT", "c_ones"], w=[kb])
        P.I("dve", "tensor_tensor", t2[:, ts], pb[0:64, 0:NB], vT[:, ts], ALU.mult, r=[kb, "vT"], w=["bT"])
        P.I("dve", "scalar_tensor_tensor", yT[0:64, ts], yT[0:64, ts], pv[:, 6:7], t2[:, ts], ALU.add, ALU.add, r=["raw", "pv", "bT"], w=["raw"])
        P.I("dve", "tensor_tensor", yT[0:64, ts], yT[0:64, ts], gT[:, ts], ALU.mult, r=["raw", "gT"], w=["raw"])
    P.dma("sp", yd, yT[0:64, :], reads=["raw"], is_output=True)
    P.finish()
    return nc


def host_B3_inputs(pc_, L, b, head, prm):
    p = pc_[b]
    hc = slice(head * 64, (head + 1) * 64)
    mu = prm['rw_mu'][L]
    def seg(o, n):
        cols = np.arange(o, o + n); m = mu[cols]; cl = cols % 4
        tab = np.zeros((n, 8), np.float32)
        tab[:, 0] = m
        for j in range(4):
            tab[:, 1 + j] = np.where(cl == j, m, 0.0)
        tab[:, 5] = np.where(cl % 2 == 0, m, 0.0); tab[:, 6] = np.where(cl % 2 == 1, m, 0.0)
        return p[:, cols].T, tab
    s64 = [seg(head * 64, 64), seg(256 + head * 64, 64), seg(512 + head * 64, 64), seg(896, 64)]
    s128 = [seg(768, 128), seg(960, 128)]
    pv = np.zeros((64, 8), np.float32)
    pv[:, 0] = prm['rw_a0'][L][hc]; pv[:, 1] = prm['rw_k_k'][L][hc]; pv[:, 2] = prm['rw_k_a'][L][hc]
    pv[:, 3] = prm['rw_w0'][L][0][hc]; pv[:, 4] = prm['rw_w0'][L][1][hc]
    pv[:, 5] = prm['rw_ln_w'][L][hc]; pv[:, 6] = prm['rw_ln_b'][L][hc]; pv[:, 7] = prm['rw_r_k'][L][head]
    w2pad = np.zeros((2, 128, 64), np.float32)
    for j in range(2):
        w2pad[j, j * 64:(j + 1) * 64] = prm['rw_w2'][L][j][:, hc]
    A = lambda a: np.ascontiguousarray(a, dtype=np.float32)
    return {"p64": A(np.stack([s[0] for s in s64])), "p128": A(np.stack([s[0] for s in s128])),
            "mu64": A(np.stack([s[1] for s in s64])), "mu128": A(np.stack([s[1] for s in s128])),
            "pv": pv, "a2h": A(prm['rw_a2'][L][:, hc]), "g2h": A(prm['rw_g2'][L][:, hc]), "w2pad": w2pad,
            "consts": host_consts64()}


NTT = 17


def build_C1():
    nc = bass.Bass("TRN2", target_bir_lowering=False)
    D = lambda n, s: nc.dram_tensor(n, s, F32, kind="ExternalInput").ap()
    xTd = D("xT", [128, 8, NTOK]); yTd = D("yT", [128, 8, NTOK]); woutd = D("wout", [128, 8, 1024])
    modd = D("mod", [128, 8, 6]); nwd = D("nw", [128, 8]); wrd = D("wr", [128, 8, 32]); brd = D("br", [128, 32])
    O = lambda n, s: nc.dram_tensor(n, s, F32, kind="ExternalOutput").ap()
    xmd = O("xmT", [128, 8, NTOK]); h2d = O("h2T", [128, 8, NTOK]); Gd = O("G", [128, NTT, 32])
    P = Prog(nc)
    xT = P.sb("xT", [128, 8, NTOK]); ybf = P.sb("ybf", [128, 8, NTOK], BF16)
    hT32 = xT
    mod = P.sb("mod", [128, 8, 6]); nw = P.sb("nw", [128, 8]); wbf = P.sb("wbf", [128, 8, 1024], BF16)
    wr = P.sb("wr", [128, 8, 32]); br = P.sb("br", [128, 32])
    ones_bf = P.sb("ones_bf", [128, 128], BF16)
    P.I("pool", "memset", ones_bf[:], 1.0, w=["ones_bf"])
    for k in range(8):
        P.dma("sp", xT[:, k, :], xTd[:, k, :], writes=["xT"])
        P.dma("pool", ybf[:, k, :], yTd[:, k, :], writes=["ybf"])
        P.dma("pool", wbf[:, k, :], woutd[:, k, :], writes=["wbf"])
    for t, d, kk in ((mod, modd, "mod"), (nw, nwd, "nw"), (wr, wrd, "wr"), (br, brd, "br")):
        P.dma("sp", t[:], d, writes=[kk])
    pp = [P.ps(f"bank{i}", [128, 512]) for i in range(8)]
    i = 0
    for m in range(8):
        for it, (t0, n) in enumerate(TILES):
            b = i % 4; i += 1
            for k in range(8):
                P.I("pe", "matmul", pp[b][:, 0:n], wbf[:, k, m * 128:(m + 1) * 128], ybf[:, k, t0:t0 + n], start=(k == 0), stop=(k == 7),
                    r=["wbf", "ybf"], w=[f"bk{b}"])
            gcol = 3 if it == 0 else 0
            P.I("dve", "scalar_tensor_tensor", xT[:, m, t0:t0 + n], pp[b][:, 0:n], mod[:, m, gcol:gcol + 1], xT[:, m, t0:t0 + n], ALU.mult, ALU.add,
                r=[f"bk{b}", "mod", "xT"], w=["xT"])
    for k in range(8):
        P.dma("sp", xmd[:, k, :], xT[:, k, :], reads=["xT"], is_output=True)
    rms_modulate(P, xT, xT, mod, nw, ones_bf, shift_i=(1, 4), scale_i=(2, 5), tagp="n2", psb=(pp[4], pp[5]), hkey="xT")
    for k in range(8):
        P.dma("sp", h2d[:, k, :], hT32[:, k, :], reads=["xT"], is_output=True)
    G = P.sb("G", [128, NTT, 32]); lg = P.sb("lg", [128, NTT, 32]); m8 = P.sb("m8", [128, NTT, 8]); nmx = P.sb("nmx", [128, NTT])
    msk = P.sb("msk", [128, NTT, 32]); ssum = P.sb("ssum", [128, NTT])
    for tt in range(NTT):
        b = 6 + tt % 2
        for k in range(8):
            P.I("pe", "matmul", pp[b][:, 0:32], hT32[:, k, tt * 128:(tt + 1) * 128], wr[:, k, :], start=(k == 0), stop=(k == 7),
                r=["xT", "wr"], w=[f"bk{b}"])
        P.I("dve", "tensor_tensor", lg[:, tt, :], pp[b][:, 0:32], br[:], ALU.add, r=[f"bk{b}", "br"], w=["lg"])
        P.I("dve", "max", m8[:, tt, :], lg[:, tt, :], r=["lg"], w=["m8"])
        P.I("dve", "tensor_scalar", msk[:, tt, :], lg[:, tt, :], m8[:, tt, 3:4], None, ALU.is_ge, r=["lg", "m8"], w=["msk"])
        P.I("dve", "tensor_scalar", nmx[:, tt:tt + 1], m8[:, tt, 0:1], -1.0, None, ALU.mult, r=["m8"], w=["nmx"])
        P.I("act", "activation", G[:, tt, :], lg[:, tt, :], AF.Exp, bias=nmx[:, tt:tt + 1], r=["lg", "nmx"], w=["G"])
        P.I("dve", "tensor_tensor", G[:, tt, :], G[:, tt, :], msk[:, tt, :], ALU.mult, r=["G", "msk"], w=["G"])
        P.I("dve", "tensor_reduce", ssum[:, tt:tt + 1], G[:, tt, :], AX.X, ALU.add, r=["G"], w=["ssum"])
        P.I("dve", "reciprocal", ssum[:, tt:tt + 1], ssum[:, tt:tt + 1], r=["ssum"], w=["ssum"])
        P.I("dve", "tensor_scalar", G[:, tt, :], G[:, tt, :], ssum[:, tt:tt + 1], None, ALU.mult, r=["G", "ssum"], w=["G"])
    P.dma("sp", Gd, G[:], reads=["G"], is_output=True)
    P.finish()
    return nc


NT2 = 1088
T2 = [(0, 512), (512, 512), (1024, 64)]
ST2 = [(i * 128, 128) for i in range(8)] + [(1024, 64)]


def build_C2(NB=16, NE=4):
    nc = bass.Bass("TRN2", target_bir_lowering=False)
    D = lambda n, s: nc.dram_tensor(n, s, F32, kind="ExternalInput").ap()
    h2d = D("h2T", [128, 8, NB * NT2]); Gd = D("G", [128, NB, 9, NE]); wgud = D("wgu", [NE, 128, 8, 2048]); wdd = D("wd", [NE, 128, 8, 1024])
    bgud = D("bgu", [128, NE, 16]); bdd = D("bd", [NE, 1024]); idd = D("ident", [128, 128])
    fd = nc.dram_tensor("f", [NB, 128, 9, 1024], F32, kind="ExternalOutput").ap()
    P = Prog(nc)
    hbf = [P.sb(f"hbf{i}", [128, 8, NT2], BF16) for i in range(2)]
    G = P.sb("G", [128, NB, 9, NE]); bgu = P.sb("bgu", [128, NE, 16]); bd = P.sb("bd", [NE, 1024]); ident = P.sb("ident", [128, 128])
    P.dma("sp", G[:], Gd, writes=["G"]); P.dma("sp", bgu[:], bgud, writes=["bgu"]); P.dma("sp", bd[:], bdd, writes=["bd"]); P.dma("sp", ident[:], idd, writes=["ident"])
    wgu = [P.sb(f"wgu{i}", [128, 8, 2048], BF16) for i in range(2)]; wd = [P.sb(f"wd{i}", [128, 8, 1024], BF16) for i in range(2)]
    act = P.sb("act", [128, 8, NT2], BF16); acc = P.sb("acc", [128, 9, 1024])
    gc_ = [P.sb(f"gc{i}", [128, 512]) for i in range(2)]; sg = [P.sb(f"sg{i}", [128, 512]) for i in range(2)]
    uc = [P.sb(f"uc{i}", [128, 512]) for i in range(2)]
    GT = P.sb("GT", [NE, 128])
    pp = [P.ps(f"bank{i}", [128, 512]) for i in range(8)]

    wgus = [nc.dram_tensor(f"wgu_bf{e}", [128, 8, 2048], BF16).ap() for e in range(NE)]
    wds = [nc.dram_tensor(f"wd_bf{e}", [128, 8, 1024], BF16).ap() for e in range(NE)]
    for e in range(NE):
        for k in range(8):
            P.dma("pool", wgus[e][:, k, :], wgud[e, :, k, :], writes=[f"wgus{e}"])
            P.dma("pool", wds[e][:, k, :], wdd[e, :, k, :], writes=[f"wds{e}"])

    def load_w(j):
        e = j % NE; b = j % 2
        for k in range(0, 8, 2):
            P.dma("sp", wgu[b][:, k:k + 2, :], wgus[e][:, k:k + 2, :], reads=[f"wgus{e}"], writes=[f"wgu{b}"])
        for k in range(0, 8, 4):
            P.dma("act", wd[b][:, k:k + 4, :], wds[e][:, k:k + 4, :], reads=[f"wds{e}"], writes=[f"wd{b}"])

    def load_h(blk):
        for k in range(8):
            P.dma("pool", hbf[blk % 2][:, k, :], h2d[:, k, blk * NT2:(blk + 1) * NT2], writes=[f"hbf{blk%2}"])
    load_h(0); load_w(0)
    it = 0; jt = 0; j = 0
    for blk in range(NB):
        hb = hbf[blk % 2]; hk = f"hbf{blk%2}"
        if blk + 1 < NB:
            load_h(blk + 1)
        for e in range(NE):
            b = j % 2
            if j + 1 < NB * NE:
                load_w(j + 1)
            j += 1
            for fc in range(8):
                for (t0, n) in T2:
                    s = it % 2; it += 1
                    pg, pu = pp[2 * s], pp[2 * s + 1]; kg, ku = f"bk{2*s}", f"bk{2*s+1}"
                    for k in range(8):
                        P.I("pe", "matmul", pg[:, 0:n], wgu[b][:, k, fc * 128:(fc + 1) * 128], hb[:, k, t0:t0 + n], start=(k == 0), stop=(k == 7),
                            r=[f"wgu{b}", hk], w=[kg])
                    for k in range(8):
                        P.I("pe", "matmul", pu[:, 0:n], wgu[b][:, k, 1024 + fc * 128:1024 + (fc + 1) * 128], hb[:, k, t0:t0 + n], start=(k == 0), stop=(k == 7),
                            r=[f"wgu{b}", hk], w=[ku])
                    P.I("dve", "tensor_scalar", gc_[s][:, 0:n], pg[:, 0:n], bgu[:, e, fc:fc + 1], 7.0, ALU.add, ALU.min, r=[kg, "bgu"], w=[f"gc{s}"])
                    P.I("act", "activation", sg[s][:, 0:n], gc_[s][:, 0:n], AF.Sigmoid, scale=1.702, r=[f"gc{s}"], w=[f"sg{s}"])
                    P.I("dve", "tensor_scalar", uc[s][:, 0:n], pu[:, 0:n], bgu[:, e, 8 + fc:9 + fc], 7.0, ALU.add, ALU.min, r=[ku, "bgu"], w=[f"uc{s}"])
                    P.I("dve", "tensor_scalar", uc[s][:, 0:n], uc[s][:, 0:n], -7.0, 1.0, ALU.max, ALU.add, r=[f"uc{s}"], w=[f"uc{s}"])
                    P.I("dve", "tensor_tensor", gc_[s][:, 0:n], gc_[s][:, 0:n], sg[s][:, 0:n], ALU.mult, r=[f"gc{s}", f"sg{s}"], w=[f"gc{s}"])
                    P.I("dve", "tensor_tensor", act[:, fc, t0:t0 + n], gc_[s][:, 0:n], uc[s][:, 0:n], ALU.mult, r=[f"gc{s}", f"uc{s}"], w=["act"])
            for si, (s0, sn) in enumerate(ST2):
                for half in range(2):
                    pb = 4 + jt % 4; jt += 1
                    hs = slice(half * 512, (half + 1) * 512)
                    for fc in range(8):
                        P.I("pe", "matmul", pp[pb][0:sn, 0:512], act[:, fc, s0:s0 + sn], wd[b][:, fc, hs], start=(fc == 0), stop=(fc == 7),
                            r=["act", f"wd{b}"], w=[f"bk{pb}"])
                    if e == 0:
                        P.I("dve", "tensor_scalar", acc[0:sn, si, hs], pp[pb][0:sn, 0:512], G[0:sn, blk, si, e:e + 1], None, ALU.mult,
                            r=[f"bk{pb}", "G"], w=["acc"])
                    else:
                        P.I("dve", "scalar_tensor_tensor", acc[0:sn, si, hs], pp[pb][0:sn, 0:512], G[0:sn, blk, si, e:e + 1], acc[0:sn, si, hs],
                            ALU.mult, ALU.add, r=[f"bk{pb}", "G", "acc"], w=["acc"])
        for si, (s0, sn) in enumerate(ST2):
            P.I("pe", "transpose", pp[0][0:NE, 0:sn], G[0:sn, blk, si, :], ident[0:sn, 0:sn], r=["G", "ident"], w=["bk0"])
            P.I("dve", "tensor_copy", GT[:, 0:sn], pp[0][0:NE, 0:sn], r=["bk0"], w=["GT"])
            for half in range(2):
                pb = 1 + half; hs = slice(half * 512, (half + 1) * 512)
                P.I("pe", "matmul", pp[pb][0:sn, 0:512], GT[:, 0:sn], bd[:, hs], start=True, stop=True, r=["GT", "bd"], w=[f"bk{pb}"])
                P.I("dve", "tensor_tensor", acc[0:sn, si, hs], acc[0:sn, si, hs], pp[pb][0:sn, 0:512], ALU.add, r=[f"bk{pb}", "acc"], w=["acc"])
        P.I("pool", "memset", acc[64:128, 8, :], 0.0, r=["acc"], w=["acc"]) if blk == 0 else None
        P.dma("sp", fd[blk], acc[:], reads=["acc"], is_output=True)
    P.finish()
    return nc


def build_D():
    nc = bass.Bass("TRN2", target_bir_lowering=False)
    D = lambda n, s: nc.dram_tensor(n, s, F32, kind="ExternalInput").ap()
    xmd = D("xmT", [128, 8, NTOK]); fTd = D("fT", [8, 128, 8, NTOK]); modd = D("mod", [128, 8, 2]); nwd = D("nw", [128, 8])
    od = nc.dram_tensor("oT", [128, 8, NTOK], F32, kind="ExternalOutput").ap()
    P = Prog(nc)
    xT = P.sb("xT", [128, 8, NTOK]); fT = P.sb("fT", [128, 8, NTOK]); mod = P.sb("mod", [128, 8, 2]); nw = P.sb("nw", [128, 8])
    ones_bf = P.sb("ones_bf", [128, 128], BF16)
    P.I("pool", "memset", ones_bf[:], 1.0, w=["ones_bf"])
    for k in range(8):
        P.dma("sp", xT[:, k, :], xmd[:, k, :], writes=["xT"])
    P.dma("sp", mod[:], modd, writes=["mod"]); P.dma("sp", nw[:], nwd, writes=["nw"])
    for c in range(8):
        for k in range(8):
            P.dma("sp", fT[:, k, :], fTd[c, :, k, :], writes=["fT"])
        add_gated(P, xT, fT, mod, 0, 1)
    sq = [P.sb(f"sq{i}", [128, 8, 512], BF16) for i in range(2)]; rs = [P.sb(f"rs{i}", [128, 512]) for i in range(2)]
    ss = [P.ps(f"bank{i}", [128, 512]) for i in range(2)]
    for it, (t0, n) in enumerate(TILES):
        b = it % 2
        for k in range(8):
            P.I("act", "activation", sq[b][:, k, 0:n], xT[:, k, t0:t0 + n], AF.Square, r=["xT"], w=[f"sq{b}"])
        for k in range(8):
            P.I("pe", "matmul", ss[b][:, 0:n], ones_bf[:], sq[b][:, k, 0:n], start=(k == 0), stop=(k == 7), r=[f"sq{b}", "ones_bf"], w=[f"bk{b}"])
        P.I("dve", "tensor_scalar", rs[b][:, 0:n], ss[b][:, 0:n], 1.0 / 1024, 1e-6, ALU.mult, ALU.add, r=[f"bk{b}"], w=[f"rs{b}"])
        P.I("dve", "reciprocal", rs[b][:, 0:n], rs[b][:, 0:n], r=[f"rs{b}"], w=[f"rs{b}"])
        P.I("act", "activation", rs[b][:, 0:n], rs[b][:, 0:n], AF.Sqrt, r=[f"rs{b}"], w=[f"rs{b}"])
        for k in range(8):
            P.I("dve", "scalar_tensor_tensor", fT[:, k, t0:t0 + n], xT[:, k, t0:t0 + n], nw[:, k:k + 1], rs[b][:, 0:n], ALU.mult, ALU.mult,
                r=["xT", "nw", f"rs{b}"], w=["fT"])
    for k in range(8):
        P.dma("sp", od[:, k, :], fT[:, k, :], reads=["fT"], is_output=True)
    P.finish()
    return nc


def add_gated(P, xT, fT, mod, col_l, col_c):
    for k in range(8):
        P.I("dve", "scalar_tensor_tensor", xT[:, k, 0:128], fT[:, k, 0:128], mod[:, k, col_c:col_c + 1], xT[:, k, 0:128], ALU.mult, ALU.add,
            r=["fT", "mod", "xT"], w=["xT"])
        P.I("dve", "scalar_tensor_tensor", xT[:, k, 128:NTOK], fT[:, k, 128:NTOK], mod[:, k, col_l:col_l + 1], xT[:, k, 128:NTOK], ALU.mult, ALU.add,
            r=["fT", "mod", "xT"], w=["xT"])


I32 = mybir.dt.int32


def build_C2s(NTILE=136, NE=4, caps=(4608, 4608, 4608, 4608)):
    NTOKA = NTILE * 128; NJs = [c_ // 128 for c_ in caps]; NJ = max(NJs); HALF = 9 * 128
    nc = bass.Bass("TRN2", target_bir_lowering=False)
    D = lambda n, s: nc.dram_tensor(n, s, F32, kind="ExternalInput").ap()
    h2d = D("h2tok", [NTOKA + 128, 1024]); Gd = D("Gm", [128, NTILE, NE]); tokd = D("tokid", [128, NTILE]); Ld = D("lst", [128, 128])
    padd = D("padtab", [128, NJ, 2]); idd = D("ident", [128, 128]); dumpd = D("dump", [128, NE])
    wgud = D("wgu", [NE, 128, 8, 2048]); wdd = D("wd", [NE, 128, 8, 1024]); bgud = D("bgu", [128, NE, 16]); bdbd = D("bdb", [NE, 128, 1024])
    fd = nc.dram_tensor("fpart", [NTOKA + 128, 1024], F32, kind="ExternalOutput").ap()
    tab = [nc.dram_tensor(f"slot_tab{e}", [caps[e] + 128, 2], F32).ap() for e in range(NE)]
    P = Prog(nc)
    pp = [P.ps(f"bank{i}", [128, 512]) for i in range(8)]
    z = P.sb("z", [128, 1024]); ones = P.sb("ones", [128, 128]); lst = P.sb("lst", [128, 128]); ident = P.sb("ident", [128, 128])
    P.I("pool", "memset", z[:], 0.0, w=["z"]); P.I("pool", "memset", ones[:], 1.0, w=["ones"])
    P.dma("sp", lst[:], Ld, writes=["lst"]); P.dma("sp", ident[:], idd, writes=["ident"])
    for r in range(NTILE + 1):
        P.dma("sp", fd[r * 128:(r + 1) * 128, :], z[:], reads=["z"], writes=["fpart"])
    Gm = P.sb("Gm", [128, NTILE, NE]); tokid = P.sb("tokid", [128, NTILE]); padt = P.sb("padt", [128, NJ, 2]); bgu = P.sb("bgu", [128, NE, 16])
    P.dma("act", Gm[:], Gd, writes=["Gm"]); P.dma("act", tokid[:], tokd, writes=["tokid"]); P.dma("act", padt[:], padd, writes=["padt"])
    P.dma("act", bgu[:], bgud, writes=["bgu"])
    dump = P.sb("dump", [128, NE]); P.dma("act", dump[:], dumpd, writes=["dump"])
    wgu = [P.sb(f"wgu{i}", [128, 8, 2048], BF16) for i in range(2)]; wd = [P.sb(f"wd{i}", [128, 8, 1024], BF16) for i in range(2)]
    bdb = [P.sb(f"bdb{i}", [128, 1024]) for i in range(2)]
    hsel = P.sb("hsel", [128, 8, HALF], BF16); act = P.sb("act", [128, 8, HALF], BF16)
    hg = [P.sb(f"hg{i}", [128, 1024]) for i in range(2)]; yst = [P.sb(f"yst{i}", [128, 1024]) for i in range(2)]
    gc_ = [P.sb(f"gc{i}", [128, 512]) for i in range(2)]; sg = [P.sb(f"sg{i}", [128, 512]) for i in range(2)]
    uc = [P.sb(f"uc{i}", [128, 512]) for i in range(2)]

    def load_w(e):
        b = e % 2
        for k in range(8):
            P.dma("pool", wgu[b][:, k, :], wgud[e, :, k, :], writes=[f"wgu{b}"])
        for k in range(8):
            P.dma("pool", wd[b][:, k, :], wdd[e, :, k, :], writes=[f"wd{b}"])
        P.dma("act", bdb[b][:], bdbd[e], writes=[f"bdb{b}"])
    load_w(0)
    m = P.sb("m", [128, NE, NTILE]); cs = P.sb("cs", [128, NE, NTILE]); inc = P.sb("inc", [128, NE, NTILE]); rk = P.sb("rk", [128, NE, NTILE])
    idx = P.sb("idx", [128, NE, NTILE], I32); pr = P.sb("pr", [128, NTILE, NE, 2]); onesw = P.sb("onesw", [128, NTILE])
    P.I("pool", "memset", onesw[:], 1.0, w=["onesw"])
    for e in range(NE):
        P.I("dve", "tensor_scalar", m[:, e, :], Gm[:, :, e], 0.0, None, ALU.is_gt, r=["Gm"], w=["m"])
        P.I("pe", "matmul", pp[0][:, 0:NTILE], lst[:], m[:, e, :], start=True, stop=True, r=["lst", "m"], w=["bk0"])
        P.I("pe", "matmul", pp[1][:, 0:NTILE], ones[:], m[:, e, :], start=True, stop=True, r=["ones", "m"], w=["bk1"])
        P.I("act", "activation", cs[:, e, :], pp[1][:, 0:NTILE], AF.Copy, r=["bk1"], w=["cs"])
        P.I("dve", "tensor_tensor_scan", inc[:, e, :], onesw[:], cs[:, e, :], 0.0, ALU.mult, ALU.add, r=["onesw", "cs"], w=["inc"])
        P.I("dve", "tensor_tensor", rk[:, e, :], pp[0][:, 0:NTILE], inc[:, e, :], ALU.add, r=["bk0", "inc"], w=["rk"])
        P.I("dve", "tensor_tensor", rk[:, e, :], rk[:, e, :], cs[:, e, :], ALU.subtract, r=["rk", "cs"], w=["rk"])
        P.I("dve", "tensor_scalar", cs[:, e, :], rk[:, e, :], float(caps[e]), None, ALU.is_lt, r=["rk", "cs"], w=["cs"])
        P.I("dve", "tensor_tensor", cs[:, e, :], cs[:, e, :], m[:, e, :], ALU.mult, r=["cs", "m"], w=["cs"])
        P.I("dve", "tensor_scalar", rk[:, e, :], rk[:, e, :], dump[:, e:e + 1], None, ALU.subtract, r=["rk", "dump"], w=["rk"])
        P.I("dve", "tensor_tensor", rk[:, e, :], rk[:, e, :], cs[:, e, :], ALU.mult, r=["rk", "cs"], w=["rk"])
        P.I("dve", "tensor_scalar", rk[:, e, :], rk[:, e, :], dump[:, e:e + 1], None, ALU.add, r=["rk", "dump"], w=["rk"])
        P.I("dve", "tensor_copy", idx[:, e, :], rk[:, e, :], r=["rk"], w=["idx"])
        P.I("pool", "tensor_copy", pr[:, :, e, 0], tokid[:], r=["tokid"], w=["pr"])
        P.I("pool", "tensor_copy", pr[:, :, e, 1], Gm[:, :, e], r=["Gm"], w=["pr"])
    for e in range(NE):
        P.dma("act", tab[e][0:caps[e], :].rearrange("(p j) c -> p j c", j=NJs[e]), padt[:, 0:NJs[e], :], reads=["padt"], writes=[f"tabinit{e}"])
    for t in range(NTILE):
        for e in range(NE):
            P.idma(tab[e], pr[:, t, e, :], out_idx=idx[:, e, t:t + 1], reads=["pr", "idx", f"tabinit{e}"], writes=[f"sc{e}_{t}"])
    tabsb = P.sb("tabsb", [128, NE, NJ, 2]); tok_i = P.sb("tok_i", [128, NE, NJ], I32); gate = P.sb("gate", [128, NE, NJ])
    for e in range(NE):
        P.dma("act", tabsb[:, e, 0:NJs[e], :], tab[e][0:caps[e], :].rearrange("(p j) c -> p j c", j=NJs[e]), reads=[f"sc{e}_{t}" for t in range(NTILE)], writes=["tabsb"])
    P.I("pool", "memset", tabsb[:], 0.0, w=["tabsb"]) if False else None
    P.I("dve", "tensor_copy", tok_i[:], tabsb[:, :, :, 0], r=["tabsb"], w=["tok_i"])
    P.I("dve", "tensor_copy", gate[:], tabsb[:, :, :, 1], r=["tabsb"], w=["gate"])
    it = 0; jt = 0; gi = 0; ti = 0
    for e in range(NE):
        b = e % 2
        if e + 1 < NE:
            load_w(e + 1)
        for j0 in range(0, NJs[e], 9):
            NJH = min(9, NJs[e] - j0); SPN = NJH * 128
            T3 = [(t0, min(512, SPN - t0)) for t0 in range(0, SPN, 512)]
            for jj in range(NJH):
                j = j0 + jj; g = gi % 2; gi += 1
                P.idma(hg[g][:], h2d, in_idx=tok_i[:, e, j:j + 1], reads=["tok_i"], writes=[f"hg{g}"])
                for k in range(8):
                    pb = 4 + ti % 4; ti += 1
                    P.I("pe", "transpose", pp[pb][:, 0:128], hg[g][:, k * 128:(k + 1) * 128], ident[:], r=[f"hg{g}", "ident"], w=[f"bk{pb}"])
                    if ti % 2:
                        P.I("act", "activation", hsel[:, k, jj * 128:(jj + 1) * 128], pp[pb][:, 0:128], AF.Copy, r=[f"bk{pb}"], w=["hsel"])
                    else:
                        P.I("dve", "tensor_copy", hsel[:, k, jj * 128:(jj + 1) * 128], pp[pb][:, 0:128], r=[f"bk{pb}"], w=["hsel"])
            for fc in range(8):
                for (t0, n) in T3:
                    s = it % 2; it += 1
                    pg, pu = pp[2 * s], pp[2 * s + 1]; kg, ku = f"bk{2*s}", f"bk{2*s+1}"
                    for k in range(8):
                        P.I("pe", "matmul", pg[:, 0:n], wgu[b][:, k, fc * 128:(fc + 1) * 128], hsel[:, k, t0:t0 + n], start=(k == 0), stop=(k == 7),
                            r=[f"wgu{b}", "hsel"], w=[kg])
                    for k in range(8):
                        P.I("pe", "matmul", pu[:, 0:n], wgu[b][:, k, 1024 + fc * 128:1024 + (fc + 1) * 128], hsel[:, k, t0:t0 + n], start=(k == 0), stop=(k == 7),
                            r=[f"wgu{b}", "hsel"], w=[ku])
                    P.I("dve", "tensor_scalar", gc_[s][:, 0:n], pg[:, 0:n], bgu[:, e, fc:fc + 1], 7.0, ALU.add, ALU.min, r=[kg, "bgu"], w=[f"gc{s}"])
                    P.I("act", "activation", sg[s][:, 0:n], gc_[s][:, 0:n], AF.Sigmoid, scale=1.702, r=[f"gc{s}"], w=[f"sg{s}"])
                    P.I("dve", "tensor_scalar", uc[s][:, 0:n], pu[:, 0:n], bgu[:, e, 8 + fc:9 + fc], 7.0, ALU.add, ALU.min, r=[ku, "bgu"], w=[f"uc{s}"])
                    P.I("dve", "tensor_scalar", uc[s][:, 0:n], uc[s][:, 0:n], -7.0, 1.0, ALU.max, ALU.add, r=[f"uc{s}"], w=[f"uc{s}"])
                    P.I("dve", "tensor_tensor", gc_[s][:, 0:n], gc_[s][:, 0:n], sg[s][:, 0:n], ALU.mult, r=[f"gc{s}", f"sg{s}"], w=[f"gc{s}"])
                    P.I("dve", "tensor_tensor", act[:, fc, t0:t0 + n], gc_[s][:, 0:n], uc[s][:, 0:n], ALU.mult, r=[f"gc{s}", f"uc{s}"], w=["act"])
            for jj in range(NJH):
                j = j0 + jj; y = jt % 2
                for hh in range(2):
                    pb = 4 + jt % 4; jt += 1
                    hs = slice(hh * 512, (hh + 1) * 512)
                    for fc in range(8):
                        P.I("pe", "matmul", pp[pb][:, 0:512], act[:, fc, jj * 128:(jj + 1) * 128], wd[b][:, fc, hs], start=(fc == 0), stop=(fc == 7),
                            r=["act", f"wd{b}"], w=[f"bk{pb}"])
                    P.I("dve", "tensor_tensor", yst[jj % 2][:, hs], pp[pb][:, 0:512], bdb[b][:, hs], ALU.add, r=[f"bk{pb}", f"bdb{b}"], w=[f"yst{jj%2}"])
                P.I("pool", "tensor_scalar", yst[jj % 2][:], yst[jj % 2][:], gate[:, e, j:j + 1], None, ALU.mult, r=[f"yst{jj%2}", "gate"], w=[f"yst{jj%2}"])
                P.idma(fd, yst[jj % 2][:], out_idx=tok_i[:, e, j:j + 1], reads=[f"yst{jj%2}", "tok_i", "fpart"], writes=["fpart"], is_output=True, compute_op=ALU.add)
    P.finish()
    return nc


def host_C2s_consts(NTILE=136, caps=(4608, 4608, 4608, 4608)):
    NJ = max(caps) // 128
    tokid = (np.arange(NTILE)[None, :] * 128 + np.arange(128)[:, None]).astype(np.float32)
    p_ = np.arange(128)[:, None]; q_ = np.arange(128)[None, :]
    lst = (p_ < q_).astype(np.float32)
    padtab = np.zeros((128, NJ, 2), np.float32); padtab[:, :, 0] = NTILE * 128 + np.arange(128)[:, None]
    return {"tokid": tokid, "lst": lst, "padtab": padtab, "ident": np.eye(128, dtype=np.float32), "dump": np.stack([c_ + np.arange(128, dtype=np.float32) for c_ in caps], 1)}


def _fm(tok):
    return np.ascontiguousarray(tok.T.reshape(8, 128, -1).transpose(1, 0, 2))


def _tok(fm):
    return fm.transpose(2, 1, 0).reshape(fm.shape[2], -1)


def _vec(v):
    return np.ascontiguousarray(np.asarray(v, np.float32).reshape(8, 128).T)


_PROGS = {}
C2S_CAP = 6144
MOE_SPARSE = True


def _prog(name, builder, *a):
    key = (name,) + a
    if key not in _PROGS:
        _PROGS[key] = builder(*a)
    return _PROGS[key]


def _run(nc, maps):
    res = run_bass_kernel_spmd(nc, maps, core_ids=list(range(len(maps))))
    return res.results


def kernel(**inp):
    prm = {k: np.asarray(v, dtype=np.float32) for k, v in inp.items()}
    x, c, ctx, c_ctx = prm['x'], prm['c'], prm['ctx'], prm['c_ctx']
    NC = 8
    A_ = lambda a: np.ascontiguousarray(a, dtype=np.float32)
    cs = np.zeros((128, 8, 5), np.float32)
    for v in range(4):
        cs[:, :, v] = _vec(c[v])
    cs[:, :, 4] = _vec(c_ctx)
    items = [(l, fc) for l in range(2) for fc in range(48)]
    maps = []
    for i in range(NC):
        its = items[i * 12:(i + 1) * 12]
        wm = np.stack([prm['w_mod'][l][:, fc * 128:(fc + 1) * 128].reshape(8, 128, 128).transpose(1, 0, 2) for l, fc in its])
        bm = np.stack([prm['b_mod'][l][fc * 128:(fc + 1) * 128] for l, fc in its], 1)
        maps.append({"cs": cs, "wm": A_(wm), "bm": A_(bm)})
    res = _run(_prog("M", build_M), maps)
    modv = {}
    for i in range(NC):
        for j, (l, fc) in enumerate(items[i * 12:(i + 1) * 12]):
            modv[(l, fc)] = res[i]["modT"][:, j, :]
    mod6 = [[np.stack([modv[(l, i6 * 8 + k)] for k in range(8)], 1) for i6 in range(6)] for l in range(2)]

    def core_tokens(arr_c, arr_l, b, half):
        return np.concatenate([arr_c[b, half * 128:(half + 1) * 128], arr_l[b, half * 2048:(half + 1) * 2048]], 0)

    xm = [_fm(core_tokens(ctx, x, j // 2, j % 2)) for j in range(NC)]
    fparts = None
    consts64 = host_consts64()
    for l in range(2):
        win = np.zeros((1024, NCT * 128), np.float32); win[:, :4184] = prm['w_in'][l]
        win = A_(win.reshape(8, 128, NCT * 128).transpose(1, 0, 2)); nw1 = _vec(prm['norm1_w'][l])
        maps = []
        for j in range(NC):
            b = j // 2
            g5l = mod6[l - 1][5][:, :, b] if l > 0 else np.zeros((128, 8), np.float32)
            g5c = mod6[l - 1][5][:, :, 4] if l > 0 else np.zeros((128, 8), np.float32)
            mod = np.stack([mod6[l][0][:, :, b], mod6[l][1][:, :, b], mod6[l][0][:, :, 4], mod6[l][1][:, :, 4], g5l, g5c], -1)
            m = {"xT": xm[j], "mod": A_(mod), "nw": nw1, "win": win}
            if l > 0:
                m["fT"] = A_(np.stack([_fm(fparts[cc][j * NTOK:(j + 1) * NTOK]) for cc in range(NC)]))
            maps.append(m)
        res = _run(_prog("A", build_A, l == 0), maps)
        xcur = [res[j]["xout"] for j in range(NC)]
        ptok = [res[j]["pT"].reshape(NCT * 128, NTOK).T[:, :4184] for j in range(NC)]
        del res
        pfull = np.stack([np.concatenate([ptok[2 * b][:128], ptok[2 * b + 1][:128], ptok[2 * b][128:], ptok[2 * b + 1][128:]], 0) for b in range(4)])
        del ptok
        yall = np.zeros((4, TSEQ, 1024), np.float32)
        pa = pfull[:, :, 0:1032]
        res = _run(_prog("B1", build_B1), [host_B1_inputs(pa, l, j // 2, j % 2, prm) for j in range(NC)])
        for j in range(NC):
            yall[j // 2, :, (j % 2) * 128:(j % 2 + 1) * 128] = res[j]["yT"].T
        pb = pfull[:, :, 1032:3096]
        for rnd in range(2):
            its = [(i // 4, i % 4) for i in range(rnd * 8, rnd * 8 + 8)]
            maps = [host_B2_inputs(pb, l, b, h, prm) for b, h in its]
            for m in maps:
                m["consts"] = consts64
            res = _run(_prog("B2", build_B2), maps)
            for (b, h), r in zip(its, res):
                yall[b, :, 256 + h * 128:256 + (h + 1) * 128] = host_B2_output(r["y"])
        pc = pfull[:, :, 3096:4184]
        for rnd in range(2):
            its = [(i // 4, i % 4) for i in range(rnd * 8, rnd * 8 + 8)]
            maps = [host_B3_inputs(pc, l, b, h, prm) for b, h in its]
            for m in maps:
                m["consts"] = consts64
            res = _run(_prog("B3", build_B3), maps)
            for (b, h), r in zip(its, res):
                yall[b, :, 768 + h * 64:768 + (h + 1) * 64] = r["y"].T
        del pfull
        wout = A_(prm['w_out'][l].reshape(8, 128, 1024).transpose(1, 0, 2)); nw2 = _vec(prm['norm2_w'][l])
        wr = A_(prm['w_router'][l].reshape(8, 128, 32).transpose(1, 0, 2)); br = A_(np.broadcast_to(prm['b_router'][l][None], (128, 32)))
        maps = []
        for j in range(NC):
            b, half = j // 2, j % 2
            ytok = np.concatenate([yall[b, half * 128:(half + 1) * 128], yall[b, 256 + half * 2048:256 + (half + 1) * 2048]], 0)
            mod = np.stack([mod6[l][2][:, :, b], mod6[l][3][:, :, b], mod6[l][4][:, :, b], mod6[l][2][:, :, 4], mod6[l][3][:, :, 4], mod6[l][4][:, :, 4]], -1)
            maps.append({"xT": xcur[j], "yT": _fm(ytok), "wout": wout, "mod": A_(mod), "nw": nw2, "wr": wr, "br": br})
        res = _run(_prog("C1", build_C1), maps)
        xm = [res[j]["xmT"] for j in range(NC)]
        Gall = np.concatenate([res[j]["G"].transpose(1, 0, 2).reshape(NTOK, 32) for j in range(NC)], 0)
        loads = np.count_nonzero(Gall, axis=0)
        caps = tuple(int(-(-max(int(loads[4 * cc + e]) for cc in range(NC)) // 128) * 128) for e in range(4))
        use_sparse = MOE_SPARSE and max(caps) <= C2S_CAP
        print(f"[moe] layer {l}: max expert load {int(loads.max())} (mean {float(loads.mean()):.0f}) -> {'dispatch' if use_sparse else 'dense'} caps {caps}", flush=True)
        if use_sparse:
            h2tok = np.concatenate([_tok(res[j]["h2T"]) for j in range(NC)] + [np.zeros((128, 1024), np.float32)], 0)
        else:
            h2all = np.ascontiguousarray(np.concatenate([res[j]["h2T"] for j in range(NC)], 2))
        del res, yall
        if use_sparse:
            maps = []
            c2c = host_C2s_consts(136, caps)
            for cc in range(NC):
                es = slice(4 * cc, 4 * cc + 4)
                m_ = {"h2tok": h2tok, "Gm": A_(Gall[:, es].reshape(136, 128, 4).transpose(1, 0, 2)),
                      "wgu": A_(prm['w_gate_up'][l][es].reshape(4, 8, 128, 2048).transpose(0, 2, 1, 3)),
                      "wd": A_(prm['w_down'][l][es].reshape(4, 8, 128, 1024).transpose(0, 2, 1, 3)),
                      "bgu": A_(prm['b_gate_up'][l][es].reshape(4, 16, 128).transpose(2, 0, 1)),
                      "bdb": A_(np.broadcast_to(prm['b_down'][l][es][:, None, :], (4, 128, 1024)))}
                m_.update(c2c)
                maps.append(m_)
            res = _run(_prog("C2s", build_C2s, 136, 4, caps), maps)
            del maps, h2tok
            fparts = [res[cc]["fpart"][:16 * NT2] for cc in range(NC)]
            del res
        else:
            maps = []
            ident = np.eye(128, dtype=np.float32)
            for cc in range(NC):
                es = slice(4 * cc, 4 * cc + 4)
                Gp = np.zeros((16, 1152, 4), np.float32); Gp[:, :NT2] = Gall[:, es].reshape(16, NT2, 4)
                maps.append({"h2T": h2all, "G": A_(Gp.reshape(16, 9, 128, 4).transpose(2, 0, 1, 3)),
                             "wgu": A_(prm['w_gate_up'][l][es].reshape(4, 8, 128, 2048).transpose(0, 2, 1, 3)),
                             "wd": A_(prm['w_down'][l][es].reshape(4, 8, 128, 1024).transpose(0, 2, 1, 3)),
                             "bgu": A_(prm['b_gate_up'][l][es].reshape(4, 16, 128).transpose(2, 0, 1)), "bd": A_(prm['b_down'][l][es]), "ident": ident})
            res = _run(_prog("C2", build_C2), maps)
            del maps, h2all
            fparts = [res[cc]["f"].transpose(0, 2, 1, 3).reshape(16, 1152, 1024)[:, :NT2].reshape(16 * NT2, 1024) for cc in range(NC)]
            del res
    nwf = _vec(prm['norm_f_w'])
    maps = []
    for j in range(NC):
        b = j // 2
        mod = np.stack([mod6[1][5][:, :, b], mod6[1][5][:, :, 4]], -1)
        maps.append({"xmT": xm[j], "fT": A_(np.stack([_fm(fparts[cc][j * NTOK:(j + 1) * NTOK]) for cc in range(NC)])), "mod": A_(mod), "nw": nwf})
    res = _run(_prog("D", build_D), maps)
    out = np.zeros((4, 4096, 1024), np.float32)
    for j in range(NC):
        b, half = j // 2, j % 2
        out[b, half * 2048:(half + 1) * 2048] = _tok(res[j]["oT"])[128:]
    return out
```
